# Optimizing a Trainium2 kernel written in Bass

```python
import math
import jax
import jax.numpy as jnp
from jax import lax
import numpy as np


D_MODEL = 1024
BATCH = 32
SEQ = 2048
DEPTH = 2

GRID_W = 64
CTX_LEN = 256
N_EVEN = (DEPTH + 1) // 2
N_ODD = DEPTH // 2
DEEPNORM_ALPHA = (2.0 * DEPTH) ** 0.25
DEEPNORM_BETA = (8.0 * DEPTH) ** -0.25
LN_EPS = 1e-5
RMS_EPS = 1e-6
ROPE_BASE = 10000.0

B_HEADS = 4
B_HEAD_DIM = 128
B_WIDTH = B_HEADS * B_HEAD_DIM
B_CONV = 3
B_CHUNK = 64
A_WIDTH = D_MODEL - B_WIDTH
A_CONV = 3
C_Q_HEADS = 8
C_KV_HEADS = 2
C_HEAD_DIM = 64
C_WINDOW = 128
D_HEADS = 8
D_NOPE = 64
D_ROPE = 32
D_V = 64
D_Q_RANK = 384
D_KV_RANK = 256
Q_BLOCK = 128
N_EXPERTS = 16
N_GROUPS = 4
TOP_K = 2
D_EXPERT = 512

EVEN_COLS = (A_WIDTH, A_WIDTH, A_WIDTH, B_WIDTH, B_WIDTH, B_WIDTH, B_WIDTH, 2 * B_HEADS, 2 * B_HEADS)
ODD_COLS = (C_Q_HEADS * C_HEAD_DIM, C_KV_HEADS * C_HEAD_DIM, C_KV_HEADS * C_HEAD_DIM, D_Q_RANK, D_KV_RANK, D_ROPE)
EVEN_IN = sum(EVEN_COLS)
ODD_IN = sum(ODD_COLS)
EVEN_OUT = A_WIDTH + B_WIDTH
ODD_OUT = C_Q_HEADS * C_HEAD_DIM + D_HEADS * D_V

kernel_name = 'hybrid_prefix_diffusion_block'

F32 = jnp.float32


def layer_norm(x, g, b):
    xf = x.astype(F32)
    mu = jnp.mean(xf, -1, keepdims=True)
    var = jnp.mean(jnp.square(xf - mu), -1, keepdims=True)
    return ((xf - mu) * lax.rsqrt(var + LN_EPS) * g.astype(F32) + b.astype(F32)).astype(x.dtype)


def rms_norm(x, g):
    xf = x.astype(F32)
    y = xf * lax.rsqrt(jnp.mean(jnp.square(xf), -1, keepdims=True) + RMS_EPS)
    return (y * g.astype(F32)).astype(x.dtype)


def l2_normalize(x):
    return x * lax.rsqrt(jnp.sum(jnp.square(x), -1, keepdims=True) + 1e-6)


def modulate(x, shift, scale):
    return x * (1 + scale) + shift


def split_cols(p, sizes):
    offsets = [int(o) for o in np.cumsum(sizes)[:-1]]
    return jnp.split(p, offsets, axis=-1)


def flip_seq(t, rev):
    return jnp.flip(t, axis=1) if rev else t


def depthwise_conv(x, w):
    width = w.shape[0]
    return lax.conv_general_dilated(
        x, w[:, None, :].astype(x.dtype), window_strides=(1,), padding=[(width // 2, width // 2)],
        dimension_numbers=('NWC', 'WIO', 'NWC'), feature_group_count=x.shape[-1])


def axial_rope(n_tokens, rot_dim, dtype):
    n_rows = n_tokens // GRID_W
    t = jnp.arange(n_rows * GRID_W)
    rows = (t // GRID_W).astype(F32)
    cols = (t % GRID_W).astype(F32)
    n_freq = rot_dim // 4
    inv_freq = ROPE_BASE ** (-jnp.arange(n_freq, dtype=F32) / n_freq)
    ang = jnp.concatenate([rows[:, None] * inv_freq, cols[:, None] * inv_freq], -1)
    return jnp.cos(ang).astype(dtype), jnp.sin(ang).astype(dtype)


def apply_rope(x, cos, sin):
    bshape = cos.shape[:1] + (1,) * (x.ndim - 3) + cos.shape[1:]
    cos, sin = cos.reshape(bshape), sin.reshape(bshape)
    x1, x2 = jnp.split(x, 2, axis=-1)
    return jnp.concatenate([x1 * cos - x2 * sin, x2 * cos + x1 * sin], -1)


def gated_delta_chunked(q, k, v, beta, g, state):
    bsz, seq, heads, dk = q.shape
    dv = v.shape[-1]
    cs = B_CHUNK
    n_chunks = seq // cs

    def chunks(t):
        t = t.reshape((bsz, n_chunks, cs, heads) + t.shape[3:])
        return jnp.moveaxis(jnp.moveaxis(t, 3, 2), 1, 0)

    q = chunks(q) * dk ** -0.5
    k = chunks(k)
    v = chunks(v)
    beta = chunks(beta)
    gc = jnp.cumsum(chunks(g), axis=-1)
    idx = jnp.arange(cs)
    lower = idx[:, None] >= idx[None, :]
    strict = idx[:, None] > idx[None, :]
    decay = jnp.exp(jnp.where(lower, gc[..., :, None] - gc[..., None, :], -jnp.inf))
    kb = k * beta[..., None]
    a = jnp.where(strict, jnp.einsum('nbhid,nbhjd->nbhij', kb, k) * decay, 0.0)
    m = a + jnp.eye(cs, dtype=a.dtype)
    u = lax.linalg.triangular_solve(m, v * beta[..., None], left_side=True, lower=True, unit_diagonal=True)
    w = lax.linalg.triangular_solve(m, kb * jnp.exp(gc)[..., None], left_side=True, lower=True, unit_diagonal=True)
    attn = jnp.einsum('nbhid,nbhjd->nbhij', q, k) * decay

    def step(s, xs):
        q_n, k_n, u_n, w_n, gc_n, attn_n = xs
        v_new = u_n - jnp.einsum('bhcd,bhde->bhce', w_n, s)
        o_n = (jnp.einsum('bhcd,bhde->bhce', q_n * jnp.exp(gc_n)[..., None], s)
               + jnp.einsum('bhij,bhje->bhie', attn_n, v_new))
        g_last = gc_n[..., -1:]
        s = (s * jnp.exp(g_last)[..., None]
             + jnp.einsum('bhcd,bhce->bhde', k_n * jnp.exp(g_last - gc_n)[..., None], v_new))
        return s, o_n

    state, o = lax.scan(step, state, (q, k, u, w, gc, attn))
    o = jnp.swapaxes(jnp.moveaxis(o, 0, 1), 2, 3).reshape(bsz, seq, heads, dv)
    return o, state


def softmax_with_sink(scores, sink):
    g, r = scores.shape[1], scores.shape[2]
    sink_col = jnp.broadcast_to(sink.astype(F32).reshape(1, g, r, 1, 1), scores.shape[:-1] + (1,))
    p = jax.nn.softmax(jnp.concatenate([scores, sink_col], -1), axis=-1)
    return p[..., :-1]


def window_attention(q, k, v, k_ctx, v_ctx, sink):
    bsz, seq, g, r, d = q.shape
    wdw = C_WINDOW
    n_blk = seq // wdw
    scale = d ** -0.5
    kp = jnp.pad(k, ((0, 0), (wdw, wdw), (0, 0), (0, 0)))
    vp = jnp.pad(v, ((0, 0), (wdw, wdw), (0, 0), (0, 0)))
    qb = jnp.moveaxis(q.reshape(bsz, n_blk, wdw, g, r, d), 1, 0)
    rel = jnp.arange(3 * wdw)[None, :] - wdw - jnp.arange(wdw)[:, None]
    near = jnp.abs(rel) <= wdw
    n_local = 3 * wdw

    def block(args):
        q_blk, i = args
        k_blk = lax.dynamic_slice_in_dim(kp, i * wdw, n_local, axis=1)
        v_blk = lax.dynamic_slice_in_dim(vp, i * wdw, n_local, axis=1)
        kpos = (i - 1) * wdw + jnp.arange(n_local)
        valid = near & ((kpos >= 0) & (kpos < seq))[None, :]
        s_loc = jnp.einsum('bqgrd,bkgd->bgrqk', q_blk, k_blk).astype(F32) * scale
        s_loc = jnp.where(valid, s_loc, -jnp.inf)
        s_ctx = jnp.einsum('bqgrd,bcgd->bgrqc', q_blk, k_ctx).astype(F32) * scale
        p = softmax_with_sink(jnp.concatenate([s_loc, s_ctx], -1), sink).astype(v.dtype)
        return (jnp.einsum('bgrqk,bkgd->bqgrd', p[..., :n_local], v_blk)
                + jnp.einsum('bgrqc,bcgd->bqgrd', p[..., n_local:], v_ctx))

    o = lax.map(block, (qb, jnp.arange(n_blk)))
    return jnp.moveaxis(o, 0, 1).reshape(bsz, seq, g * r * d)


def context_gqa(q, k, v, sink):
    s = jnp.einsum('bqgrd,bkgd->bgrqk', q, k).astype(F32) * C_HEAD_DIM ** -0.5
    p = softmax_with_sink(s, sink).astype(v.dtype)
    o = jnp.einsum('bgrqk,bkgd->bqgrd', p, v)
    return o.reshape(o.shape[0], o.shape[1], -1)


def latent_attention(q_nope, q_rope, k_nope, k_rope, v, q_block):
    bsz, lq, heads, dn = q_nope.shape
    dr = q_rope.shape[-1]
    n_blk = lq // q_block
    scale = (D_NOPE + D_ROPE) ** -0.5
    qn_b = jnp.moveaxis(q_nope.reshape(bsz, n_blk, q_block, heads, dn), 1, 0)
    qr_b = jnp.moveaxis(q_rope.reshape(bsz, n_blk, q_block, heads, dr), 1, 0)

    def block(args):
        qn, qr = args
        s = (jnp.einsum('bqhd,bkhd->bhqk', qn, k_nope)
             + jnp.einsum('bqhd,bkd->bhqk', qr, k_rope)).astype(F32) * scale
        p = jax.nn.softmax(s, axis=-1).astype(v.dtype)
        return jnp.einsum('bhqk,bkhd->bqhd', p, v)

    o = lax.map(block, (qn_b, qr_b))
    return jnp.moveaxis(o, 0, 1).reshape(bsz, lq, heads * v.shape[-1])


def even_mixer(hl, hc, w_in, a_conv, b_conv, b_alog, b_dtbias, b_norm, w_out, ctx_out):
    pl = split_cols(hl @ w_in, EVEN_COLS)
    pc = split_cols(hc @ w_in, EVEN_COLS)

    def short_conv(p):
        return p[0] * depthwise_conv(p[1] * p[2], a_conv)

    def delta_inputs(p):
        q, k, v, beta_logit, a = p[3], p[4], p[5], p[7], p[8]
        bsz, n = q.shape[:2]
        qkv = jax.nn.silu(depthwise_conv(jnp.concatenate([q, k, v], -1), b_conv)).astype(F32)
        q, k, v = [t.reshape(bsz, n, B_HEADS, B_HEAD_DIM) for t in jnp.split(qkv, 3, -1)]
        beta = jax.nn.sigmoid(beta_logit.astype(F32)).reshape(bsz, n, 2, B_HEADS)
        g = -jnp.exp(b_alog.astype(F32)) * jax.nn.softplus(
            a.astype(F32).reshape(bsz, n, 2, B_HEADS) + b_dtbias.astype(F32))
        return l2_normalize(q), l2_normalize(k), v, beta, g

    def delta_output(o, p):
        bsz, n = o.shape[:2]
        gate = jax.nn.silu(p[6].astype(F32)).reshape(bsz, n, B_HEADS, B_HEAD_DIM)
        return (rms_norm(o, b_norm) * gate).reshape(bsz, n, B_WIDTH).astype(hl.dtype)

    ql, kl, vl, beta_l, g_l = delta_inputs(pl)
    qc, kc, vc, beta_c, g_c = delta_inputs(pc)
    state0 = jnp.zeros((hl.shape[0], B_HEADS, B_HEAD_DIM, B_HEAD_DIM), F32)
    o_l = jnp.zeros(vl.shape, F32)
    o_c = jnp.zeros(vc.shape, F32)
    for direction in range(2):
        rev = direction == 1
        oc_d, s_ctx = gated_delta_chunked(
            flip_seq(qc, rev), flip_seq(kc, rev), flip_seq(vc, rev),
            flip_seq(beta_c[:, :, direction], rev), flip_seq(g_c[:, :, direction], rev), state0)
        ol_d, _ = gated_delta_chunked(
            flip_seq(ql, rev), flip_seq(kl, rev), flip_seq(vl, rev),
            flip_seq(beta_l[:, :, direction], rev), flip_seq(g_l[:, :, direction], rev), s_ctx)
        o_l = o_l + flip_seq(ol_d, rev)
        o_c = o_c + flip_seq(oc_d, rev)
    yl = jnp.concatenate([short_conv(pl), delta_output(o_l, pl)], -1) @ w_out
    yc = jnp.concatenate([short_conv(pc), delta_output(o_c, pc)], -1) @ w_out if ctx_out else None
    return yl, yc


def odd_mixer(hl, hc, w_in, c_sink, d_qnorm, d_kvnorm, d_wuq, d_wukv, w_out, ctx_out):
    def project(h, positional):
        bsz, n = h.shape[:2]
        cq, ck, cv, dq, dkv, k_rope = split_cols(h @ w_in, ODD_COLS)
        cq = cq.reshape(bsz, n, C_KV_HEADS, C_Q_HEADS // C_KV_HEADS, C_HEAD_DIM)
        ck = ck.reshape(bsz, n, C_KV_HEADS, C_HEAD_DIM)
        cv = cv.reshape(bsz, n, C_KV_HEADS, C_HEAD_DIM)
        q = (rms_norm(dq, d_qnorm) @ d_wuq).reshape(bsz, n, D_HEADS, D_NOPE + D_ROPE)
        kv = (rms_norm(dkv, d_kvnorm) @ d_wukv).reshape(bsz, n, D_HEADS, D_NOPE + D_V)
        q_nope, q_rope = q[..., :D_NOPE], q[..., D_NOPE:]
        k_nope, v = kv[..., :D_NOPE], kv[..., D_NOPE:]
        if positional:
            cos, sin = axial_rope(n, C_HEAD_DIM, h.dtype)
            cq, ck = apply_rope(cq, cos, sin), apply_rope(ck, cos, sin)
            cos, sin = axial_rope(n, D_ROPE, h.dtype)
            q_rope, k_rope = apply_rope(q_rope, cos, sin), apply_rope(k_rope, cos, sin)
        return cq, ck, cv, q_nope, q_rope, k_nope, k_rope, v

    cq_l, ck_l, cv_l, qn_l, qr_l, kn_l, kr_l, v_l = project(hl, True)
    cq_c, ck_c, cv_c, qn_c, qr_c, kn_c, kr_c, v_c = project(hc, False)
    y_win = window_attention(cq_l, ck_l, cv_l, ck_c, cv_c, c_sink)
    y_mla = latent_attention(qn_l, qr_l, jnp.concatenate([kn_c, kn_l], 1),
                             jnp.concatenate([kr_c, kr_l], 1), jnp.concatenate([v_c, v_l], 1), Q_BLOCK)
    yl = jnp.concatenate([y_win, y_mla], -1) @ w_out
    if ctx_out:
        yc_win = context_gqa(cq_c, ck_c, cv_c, c_sink)
        yc_mla = latent_attention(qn_c, qr_c, kn_c, kr_c, v_c, hc.shape[1])
        yc = jnp.concatenate([yc_win, yc_mla], -1) @ w_out
    else:
        yc = None
    return yl, yc


def moe(h, router_w, router_bias, w_gate, w_up, w_down):
    affinity = jax.nn.sigmoid(jnp.einsum('bld,de->ble', h, router_w).astype(F32))
    sel = affinity + router_bias.astype(F32)
    per_group = N_EXPERTS // N_GROUPS
    grp = sel.reshape(sel.shape[:-1] + (N_GROUPS, per_group))
    group_score = jnp.sum(lax.top_k(grp, 2)[0], -1)
    best = jnp.argmax(group_score, -1)
    in_group = (jnp.arange(N_EXPERTS) // per_group) == best[..., None]
    _, idx = lax.top_k(jnp.where(in_group, sel, -jnp.inf), TOP_K)
    wts = jnp.take_along_axis(affinity, idx, -1)
    wts = wts / jnp.sum(wts, -1, keepdims=True)
    gates = jnp.sum(jax.nn.one_hot(idx, N_EXPERTS, dtype=F32) * wts[..., None], -2).astype(h.dtype)
    out = jnp.zeros_like(h)
    for e in range(N_EXPERTS):
        act = jax.nn.silu(h @ w_gate[e]) * (h @ w_up[e])
        out = out + gates[..., e:e + 1] * (act @ w_down[e])
    return out


def setup_inputs(seed: int = 0) -> dict:
    key = jax.random.key(seed)
    ks = jax.random.split(key, 27)
    d = D_MODEL

    def nrm(i, shape, scale):
        return jax.random.normal(ks[i], shape, F32) * scale

    dt = jnp.exp(jax.random.uniform(ks[12], (N_EVEN, 2, B_HEADS), F32, math.log(1e-3), math.log(1e-1)))
    return {
        'x': nrm(0, (BATCH, SEQ, d), 1.0),
        'c': nrm(1, (BATCH, d), 1.0),
        'ctx': nrm(2, (BATCH, CTX_LEN, d), 1.0),
        'c_ctx': nrm(3, (d,), 1.0),
        'ada_w': nrm(4, (DEPTH, d, 6 * d), 0.5 * d ** -0.5),
        'ada_b': nrm(5, (DEPTH, 6 * d), 0.02),
        'ln_g': 1.0 + nrm(6, (DEPTH, 2, d), 0.02),
        'ln_b': nrm(7, (DEPTH, 2, d), 0.02),
        'ev_w_in': nrm(8, (N_EVEN, d, EVEN_IN), d ** -0.5),
        'ev_a_conv': nrm(9, (N_EVEN, A_CONV, A_WIDTH), A_CONV ** -0.5),
        'ev_b_conv': nrm(10, (N_EVEN, B_CONV, 3 * B_WIDTH), B_CONV ** -0.5),
        'ev_b_alog': jnp.log(jax.random.uniform(ks[11], (N_EVEN, 2, B_HEADS), F32, 1.0, 16.0)),
        'ev_b_dtbias': dt + jnp.log(-jnp.expm1(-dt)),
        'ev_b_norm': 1.0 + nrm(13, (N_EVEN, B_HEAD_DIM), 0.02),
        'ev_w_out': nrm(14, (N_EVEN, EVEN_OUT, d), DEEPNORM_BETA * EVEN_OUT ** -0.5),
        'od_w_in': nrm(15, (N_ODD, d, ODD_IN), d ** -0.5),
        'od_c_sink': nrm(16, (N_ODD, C_Q_HEADS), 1.0),
        'od_d_qnorm': 1.0 + nrm(17, (N_ODD, D_Q_RANK), 0.02),
        'od_d_kvnorm': 1.0 + nrm(18, (N_ODD, D_KV_RANK), 0.02),
        'od_d_wuq': nrm(19, (N_ODD, D_Q_RANK, D_HEADS * (D_NOPE + D_ROPE)), D_Q_RANK ** -0.5),
        'od_d_wukv': nrm(20, (N_ODD, D_KV_RANK, D_HEADS * (D_NOPE + D_V)), D_KV_RANK ** -0.5),
        'od_w_out': nrm(21, (N_ODD, ODD_OUT, d), DEEPNORM_BETA * ODD_OUT ** -0.5),
        'router_w': nrm(22, (d, N_EXPERTS), d ** -0.5),
        'router_bias': nrm(23, (N_EXPERTS,), 0.01),
        'moe_w_gate': nrm(24, (DEPTH, N_EXPERTS, d, D_EXPERT), d ** -0.5),
        'moe_w_up': nrm(25, (DEPTH, N_EXPERTS, d, D_EXPERT), d ** -0.5),
        'moe_w_down': nrm(26, (DEPTH, N_EXPERTS, D_EXPERT, d), DEEPNORM_BETA * D_EXPERT ** -0.5),
    }


def reference(x, c, ctx, c_ctx, ada_w, ada_b, ln_g, ln_b, ev_w_in, ev_a_conv, ev_b_conv, ev_b_alog,
              ev_b_dtbias, ev_b_norm, ev_w_out, od_w_in, od_c_sink, od_d_qnorm, od_d_kvnorm, od_d_wuq,
              od_d_wukv, od_w_out, router_w, router_bias, moe_w_gate, moe_w_up, moe_w_down):
    xl, xc = x, ctx
    for layer in range(DEPTH):
        last = layer == DEPTH - 1
        j = layer // 2
        mod_l = (jax.nn.silu(c) @ ada_w[layer] + ada_b[layer])[:, None, :]
        mod_c = (jax.nn.silu(c_ctx) @ ada_w[layer] + ada_b[layer])[None, None, :]
        sh1_l, sc1_l, g1_l, sh2_l, sc2_l, g2_l = jnp.split(mod_l, 6, -1)
        sh1_c, sc1_c, g1_c, sh2_c, sc2_c, g2_c = jnp.split(mod_c, 6, -1)
        hl = modulate(xl, sh1_l, sc1_l)
        hc = modulate(xc, sh1_c, sc1_c)
        if layer % 2 == 0:
            yl, yc = even_mixer(hl, hc, ev_w_in[j], ev_a_conv[j], ev_b_conv[j], ev_b_alog[j],
                                ev_b_dtbias[j], ev_b_norm[j], ev_w_out[j], not last)
        else:
            yl, yc = odd_mixer(hl, hc, od_w_in[j], od_c_sink[j], od_d_qnorm[j], od_d_kvnorm[j],
                               od_d_wuq[j], od_d_wukv[j], od_w_out[j], not last)
        xl = layer_norm(DEEPNORM_ALPHA * xl + g1_l * yl, ln_g[layer, 0], ln_b[layer, 0])
        hl = modulate(xl, sh2_l, sc2_l)
        if last:
            f_l = moe(hl, router_w, router_bias, moe_w_gate[layer], moe_w_up[layer], moe_w_down[layer])
        else:
            xc = layer_norm(DEEPNORM_ALPHA * xc + g1_c * yc, ln_g[layer, 0], ln_b[layer, 0])
            hc = modulate(xc, sh2_c, sc2_c)
            n_ctx = xc.shape[1]
            f_all = moe(jnp.concatenate([hc, hl], 1), router_w, router_bias,
                        moe_w_gate[layer], moe_w_up[layer], moe_w_down[layer])
            f_l = f_all[:, n_ctx:]
            xc = layer_norm(DEEPNORM_ALPHA * xc + g2_c * f_all[:, :n_ctx], ln_g[layer, 1], ln_b[layer, 1])
        xl = layer_norm(DEEPNORM_ALPHA * xl + g2_l * f_l, ln_g[layer, 1], ln_b[layer, 1])
    return xl
```

```python
from concourse.bass_utils import run_bass_kernel_spmd
import numpy as np
import concourse.bass as bass
import concourse.mybir as mybir
from contextlib import ExitStack

F32 = mybir.dt.float32
BF16 = mybir.dt.bfloat16
AF = mybir.ActivationFunctionType
ALU = mybir.AluOpType
AX = mybir.AxisListType

N_DMA_SEMS = 40
SEM_ROLL = 30000


class Tok:
    __slots__ = ("sem", "val", "eng")

    def __init__(self, sem, val, eng):
        self.sem = sem
        self.val = val
        self.eng = eng


class Sched:
    def __init__(self, nc, stack):
        self.nc = nc
        self.stack = stack
        self.cengs = ["pe", "act", "dve", "pool"]
        self.all = ["pe", "act", "dve", "pool", "sp"]
        self.prog = {e: [] for e in self.all}
        self.nsem = 0
        self.sem = {e: self._newsem() for e in self.cengs}
        self.cnt = {e: 0 for e in self.cengs}
        self.waited = {e: {} for e in self.all}
        self.dsem = [self._newsem() for _ in range(N_DMA_SEMS)]
        self.dcnt = [0] * N_DMA_SEMS
        self.drr = 0
        self.res = {}
        self.pend = {e: {} for e in self.all}
        self.excl = set()
        self.old = []

    def _newsem(self):
        self.nsem += 1
        return self.stack.enter_context(self.nc.semaphore("s%d" % self.nsem))

    def _st(self, r):
        if isinstance(r, tuple):
            k = tuple(x if isinstance(x, (str, int)) else id(x) for x in r)
        elif isinstance(r, str):
            k = r
        else:
            k = id(r)
        st = self.res.get(k)
        if st is None:
            st = {"w": None, "r": {}}
            self.res[k] = st
        return st

    def _emit(self, eng, fn, reads, writes, dma):
        if self.excl:
            ex = [r for r in reads if (not isinstance(r, (tuple, str))) and id(r) in self.excl]
            if ex:
                reads = [r for r in reads if not ((not isinstance(r, (tuple, str))) and id(r) in self.excl)]
                writes = list(writes) + ex
        deps = []
        for r in reads:
            st = self._st(r)
            if st["w"] is not None:
                deps.append(st["w"])
        for w in writes:
            st = self._st(w)
            if st["w"] is not None:
                deps.append(st["w"])
            deps.extend(st["r"].values())
        need = {}
        for t in deps:
            if t.eng == eng and eng == "pe" and not dma:
                continue
            cur = need.get(id(t.sem))
            if cur is None or cur[1] < t.val:
                need[id(t.sem)] = (t.sem, t.val)
        if self.pend[eng]:
            for sid, (s_, v_) in self.pend[eng].items():
                cur = need.get(sid)
                if cur is None or cur[1] < v_:
                    need[sid] = (s_, v_)
            self.pend[eng] = {}
        if dma:
            i = self.drr
            self.drr = (self.drr + 1) % N_DMA_SEMS
            prev = self.dcnt[i]
            if prev > 0:
                cur = need.get(id(self.dsem[i]))
                if cur is None or cur[1] < prev:
                    need[id(self.dsem[i])] = (self.dsem[i], prev)
            self.dcnt[i] += 16
            tok = Tok(self.dsem[i], self.dcnt[i], "dma")
            inc = (self.dsem[i], 16)
        else:
            if self.cnt[eng] >= SEM_ROLL:
                self.old.append((self.sem[eng], self.cnt[eng]))
                self.sem[eng] = self._newsem()
                self.cnt[eng] = 0
            self.cnt[eng] += 1
            tok = Tok(self.sem[eng], self.cnt[eng], eng)
            inc = (self.sem[eng], 1)
        waits = []
        wd = self.waited[eng]
        for sid, (s, v) in need.items():
            if wd.get(sid, 0) >= v:
                continue
            wd[sid] = v
            waits.append((s, v))
        for r in reads:
            self._st(r)["r"][id(tok.sem)] = tok
        for w in writes:
            st = self._st(w)
            st["w"] = tok
            st["r"] = {}
        self.prog[eng].append((waits, fn, inc))
        return tok

    def op(self, eng, fn, reads=(), writes=()):
        return self._emit(eng, fn, reads, writes, False)

    def all_tokens(self):
        toks = {}
        for e in self.cengs:
            if self.cnt[e] > 0:
                toks[id(self.sem[e])] = (self.sem[e], self.cnt[e])
        for i in range(N_DMA_SEMS):
            if self.dcnt[i] > 0:
                toks[id(self.dsem[i])] = (self.dsem[i], self.dcnt[i])
        return toks

    def barrier(self):
        toks = self.all_tokens()
        for e in self.all:
            self.pend[e] = dict(toks)
        self.res = {}

    def dma(self, out, in_, reads=(), writes=(), q="sp", **kw):
        return self._emit(q, lambda e: e.dma_start(out=out, in_=in_, **kw), reads, writes, True)

    def mm(self, out, lhsT, rhs, start, stop, reads=(), writes=()):
        return self.op("pe", lambda e: e.matmul(out, lhsT, rhs, start=start, stop=stop), reads, writes)

    def tr(self, out, in_, ident, reads=(), writes=()):
        return self.op("pe", lambda e: e.transpose(out, in_, ident), reads, writes)

    def act(self, out, in_, func, bias=None, scale=None, accum_out=None, reads=(), writes=(), eng="act"):
        kw = {}
        if bias is not None:
            kw["bias"] = bias
        if scale is not None:
            kw["scale"] = scale
        if accum_out is not None:
            kw["accum_out"] = accum_out
        return self.op(eng, lambda e: e.activation(out, in_, func, **kw), reads, writes)

    def tt(self, eng, out, in0, in1, op, reads=(), writes=()):
        return self.op(eng, lambda e: e.tensor_tensor(out, in0, in1, op), reads, writes)

    def ts(self, eng, out, in0, s1, s2, op0, op1=None, reads=(), writes=(), accum_out=None):
        kw = {}
        if accum_out is not None:
            kw["accum_out"] = accum_out
        if op1 is None:
            return self.op(eng, lambda e: e.tensor_scalar(out, in0, s1, None, op0, **kw), reads, writes)
        return self.op(eng, lambda e: e.tensor_scalar(out, in0, s1, s2, op0, op1, **kw), reads, writes)

    def stt(self, eng, out, in0, scalar, in1, op0, op1, reads=(), writes=()):
        return self.op(eng, lambda e: e.scalar_tensor_tensor(out, in0, scalar, in1, op0, op1), reads, writes)

    def copy(self, eng, out, in_, reads=(), writes=()):
        if eng == "act":
            return self.op(eng, lambda e: e.copy(out, in_), reads, writes)
        return self.op(eng, lambda e: e.tensor_copy(out, in_), reads, writes)

    def memset(self, eng, ap, val, writes=()):
        return self.op(eng, lambda e: e.memset(ap, val), (), writes)

    def finish(self, final_tokens):
        nc = self.nc
        prog = self.prog
        engmap = {"pe": "tensor", "act": "scalar", "dve": "vector", "pool": "gpsimd", "sp": "sync"}
        fin = self.all_tokens()
        with nc.Block() as block:
            for ename in self.all:
                entries = prog[ename]
                is_sp = ename == "sp"

                def body(e, entries=entries, is_sp=is_sp):
                    for waits, fn, inc in entries:
                        for wi_, (s, v) in enumerate(waits):
                            e.wait_ge(s, v)
                            if wi_ + 1 < len(waits):
                                e.nop(nofuse=True)
                        ins = fn(e)
                        ins.then_inc(inc[0], inc[1])
                    if is_sp:
                        for (s, v) in fin.values():
                            e.wait_ge(s, v)

                getattr(block, engmap[ename])(body)


D = 1024
T = 2304
NT = 18
LT = 2048
ALPHA = (2.0 * 2) ** 0.25
NEG = -30000.0


def tcol(ti):
    return 1 + 128 * ti if ti < 2 else 2 + 128 * ti


BLOCKS = [(1, 256, 0)] + [(258 + 512 * j, 512, 256 + 512 * j) for j in range(4)]


ORDER = ['pw', 'pm', 'A', 'B', 'C', 'D', 'E', 'F', 'G', 'L1A', 'L1P', 'L1W', 'L1WA', 'L1U', 'L1MA', 'L1E', 'L1F', 'L1']


def build(NB, dbg=False, only0=False, stop='L1'):
    def want(nm):
        return ORDER.index(nm) <= ORDER.index(stop)

    nc = bass.Bass("TRN2", target_bir_lowering=False)
    R = NB + 1
    dd = {}

    def din(name, shape, dt=F32):
        dd[name] = nc.dram_tensor(name, list(shape), dt, kind="ExternalInput").ap()
        return dd[name]

    def dscr(name, shape, dt=F32, out=False):
        return nc.dram_tensor(name, list(shape), dt, kind="ExternalOutput" if out else "Internal").ap()

    x_d = din("x", [NB, LT, D]); ctx_d = din("ctx", [NB, 256, D]); csT_d = din("csT", [128, 8, R])
    adaw_d = din("ada_w", [2, D, 6144]); adab_d = din("ada_b", [2, 6144])
    lng_d = din("ln_g", [4, D]); lnb_d = din("ln_b", [4, D])
    evwin_d = din("ev_w_in", [D, 3600]); aconvT_d = din("a_convT", [128, 4, 3]); bconv_d = din("ev_b_conv", [3, 1536])
    alog_d = din("alog", [1, 8]); dtb_d = din("dtbias", [1, 8]); bnorm_d = din("bnorm", [1, 128]); evwout_d = din("ev_w_out", [D, D])
    odwin_d = din("od_w_in", [D, 1440]); odwinsw_d = din("od_w_in_sw", [D, 1440])
    wkr_d = din("wkr", [D, 96]); wkrsw_d = din("wkr_sw", [D, 96])
    sink_d = din("sink", [1, 8]); qnT_d = din("qnormT", [128, 3]); kvnT_d = din("kvnormT", [128, 2])
    wuq_d = din("wuq", [384, 768]); wuqsw_d = din("wuq_sw", [384, 768]); wukv_d = din("wukv", [256, 1024]); odwout_d = din("od_w_out", [D, D])
    rw_d = din("router_w", [D, 16]); rb_d = din("router_bias", [1, 16])
    mg_d = din("moe_g", [32768, 512]); mu_d = din("moe_u", [32768, 512]); md_d = din("moe_d", [16384, 1024])
    idn_d = din("idn", [128, 128]); masks_d = din("masks", [14, 128, 128])
    rope64_d = din("rope64", [2, 64, LT]); rope32_d = din("rope32", [2, 96, LT])
    out_d = dscr("out", [NB, LT, D], F32, out=True)

    evwin_b = dscr("evwin_b", [D, 3600], BF16); wB_b = [dscr("wB%d_b" % j, [D, 1536], BF16) for j in range(3)]
    evwout_b = dscr("evwout_b", [D, D], BF16)
    odwin_b = dscr("odwin_b", [D, 1440], BF16); odwinsw_b = dscr("odwinsw_b", [D, 1440], BF16)
    wkr_b = dscr("wkr_b", [D, 96], BF16); wkrsw_b = dscr("wkrsw_b", [D, 96], BF16)
    wuq_b = dscr("wuq_b", [384, 768], BF16); wuqsw_b = dscr("wuqsw_b", [384, 768], BF16); wukv_b = dscr("wukv_b", [256, 1024], BF16)
    odwout_b = dscr("odwout_b", [D, D], BF16)
    mg_b = dscr("mg_b", [32768, 512], BF16); mu_b = dscr("mu_b", [32768, 512], BF16); md_b = dscr("md_b", [16384, 1024], BF16)
    modrow_s = dscr("modrow_s", [2, R, 6144], F32, out=dbg)
    qT_s = dscr("qT_s", [4, 128, T], BF16); kT_s = dscr("kT_s", [4, 128, T], BF16)
    k_s = dscr("k_s", [T, 512], BF16); v_s = dscr("v_s", [T, 512], BF16); gate_s = dscr("gate_s", [T, 512]); o_s = dscr("o_s", [T, 512])
    xa_s = dscr("xa_s", [NB, 2, T, D], F32, out=dbg)
    xb_s = dscr("xb_s", [NB, T, D], F32, out=dbg)
    h2T_s = dscr("h2T_s", [8, 128, T], BF16)
    cat_dbg = dscr("cat_dbg", [128, 8, LT], BF16, out=True) if dbg else None
    ymix_s = dscr("ymix_s", [NB, 2, T, D], F32, out=dbg) if dbg else None

    with ExitStack() as st0:
        S = Sched(nc, st0)
        cnt = [0]

        def sb(stack, shape, dt=F32, name=None):
            cnt[0] += 1
            return stack.enter_context(nc.sbuf_tensor("%s_%d" % (name or "t", cnt[0]), list(shape), dt))

        PB = [st0.enter_context(nc.psum_tensor("pb%d" % i, [128, 512], F32)) for i in range(8)]
        pbi = [0]
        S.excl = set(id(p_) for p_ in PB)

        def pb():
            p = PB[pbi[0] % 8]
            pbi[0] += 1
            return p

        ld_rr = [0]

        def rr3():
            ld_rr[0] += 1
            return ("dve", "pool", "act")[ld_rr[0] % 3]

        ident = sb(st0, [128, 128], name="ident"); S.dma(ident[:], idn_d, writes=[ident])
        ones = sb(st0, [128, 128], name="ones"); S.memset("pool", ones[:], 1.0, writes=[ones])
        MK = sb(st0, [128, 14, 128], name="masks")
        S.dma(MK[:], masks_d.rearrange("m p f -> p m f"), writes=[MK])
        modT = sb(st0, [128, 2, 48, R], name="modT")
        MKb = sb(st0, [128, 14, 128], BF16, name="masksb"); S.copy("dve", MKb[:], MK[:], reads=[MK], writes=[MKb])
        identb = sb(st0, [128, 128], BF16, name="identb"); S.copy("dve", identb[:], ident[:], reads=[ident], writes=[identb])
        onesb = sb(st0, [128, 128], BF16, name="onesb"); S.memset("pool", onesb[:], 1.0, writes=[onesb])
        rwt = sb(st0, [128, 8, 16], name="rw"); S.dma(rwt[:], rw_d.rearrange("(kc p) e -> p kc e", p=128), writes=[rwt])
        rbias = sb(st0, [128, 16], name="rbias"); S.dma(rbias[:], rb_d.to_broadcast([128, 16]), writes=[rbias])
        aconvT = sb(st0, [128, 4, 3], name="aconvT"); S.dma(aconvT[:], aconvT_d, writes=[aconvT])
        negexpA = sb(st0, [128, 8], name="negexpA"); dtb = sb(st0, [128, 8], name="dtb")
        S.dma(negexpA[:], alog_d.to_broadcast([128, 8]), writes=[negexpA]); S.dma(dtb[:], dtb_d.to_broadcast([128, 8]), writes=[dtb])
        S.act(negexpA[:], negexpA[:], AF.Exp, reads=[negexpA], writes=[negexpA])
        S.ts("dve", negexpA[:], negexpA[:], -1.0, None, ALU.mult, reads=[negexpA], writes=[negexpA])
        bnorm = sb(st0, [128, 128], name="bnorm"); S.dma(bnorm[:], bnorm_d.to_broadcast([128, 128]), writes=[bnorm])
        expsink = sb(st0, [128, 8], name="expsink"); S.dma(expsink[:], sink_d.to_broadcast([128, 8]), writes=[expsink])
        S.act(expsink[:], expsink[:], AF.Exp, reads=[expsink], writes=[expsink])
        qnT = sb(st0, [128, 3], name="qnT"); S.dma(qnT[:], qnT_d, writes=[qnT])
        kvnT = sb(st0, [128, 2], name="kvnT"); S.dma(kvnT[:], kvnT_d, writes=[kvnT])

        with ExitStack() as st:
            stg = [sb(st, [128, 4096], F32, "stg") for _ in range(3)]
            stgb = [sb(st, [128, 4096], BF16, "stgb") for _ in range(3)]
            bcv = sb(st, [128, 3, 1536], F32, "bcv")
            for j in range(3):
                S.dma(bcv[:, j, :], bconv_d[j:j + 1, :].to_broadcast([128, 1536]), writes=[(bcv, j)])
            ui = [0]

            def conv(src, dst, rows, cols, scale=None):
                nrc = rows // 128
                G = max(1, min(nrc, 4096 // cols)) if cols <= 4096 else 1
                if scale is not None:
                    G = 1
                while nrc % G:
                    G -= 1
                for r0 in range(0, nrc, G):
                    i = ui[0] % 3
                    ui[0] += 1
                    eng = ("dve", "pool", "act")[i]
                    a = stg[i][:, 0:G * cols].rearrange("p (g c) -> p g c", g=G)
                    b = stgb[i][:, 0:G * cols].rearrange("p (g c) -> p g c", g=G)
                    sv = src[r0 * 128:(r0 + G) * 128, :].rearrange("(g p) c -> p g c", p=128)
                    dv = dst[r0 * 128:(r0 + G) * 128, :].rearrange("(g p) c -> p g c", p=128)
                    S.dma(a, sv, writes=[stg[i]])
                    if scale is None:
                        S.copy(eng, b, a, reads=[stg[i]], writes=[stgb[i]])
                    else:
                        e2 = "pool" if eng == "act" else eng
                        S.tt(e2, b[:, 0, :], a[:, 0, :], scale, ALU.mult, reads=[stg[i], (bcv, 0), (bcv, 1), (bcv, 2)], writes=[stgb[i]])
                    S.dma(dv, b, reads=[stgb[i]])

            conv(evwin_d, evwin_b, D, 3600)
            for j in range(3):
                conv(evwin_d[:, 1536:3072], wB_b[j], D, 1536, scale=bcv[:, j, :])
            conv(evwout_d, evwout_b, D, D)
            conv(odwin_d, odwin_b, D, 1440); conv(odwinsw_d, odwinsw_b, D, 1440)
            conv(wkr_d, wkr_b, D, 96); conv(wkrsw_d, wkrsw_b, D, 96)
            conv(wuq_d, wuq_b, 384, 768); conv(wuqsw_d, wuqsw_b, 384, 768); conv(wukv_d, wukv_b, 256, 1024)
            conv(odwout_d, odwout_b, D, D)
            S.barrier()
        with ExitStack() as st:
          if want('pm'):
            csT = sb(st, [128, 8, R], F32, "csT")
            S.dma(csT[:], csT_d, writes=[csT])
            S.act(csT[:], csT[:], AF.Silu, reads=[csT], writes=[csT])
            awt = [sb(st, [128, 8, 1536], F32, "awt") for _ in range(2)]
            modrow = sb(st, [R, 6144], F32, "modrow")
            abr = sb(st, [R, 6144], F32, "abr")
            for l in range(2):
                S.dma(abr[:], adab_d[l:l + 1, :].to_broadcast([R, 6144]), reads=[modrow], writes=[abr])
                for q in range(4):
                    aw = awt[q % 2]
                    S.dma(aw[:], adaw_d[l, :, q * 1536:(q + 1) * 1536].rearrange("(kc p) n -> p kc n", p=128), writes=[aw])
                    for nb_ in range(3):
                        p = pb()
                        c0 = q * 1536 + nb_ * 512
                        for kc in range(8):
                            S.mm(p[0:R, :], csT[:, kc, :], aw[:, kc, nb_ * 512:(nb_ + 1) * 512], kc == 0, kc == 7, reads=[csT, aw], writes=[p])
                        S.tt("dve", modrow[:, c0:c0 + 512], p[0:R, :], abr[:, c0:c0 + 512], ALU.add, reads=[p, abr], writes=[modrow])
                S.dma(modrow_s[l], modrow[:], reads=[modrow])
                p = pb()
                for ch in range(48):
                    S.tr(p[:, ch * R:(ch + 1) * R], modrow[0:R, ch * 128:(ch + 1) * 128], ident[0:R, 0:R], reads=[modrow, ident], writes=[p])
                S.copy("dve", modT[:, l, :, :], p[:, 0:48 * R].rearrange("p (c r) -> p c r", r=R), reads=[p], writes=[modT])
                for c0 in (8, 32):
                    S.ts("dve", modT[:, l, c0:c0 + 8, :], modT[:, l, c0:c0 + 8, :], 1.0, None, ALU.add, reads=[modT], writes=[modT])
            S.barrier()

        bgs = {"gen": None}

        def moe_conv_gen(stg, stgb):
            ui = 0
            for (src, dst, rows, cols) in ((mg_d, mg_b, 32768, 512), (mu_d, mu_b, 32768, 512), (md_d, md_b, 16384, 1024)):
                G = 2048 // cols
                for r0 in range(0, rows // 128, G):
                    i = ui % len(stg)
                    ui += 1
                    a = stg[i][:, 0:2048].rearrange("p (g c) -> p g c", g=G)
                    bq = stgb[i][:, 0:2048].rearrange("p (g c) -> p g c", g=G)
                    sv = src[r0 * 128:(r0 + G) * 128, :].rearrange("(g p) c -> p g c", p=128)
                    dv = dst[r0 * 128:(r0 + G) * 128, :].rearrange("(g p) c -> p g c", p=128)
                    S.dma(a, sv, writes=[stg[i]])
                    S.copy("pool", bq, a, reads=[stg[i]], writes=[stgb[i]])
                    S.dma(dv, bq, reads=[stgb[i]])
                    yield

        def bg_step(k=1):
            for _ in range(k):
                g_ = bgs["gen"]
                if g_ is None:
                    return
                try:
                    next(g_)
                except StopIteration:
                    bgs["gen"] = None
                    return

        def xsrc(layer, b, ti):
            if layer == 0:
                return ctx_d[b, ti * 128:(ti + 1) * 128, :] if ti < 2 else x_d[b, (ti - 2) * 128:(ti - 1) * 128, :]
            return xb_s[b, ti * 128:(ti + 1) * 128, :]

        def phase_A(layer, b, hT, tiles, st):
            xin = [sb(st, [128, D], F32, "xin") for _ in range(2)]
            for n, ti in enumerate(tiles):
                xt = xin[n % 2]
                r = NB if ti < 2 else b
                bg_step(1)
                S.dma(xt[:], xsrc(layer, b, ti), writes=[xt])
                for half in range(2):
                    p = pb()
                    for k4 in range(4):
                        kc = half * 4 + k4
                        S.tr(p[:, k4 * 128:(k4 + 1) * 128], xt[:, kc * 128:(kc + 1) * 128], ident[:], reads=[xt], writes=[p])
                    for k4 in range(4):
                        kc = half * 4 + k4
                        dst = hT[:, kc, tcol(ti):tcol(ti) + 128]
                        if half == 0:
                            S.act(dst, p[:, k4 * 128:(k4 + 1) * 128], AF.Identity, bias=modT[:, layer, kc, r:r + 1],
                                  scale=modT[:, layer, 8 + kc, r:r + 1], reads=[p], writes=[(hT, ti, kc)])
                        else:
                            S.ts("dve", dst, p[:, k4 * 128:(k4 + 1) * 128], modT[:, layer, 8 + kc, r:r + 1], modT[:, layer, kc, r:r + 1],
                                 ALU.mult, ALU.add, reads=[p], writes=[(hT, ti, kc)])

        def layer_norm_tile(st_tiles, rt, lng, lnb, outt):
            stats, ag, sm = st_tiles
            for hf in range(2):
                S.op("dve", lambda e, hf=hf: e.bn_stats(stats[:, hf, :], rt[:, hf * 512:(hf + 1) * 512]), reads=[rt], writes=[stats])
            S.op("dve", lambda e: e.bn_aggr(ag[:], stats[:].rearrange('p a b -> p (a b)')), reads=[stats], writes=[ag])
            S.act(sm[:, 0:1], ag[:, 1:2], AF.Sqrt, bias=1e-5, reads=[ag], writes=[sm])
            S.op("dve", lambda e: e.reciprocal(sm[:, 1:2], sm[:, 0:1]), reads=[sm], writes=[sm])
            S.stt("dve", sm[:, 2:3], ag[:, 0:1], -1.0, sm[:, 1:2], ALU.mult, ALU.mult, reads=[ag, sm], writes=[sm])
            S.act(rt[:], rt[:], AF.Identity, bias=sm[:, 2:3], scale=sm[:, 1:2], reads=[rt, sm], writes=[rt])
            S.tt("pool", rt[:], rt[:], lng[:], ALU.mult, reads=[rt, lng], writes=[rt])
            S.tt("pool", outt[:], rt[:], lnb[:], ALU.add, reads=[rt, lnb], writes=[outt])

        def run_pipe(gens, depth):
            active = []
            it = iter(gens)
            fin = False
            while True:
                while len(active) < depth and not fin:
                    try:
                        active.append(next(it))
                    except StopIteration:
                        fin = True
                if not active:
                    break
                for g_ in list(active):
                    try:
                        next(g_)
                    except StopIteration:
                        active.remove(g_)

        def ln_stats(rt, stats, ag, sm):
            for hf in range(2):
                S.op("dve", lambda e, hf=hf: e.bn_stats(stats[:, hf, :], rt[:, hf * 512:(hf + 1) * 512]), reads=[rt], writes=[stats])
            S.op("dve", lambda e: e.bn_aggr(ag[:], stats[:].rearrange('p a b -> p (a b)')), reads=[stats], writes=[ag])
            S.act(sm[:, 0:1], ag[:, 1:2], AF.Sqrt, bias=1e-5, reads=[ag], writes=[sm])
            S.op("dve", lambda e: e.reciprocal(sm[:, 1:2], sm[:, 0:1]), reads=[sm], writes=[sm])
            S.stt("dve", sm[:, 2:3], ag[:, 0:1], -1.0, sm[:, 1:2], ALU.mult, ALU.mult, reads=[ag, sm], writes=[sm])

        def ln_apply(rt, sm, lng, lnb, outt):
            S.act(rt[:], rt[:], AF.Identity, bias=sm[:, 2:3], scale=sm[:, 1:2], reads=[rt, sm], writes=[rt])
            S.tt("pool", rt[:], rt[:], lng[:], ALU.mult, reads=[rt, lng], writes=[rt])
            S.tt("pool", outt[:], rt[:], lnb[:], ALU.add, reads=[rt, lnb], writes=[outt])

        def phase_E(layer, b, catT, tiles, gates, st):
            nt = len(tiles)
            NBUF = 4
            wo = sb(st, [128, 8, D], BF16, "wo")
            wsrc = evwout_b if layer == 0 else odwout_b
            S.dma(wo[:], wsrc.rearrange("(kc p) n -> p kc n", p=128), writes=[wo])
            lng = sb(st, [128, D], F32, "lng"); lnb = sb(st, [128, D], F32, "lnb")
            S.dma(lng[:], lng_d[2 * layer:2 * layer + 1, :].to_broadcast([128, D]), writes=[lng])
            S.dma(lnb[:], lnb_d[2 * layer:2 * layer + 1, :].to_broadcast([128, D]), writes=[lnb])
            g1 = [sb(st, [128, D], F32, "g1") for _ in range(2)]
            S.dma(g1[0][:], modrow_s[layer, NB:NB + 1, 2048:3072].to_broadcast([128, D]), writes=[g1[0]])
            S.dma(g1[1][:], modrow_s[layer, b:b + 1, 2048:3072].to_broadcast([128, D]), writes=[g1[1]])
            xin = [sb(st, [128, D], F32, "xin") for _ in range(NBUF)]
            rts = [sb(st, [128, D], F32, "rt") for _ in range(NBUF)]
            xns = [sb(st, [128, D], F32, "xn") for _ in range(NBUF)]
            h2f = [sb(st, [128, 8, 128], F32, "h2f") for _ in range(NBUF)]
            h2b = [sb(st, [128, 8, 128], BF16, "h2b") for _ in range(NBUF)]
            statsL = [sb(st, [128, 2, 6], F32, "stats") for _ in range(NBUF)]
            agL = [sb(st, [128, 2], F32, "ag") for _ in range(NBUF)]
            smL = [sb(st, [128, 4], F32, "sm") for _ in range(NBUF)]
            aff = sb(st, [128, nt, 16], F32, "aff")

            def tile_gen(n, ti):
                k = n % NBUF
                xt = xin[k]; rt = rts[k]; xn = xns[k]; hf_ = h2f[k]; hb_ = h2b[k]; stats = statsL[k]; ag = agL[k]; sm = smL[k]
                r = NB if ti < 2 else b
                gt = g1[0] if ti < 2 else g1[1]
                bg_step(2)
                S.dma(xt[:], xsrc(layer, b, ti), writes=[xt])
                c0 = tcol(ti) if layer == 0 else (ti - 2) * 128
                for hf in range(2):
                    p = pb()
                    for kc in range(8):
                        S.mm(p[:], catT[:, kc, c0:c0 + 128], wo[:, kc, hf * 512:(hf + 1) * 512], kc == 0, kc == 7, reads=[wo], writes=[p])
                    if dbg:
                        S.copy("act", rt[:, hf * 512:(hf + 1) * 512], p[:], reads=[p], writes=[rt])
                        S.dma(ymix_s[b, layer, ti * 128:(ti + 1) * 128, hf * 512:(hf + 1) * 512], rt[:, hf * 512:(hf + 1) * 512], reads=[rt])
                    S.tt("dve", rt[:, hf * 512:(hf + 1) * 512], p[:], gt[:, hf * 512:(hf + 1) * 512], ALU.mult, reads=[p, gt], writes=[rt])
                yield
                S.stt("dve", rt[:], xt[:], ALPHA, rt[:], ALU.mult, ALU.add, reads=[xt, rt], writes=[rt])
                ln_stats(rt, stats, ag, sm)
                yield
                ln_apply(rt, sm, lng, lnb, xn)
                S.dma(xa_s[b, layer, ti * 128:(ti + 1) * 128, :], xn[:], reads=[xn])
                yield
                for half in range(2):
                    p = pb()
                    for k4 in range(4):
                        kc = half * 4 + k4
                        S.tr(p[:, k4 * 128:(k4 + 1) * 128], xn[:, kc * 128:(kc + 1) * 128], ident[:], reads=[xn], writes=[p])
                    for k4 in range(4):
                        kc = half * 4 + k4
                        if half == 0:
                            S.act(hf_[:, kc, :], p[:, k4 * 128:(k4 + 1) * 128], AF.Identity, bias=modT[:, layer, 24 + kc, r:r + 1],
                                  scale=modT[:, layer, 32 + kc, r:r + 1], reads=[p], writes=[hf_])
                        else:
                            S.ts("dve", hf_[:, kc, :], p[:, k4 * 128:(k4 + 1) * 128], modT[:, layer, 32 + kc, r:r + 1], modT[:, layer, 24 + kc, r:r + 1],
                                 ALU.mult, ALU.add, reads=[p], writes=[hf_])
                yield
                S.copy("pool", hb_[:], hf_[:], reads=[hf_], writes=[hb_])
                S.dma(h2T_s.rearrange("k p t -> p k t")[:, :, n * 128:(n + 1) * 128], hb_[:], reads=[hb_])
                p = pb()
                for kc in range(8):
                    S.mm(p[:, 0:16], hf_[:, kc, :], rwt[:, kc, :], kc == 0, kc == 7, reads=[hf_], writes=[p])
                S.act(aff[:, n, :], p[:, 0:16], AF.Sigmoid, reads=[p], writes=[(aff, n)])

            run_pipe((tile_gen(n, ti) for n, ti in enumerate(tiles)), 3)
            router_batched(aff, gates, nt, st)

        def router_batched(aff, gates, nt, st):
            G4 = nt * 4
            sel = sb(st, [128, nt, 16], F32, "r_sel"); t1 = sb(st, [128, nt, 16], F32, "r_t1"); t2 = sb(st, [128, nt, 16], F32, "r_t2")
            m1 = sb(st, [128, G4], F32, "r_m1"); sec = sb(st, [128, G4], F32, "r_sec"); gs = sb(st, [128, G4], F32, "r_gs")
            gm = sb(st, [128, G4], F32, "r_gm"); tm = sb(st, [128, G4], F32, "r_tm")
            s1 = sb(st, [128, nt], F32, "r_s1"); s2 = sb(st, [128, nt], F32, "r_s2"); den = sb(st, [128, nt], F32, "r_den")
            allaff = [(aff, n) for n in range(nt)]
            RS = "rsres"

            def g4(t):
                return t[:].rearrange("p n (g e) -> p (n g) e", g=4)

            def bc44(t):
                return t[:].unsqueeze(2).to_broadcast([128, G4, 4])

            def bc16(t):
                return t[:].unsqueeze(2).to_broadcast([128, nt, 16])

            def dv(fn, *a, **k):
                return S.op("dve", fn, reads=allaff + [RS], writes=[RS])
            dv(lambda e: e.tensor_tensor(sel[:], aff[:], rbias[:].unsqueeze(1).to_broadcast([128, nt, 16]), ALU.add))
            dv(lambda e: e.tensor_reduce(m1[:], g4(sel), AX.X, ALU.max))
            dv(lambda e: e.tensor_tensor(g4(t1), g4(sel), bc44(m1), ALU.is_lt))
            dv(lambda e: e.tensor_scalar(t2[:], t1[:], 1.0, 1e9, ALU.subtract, ALU.mult))
            dv(lambda e: e.tensor_tensor(t1[:], t1[:], sel[:], ALU.mult))
            dv(lambda e: e.tensor_tensor(t2[:], t2[:], t1[:], ALU.add))
            dv(lambda e: e.tensor_reduce(sec[:], g4(t2), AX.X, ALU.max))
            dv(lambda e: e.tensor_tensor(gs[:], m1[:], sec[:], ALU.add))
            gs3 = gs[:].rearrange("p (n g) -> p n g", g=4)
            dv(lambda e: e.tensor_reduce(s1[:], gs3, AX.X, ALU.max))
            dv(lambda e: e.tensor_tensor(gm[:].rearrange("p (n g) -> p n g", g=4), gs3, s1[:].unsqueeze(2).to_broadcast([128, nt, 4]), ALU.is_ge))
            dv(lambda e: e.tensor_tensor(g4(t1), g4(sel), bc44(gm), ALU.mult))
            dv(lambda e: e.tensor_scalar(tm[:], gm[:], 1.0, 1e9, ALU.subtract, ALU.mult))
            dv(lambda e: e.tensor_tensor(g4(t1), g4(t1), bc44(tm), ALU.add))
            dv(lambda e: e.tensor_reduce(s1[:], t1[:], AX.X, ALU.max))
            dv(lambda e: e.tensor_tensor(t2[:], t1[:], bc16(s1), ALU.is_lt))
            dv(lambda e: e.tensor_tensor(sel[:], t1[:], t2[:], ALU.mult))
            dv(lambda e: e.tensor_scalar(t2[:], t2[:], 1.0, 1e9, ALU.subtract, ALU.mult))
            dv(lambda e: e.tensor_tensor(sel[:], sel[:], t2[:], ALU.add))
            dv(lambda e: e.tensor_reduce(s2[:], sel[:], AX.X, ALU.max))
            dv(lambda e: e.tensor_tensor(t2[:], t1[:], bc16(s2), ALU.is_ge))
            dv(lambda e: e.tensor_tensor(t2[:], t2[:], aff[:], ALU.mult))
            dv(lambda e: e.tensor_reduce(den[:], t2[:], AX.X, ALU.add))
            dv(lambda e: e.reciprocal(den[:], den[:]))
            S.op("dve", lambda e: e.tensor_tensor(gates[:], t2[:], bc16(den), ALU.mult), reads=[RS], writes=[gates])

        def phase_F(layer, b, h2T, gates, ntile, outacc, st):
            wgu = [sb(st, [128, 2, 8, 512], BF16, "wgu") for _ in range(2)]
            wdn = [sb(st, [128, 4, D], BF16, "wdn") for _ in range(2)]
            actT = [sb(st, [128, 4, 512], BF16, "actT") for _ in range(2)]
            sl = [sb(st, [128, 512], F32, "sl") for _ in range(2)]
            ntok = ntile * 128
            blocks = [(c, min(512, ntok - c)) for c in range(0, ntok, 512)]
            it = 0
            for e in range(16):
                wg = wgu[e % 2]; wd = wdn[e % 2]
                r0 = (layer * 16 + e) * 1024
                S.dma(wg[:, 0], mg_b[r0:r0 + 1024, :].rearrange("(kc p) f -> p kc f", p=128), writes=[wg])
                S.dma(wg[:, 1], mu_b[r0:r0 + 1024, :].rearrange("(kc p) f -> p kc f", p=128), writes=[wg])
                r1 = (layer * 16 + e) * 512
                S.dma(wd[:], md_b[r1:r1 + 512, :].rearrange("(fc p) n -> p fc n", p=128), writes=[wd])
                for (c0, w) in blocks:
                    at = actT[it % 2]; it += 1
                    for fc in range(4):
                        pg = pb(); pu = pb()
                        for kc in range(8):
                            S.mm(pg[:, 0:w], wg[:, 0, kc, fc * 128:(fc + 1) * 128], h2T[:, kc, c0:c0 + w], kc == 0, kc == 7, reads=[wg], writes=[pg])
                        for kc in range(8):
                            S.mm(pu[:, 0:w], wg[:, 1, kc, fc * 128:(fc + 1) * 128], h2T[:, kc, c0:c0 + w], kc == 0, kc == 7, reads=[wg], writes=[pu])
                        s_ = sl[fc % 2]
                        S.act(s_[:, 0:w], pg[:, 0:w], AF.Silu, reads=[pg], writes=[s_])
                        S.tt("dve", at[:, fc, 0:w], s_[:, 0:w], pu[:, 0:w], ALU.mult, reads=[s_, pu], writes=[(at, fc)])
                    for j in range(w // 128):
                        n = c0 // 128 + j
                        for hf in range(2):
                            pd = pb()
                            for fc in range(4):
                                S.mm(pd[:], at[:, fc, j * 128:(j + 1) * 128], wd[:, fc, hf * 512:(hf + 1) * 512], fc == 0, fc == 3,
                                     reads=[wd, (at, 0), (at, 1), (at, 2), (at, 3)], writes=[pd])
                            dst = outacc[:, n, hf * 512:(hf + 1) * 512]
                            if e == 0:
                                S.ts("dve", dst, pd[:], gates[:, n, e:e + 1], None, ALU.mult, reads=[pd], writes=[(outacc, n, hf)])
                            else:
                                S.stt("dve", dst, pd[:], gates[:, n, e:e + 1], dst, ALU.mult, ALU.add, reads=[pd], writes=[(outacc, n, hf)])

        def phase_G(layer, b, tiles, outacc, st):
            NBUF = 4
            lng = sb(st, [128, D], F32, "lng2"); lnb = sb(st, [128, D], F32, "lnb2")
            S.dma(lng[:], lng_d[2 * layer + 1:2 * layer + 2, :].to_broadcast([128, D]), writes=[lng])
            S.dma(lnb[:], lnb_d[2 * layer + 1:2 * layer + 2, :].to_broadcast([128, D]), writes=[lnb])
            g2 = [sb(st, [128, D], F32, "g2") for _ in range(2)]
            S.dma(g2[0][:], modrow_s[layer, NB:NB + 1, 5120:6144].to_broadcast([128, D]), writes=[g2[0]])
            S.dma(g2[1][:], modrow_s[layer, b:b + 1, 5120:6144].to_broadcast([128, D]), writes=[g2[1]])
            xin = [sb(st, [128, D], F32, "xin2") for _ in range(NBUF)]
            rts = [sb(st, [128, D], F32, "rt2") for _ in range(NBUF)]
            xns = [sb(st, [128, D], F32, "xn2") for _ in range(NBUF)]
            statsL = [sb(st, [128, 2, 6], F32, "stats2") for _ in range(NBUF)]
            agL = [sb(st, [128, 2], F32, "ag2") for _ in range(NBUF)]
            smL = [sb(st, [128, 4], F32, "sm2") for _ in range(NBUF)]

            def tile_gen(n, ti):
                k = n % NBUF
                xt = xin[k]; rt = rts[k]; xn = xns[k]; stats = statsL[k]; ag = agL[k]; sm = smL[k]
                gt = g2[0] if ti < 2 else g2[1]
                S.dma(xt[:], xa_s[b, layer, ti * 128:(ti + 1) * 128, :], writes=[xt])
                S.tt("pool", rt[:], outacc[:, n, :], gt[:], ALU.mult, reads=[gt, (outacc, n, 0), (outacc, n, 1)], writes=[rt])
                yield
                S.stt("dve", rt[:], xt[:], ALPHA, rt[:], ALU.mult, ALU.add, reads=[xt, rt], writes=[rt])
                ln_stats(rt, stats, ag, sm)
                yield
                ln_apply(rt, sm, lng, lnb, xn)
                if layer == 0:
                    S.dma(xb_s[b, ti * 128:(ti + 1) * 128, :], xn[:], reads=[xn])
                else:
                    S.dma(out_d[b, (ti - 2) * 128:(ti - 1) * 128, :], xn[:], reads=[xn])

            run_pipe((tile_gen(n, ti) for n, ti in enumerate(tiles)), 3)

        def post_E(layer, b, catT, tiles, gates):
            if not want('E'):
                return
            with ExitStack() as st2:
                phase_E(layer, b, catT, tiles, gates, st2)
                S.barrier()

        def post_FG(layer, b, tiles, gates):
            nt = len(tiles)
            if not want('F'):
                return
            with ExitStack() as st:
                h2T = sb(st, [128, 8, nt * 128], BF16, "h2T")
                for kc in range(8):
                    S.dma(h2T[:, kc, :], h2T_s[kc, :, 0:nt * 128], writes=[h2T])
                outacc = sb(st, [128, nt, D], F32, "outacc")
                with ExitStack() as st2:
                    phase_F(layer, b, h2T, gates, nt, outacc, st2)
                    S.barrier()
                if not want('G'):
                    return
                with ExitStack() as st2:
                    phase_G(layer, b, tiles, outacc, st2)
                    S.barrier()

        def phase_B(b, hT, catT, st):
            wA = [sb(st, [128, 3, 8, 128], BF16, "wA") for _ in range(2)]
            u = sb(st, [128, 2307], F32, "u"); p0s = sb(st, [128, 2307], F32, "p0s"); y = sb(st, [128, 2307], F32, "y")
            t1 = [sb(st, [128, 512], F32, "t1") for _ in range(2)]
            for c in (0, 257, 2306):
                S.memset("pool", u[:, c:c + 1], 0.0, writes=[(u, "pad")])
            bi = 0
            for ch in range(4):
                w = wA[ch % 2]
                for which in range(3):
                    cc = which * 512 + ch * 128
                    S.dma(w[:, which], evwin_b[:, cc:cc + 128].rearrange("(kc p) n -> p kc n", p=128), writes=[(w, which)])
                for (c0, wd_, t0) in BLOCKS:
                    bg_step(1)
                    pp = [pb(), pb(), pb()]
                    for which in range(3):
                        for kc in range(8):
                            S.mm(pp[which][:, 0:wd_], w[:, which, kc, :], hT[:, kc, c0:c0 + wd_], kc == 0, kc == 7, reads=[(w, which)], writes=[pp[which]])
                    tt_ = t1[bi % 2]; bi += 1
                    S.copy("act", tt_[:, 0:wd_], pp[1][:, 0:wd_], reads=[pp[1]], writes=[tt_])
                    S.tt("dve", u[:, c0:c0 + wd_], tt_[:, 0:wd_], pp[2][:, 0:wd_], ALU.mult, reads=[tt_, pp[2]], writes=[(u, c0)])
                    S.copy("act", p0s[:, c0:c0 + wd_], pp[0][:, 0:wd_], reads=[pp[0]], writes=[(p0s, c0)])
                allu = [(u, c0) for c0, _, _ in BLOCKS] + [(u, "pad")]
                allp = [(p0s, c0) for c0, _, _ in BLOCKS]
                S.ts("dve", y[:, 1:2306], u[:, 1:2306], aconvT[:, ch, 1:2], None, ALU.mult, reads=allu, writes=[y])
                S.stt("dve", y[:, 1:2306], u[:, 0:2305], aconvT[:, ch, 0:1], y[:, 1:2306], ALU.mult, ALU.add, reads=allu + [y], writes=[y])
                S.stt("dve", y[:, 1:2306], u[:, 2:2307], aconvT[:, ch, 2:3], y[:, 1:2306], ALU.mult, ALU.add, reads=allu + [y], writes=[y])
                S.tt("pool", catT[:, ch, 1:2306], y[:, 1:2306], p0s[:, 1:2306], ALU.mult, reads=[y] + allp, writes=[(catT, ch)])

        def phase_C(b, hT, BG, st):
            NBUF = 4
            wB = [sb(st, [128, 3, 8, 128], BF16, "wB") for _ in range(2)]
            s_ = [sb(st, [128, 512], F32, "s_") for _ in range(NBUF)]
            sq_ = [sb(st, [128, 512], BF16, "sq_") for _ in range(NBUF)]
            rin = [sb(st, [128, 512], F32, "rin") for _ in range(NBUF)]
            kn = [sb(st, [128, 512], BF16, "kn") for _ in range(NBUF)]
            ktk = [sb(st, [128, 4, 128], BF16, "ktk") for _ in range(NBUF)]

            def qk_gen(i, which, h, w, c0, wd_, t0):
                p = pb(); n = 0
                for j in range(3):
                    for kc in range(8):
                        S.mm(p[:, 0:wd_], w[:, j, kc, :], hT[:, kc, c0 + j - 1:c0 + j - 1 + wd_], n == 0, n == 23, reads=[(w, j)], writes=[p])
                        n += 1
                s = s_[i % NBUF]; sq = sq_[i % NBUF]; ri = rin[i % NBUF]; qn = kn[i % NBUF]; kt = ktk[i % NBUF]
                S.act(s[:, 0:wd_], p[:, 0:wd_], AF.Silu, reads=[p], writes=[s])
                S.tt("pool", sq[:, 0:wd_], s[:, 0:wd_], s[:, 0:wd_], ALU.mult, reads=[s], writes=[sq])
                yield
                p2 = pb()
                S.mm(p2[:, 0:wd_], onesb[:], sq[:, 0:wd_], True, True, reads=[sq], writes=[p2])
                S.act(ri[:, 0:wd_], p2[:, 0:wd_], AF.Sqrt, bias=1e-6, reads=[p2], writes=[ri])
                S.op("dve", lambda e: e.reciprocal(ri[:, 0:wd_], ri[:, 0:wd_]), reads=[ri], writes=[ri])
                if which == 0:
                    S.stt("dve", qn[:, 0:wd_], s[:, 0:wd_], 128.0 ** -0.5, ri[:, 0:wd_], ALU.mult, ALU.mult, reads=[s, ri], writes=[qn])
                else:
                    S.tt("dve", qn[:, 0:wd_], s[:, 0:wd_], ri[:, 0:wd_], ALU.mult, reads=[s, ri], writes=[qn])
                dstT = (qT_s if which == 0 else kT_s)[h][:, t0:t0 + wd_]
                S.dma(dstT, qn[:, 0:wd_], reads=[qn])
                if which == 1:
                    yield
                    p3 = pb()
                    na = wd_ // 128
                    for j4 in range(na):
                        S.mm(p3[:, j4 * 128:(j4 + 1) * 128], qn[:, j4 * 128:(j4 + 1) * 128], identb[:], True, True, reads=[qn], writes=[p3])
                    S.copy("act", kt[:, 0:na, :], p3[:, 0:wd_].rearrange("p (a b) -> p a b", b=128), reads=[p3], writes=[kt])
                    S.dma(k_s[t0:t0 + wd_, h * 128:(h + 1) * 128].rearrange("(a p) d -> p a d", p=128), kt[:, 0:na, :], reads=[kt])

            def all_qk():
                i = 0
                wi = 0
                for which in range(2):
                    for h in range(4):
                        w = wB[wi % 2]; wi += 1
                        col = which * 512 + h * 128
                        for j in range(3):
                            S.dma(w[:, j], wB_b[j][:, col:col + 128].rearrange("(kc p) n -> p kc n", p=128), writes=[(w, j)])
                        for (c0, wd_, t0) in BLOCKS:
                            bg_step(1)
                            yield qk_gen(i, which, h, w, c0, wd_, t0)
                            i += 1
            run_pipe(all_qk(), 3)
            wV = sb(st, [128, 3, 8, 512], BF16, "wV")
            for j in range(3):
                S.dma(wV[:, j], wB_b[j][:, 1024:1536].rearrange("(kc p) n -> p kc n", p=128), writes=[(wV, j)])
            wG = sb(st, [128, 8, 512], BF16, "wG")
            S.dma(wG[:], evwin_b[:, 3072:3584].rearrange("(kc p) n -> p kc n", p=128), writes=[wG])
            wba = sb(st, [128, 8, 16], BF16, "wba")
            S.dma(wba[:], evwin_b[:, 3584:3600].rearrange("(kc p) n -> p kc n", p=128), writes=[wba])
            vt = [sb(st, [128, 512], BF16, "vt") for _ in range(2)]
            gt = [sb(st, [128, 512], F32, "gt") for _ in range(2)]
            for ti in range(NT):
                bg_step(1)
                c = tcol(ti)
                p = pb(); n = 0
                for j in range(3):
                    for kc in range(8):
                        S.mm(p[:], hT[:, kc, c + j - 1:c + j - 1 + 128], wV[:, j, kc, :], n == 0, n == 23, reads=[(wV, j)], writes=[p])
                        n += 1
                v = vt[ti % 2]
                S.act(v[:], p[:], AF.Silu, reads=[p], writes=[v])
                S.dma(v_s[ti * 128:(ti + 1) * 128, :], v[:], reads=[v])
                p = pb()
                for kc in range(8):
                    S.mm(p[:], hT[:, kc, c:c + 128], wG[:, kc, :], kc == 0, kc == 7, reads=[wG], writes=[p])
                g = gt[ti % 2]
                S.act(g[:], p[:], AF.Silu, reads=[p], writes=[g])
                S.dma(gate_s[ti * 128:(ti + 1) * 128, :], g[:], reads=[g])
                p = pb()
                for kc in range(8):
                    S.mm(p[:, 0:16], hT[:, kc, c:c + 128], wba[:, kc, :], kc == 0, kc == 7, reads=[wba], writes=[p])
                S.copy("dve", BG[:, ti, :], p[:, 0:16], reads=[p], writes=[(BG, ti)])
            allbg = [(BG, ti) for ti in range(NT)]
            smb = sb(st, [128, NT, 8], F32, "smb")
            S.act(BG[:, :, 0:8], BG[:, :, 0:8], AF.Sigmoid, reads=allbg, writes=[(BG, "beta")])
            S.tt("dve", smb[:], BG[:, :, 8:16], dtb[:].unsqueeze(1).to_broadcast([128, NT, 8]), ALU.add, reads=allbg, writes=[smb])
            S.ts("dve", smb[:], smb[:], 30.0, None, ALU.min, reads=[smb], writes=[smb])
            S.act(smb[:], smb[:], AF.Exp, reads=[smb], writes=[smb])
            S.act(smb[:], smb[:], AF.Ln, bias=1.0, reads=[smb], writes=[smb])
            S.tt("dve", BG[:, :, 8:16], smb[:], negexpA[:].unsqueeze(1).to_broadcast([128, NT, 8]), ALU.mult, reads=[smb] + allbg, writes=[(BG, "g")])

        def phase_D(b, BG, catT, st):
            Sst = sb(st, [128, 2, 4, 128], F32, "Sst")
            S.memset("pool", Sst[:], 0.0, writes=[Sst])
            names16 = ["kT", "qT", "ktok", "v", "TG", "A", "AT", "Mb", "MbT", "P", "vb", "kbg"]
            names32 = ["DT", "kd", "u", "wT", "Eg", "qg0", "qg1", "vnew", "osb", "oprev", "gate"]
            slots = []
            for d in range(2):
                sl = {nm: sb(st, [128, 4, 128], BF16, nm) for nm in names16}
                sl.update({nm: sb(st, [128, 4, 128], F32, nm) for nm in names32})
                sl["E"] = sb(st, [128, 16], F32, "E"); sl["bg2"] = sb(st, [128, 4], F32, "bg2")
                sl["ss"] = sb(st, [128, 8], F32, "ss"); sl["junk"] = sb(st, [128, 128], F32, "junk")
                S.memset("pool", sl["qg0"][:], 0.0, writes=[sl["qg0"]]); S.memset("pool", sl["qg1"][:], 0.0, writes=[sl["qg1"]])
                slots.append(sl)
            visited = set()
            identbc = ident[:].unsqueeze(1).to_broadcast([128, 4, 128])
            bnbc = bnorm[:].unsqueeze(1).to_broadcast([128, 4, 128])

            def bc4(ap):
                return ap.unsqueeze(2).to_broadcast([128, 4, 128])

            def f2(t):
                return t[:].rearrange("p h d -> p (h d)")

            def hs(h):
                return slice(h * 128, (h + 1) * 128)

            ringi = [0, 0]

            def item(ti, d, sl):
                def pb():
                    r = PB[4 * d + ringi[d] % 3]
                    ringi[d] += 1
                    return r
                mi, ms, negS, negI = (0, 1, 4, 5) if d == 0 else (2, 3, 6, 7)
                gd = BG[:, ti, 8 + 4 * d:12 + 4 * d]
                bd = BG[:, ti, 4 * d:4 * d + 4]
                rows = slice(ti * 128, (ti + 1) * 128)
                kT, qT, ktok, v, TG, A, AT, P, DT = sl["kT"], sl["qT"], sl["ktok"], sl["v"], sl["TG"], sl["A"], sl["AT"], sl["P"], sl["DT"]
                E = sl["E"]
                S.dma(kT[:], kT_s.rearrange("h d t -> d h t")[:, :, rows], writes=[kT])
                S.dma(qT[:], qT_s.rearrange("h d t -> d h t")[:, :, rows], writes=[qT])
                S.dma(f2(ktok), k_s[rows, :], writes=[ktok])
                S.dma(f2(v), v_s[rows, :], writes=[v])
                yield
                ps_ = pb()
                S.mm(ps_[:, 0:4], MK[:, mi, :], gd, True, True, reads=[(BG, ti)], writes=[ps_])
                S.mm(ps_[:, 4:8], MK[:, ms, :], gd, True, True, reads=[(BG, ti)], writes=[ps_])
                S.mm(ps_[:, 8:12], MK[:, 8, :], gd, True, True, reads=[(BG, ti)], writes=[ps_])
                S.mm(ps_[:, 12:16], MK[:, 9, :], gd, True, True, reads=[(BG, ti)], writes=[ps_])
                S.act(E[:], ps_[:, 0:16], AF.Exp, reads=[ps_], writes=[E])
                S.tt("dve", sl["bg2"][:], bd, E[:, 0:4], ALU.mult, reads=[E, (BG, ti)], writes=[sl["bg2"]])
                for h in range(4):
                    S.ts("dve", TG[:, h, :], MK[:, mi, :], gd[:, h:h + 1], None, ALU.mult, reads=[(BG, ti)], writes=[TG])
                yield
                pKK = pb(); pL = pb()
                for h in range(4):
                    S.mm(pKK[:, hs(h)], kT[:, h, :], kT[:, h, :], True, True, reads=[kT], writes=[pKK])
                for h in range(4):
                    S.mm(pL[:, hs(h)], TG[:, h, :], MKb[:, ms, :], True, False, reads=[TG], writes=[pL])
                    S.mm(pL[:, hs(h)], identb[:], MKb[:, negS, :], False, True, reads=[TG], writes=[pL])
                S.act(f2(A), pL[:], AF.Exp, reads=[pL], writes=[A])
                for h in range(4):
                    S.stt("dve", A[:, h, :], pKK[:, hs(h)], bd[:, h:h + 1], A[:, h, :], ALU.mult, ALU.mult, reads=[pKK, A, (BG, ti)], writes=[A])
                yield
                pAT = pb()
                for h in range(4):
                    S.mm(pAT[:, hs(h)], A[:, h, :], identb[:], True, True, reads=[A], writes=[pAT])
                S.copy("act", f2(AT), pAT[:], reads=[pAT], writes=[AT])
                S.stt("dve", P[:], pAT[:].rearrange("p (h d) -> p h d", h=4), -1.0, identbc, ALU.mult, ALU.add, reads=[pAT], writes=[P])
                pLT = pb(); pQK = pb()
                for h in range(4):
                    S.mm(pLT[:, hs(h)], MKb[:, ms, :], TG[:, h, :], True, False, reads=[TG], writes=[pLT])
                    S.mm(pLT[:, hs(h)], identb[:], MKb[:, negI, :], False, True, reads=[TG], writes=[pLT])
                for h in range(4):
                    S.mm(pQK[:, hs(h)], kT[:, h, :], qT[:, h, :], True, True, reads=[kT, qT], writes=[pQK])
                S.act(f2(DT), pLT[:], AF.Exp, reads=[pLT], writes=[DT])
                S.tt("dve", f2(DT), pQK[:], f2(DT), ALU.mult, reads=[pQK, DT], writes=[DT])
                yield
                N_, NT_ = AT, A
                Y_, YT_ = sl["Mb"], sl["MbT"]
                prevYT = None
                for lev in range(6):
                    pP = None
                    if prevYT is not None:
                        pP = pb()
                        for h in range(4):
                            S.mm(pP[:, hs(h)], prevYT[:, h, :], P[:, h, :], True, True, reads=[prevYT, P], writes=[pP])
                    if lev < 5:
                        last = lev == 4
                        pMT = pb()
                        pM = None if last else pb()
                        for h in range(4):
                            if not last:
                                S.mm(pM[:, hs(h)], NT_[:, h, :], N_[:, h, :], True, True, reads=[N_, NT_], writes=[pM])
                            S.mm(pMT[:, hs(h)], N_[:, h, :], NT_[:, h, :], True, True, reads=[N_, NT_], writes=[pMT])
                        if not last:
                            S.copy("act", f2(Y_), pM[:], reads=[pM], writes=[Y_])
                        S.copy("act" if last else "dve", f2(YT_), pMT[:], reads=[pMT], writes=[YT_])
                    if pP is not None:
                        S.tt("dve", f2(P), f2(P), pP[:], ALU.add, reads=[pP, P], writes=[P])
                    if lev < 5:
                        prevYT = YT_
                        N_, NT_, Y_, YT_ = Y_, YT_, N_, NT_
                    yield
                vb, kbg, kd, u, wT, Eg, vnew = sl["vb"], sl["kbg"], sl["kd"], sl["u"], sl["wT"], sl["Eg"], sl["vnew"]
                S.tt("dve", vb[:], v[:], bc4(bd), ALU.mult, reads=[v, (BG, ti)], writes=[vb])
                S.tt("pool", kbg[:], ktok[:], bc4(sl["bg2"][:]), ALU.mult, reads=[ktok, sl["bg2"]], writes=[kbg])
                S.tt("pool", kd[:], ktok[:], bc4(E[:, 4:8]), ALU.mult, reads=[ktok, E], writes=[kd])
                pu = pb(); pw = pb(); pE = pb()
                for h in range(4):
                    S.mm(pu[:, hs(h)], P[:, h, :], vb[:, h, :], True, True, reads=[P, vb], writes=[pu])
                for h in range(4):
                    S.mm(pw[:, hs(h)], kbg[:, h, :], P[:, h, :], True, True, reads=[P, kbg], writes=[pw])
                for h in range(4):
                    S.mm(pE[:, hs(h)], onesb[:], TG[:, h, :], True, True, reads=[TG], writes=[pE])
                S.copy("act", f2(u), pu[:], reads=[pu], writes=[u])
                S.copy("dve", f2(wT), pw[:], reads=[pw], writes=[wT])
                S.act(f2(Eg), pE[:], AF.Exp, reads=[pE], writes=[Eg])
                S.tt("dve", sl["qg0"][:, :, 0:64], qT[:, :, 0:64], Eg[:, :, 0:64], ALU.mult, reads=[qT, Eg], writes=[sl["qg0"]])
                S.tt("pool", sl["qg1"][:, :, 64:128], qT[:, :, 64:128], Eg[:, :, 64:128], ALU.mult, reads=[qT, Eg], writes=[sl["qg1"]])
                yield
                po = PB[4 * d + 3]
                S.memset("dve", po[:], 0.0, writes=[po])
                for c in ([0, 1] if d == 0 else [1, 0]):
                    Rr = slice(64 * c, 64 * c + 64)
                    pws = pb()
                    for h in range(4):
                        S.mm(pws[:, hs(h)], wT[:, h, :], Sst[:, d, h, :], True, True, reads=[wT, (Sst, d)], writes=[pws])
                    S.tt("dve", f2(vnew)[Rr, :], f2(u)[Rr, :], pws[Rr, :], ALU.subtract, reads=[u, pws], writes=[vnew])
                    qg = sl["qg0"] if c == 0 else sl["qg1"]
                    for h in range(4):
                        S.op("pe", lambda e, h=h, qg=qg: e.matmul(po[:, hs(h)], qg[:, h, :], Sst[:, d, h, :], start=False, stop=False, skip_group_check=True),
                             reads=[qg, (Sst, d)], writes=[po])
                    pS = pb()
                    for h in range(4):
                        S.mm(pS[:, hs(h)], kd[Rr, h, :], vnew[Rr, h, :], True, True, reads=[kd, vnew], writes=[pS])
                    for h in range(4):
                        S.stt("dve", Sst[:, d, h, :], Sst[:, d, h, :], E[:, 8 + 4 * c + h:9 + 4 * c + h], pS[:, hs(h)], ALU.mult, ALU.add,
                              reads=[pS, E, (Sst, d)], writes=[(Sst, d)])
                    yield
                for h in range(4):
                    S.op("pe", lambda e, h=h: e.matmul(po[:, hs(h)], DT[:, h, :], vnew[:, h, :], start=False, stop=False, skip_group_check=True),
                         reads=[DT, vnew], writes=[po])
                osb, oprev, gate = sl["osb"], sl["oprev"], sl["gate"]
                if ti not in visited:
                    visited.add(ti)
                    S.copy("act", f2(osb), po[:], reads=[po], writes=[osb])
                    S.dma(o_s[rows, :], f2(osb), reads=[osb], writes=[("o_s", ti)])
                else:
                    ss = sl["ss"]
                    S.dma(f2(oprev), o_s[rows, :], reads=[("o_s", ti)], writes=[oprev])
                    S.dma(f2(gate), gate_s[rows, :], writes=[gate])
                    S.tt("dve", f2(osb), po[:], f2(oprev), ALU.add, reads=[po, oprev], writes=[osb])
                    S.memset("pool", ss[:], 0.0, writes=[ss])
                    for h in range(4):
                        S.act(sl["junk"][:], osb[:, h, :], AF.Square, accum_out=ss[:, h:h + 1], reads=[osb, ss], writes=[sl["junk"], ss])
                    S.act(ss[:, 4:8], ss[:, 0:4], AF.Sqrt, scale=1.0 / 128, bias=1e-6, reads=[ss], writes=[ss])
                    S.op("dve", lambda e: e.reciprocal(ss[:, 4:8], ss[:, 4:8]), reads=[ss], writes=[ss])
                    S.tt("dve", osb[:], osb[:], bc4(ss[:, 4:8]), ALU.mult, reads=[osb, ss], writes=[osb])
                    S.tt("pool", osb[:], osb[:], gate[:], ALU.mult, reads=[osb, gate], writes=[osb])
                    S.tt("pool", osb[:], osb[:], bnbc, ALU.mult, reads=[osb], writes=[osb])
                    pT = pb()
                    for h in range(4):
                        S.tr(pT[:, hs(h)], osb[:, h, :], ident[:], reads=[osb], writes=[pT])
                    S.copy("act", catT[:, 4:8, tcol(ti):tcol(ti) + 128], pT[:].rearrange("p (h t) -> p h t", h=4), reads=[pT], writes=[(catT, "B", ti)])

            fwd_order = list(range(18))
            bwd_order = [1, 0] + list(range(17, 1, -1))
            for s_i in range(18):
                bg_step(3)
                gens = [item(fwd_order[s_i], 0, slots[0]), item(bwd_order[s_i], 1, slots[1])]
                while gens:
                    for g in list(gens):
                        try:
                            next(g)
                        except StopIteration:
                            gens.remove(g)

        def layer0(b):
            tiles = list(range(18))
            with ExitStack() as stL:
                gates = sb(stL, [128, 18, 16], F32, "gates")
                with ExitStack() as stBG:
                    if b == 0 and want('F'):
                        stg = [sb(stBG, [128, 2048], F32, "bgs") for _ in range(3)]
                        stgb = [sb(stBG, [128, 2048], BF16, "bgb") for _ in range(3)]
                        bgs["gen"] = moe_conv_gen(stg, stgb)
                    with ExitStack() as stC:
                        catT = sb(stC, [128, 8, 2307], BF16, "catT")
                        BG = sb(stC, [128, 18, 16], F32, "BG")
                        with ExitStack() as st:
                            hT = sb(st, [128, 8, 2307], BF16, "hT")
                            for c in (0, 257, 2306):
                                S.memset("pool", hT[:, :, c:c + 1], 0.0, writes=[(hT, "pad", c)])
                            if want('A'):
                                with ExitStack() as st2:
                                    phase_A(0, b, hT, tiles, st2)
                                    S.barrier()
                            if want('B'):
                                with ExitStack() as st2:
                                    phase_B(b, hT, catT, st2)
                                    S.barrier()
                            if want('C'):
                                with ExitStack() as st2:
                                    phase_C(b, hT, BG, st2)
                                    S.barrier()
                        if want('D'):
                            with ExitStack() as st:
                                phase_D(b, BG, catT, st)
                                S.barrier()
                        post_E(0, b, catT, tiles, gates)
                    if bgs["gen"] is not None:
                        bg_step(10000)
                        S.barrier()
                post_FG(0, b, tiles, gates)

        LBLK = [(258 + 512 * j, 512, 256 + 512 * j, 512 * j) for j in range(4)]

        def rearr_w(ap):
            return ap.rearrange("(kc p) n -> p kc n", p=128)

        def l1_proj_mla(b, hT, dqn, dkvn, KR, st):
            wdq = sb(st, [128, 8, 384], BF16, "wdq"); S.dma(wdq[:], rearr_w(odwin_b[:, 768:1152]), writes=[wdq])
            wdkv = sb(st, [128, 8, 256], BF16, "wdkv"); S.dma(wdkv[:], rearr_w(odwin_b[:, 1152:1408]), writes=[wdkv])
            wk = sb(st, [128, 8, 96], BF16, "wk"); S.dma(wk[:], rearr_w(wkr_b), writes=[wk])
            wks = sb(st, [128, 8, 96], BF16, "wks"); S.dma(wks[:], rearr_w(wkrsw_b), writes=[wks])
            r32 = sb(st, [96, 2, LT], F32, "r32"); S.dma(r32[:], rope32_d.rearrange("a p t -> p a t"), writes=[r32])
            sq = [sb(st, [128, 512], F32, "sq1") for _ in range(3)]
            rinv = sb(st, [128, 512], F32, "rinv1")
            t1 = sb(st, [96, 512], F32, "t1a"); t2 = sb(st, [96, 512], F32, "t2a")

            def rms_proj(wt, nch, normT, dst, c0, w, d0, inv_n):
                pp = [pb() for _ in range(nch)]
                for c in range(nch):
                    for kc in range(8):
                        S.mm(pp[c][:, 0:w], wt[:, kc, c * 128:(c + 1) * 128], hT[:, kc, c0:c0 + w], kc == 0, kc == 7, reads=[wt], writes=[pp[c]])
                    S.act(sq[c][:, 0:w], pp[c][:, 0:w], AF.Square, reads=[pp[c]], writes=[sq[c]])
                pss = pb()
                for c in range(nch):
                    S.mm(pss[:, 0:w], ones[:], sq[c][:, 0:w], c == 0, c == nch - 1, reads=[sq[c]], writes=[pss])
                S.act(rinv[:, 0:w], pss[:, 0:w], AF.Sqrt, scale=inv_n, bias=1e-6, reads=[pss], writes=[rinv])
                S.op("dve", lambda e: e.reciprocal(rinv[:, 0:w], rinv[:, 0:w]), reads=[rinv], writes=[rinv])
                for c in range(nch):
                    S.stt("dve", dst[:, c, d0:d0 + w], pp[c][:, 0:w], normT[:, c:c + 1], rinv[:, 0:w], ALU.mult, ALU.mult,
                          reads=[pp[c], rinv], writes=[(dst, c, d0)])

            for bi, (c0, w, t0) in enumerate(BLOCKS):
                rms_proj(wdkv, 2, kvnT, dkvn, c0, w, t0, 1.0 / 256)
                pk = pb(); pks = pb()
                for kc in range(8):
                    S.mm(pk[0:96, 0:w], wk[:, kc, :], hT[:, kc, c0:c0 + w], kc == 0, kc == 7, reads=[wk], writes=[pk])
                if bi == 0:
                    S.copy("act", KR[64:96, t0:t0 + w], pk[64:96, 0:w], reads=[pk], writes=[(KR, t0)])
                else:
                    l0 = t0 - 256
                    for kc in range(8):
                        S.mm(pks[0:96, 0:w], wks[:, kc, :], hT[:, kc, c0:c0 + w], kc == 0, kc == 7, reads=[wks], writes=[pks])
                    S.tt("dve", t1[64:96, 0:w], pk[64:96, 0:w], r32[64:96, 0, l0:l0 + w], ALU.mult, reads=[pk, r32], writes=[t1])
                    S.tt("dve", t2[64:96, 0:w], pks[64:96, 0:w], r32[64:96, 1, l0:l0 + w], ALU.mult, reads=[pks, r32], writes=[t2])
                    S.tt("pool", KR[64:96, t0:t0 + w], t1[64:96, 0:w], t2[64:96, 0:w], ALU.add, reads=[t1, t2], writes=[(KR, t0)])
                    rms_proj(wdq, 3, qnT, dqn, c0, w, l0, 1.0 / 384)

        def l1_proj_win(b, hT, CQ, CK, CV, st):
            wq = sb(st, [128, 8, 512], BF16, "wq"); S.dma(wq[:], rearr_w(odwin_b[:, 0:512]), writes=[wq])
            wqs = sb(st, [128, 8, 512], BF16, "wqs"); S.dma(wqs[:], rearr_w(odwinsw_b[:, 0:512]), writes=[wqs])
            wkk = sb(st, [128, 8, 128], BF16, "wkk"); S.dma(wkk[:], rearr_w(odwin_b[:, 512:640]), writes=[wkk])
            wkks = sb(st, [128, 8, 128], BF16, "wkks"); S.dma(wkks[:], rearr_w(odwinsw_b[:, 512:640]), writes=[wkks])
            wv = sb(st, [128, 8, 128], BF16, "wv"); S.dma(wv[:], rearr_w(odwin_b[:, 640:768]), writes=[wv])
            r64 = sb(st, [64, 2, LT], F32, "r64"); S.dma(r64[:], rope64_d.rearrange("a p t -> p a t"), writes=[r64])
            t1 = [sb(st, [64, 512], F32, "t1w") for _ in range(2)]; t2 = [sb(st, [64, 512], F32, "t2w") for _ in range(2)]
            S.memset("pool", CV[:, :, :, 64:65], 1.0, writes=[(CV, "ones")])
            ii = 0
            for bi, (c0, w, t0) in enumerate(BLOCKS):
                l0 = t0 - 256
                for g in range(2):
                    pk = pb(); pks = pb()
                    for kc in range(8):
                        S.mm(pk[0:64, 0:w], wkk[:, kc, g * 64:(g + 1) * 64], hT[:, kc, c0:c0 + w], kc == 0, kc == 7, reads=[wkk], writes=[pk])
                    if bi == 0:
                        S.copy("act", CK[:, g, t0:t0 + w], pk[0:64, 0:w], reads=[pk], writes=[(CK, g, t0)])
                    else:
                        for kc in range(8):
                            S.mm(pks[0:64, 0:w], wkks[:, kc, g * 64:(g + 1) * 64], hT[:, kc, c0:c0 + w], kc == 0, kc == 7, reads=[wkks], writes=[pks])
                        a1 = t1[ii % 2]; a2 = t2[ii % 2]; ii += 1
                        S.tt("dve", a1[:, 0:w], pk[0:64, 0:w], r64[:, 0, l0:l0 + w], ALU.mult, reads=[pk, r64], writes=[a1])
                        S.tt("dve", a2[:, 0:w], pks[0:64, 0:w], r64[:, 1, l0:l0 + w], ALU.mult, reads=[pks, r64], writes=[a2])
                        S.tt("pool", CK[:, g, t0:t0 + w], a1[:, 0:w], a2[:, 0:w], ALU.add, reads=[a1, a2], writes=[(CK, g, t0)])
                for j in range(w // 128):
                    ti = t0 // 128 + j
                    pv = pb()
                    for kc in range(8):
                        S.mm(pv[:, 0:128], hT[:, kc, tcol(ti):tcol(ti) + 128], wv[:, kc, :], kc == 0, kc == 7, reads=[wv], writes=[pv])
                    S.copy("act", CV[:, ti, :, 0:64], pv[:, 0:128].rearrange("p (g d) -> p g d", g=2), reads=[pv], writes=[(CV, ti)])
                if bi > 0:
                    for h in range(8):
                        pq = pb(); pqs = pb()
                        for kc in range(8):
                            S.mm(pq[0:64, 0:w], wq[:, kc, h * 64:(h + 1) * 64], hT[:, kc, c0:c0 + w], kc == 0, kc == 7, reads=[wq], writes=[pq])
                        for kc in range(8):
                            S.mm(pqs[0:64, 0:w], wqs[:, kc, h * 64:(h + 1) * 64], hT[:, kc, c0:c0 + w], kc == 0, kc == 7, reads=[wqs], writes=[pqs])
                        a1 = t1[ii % 2]; a2 = t2[ii % 2]; ii += 1
                        S.tt("dve", a1[:, 0:w], pq[0:64, 0:w], r64[:, 0, l0:l0 + w], ALU.mult, reads=[pq, r64], writes=[a1])
                        S.tt("dve", a2[:, 0:w], pqs[0:64, 0:w], r64[:, 1, l0:l0 + w], ALU.mult, reads=[pqs, r64], writes=[a2])
                        S.tt("pool", CQ[:, h, l0:l0 + w], a1[:, 0:w], a2[:, 0:w], ALU.add, reads=[a1, a2], writes=[(CQ, h, l0)])

        def l1_attn_win(b, CQ, CK, CV, catT, st):
            PT = [sb(st, [128, 512], BF16, "PTw") for _ in range(2)]
            Yw = [sb(st, [128, 512], F32, "Yw") for _ in range(2)]
            den = sb(st, [128, 8], F32, "denw")
            ring = [0]

            def rb():
                r = PB[ring[0] % 6]
                ring[0] += 1
                return r
            seq = []
            for qt in range(16):
                for g in range(2):
                    keys = [(0, None), (1, None)]
                    if qt >= 1:
                        keys.append((qt + 1, 10))
                    keys.append((qt + 2, None))
                    if qt <= 14:
                        keys.append((qt + 3, 11))
                    for ki, (kt, mk) in enumerate(keys):
                        seq.append((qt, g, kt, mk, ki == 0, ki == len(keys) - 1))
            pss = {}

            def issue_S(i):
                qt, g, kt, mk, first, last = seq[i]
                ps = rb()
                S.mm(ps[:, 0:512], CK[:, g, kt * 128:(kt + 1) * 128], CQ[:, 4 * g:4 * g + 4, qt * 128:(qt + 1) * 128], True, True, reads=[], writes=[ps])
                pss[i] = ps
            issue_S(0)
            io = 0
            po = None; po3 = None
            for i, (qt, g, kt, mk, first, last) in enumerate(seq):
                yw = Yw[qt % 2]
                if first:
                    po = PB[6 + io % 2]; io += 1
                    po3 = po[:, 0:260].rearrange("p (a b) -> p a b", a=4)
                    S.memset("dve", po[:, 0:260], 0.0, writes=[po])
                if i + 1 < len(seq):
                    issue_S(i + 1)
                ps = pss.pop(i)
                pt = PT[i % 2]
                S.act(pt[:], ps[:, 0:512], AF.Exp, scale=0.125, reads=[ps], writes=[pt])
                if mk is not None:
                    pt3 = pt[:].rearrange("p (a b) -> p a b", a=4)
                    S.tt("pool", pt3, pt3, MK[:, mk, :].unsqueeze(1).to_broadcast([128, 4, 128]), ALU.mult, reads=[pt], writes=[pt])
                for hh in range(4):
                    S.op("pe", lambda e, hh=hh, pt=pt, kt=kt, po=po, g=g: e.matmul(po[:, hh * 65:(hh + 1) * 65], pt[:, hh * 128:(hh + 1) * 128], CV[:, kt, g, :],
                                                                                 start=False, stop=False, skip_group_check=True), reads=[pt], writes=[po])
                if last:
                    S.tt("dve", den[:, 0:4], po3[:, :, 64], expsink[:, 4 * g:4 * g + 4], ALU.add, reads=[po], writes=[den])
                    S.op("dve", lambda e: e.reciprocal(den[:, 4:8], den[:, 0:4]), reads=[den], writes=[den])
                    S.tt("dve", yw[:, g * 256:(g + 1) * 256].rearrange("p (a b) -> p a b", a=4), po3[:, :, 0:64],
                         den[:, 4:8].unsqueeze(2).to_broadcast([128, 4, 64]), ALU.mult, reads=[po, den], writes=[yw])
                    if g == 1:
                        pT = rb()
                        for j in range(4):
                            S.tr(pT[:, j * 128:(j + 1) * 128], yw[:, j * 128:(j + 1) * 128], ident[:], reads=[yw], writes=[pT])
                        S.copy("act", catT[:, 0:4, qt * 128:(qt + 1) * 128], pT[:, 0:512].rearrange("p (a b) -> p a b", a=4), reads=[pT], writes=[(catT, "w", qt)])

        def l1_up_mla(b, dqn, dkvn, KR, MQ, MKk, MV, st):
            wuq = sb(st, [128, 3, 768], BF16, "wuq"); S.dma(wuq[:], wuq_b.rearrange("(c p) n -> p c n", p=128), writes=[wuq])
            wuqs = sb(st, [128, 3, 768], BF16, "wuqs"); S.dma(wuqs[:], wuqsw_b.rearrange("(c p) n -> p c n", p=128), writes=[wuqs])
            wukv = sb(st, [128, 2, 1024], BF16, "wukv"); S.dma(wukv[:], wukv_b.rearrange("(c p) n -> p c n", p=128), writes=[wukv])
            r32 = sb(st, [96, 2, LT], F32, "r32b"); S.dma(r32[:], rope32_d.rearrange("a p t -> p a t"), writes=[r32])
            t1 = [sb(st, [96, 512], F32, "t1m") for _ in range(2)]; t2 = [sb(st, [96, 512], F32, "t2m") for _ in range(2)]
            S.memset("pool", MV[:, :, :, 64:65], 1.0, writes=[(MV, "ones")])
            ii = 0
            for (c0, w, t0, l0) in LBLK:
                for h in range(8):
                    pq = pb(); pqs = pb()
                    for c in range(3):
                        S.mm(pq[0:96, 0:w], wuq[:, c, h * 96:(h + 1) * 96], dqn[:, c, l0:l0 + w], c == 0, c == 2, reads=[wuq], writes=[pq])
                    for c in range(3):
                        S.mm(pqs[0:96, 0:w], wuqs[:, c, h * 96:(h + 1) * 96], dqn[:, c, l0:l0 + w], c == 0, c == 2, reads=[wuqs], writes=[pqs])
                    S.copy("act", MQ[0:64, h, l0:l0 + w], pq[0:64, 0:w], reads=[pq], writes=[(MQ, h, l0, 0)])
                    a1 = t1[ii % 2]; a2 = t2[ii % 2]; ii += 1
                    S.tt("dve", a1[64:96, 0:w], pq[64:96, 0:w], r32[64:96, 0, l0:l0 + w], ALU.mult, reads=[pq, r32], writes=[a1])
                    S.tt("dve", a2[64:96, 0:w], pqs[64:96, 0:w], r32[64:96, 1, l0:l0 + w], ALU.mult, reads=[pqs, r32], writes=[a2])
                    S.tt("pool", MQ[64:96, h, l0:l0 + w], a1[64:96, 0:w], a2[64:96, 0:w], ALU.add, reads=[a1, a2], writes=[(MQ, h, l0, 1)])
            for (c0, w, t0) in BLOCKS:
                for h in range(8):
                    pk = pb()
                    for c in range(2):
                        S.mm(pk[0:64, 0:w], wukv[:, c, h * 128:h * 128 + 64], dkvn[:, c, t0:t0 + w], c == 0, c == 1, reads=[wukv], writes=[pk])
                    if h % 2 == 0:
                        S.copy("act", MKk[0:64, h, t0:t0 + w], pk[0:64, 0:w], reads=[pk], writes=[(MKk, h, t0, 0)])
                    else:
                        S.copy("dve", MKk[0:64, h, t0:t0 + w], pk[0:64, 0:w], reads=[pk], writes=[(MKk, h, t0, 0)])
                    S.copy("pool", MKk[64:96, h, t0:t0 + w], KR[64:96, t0:t0 + w], reads=[], writes=[(MKk, h, t0, 1)])
                for j in range(w // 128):
                    ti = t0 // 128 + j
                    pv = pb()
                    wv3 = wukv[:].rearrange("p c (h x) -> p c h x", h=8)
                    for c in range(2):
                        S.mm(pv[:, 0:512], dkvn[:, c, ti * 128:(ti + 1) * 128], wv3[:, c, :, 64:128], c == 0, c == 1, reads=[wukv], writes=[pv])
                    S.copy("act", MV[:, ti, :, 0:64], pv[:, 0:512].rearrange("p (h x) -> p h x", h=8), reads=[pv], writes=[(MV, ti)])

        def l1_attn_mla(b, MQ, MKk, MV, catT, st):
            PT = [sb(st, [128, 512], BF16, "PTm") for _ in range(3)]
            Ym = sb(st, [128, 4, 512], F32, "Ym")
            den = sb(st, [128, 8], F32, "denm")
            ring = [0]

            def rb():
                r = PB[ring[0] % 6]
                ring[0] += 1
                return r
            scale = 96.0 ** -0.5
            seq = [(qb, h, kt) for qb in range(4) for h in range(8) for kt in range(18)]
            pss = {}

            def issue_S(i):
                qb, h, kt = seq[i]
                ps = rb()
                S.mm(ps[:, 0:512], MKk[:, h, kt * 128:(kt + 1) * 128], MQ[:, h, qb * 512:(qb + 1) * 512], True, True, reads=[], writes=[ps])
                pss[i] = ps
            issue_S(0)
            io = 0
            po = None; po3 = None
            for i, (qb, h, kt) in enumerate(seq):
                if kt == 0:
                    po = PB[6 + io % 2]; io += 1
                    po3 = po[:, 0:260].rearrange("p (a b) -> p a b", a=4)
                    S.memset("dve", po[:, 0:260], 0.0, writes=[po])
                if i + 1 < len(seq):
                    issue_S(i + 1)
                ps = pss.pop(i)
                pt = PT[i % 3]
                S.act(pt[:], ps[:, 0:512], AF.Exp, scale=scale, reads=[ps], writes=[pt])
                for qs in range(4):
                    S.op("pe", lambda e, qs=qs, pt=pt, kt=kt, po=po, h=h: e.matmul(po[:, qs * 65:(qs + 1) * 65], pt[:, qs * 128:(qs + 1) * 128], MV[:, kt, h, :],
                                                                                  start=False, stop=False, skip_group_check=True), reads=[pt], writes=[po])
                if kt == 17:
                    S.op("dve", lambda e, po3=po3: e.reciprocal(den[:, 0:4], po3[:, :, 64]), reads=[po], writes=[den])
                    S.tt("dve", Ym[:, :, h * 64:(h + 1) * 64], po3[:, :, 0:64], den[:, 0:4].unsqueeze(2).to_broadcast([128, 4, 64]), ALU.mult,
                         reads=[po, den], writes=[(Ym, h)])
                    if h == 7:
                        for qs in range(4):
                            pT = rb()
                            for j in range(4):
                                S.tr(pT[:, j * 128:(j + 1) * 128], Ym[:, qs, j * 128:(j + 1) * 128], ident[:], reads=[(Ym, hh) for hh in range(8)], writes=[pT])
                            qt = qb * 4 + qs
                            S.copy("act", catT[:, 4:8, qt * 128:(qt + 1) * 128], pT[:, 0:512].rearrange("p (a b) -> p a b", a=4), reads=[pT], writes=[(catT, "m", qt)])

        def layer1(b):
            tiles = list(range(2, 18))
            with ExitStack() as stL:
                gates = sb(stL, [128, 16, 16], F32, "gates1")
                with ExitStack() as stC:
                    catT = sb(stC, [128, 8, LT], BF16, "catT1")
                    dqn = sb(stC, [128, 3, LT], BF16, "dqn"); dkvn = sb(stC, [128, 2, T], BF16, "dkvn"); KR = sb(stC, [96, T], BF16, "KR")
                    with ExitStack() as stW:
                        CQ = sb(stW, [64, 8, LT], BF16, "CQ"); CK = sb(stW, [64, 2, T], BF16, "CK"); CV = sb(stW, [128, 18, 2, 65], BF16, "CV")
                        with ExitStack() as st:
                            hT = sb(st, [128, 8, 2307], BF16, "hT1")
                            with ExitStack() as st2:
                                phase_A(1, b, hT, list(range(18)), st2)
                                S.barrier()
                            if want('L1P'):
                              with ExitStack() as st2:
                                l1_proj_mla(b, hT, dqn, dkvn, KR, st2)
                                S.barrier()
                            if want('L1W'):
                              with ExitStack() as st2:
                                l1_proj_win(b, hT, CQ, CK, CV, st2)
                                S.barrier()
                        if want('L1WA'):
                          with ExitStack() as st2:
                            l1_attn_win(b, CQ, CK, CV, catT, st2)
                            S.barrier()
                    with ExitStack() as stM:
                        MQ = sb(stM, [96, 8, LT], BF16, "MQ"); MKk = sb(stM, [96, 8, T], BF16, "MKk"); MV = sb(stM, [128, 18, 8, 65], BF16, "MV")
                        if want('L1U'):
                          with ExitStack() as st2:
                            l1_up_mla(b, dqn, dkvn, KR, MQ, MKk, MV, st2)
                            S.barrier()
                        if want('L1MA'):
                          with ExitStack() as st2:
                            l1_attn_mla(b, MQ, MKk, MV, catT, st2)
                            S.barrier()
                    if dbg:
                        S.dma(cat_dbg, catT[:], reads=[])
                        S.barrier()
                    if want('L1E'):
                        post_E(1, b, catT, tiles, gates)
                if want('L1F'):
                    post_FG(1, b, tiles, gates)


        for b in range(NB):
            layer0(b)
            if not only0 and want('L1A'):
                layer1(b)
        S.finish([])
    return nc


NEGV = -30000.0


def make_consts():
    p = np.arange(128)
    ch = p // 64
    same = ch[:, None] == ch[None, :]
    a = p[:, None]; bb = p[None, :]
    M = np.zeros((14, 128, 128), np.float32)
    M[0] = same & (a <= bb)
    M[1] = same & (a > bb)
    M[2] = same & (a >= bb)
    M[3] = same & (a < bb)
    M[4] = np.where(same & (a > bb), 0.0, NEGV)
    M[5] = np.where(same & (bb >= a), 0.0, NEGV)
    M[6] = np.where(same & (a < bb), 0.0, NEGV)
    M[7] = np.where(same & (bb <= a), 0.0, NEGV)
    M[8] = (a < 64) & (bb >= 0)
    M[9] = (a >= 64) & (bb >= 0)
    M[10] = bb <= a
    M[11] = a <= bb
    t = np.arange(LT)
    rows = (t // 64).astype(np.float32); cols = (t % 64).astype(np.float32)

    def ang(rot):
        nf = rot // 4
        inv = (10000.0 ** (-np.arange(nf, dtype=np.float32) / nf)).astype(np.float32)
        return np.concatenate([rows[:, None] * inv, cols[:, None] * inv], -1).astype(np.float32)
    a64 = ang(64); a32 = ang(32)
    r64 = np.zeros((2, 64, LT), np.float32)
    r64[0] = np.concatenate([np.cos(a64), np.cos(a64)], 1).T
    r64[1] = np.concatenate([-np.sin(a64), np.sin(a64)], 1).T
    r32 = np.zeros((2, 96, LT), np.float32)
    r32[0, :64] = 1.0
    r32[0, 64:] = np.concatenate([np.cos(a32), np.cos(a32)], 1).T
    r32[1, 64:] = np.concatenate([-np.sin(a32), np.sin(a32)], 1).T
    return M, r64, r32


def prep_shared(inp):
    f = lambda a: np.ascontiguousarray(a, dtype=np.float32)
    M, r64, r32 = make_consts()
    w1 = inp["od_w_in"][0]
    w1s = w1.copy()
    for h in range(8):
        b0 = h * 64
        w1s[:, b0:b0 + 32] = w1[:, b0 + 32:b0 + 64]; w1s[:, b0 + 32:b0 + 64] = w1[:, b0:b0 + 32]
    for g in range(2):
        b0 = 512 + g * 64
        w1s[:, b0:b0 + 32] = w1[:, b0 + 32:b0 + 64]; w1s[:, b0 + 32:b0 + 64] = w1[:, b0:b0 + 32]
    wkr = np.zeros((1024, 96), np.float32); wkrs = np.zeros((1024, 96), np.float32)
    wkr[:, 64:96] = w1[:, 1408:1440]
    wkrs[:, 64:80] = w1[:, 1424:1440]; wkrs[:, 80:96] = w1[:, 1408:1424]
    wuq = inp["od_d_wuq"][0]
    wuqs = wuq.copy()
    for h in range(8):
        b0 = h * 96 + 64
        wuqs[:, b0:b0 + 16] = wuq[:, b0 + 16:b0 + 32]; wuqs[:, b0 + 16:b0 + 32] = wuq[:, b0:b0 + 16]
    d = {
        "ada_w": f(inp["ada_w"]), "ada_b": f(inp["ada_b"]),
        "ln_g": f(inp["ln_g"].reshape(4, 1024)), "ln_b": f(inp["ln_b"].reshape(4, 1024)),
        "ev_w_in": f(inp["ev_w_in"][0]),
        "a_convT": f(inp["ev_a_conv"][0].reshape(3, 4, 128).transpose(2, 1, 0)),
        "ev_b_conv": f(inp["ev_b_conv"][0]),
        "alog": f(inp["ev_b_alog"].reshape(1, 8)), "dtbias": f(inp["ev_b_dtbias"].reshape(1, 8)),
        "bnorm": f(inp["ev_b_norm"].reshape(1, 128)), "ev_w_out": f(inp["ev_w_out"][0]),
        "od_w_in": f(w1), "od_w_in_sw": f(w1s), "wkr": f(wkr), "wkr_sw": f(wkrs),
        "sink": f(inp["od_c_sink"].reshape(1, 8)),
        "qnormT": f(inp["od_d_qnorm"][0].reshape(3, 128).T), "kvnormT": f(inp["od_d_kvnorm"][0].reshape(2, 128).T),
        "wuq": f(wuq), "wuq_sw": f(wuqs), "wukv": f(inp["od_d_wukv"][0]), "od_w_out": f(inp["od_w_out"][0]),
        "router_w": f(inp["router_w"]), "router_bias": f(inp["router_bias"].reshape(1, 16)),
        "moe_g": f(inp["moe_w_gate"].reshape(32768, 512)), "moe_u": f(inp["moe_w_up"].reshape(32768, 512)),
        "moe_d": f(inp["moe_w_down"].reshape(16384, 1024)),
        "idn": np.eye(128, dtype=np.float32), "masks": M, "rope64": r64, "rope32": r32,
    }
    return d


def prep_core(inp, shared, b0, NB):
    f = lambda a: np.ascontiguousarray(a, dtype=np.float32)
    cs = np.concatenate([inp["c"][b0:b0 + NB], inp["c_ctx"][None, :]], 0)
    csT = cs.reshape(NB + 1, 8, 128).transpose(2, 1, 0)
    d = dict(shared)
    d["x"] = f(inp["x"][b0:b0 + NB]); d["ctx"] = f(inp["ctx"][b0:b0 + NB]); d["csT"] = f(csT)
    return d


_NC_CACHE = {}


def kernel(**inputs):
    inp = {k: np.asarray(v) for k, v in inputs.items()}
    NBC = 4
    if "nc" not in _NC_CACHE:
        _NC_CACHE["nc"] = build(NBC)
    nc = _NC_CACHE["nc"]
    shared = prep_shared(inp)
    in_maps = [prep_core(inp, shared, NBC * i, NBC) for i in range(8)]
    res = run_bass_kernel_spmd(nc, in_maps, core_ids=list(range(8)))
    return np.ascontiguousarray(np.concatenate([np.asarray(r["out"]) for r in res.results], 0).astype(np.float32))
```

```python
from concourse.bass_utils import run_bass_kernel_spmd
import numpy as np
import concourse.bass as bass
import concourse.mybir as mybir
from contextlib import ExitStack

F32 = mybir.dt.float32
BF16 = mybir.dt.bfloat16
AF = mybir.ActivationFunctionType
ALU = mybir.AluOpType
AX = mybir.AxisListType

N_DMA_SEMS = 40
SEM_ROLL = 30000


class Tok:
    __slots__ = ("sem", "val", "eng")

    def __init__(self, sem, val, eng):
        self.sem = sem
        self.val = val
        self.eng = eng


class Sched:
    def __init__(self, nc, stack):
        self.nc = nc
        self.stack = stack
        self.cengs = ["pe", "act", "dve", "pool"]
        self.all = ["pe", "act", "dve", "pool", "sp"]
        self.prog = {e: [] for e in self.all}
        self.nsem = 0
        self.sem = {e: self._newsem() for e in self.cengs}
        self.cnt = {e: 0 for e in self.cengs}
        self.waited = {e: {} for e in self.all}
        self.dsem = [self._newsem() for _ in range(N_DMA_SEMS)]
        self.dcnt = [0] * N_DMA_SEMS
        self.drr = 0
        self.res = {}
        self.pend = {e: {} for e in self.all}
        self.excl = set()
        self.old = []

    def _newsem(self):
        self.nsem += 1
        return self.stack.enter_context(self.nc.semaphore("s%d" % self.nsem))

    def _st(self, r):
        if isinstance(r, tuple):
            k = tuple(x if isinstance(x, (str, int)) else id(x) for x in r)
        elif isinstance(r, str):
            k = r
        else:
            k = id(r)
        st = self.res.get(k)
        if st is None:
            st = {"w": None, "r": {}}
            self.res[k] = st
        return st

    def _emit(self, eng, fn, reads, writes, dma):
        if self.excl:
            ex = [r for r in reads if (not isinstance(r, (tuple, str))) and id(r) in self.excl]
            if ex:
                reads = [r for r in reads if not ((not isinstance(r, (tuple, str))) and id(r) in self.excl)]
                writes = list(writes) + ex
        deps = []
        for r in reads:
            st = self._st(r)
            if st["w"] is not None:
                deps.append(st["w"])
        for w in writes:
            st = self._st(w)
            if st["w"] is not None:
                deps.append(st["w"])
            deps.extend(st["r"].values())
        need = {}
        for t in deps:
            if t.eng == eng and eng == "pe" and not dma:
                continue
            cur = need.get(id(t.sem))
            if cur is None or cur[1] < t.val:
                need[id(t.sem)] = (t.sem, t.val)
        if self.pend[eng]:
            for sid, (s_, v_) in self.pend[eng].items():
                cur = need.get(sid)
                if cur is None or cur[1] < v_:
                    need[sid] = (s_, v_)
            self.pend[eng] = {}
        if dma:
            i = self.drr
            self.drr = (self.drr + 1) % N_DMA_SEMS
            prev = self.dcnt[i]
            if prev > 0:
                cur = need.get(id(self.dsem[i]))
                if cur is None or cur[1] < prev:
                    need[id(self.dsem[i])] = (self.dsem[i], prev)
            self.dcnt[i] += 16
            tok = Tok(self.dsem[i], self.dcnt[i], "dma")
            inc = (self.dsem[i], 16)
        else:
            if self.cnt[eng] >= SEM_ROLL:
                self.old.append((self.sem[eng], self.cnt[eng]))
                self.sem[eng] = self._newsem()
                self.cnt[eng] = 0
            self.cnt[eng] += 1
            tok = Tok(self.sem[eng], self.cnt[eng], eng)
            inc = (self.sem[eng], 1)
        waits = []
        wd = self.waited[eng]
        for sid, (s, v) in need.items():
            if wd.get(sid, 0) >= v:
                continue
            wd[sid] = v
            waits.append((s, v))
        for r in reads:
            self._st(r)["r"][id(tok.sem)] = tok
        for w in writes:
            st = self._st(w)
            st["w"] = tok
            st["r"] = {}
        self.prog[eng].append((waits, fn, inc))
        return tok

    def op(self, eng, fn, reads=(), writes=()):
        return self._emit(eng, fn, reads, writes, False)

    def all_tokens(self):
        toks = {}
        for e in self.cengs:
            if self.cnt[e] > 0:
                toks[id(self.sem[e])] = (self.sem[e], self.cnt[e])
        for i in range(N_DMA_SEMS):
            if self.dcnt[i] > 0:
                toks[id(self.dsem[i])] = (self.dsem[i], self.dcnt[i])
        return toks

    def barrier(self):
        toks = self.all_tokens()
        for e in self.all:
            self.pend[e] = dict(toks)
        self.res = {}

    def dma(self, out, in_, reads=(), writes=(), q="sp", **kw):
        return self._emit(q, lambda e: e.dma_start(out=out, in_=in_, **kw), reads, writes, True)

    def mm(self, out, lhsT, rhs, start, stop, reads=(), writes=()):
        return self.op("pe", lambda e: e.matmul(out, lhsT, rhs, start=start, stop=stop), reads, writes)

    def tr(self, out, in_, ident, reads=(), writes=()):
        return self.op("pe", lambda e: e.transpose(out, in_, ident), reads, writes)

    def act(self, out, in_, func, bias=None, scale=None, accum_out=None, reads=(), writes=(), eng="act"):
        kw = {}
        if bias is not None:
            kw["bias"] = bias
        if scale is not None:
            kw["scale"] = scale
        if accum_out is not None:
            kw["accum_out"] = accum_out
        return self.op(eng, lambda e: e.activation(out, in_, func, **kw), reads, writes)

    def tt(self, eng, out, in0, in1, op, reads=(), writes=()):
        return self.op(eng, lambda e: e.tensor_tensor(out, in0, in1, op), reads, writes)

    def ts(self, eng, out, in0, s1, s2, op0, op1=None, reads=(), writes=(), accum_out=None):
        kw = {}
        if accum_out is not None:
            kw["accum_out"] = accum_out
        if op1 is None:
            return self.op(eng, lambda e: e.tensor_scalar(out, in0, s1, None, op0, **kw), reads, writes)
        return self.op(eng, lambda e: e.tensor_scalar(out, in0, s1, s2, op0, op1, **kw), reads, writes)

    def stt(self, eng, out, in0, scalar, in1, op0, op1, reads=(), writes=()):
        return self.op(eng, lambda e: e.scalar_tensor_tensor(out, in0, scalar, in1, op0, op1), reads, writes)

    def copy(self, eng, out, in_, reads=(), writes=()):
        if eng == "act":
            return self.op(eng, lambda e: e.copy(out, in_), reads, writes)
        return self.op(eng, lambda e: e.tensor_copy(out, in_), reads, writes)

    def memset(self, eng, ap, val, writes=()):
        return self.op(eng, lambda e: e.memset(ap, val), (), writes)

    def finish(self, final_tokens):
        nc = self.nc
        prog = self.prog
        engmap = {"pe": "tensor", "act": "scalar", "dve": "vector", "pool": "gpsimd", "sp": "sync"}
        fin = self.all_tokens()
        with nc.Block() as block:
            for ename in self.all:
                entries = prog[ename]
                is_sp = ename == "sp"

                def body(e, entries=entries, is_sp=is_sp):
                    for waits, fn, inc in entries:
                        for wi_, (s, v) in enumerate(waits):
                            e.wait_ge(s, v)
                            if wi_ + 1 < len(waits):
                                e.nop(nofuse=True)
                        ins = fn(e)
                        ins.then_inc(inc[0], inc[1])
                    if is_sp:
                        for (s, v) in fin.values():
                            e.wait_ge(s, v)

                getattr(block, engmap[ename])(body)


D = 1024
T = 2304
NT = 18
LT = 2048
ALPHA = (2.0 * 2) ** 0.25
NEG = -30000.0


def tcol(ti):
    return 1 + 128 * ti if ti < 2 else 2 + 128 * ti


BLOCKS = [(1, 256, 0)] + [(258 + 512 * j, 512, 256 + 512 * j) for j in range(4)]


ORDER = ['pw', 'pm', 'A', 'B', 'C', 'D', 'E', 'F', 'G', 'L1A', 'L1P', 'L1W', 'L1WA', 'L1U', 'L1MA', 'L1E', 'L1F', 'L1']


def build(NB, dbg=False, only0=False, stop='L1'):
    def want(nm):
        return ORDER.index(nm) <= ORDER.index(stop)

    nc = bass.Bass("TRN2", target_bir_lowering=False)
    R = NB + 1
    dd = {}

    def din(name, shape, dt=F32):
        dd[name] = nc.dram_tensor(name, list(shape), dt, kind="ExternalInput").ap()
        return dd[name]

    def dscr(name, shape, dt=F32, out=False):
        return nc.dram_tensor(name, list(shape), dt, kind="ExternalOutput" if out else "Internal").ap()

    x_d = din("x", [NB, LT, D]); ctx_d = din("ctx", [NB, 256, D]); csT_d = din("csT", [128, 8, R])
    adaw_d = din("ada_w", [2, D, 6144]); adab_d = din("ada_b", [2, 6144])
    lng_d = din("ln_g", [4, D]); lnb_d = din("ln_b", [4, D])
    evwin_d = din("ev_w_in", [D, 3600]); aconvT_d = din("a_convT", [128, 4, 3]); bconv_d = din("ev_b_conv", [3, 1536])
    alog_d = din("alog", [1, 8]); dtb_d = din("dtbias", [1, 8]); bnorm_d = din("bnorm", [1, 128]); evwout_d = din("ev_w_out", [D, D])
    odwin_d = din("od_w_in", [D, 1440]); odwinsw_d = din("od_w_in_sw", [D, 1440])
    wkr_d = din("wkr", [D, 96]); wkrsw_d = din("wkr_sw", [D, 96])
    sink_d = din("sink", [1, 8]); qnT_d = din("qnormT", [128, 3]); kvnT_d = din("kvnormT", [128, 2])
    wuq_d = din("wuq", [384, 768]); wuqsw_d = din("wuq_sw", [384, 768]); wukv_d = din("wukv", [256, 1024]); odwout_d = din("od_w_out", [D, D])
    rw_d = din("router_w", [D, 16]); rb_d = din("router_bias", [1, 16])
    mg_d = din("moe_g", [32768, 512]); mu_d = din("moe_u", [32768, 512]); md_d = din("moe_d", [16384, 1024])
    idn_d = din("idn", [128, 128]); masks_d = din("masks", [14, 128, 128])
    rope64_d = din("rope64", [2, 64, LT]); rope32_d = din("rope32", [2, 96, LT])
    out_d = dscr("out", [NB, LT, D], F32, out=True)

    evwin_b = dscr("evwin_b", [D, 3600], BF16); wB_b = [dscr("wB%d_b" % j, [D, 1536], BF16) for j in range(3)]
    evwout_b = dscr("evwout_b", [D, D], BF16)
    odwin_b = dscr("odwin_b", [D, 1440], BF16); odwinsw_b = dscr("odwinsw_b", [D, 1440], BF16)
    wkr_b = dscr("wkr_b", [D, 96], BF16); wkrsw_b = dscr("wkrsw_b", [D, 96], BF16)
    wuq_b = dscr("wuq_b", [384, 768], BF16); wuqsw_b = dscr("wuqsw_b", [384, 768], BF16); wukv_b = dscr("wukv_b", [256, 1024], BF16)
    odwout_b = dscr("odwout_b", [D, D], BF16)
    modrow_s = dscr("modrow_s", [2, R, 6144], F32, out=dbg)
    qT_s = dscr("qT_s", [4, 128, T], BF16); kT_s = dscr("kT_s", [4, 128, T], BF16)
    k_s = dscr("k_s", [T, 512], BF16); v_s = dscr("v_s", [T, 512], BF16); gate_s = dscr("gate_s", [T, 512]); o_s = dscr("o_s", [T, 512])
    xa_s = dscr("xa_s", [NB, 2, T, D], F32, out=dbg)
    xb_s = dscr("xb_s", [NB, T, D], F32, out=dbg)
    h2T_s = dscr("h2T_s", [8, 128, T], BF16)
    cat_dbg = dscr("cat_dbg", [128, 8, LT], BF16, out=True) if dbg else None
    ymix_s = dscr("ymix_s", [NB, 2, T, D], F32, out=dbg) if dbg else None

    with ExitStack() as st0:
        S = Sched(nc, st0)
        cnt = [0]

        def sb(stack, shape, dt=F32, name=None):
            cnt[0] += 1
            return stack.enter_context(nc.sbuf_tensor("%s_%d" % (name or "t", cnt[0]), list(shape), dt))

        PB = [st0.enter_context(nc.psum_tensor("pb%d" % i, [128, 512], F32)) for i in range(8)]
        pbi = [0]
        S.excl = set(id(p_) for p_ in PB)

        def pb():
            p = PB[pbi[0] % 8]
            pbi[0] += 1
            return p

        ld_rr = [0]

        def rr3():
            ld_rr[0] += 1
            return ("dve", "pool", "act")[ld_rr[0] % 3]

        ident = sb(st0, [128, 128], name="ident"); S.dma(ident[:], idn_d, writes=[ident])
        ones = sb(st0, [128, 128], name="ones"); S.memset("pool", ones[:], 1.0, writes=[ones])
        MK = sb(st0, [128, 14, 128], name="masks")
        S.dma(MK[:], masks_d.rearrange("m p f -> p m f"), writes=[MK])
        modT = sb(st0, [128, 2, 48, R], name="modT")
        MKb = sb(st0, [128, 14, 128], BF16, name="masksb"); S.copy("dve", MKb[:], MK[:], reads=[MK], writes=[MKb])
        identb = sb(st0, [128, 128], BF16, name="identb"); S.copy("dve", identb[:], ident[:], reads=[ident], writes=[identb])
        onesb = sb(st0, [128, 128], BF16, name="onesb"); S.memset("pool", onesb[:], 1.0, writes=[onesb])
        rwt = sb(st0, [128, 8, 16], name="rw"); S.dma(rwt[:], rw_d.rearrange("(kc p) e -> p kc e", p=128), writes=[rwt])
        rbias = sb(st0, [128, 16], name="rbias"); S.dma(rbias[:], rb_d.to_broadcast([128, 16]), writes=[rbias])
        aconvT = sb(st0, [128, 4, 3], name="aconvT"); S.dma(aconvT[:], aconvT_d, writes=[aconvT])
        negexpA = sb(st0, [128, 8], name="negexpA"); dtb = sb(st0, [128, 8], name="dtb")
        S.dma(negexpA[:], alog_d.to_broadcast([128, 8]), writes=[negexpA]); S.dma(dtb[:], dtb_d.to_broadcast([128, 8]), writes=[dtb])
        S.act(negexpA[:], negexpA[:], AF.Exp, reads=[negexpA], writes=[negexpA])
        S.ts("dve", negexpA[:], negexpA[:], -1.0, None, ALU.mult, reads=[negexpA], writes=[negexpA])
        bnorm = sb(st0, [128, 128], name="bnorm"); S.dma(bnorm[:], bnorm_d.to_broadcast([128, 128]), writes=[bnorm])
        expsink = sb(st0, [128, 8], name="expsink"); S.dma(expsink[:], sink_d.to_broadcast([128, 8]), writes=[expsink])
        S.act(expsink[:], expsink[:], AF.Exp, reads=[expsink], writes=[expsink])
        qnT = sb(st0, [128, 3], name="qnT"); S.dma(qnT[:], qnT_d, writes=[qnT])
        kvnT = sb(st0, [128, 2], name="kvnT"); S.dma(kvnT[:], kvnT_d, writes=[kvnT])

        with ExitStack() as st:
            NSTG = 6
            stg = [sb(st, [128, 4096], F32, "stg") for _ in range(NSTG)]
            stgb = [sb(st, [128, 4096], BF16, "stgb") for _ in range(NSTG)]
            bcv = sb(st, [128, 3, 1536], F32, "bcv")
            for j in range(3):
                S.dma(bcv[:, j, :], bconv_d[j:j + 1, :].to_broadcast([128, 1536]), writes=[(bcv, j)])
            ui = [0]

            def conv(src, dst, rows, cols, scale=None):
                nrc = rows // 128
                G = max(1, min(nrc, 4096 // cols)) if cols <= 4096 else 1
                if scale is not None:
                    G = 1
                while nrc % G:
                    G -= 1
                for r0 in range(0, nrc, G):
                    i = ui[0] % NSTG
                    ui[0] += 1
                    eng = ("dve", "pool", "act")[i % 3]
                    a = stg[i][:, 0:G * cols].rearrange("p (g c) -> p g c", g=G)
                    b = stgb[i][:, 0:G * cols].rearrange("p (g c) -> p g c", g=G)
                    sv = src[r0 * 128:(r0 + G) * 128, :].rearrange("(g p) c -> p g c", p=128)
                    dv = dst[r0 * 128:(r0 + G) * 128, :].rearrange("(g p) c -> p g c", p=128)
                    S.dma(a, sv, writes=[stg[i]])
                    if scale is None:
                        S.copy(eng, b, a, reads=[stg[i]], writes=[stgb[i]])
                    else:
                        e2 = "pool" if eng == "act" else eng
                        S.tt(e2, b[:, 0, :], a[:, 0, :], scale, ALU.mult, reads=[stg[i], (bcv, 0), (bcv, 1), (bcv, 2)], writes=[stgb[i]])
                    S.dma(dv, b, reads=[stgb[i]])

            conv(evwin_d, evwin_b, D, 3600)
            for j in range(3):
                conv(evwin_d[:, 1536:3072], wB_b[j], D, 1536, scale=bcv[:, j, :])
            conv(evwout_d, evwout_b, D, D)
            conv(odwin_d, odwin_b, D, 1440); conv(odwinsw_d, odwinsw_b, D, 1440)
            conv(wkr_d, wkr_b, D, 96); conv(wkrsw_d, wkrsw_b, D, 96)
            conv(wuq_d, wuq_b, 384, 768); conv(wuqsw_d, wuqsw_b, 384, 768); conv(wukv_d, wukv_b, 256, 1024)
            conv(odwout_d, odwout_b, D, D)
            S.barrier()
        with ExitStack() as st:
          if want('pm'):
            csT = sb(st, [128, 8, R], F32, "csT")
            S.dma(csT[:], csT_d, writes=[csT])
            S.act(csT[:], csT[:], AF.Silu, reads=[csT], writes=[csT])
            awt = [sb(st, [128, 8, 1536], F32, "awt") for _ in range(2)]
            modrow = sb(st, [R, 6144], F32, "modrow")
            abr = sb(st, [R, 6144], F32, "abr")
            for l in range(2):
                S.dma(abr[:], adab_d[l:l + 1, :].to_broadcast([R, 6144]), reads=[modrow], writes=[abr])
                for q in range(4):
                    aw = awt[q % 2]
                    S.dma(aw[:], adaw_d[l, :, q * 1536:(q + 1) * 1536].rearrange("(kc p) n -> p kc n", p=128), writes=[aw])
                    for nb_ in range(3):
                        p = pb()
                        c0 = q * 1536 + nb_ * 512
                        for kc in range(8):
                            S.mm(p[0:R, :], csT[:, kc, :], aw[:, kc, nb_ * 512:(nb_ + 1) * 512], kc == 0, kc == 7, reads=[csT, aw], writes=[p])
                        S.tt("dve", modrow[:, c0:c0 + 512], p[0:R, :], abr[:, c0:c0 + 512], ALU.add, reads=[p, abr], writes=[modrow])
                S.dma(modrow_s[l], modrow[:], reads=[modrow])
                p = pb()
                for ch in range(48):
                    S.tr(p[:, ch * R:(ch + 1) * R], modrow[0:R, ch * 128:(ch + 1) * 128], ident[0:R, 0:R], reads=[modrow, ident], writes=[p])
                S.copy("dve", modT[:, l, :, :], p[:, 0:48 * R].rearrange("p (c r) -> p c r", r=R), reads=[p], writes=[modT])
                for c0 in (8, 32):
                    S.ts("dve", modT[:, l, c0:c0 + 8, :], modT[:, l, c0:c0 + 8, :], 1.0, None, ALU.add, reads=[modT], writes=[modT])
            S.barrier()

        def xsrc(layer, b, ti):
            if layer == 0:
                return ctx_d[b, ti * 128:(ti + 1) * 128, :] if ti < 2 else x_d[b, (ti - 2) * 128:(ti - 1) * 128, :]
            return xb_s[b, ti * 128:(ti + 1) * 128, :]

        def phase_A(layer, b, hT, tiles, st):
            xin = [sb(st, [128, D], F32, "xin") for _ in range(2)]
            for n, ti in enumerate(tiles):
                xt = xin[n % 2]
                r = NB if ti < 2 else b
                S.dma(xt[:], xsrc(layer, b, ti), writes=[xt])
                for half in range(2):
                    p = pb()
                    for k4 in range(4):
                        kc = half * 4 + k4
                        S.tr(p[:, k4 * 128:(k4 + 1) * 128], xt[:, kc * 128:(kc + 1) * 128], ident[:], reads=[xt], writes=[p])
                    for k4 in range(4):
                        kc = half * 4 + k4
                        dst = hT[:, kc, tcol(ti):tcol(ti) + 128]
                        if half == 0:
                            S.act(dst, p[:, k4 * 128:(k4 + 1) * 128], AF.Identity, bias=modT[:, layer, kc, r:r + 1],
                                  scale=modT[:, layer, 8 + kc, r:r + 1], reads=[p], writes=[(hT, ti, kc)])
                        else:
                            S.ts("dve", dst, p[:, k4 * 128:(k4 + 1) * 128], modT[:, layer, 8 + kc, r:r + 1], modT[:, layer, kc, r:r + 1],
                                 ALU.mult, ALU.add, reads=[p], writes=[(hT, ti, kc)])

        def layer_norm_tile(st_tiles, rt, lng, lnb, outt):
            stats, ag, sm = st_tiles
            for hf in range(2):
                S.op("dve", lambda e, hf=hf: e.bn_stats(stats[:, hf, :], rt[:, hf * 512:(hf + 1) * 512]), reads=[rt], writes=[stats])
            S.op("dve", lambda e: e.bn_aggr(ag[:], stats[:].rearrange('p a b -> p (a b)')), reads=[stats], writes=[ag])
            S.act(sm[:, 0:1], ag[:, 1:2], AF.Sqrt, bias=1e-5, reads=[ag], writes=[sm])
            S.op("dve", lambda e: e.reciprocal(sm[:, 1:2], sm[:, 0:1]), reads=[sm], writes=[sm])
            S.stt("dve", sm[:, 2:3], ag[:, 0:1], -1.0, sm[:, 1:2], ALU.mult, ALU.mult, reads=[ag, sm], writes=[sm])
            S.act(rt[:], rt[:], AF.Identity, bias=sm[:, 2:3], scale=sm[:, 1:2], reads=[rt, sm], writes=[rt])
            S.tt("pool", rt[:], rt[:], lng[:], ALU.mult, reads=[rt, lng], writes=[rt])
            S.tt("pool", outt[:], rt[:], lnb[:], ALU.add, reads=[rt, lnb], writes=[outt])

        def run_pipe(gens, depth):
            active = []
            it = iter(gens)
            fin = False
            while True:
                while len(active) < depth and not fin:
                    try:
                        active.append(next(it))
                    except StopIteration:
                        fin = True
                if not active:
                    break
                for g_ in list(active):
                    try:
                        next(g_)
                    except StopIteration:
                        active.remove(g_)

        def ln_stats(rt, stats, ag, sm):
            for hf in range(2):
                S.op("dve", lambda e, hf=hf: e.bn_stats(stats[:, hf, :], rt[:, hf * 512:(hf + 1) * 512]), reads=[rt], writes=[stats])
            S.op("dve", lambda e: e.bn_aggr(ag[:], stats[:].rearrange('p a b -> p (a b)')), reads=[stats], writes=[ag])
            S.act(sm[:, 0:1], ag[:, 1:2], AF.Sqrt, bias=1e-5, reads=[ag], writes=[sm])
            S.op("dve", lambda e: e.reciprocal(sm[:, 1:2], sm[:, 0:1]), reads=[sm], writes=[sm])
            S.stt("dve", sm[:, 2:3], ag[:, 0:1], -1.0, sm[:, 1:2], ALU.mult, ALU.mult, reads=[ag, sm], writes=[sm])

        def ln_apply(rt, sm, lng, lnb, outt):
            S.act(rt[:], rt[:], AF.Identity, bias=sm[:, 2:3], scale=sm[:, 1:2], reads=[rt, sm], writes=[rt])
            S.tt("pool", rt[:], rt[:], lng[:], ALU.mult, reads=[rt, lng], writes=[rt])
            S.tt("pool", outt[:], rt[:], lnb[:], ALU.add, reads=[rt, lnb], writes=[outt])

        def phase_E(layer, b, catT, tiles, gates, st):
            nt = len(tiles)
            NBUF = 4
            wo = sb(st, [128, 8, D], BF16, "wo")
            wsrc = evwout_b if layer == 0 else odwout_b
            S.dma(wo[:], wsrc.rearrange("(kc p) n -> p kc n", p=128), writes=[wo])
            lng = sb(st, [128, D], F32, "lng"); lnb = sb(st, [128, D], F32, "lnb")
            S.dma(lng[:], lng_d[2 * layer:2 * layer + 1, :].to_broadcast([128, D]), writes=[lng])
            S.dma(lnb[:], lnb_d[2 * layer:2 * layer + 1, :].to_broadcast([128, D]), writes=[lnb])
            g1 = [sb(st, [128, D], F32, "g1") for _ in range(2)]
            S.dma(g1[0][:], modrow_s[layer, NB:NB + 1, 2048:3072].to_broadcast([128, D]), writes=[g1[0]])
            S.dma(g1[1][:], modrow_s[layer, b:b + 1, 2048:3072].to_broadcast([128, D]), writes=[g1[1]])
            xin = [sb(st, [128, D], F32, "xin") for _ in range(NBUF)]
            rts = [sb(st, [128, D], F32, "rt") for _ in range(NBUF)]
            xns = [sb(st, [128, D], F32, "xn") for _ in range(NBUF)]
            h2f = [sb(st, [128, 8, 128], F32, "h2f") for _ in range(NBUF)]
            h2b = [sb(st, [128, 8, 128], BF16, "h2b") for _ in range(NBUF)]
            statsL = [sb(st, [128, 2, 6], F32, "stats") for _ in range(NBUF)]
            agL = [sb(st, [128, 2], F32, "ag") for _ in range(NBUF)]
            smL = [sb(st, [128, 4], F32, "sm") for _ in range(NBUF)]
            aff = sb(st, [128, nt, 16], F32, "aff")

            def tile_gen(n, ti):
                k = n % NBUF
                xt = xin[k]; rt = rts[k]; xn = xns[k]; hf_ = h2f[k]; hb_ = h2b[k]; stats = statsL[k]; ag = agL[k]; sm = smL[k]
                r = NB if ti < 2 else b
                gt = g1[0] if ti < 2 else g1[1]
                S.dma(xt[:], xsrc(layer, b, ti), writes=[xt])
                c0 = tcol(ti) if layer == 0 else (ti - 2) * 128
                for hf in range(2):
                    p = pb()
                    for kc in range(8):
                        S.mm(p[:], catT[:, kc, c0:c0 + 128], wo[:, kc, hf * 512:(hf + 1) * 512], kc == 0, kc == 7, reads=[wo], writes=[p])
                    if dbg:
                        S.copy("act", rt[:, hf * 512:(hf + 1) * 512], p[:], reads=[p], writes=[rt])
                        S.dma(ymix_s[b, layer, ti * 128:(ti + 1) * 128, hf * 512:(hf + 1) * 512], rt[:, hf * 512:(hf + 1) * 512], reads=[rt])
                    S.tt("dve", rt[:, hf * 512:(hf + 1) * 512], p[:], gt[:, hf * 512:(hf + 1) * 512], ALU.mult, reads=[p, gt], writes=[rt])
                yield
                S.stt("dve", rt[:], xt[:], ALPHA, rt[:], ALU.mult, ALU.add, reads=[xt, rt], writes=[rt])
                ln_stats(rt, stats, ag, sm)
                yield
                ln_apply(rt, sm, lng, lnb, xn)
                S.dma(xa_s[b, layer, ti * 128:(ti + 1) * 128, :], xn[:], reads=[xn])
                yield
                for half in range(2):
                    p = pb()
                    for k4 in range(4):
                        kc = half * 4 + k4
                        S.tr(p[:, k4 * 128:(k4 + 1) * 128], xn[:, kc * 128:(kc + 1) * 128], ident[:], reads=[xn], writes=[p])
                    for k4 in range(4):
                        kc = half * 4 + k4
                        if half == 0:
                            S.act(hf_[:, kc, :], p[:, k4 * 128:(k4 + 1) * 128], AF.Identity, bias=modT[:, layer, 24 + kc, r:r + 1],
                                  scale=modT[:, layer, 32 + kc, r:r + 1], reads=[p], writes=[hf_])
                        else:
                            S.ts("dve", hf_[:, kc, :], p[:, k4 * 128:(k4 + 1) * 128], modT[:, layer, 32 + kc, r:r + 1], modT[:, layer, 24 + kc, r:r + 1],
                                 ALU.mult, ALU.add, reads=[p], writes=[hf_])
                yield
                S.copy("pool", hb_[:], hf_[:], reads=[hf_], writes=[hb_])
                S.dma(h2T_s.rearrange("k p t -> p k t")[:, :, n * 128:(n + 1) * 128], hb_[:], reads=[hb_])
                p = pb()
                for kc in range(8):
                    S.mm(p[:, 0:16], hf_[:, kc, :], rwt[:, kc, :], kc == 0, kc == 7, reads=[hf_], writes=[p])
                S.act(aff[:, n, :], p[:, 0:16], AF.Sigmoid, reads=[p], writes=[(aff, n)])

            run_pipe((tile_gen(n, ti) for n, ti in enumerate(tiles)), 3)
            router_batched(aff, gates, nt, st)

        def router_batched(aff, gates, nt, st):
            G4 = nt * 4
            sel = sb(st, [128, nt, 16], F32, "r_sel"); t1 = sb(st, [128, nt, 16], F32, "r_t1"); t2 = sb(st, [128, nt, 16], F32, "r_t2")
            m1 = sb(st, [128, G4], F32, "r_m1"); sec = sb(st, [128, G4], F32, "r_sec"); gs = sb(st, [128, G4], F32, "r_gs")
            gm = sb(st, [128, G4], F32, "r_gm"); tm = sb(st, [128, G4], F32, "r_tm")
            s1 = sb(st, [128, nt], F32, "r_s1"); s2 = sb(st, [128, nt], F32, "r_s2"); den = sb(st, [128, nt], F32, "r_den")
            allaff = [(aff, n) for n in range(nt)]
            RS = "rsres"

            def g4(t):
                return t[:].rearrange("p n (g e) -> p (n g) e", g=4)

            def bc44(t):
                return t[:].unsqueeze(2).to_broadcast([128, G4, 4])

            def bc16(t):
                return t[:].unsqueeze(2).to_broadcast([128, nt, 16])

            def dv(fn, *a, **k):
                return S.op("dve", fn, reads=allaff + [RS], writes=[RS])
            dv(lambda e: e.tensor_tensor(sel[:], aff[:], rbias[:].unsqueeze(1).to_broadcast([128, nt, 16]), ALU.add))
            dv(lambda e: e.tensor_reduce(m1[:], g4(sel), AX.X, ALU.max))
            dv(lambda e: e.tensor_tensor(g4(t1), g4(sel), bc44(m1), ALU.is_lt))
            dv(lambda e: e.tensor_scalar(t2[:], t1[:], 1.0, 1e9, ALU.subtract, ALU.mult))
            dv(lambda e: e.tensor_tensor(t1[:], t1[:], sel[:], ALU.mult))
            dv(lambda e: e.tensor_tensor(t2[:], t2[:], t1[:], ALU.add))
            dv(lambda e: e.tensor_reduce(sec[:], g4(t2), AX.X, ALU.max))
            dv(lambda e: e.tensor_tensor(gs[:], m1[:], sec[:], ALU.add))
            gs3 = gs[:].rearrange("p (n g) -> p n g", g=4)
            dv(lambda e: e.tensor_reduce(s1[:], gs3, AX.X, ALU.max))
            dv(lambda e: e.tensor_tensor(gm[:].rearrange("p (n g) -> p n g", g=4), gs3, s1[:].unsqueeze(2).to_broadcast([128, nt, 4]), ALU.is_ge))
            dv(lambda e: e.tensor_tensor(g4(t1), g4(sel), bc44(gm), ALU.mult))
            dv(lambda e: e.tensor_scalar(tm[:], gm[:], 1.0, 1e9, ALU.subtract, ALU.mult))
            dv(lambda e: e.tensor_tensor(g4(t1), g4(t1), bc44(tm), ALU.add))
            dv(lambda e: e.tensor_reduce(s1[:], t1[:], AX.X, ALU.max))
            dv(lambda e: e.tensor_tensor(t2[:], t1[:], bc16(s1), ALU.is_lt))
            dv(lambda e: e.tensor_tensor(sel[:], t1[:], t2[:], ALU.mult))
            dv(lambda e: e.tensor_scalar(t2[:], t2[:], 1.0, 1e9, ALU.subtract, ALU.mult))
            dv(lambda e: e.tensor_tensor(sel[:], sel[:], t2[:], ALU.add))
            dv(lambda e: e.tensor_reduce(s2[:], sel[:], AX.X, ALU.max))
            dv(lambda e: e.tensor_tensor(t2[:], t1[:], bc16(s2), ALU.is_ge))
            dv(lambda e: e.tensor_tensor(t2[:], t2[:], aff[:], ALU.mult))
            dv(lambda e: e.tensor_reduce(den[:], t2[:], AX.X, ALU.add))
            dv(lambda e: e.reciprocal(den[:], den[:]))
            S.op("dve", lambda e: e.tensor_tensor(gates[:], t2[:], bc16(den), ALU.mult), reads=[RS], writes=[gates])

        def phase_F(layer, b, h2T, gates, ntile, outacc, st):
            wgu = [sb(st, [128, 2, 8, 512], BF16, "wgu") for _ in range(2)]
            wdn = [sb(st, [128, 4, D], BF16, "wdn") for _ in range(2)]
            actT = [sb(st, [128, 4, 512], BF16, "actT") for _ in range(2)]
            sl = [sb(st, [128, 512], F32, "sl") for _ in range(2)]
            stgF = [sb(st, [128, 2048], F32, "stgF") for _ in range(2)]
            ntok = ntile * 128
            blocks = [(c, min(512, ntok - c)) for c in range(0, ntok, 512)]
            pi = [0]

            def pieces(e):
                wg = wgu[e % 2]; wd = wdn[e % 2]
                r0 = (layer * 16 + e) * 1024
                r1 = (layer * 16 + e) * 512
                out = []
                for which, src in ((0, mg_d), (1, mu_d)):
                    for half in range(2):
                        src_ap = src[r0 + half * 512:r0 + (half + 1) * 512, :].rearrange("(kc p) f -> p kc f", p=128)
                        out.append((wg[:, which, half * 4:(half + 1) * 4, :], src_ap, (wg, which, half), 4))
                for half in range(2):
                    src_ap = md_d[r1 + half * 256:r1 + (half + 1) * 256, :].rearrange("(fc p) n -> p fc n", p=128)
                    out.append((wd[:, half * 2:(half + 1) * 2, :], src_ap, (wd, half), 2))
                return out

            def load_cast(pc):
                dst_ap, src_ap, key, G = pc
                sg = stgF[pi[0] % 2]; pi[0] += 1
                sgv = sg[:, 0:2048].rearrange("p (g c) -> p g c", g=G)
                S.dma(sgv, src_ap, writes=[sg])
                S.copy("pool", dst_ap, sgv, reads=[sg], writes=[key])

            def down(at, wd, e, c0, w):
                for j in range(w // 128):
                    n = c0 // 128 + j
                    for hf in range(2):
                        pd = pb()
                        for fc in range(4):
                            S.mm(pd[:], at[:, fc, j * 128:(j + 1) * 128], wd[:, fc, hf * 512:(hf + 1) * 512], fc == 0, fc == 3,
                                 reads=[(wd, fc // 2), (at, 0), (at, 1), (at, 2), (at, 3)], writes=[pd])
                        dst = outacc[:, n, hf * 512:(hf + 1) * 512]
                        if e == 0:
                            S.ts("dve", dst, pd[:], gates[:, n, e:e + 1], None, ALU.mult, reads=[pd], writes=[(outacc, n, hf)])
                        else:
                            S.stt("dve", dst, pd[:], gates[:, n, e:e + 1], dst, ALU.mult, ALU.add, reads=[pd], writes=[(outacc, n, hf)])

            for pc in pieces(0):
                load_cast(pc)
            it = 0
            pending = None
            for e in range(16):
                wg = wgu[e % 2]; wd = wdn[e % 2]
                nxt = pieces(e + 1) if e + 1 < 16 else []
                for bi, (c0, w) in enumerate(blocks):
                    at = actT[it % 2]; it += 1
                    for fc in range(4):
                        pg = pb(); pu = pb()
                        for kc in range(8):
                            S.mm(pg[:, 0:w], wg[:, 0, kc, fc * 128:(fc + 1) * 128], h2T[:, kc, c0:c0 + w], kc == 0, kc == 7, reads=[(wg, 0, kc // 4)], writes=[pg])
                        for kc in range(8):
                            S.mm(pu[:, 0:w], wg[:, 1, kc, fc * 128:(fc + 1) * 128], h2T[:, kc, c0:c0 + w], kc == 0, kc == 7, reads=[(wg, 1, kc // 4)], writes=[pu])
                        s_ = sl[fc % 2]
                        S.act(s_[:, 0:w], pg[:, 0:w], AF.Silu, reads=[pg], writes=[s_])
                        S.tt("dve", at[:, fc, 0:w], s_[:, 0:w], pu[:, 0:w], ALU.mult, reads=[s_, pu], writes=[(at, fc)])
                    if pending is not None:
                        down(*pending)
                    pending = (at, wd, e, c0, w)
                    k = 2 if bi == 0 else 1
                    for _ in range(k):
                        if nxt:
                            load_cast(nxt.pop(0))
                while nxt:
                    load_cast(nxt.pop(0))
            down(*pending)

        def phase_G(layer, b, tiles, outacc, st):
            NBUF = 4
            lng = sb(st, [128, D], F32, "lng2"); lnb = sb(st, [128, D], F32, "lnb2")
            S.dma(lng[:], lng_d[2 * layer + 1:2 * layer + 2, :].to_broadcast([128, D]), writes=[lng])
            S.dma(lnb[:], lnb_d[2 * layer + 1:2 * layer + 2, :].to_broadcast([128, D]), writes=[lnb])
            g2 = [sb(st, [128, D], F32, "g2") for _ in range(2)]
            S.dma(g2[0][:], modrow_s[layer, NB:NB + 1, 5120:6144].to_broadcast([128, D]), writes=[g2[0]])
            S.dma(g2[1][:], modrow_s[layer, b:b + 1, 5120:6144].to_broadcast([128, D]), writes=[g2[1]])
            xin = [sb(st, [128, D], F32, "xin2") for _ in range(NBUF)]
            rts = [sb(st, [128, D], F32, "rt2") for _ in range(NBUF)]
            xns = [sb(st, [128, D], F32, "xn2") for _ in range(NBUF)]
            statsL = [sb(st, [128, 2, 6], F32, "stats2") for _ in range(NBUF)]
            agL = [sb(st, [128, 2], F32, "ag2") for _ in range(NBUF)]
            smL = [sb(st, [128, 4], F32, "sm2") for _ in range(NBUF)]

            def tile_gen(n, ti):
                k = n % NBUF
                xt = xin[k]; rt = rts[k]; xn = xns[k]; stats = statsL[k]; ag = agL[k]; sm = smL[k]
                gt = g2[0] if ti < 2 else g2[1]
                S.dma(xt[:], xa_s[b, layer, ti * 128:(ti + 1) * 128, :], writes=[xt])
                S.tt("pool", rt[:], outacc[:, n, :], gt[:], ALU.mult, reads=[gt, (outacc, n, 0), (outacc, n, 1)], writes=[rt])
                yield
                S.stt("dve", rt[:], xt[:], ALPHA, rt[:], ALU.mult, ALU.add, reads=[xt, rt], writes=[rt])
                ln_stats(rt, stats, ag, sm)
                yield
                ln_apply(rt, sm, lng, lnb, xn)
                if layer == 0:
                    S.dma(xb_s[b, ti * 128:(ti + 1) * 128, :], xn[:], reads=[xn])
                else:
                    S.dma(out_d[b, (ti - 2) * 128:(ti - 1) * 128, :], xn[:], reads=[xn])

            run_pipe((tile_gen(n, ti) for n, ti in enumerate(tiles)), 3)

        def post_E(layer, b, catT, tiles, gates):
            if not want('E'):
                return
            with ExitStack() as st2:
                phase_E(layer, b, catT, tiles, gates, st2)
                S.barrier()

        def post_FG(layer, b, tiles, gates):
            nt = len(tiles)
            if not want('F'):
                return
            with ExitStack() as st:
                h2T = sb(st, [128, 8, nt * 128], BF16, "h2T")
                for kc in range(8):
                    S.dma(h2T[:, kc, :], h2T_s[kc, :, 0:nt * 128], writes=[h2T])
                outacc = sb(st, [128, nt, D], F32, "outacc")
                with ExitStack() as st2:
                    phase_F(layer, b, h2T, gates, nt, outacc, st2)
                    S.barrier()
                if not want('G'):
                    return
                with ExitStack() as st2:
                    phase_G(layer, b, tiles, outacc, st2)
                    S.barrier()

        def phase_B(b, hT, catT, st):
            wA = [sb(st, [128, 3, 8, 128], BF16, "wA") for _ in range(2)]
            u = sb(st, [128, 2307], F32, "u"); p0s = sb(st, [128, 2307], F32, "p0s"); y = sb(st, [128, 2307], F32, "y")
            t1 = [sb(st, [128, 512], F32, "t1") for _ in range(2)]
            for c in (0, 257, 2306):
                S.memset("pool", u[:, c:c + 1], 0.0, writes=[(u, "pad")])
            bi = 0
            for ch in range(4):
                w = wA[ch % 2]
                for which in range(3):
                    cc = which * 512 + ch * 128
                    S.dma(w[:, which], evwin_b[:, cc:cc + 128].rearrange("(kc p) n -> p kc n", p=128), writes=[(w, which)])
                for (c0, wd_, t0) in BLOCKS:
                    pp = [pb(), pb(), pb()]
                    for which in range(3):
                        for kc in range(8):
                            S.mm(pp[which][:, 0:wd_], w[:, which, kc, :], hT[:, kc, c0:c0 + wd_], kc == 0, kc == 7, reads=[(w, which)], writes=[pp[which]])
                    tt_ = t1[bi % 2]; bi += 1
                    S.copy("act", tt_[:, 0:wd_], pp[1][:, 0:wd_], reads=[pp[1]], writes=[tt_])
                    S.tt("dve", u[:, c0:c0 + wd_], tt_[:, 0:wd_], pp[2][:, 0:wd_], ALU.mult, reads=[tt_, pp[2]], writes=[(u, c0)])
                    S.copy("act", p0s[:, c0:c0 + wd_], pp[0][:, 0:wd_], reads=[pp[0]], writes=[(p0s, c0)])
                allu = [(u, c0) for c0, _, _ in BLOCKS] + [(u, "pad")]
                allp = [(p0s, c0) for c0, _, _ in BLOCKS]
                S.ts("dve", y[:, 1:2306], u[:, 1:2306], aconvT[:, ch, 1:2], None, ALU.mult, reads=allu, writes=[y])
                S.stt("dve", y[:, 1:2306], u[:, 0:2305], aconvT[:, ch, 0:1], y[:, 1:2306], ALU.mult, ALU.add, reads=allu + [y], writes=[y])
                S.stt("dve", y[:, 1:2306], u[:, 2:2307], aconvT[:, ch, 2:3], y[:, 1:2306], ALU.mult, ALU.add, reads=allu + [y], writes=[y])
                S.tt("pool", catT[:, ch, 1:2306], y[:, 1:2306], p0s[:, 1:2306], ALU.mult, reads=[y] + allp, writes=[(catT, ch)])

        def phase_C(b, hT, BG, st):
            NBUF = 4
            wB = [sb(st, [128, 3, 8, 128], BF16, "wB") for _ in range(2)]
            s_ = [sb(st, [128, 512], F32, "s_") for _ in range(NBUF)]
            sq_ = [sb(st, [128, 512], BF16, "sq_") for _ in range(NBUF)]
            rin = [sb(st, [128, 512], F32, "rin") for _ in range(NBUF)]
            kn = [sb(st, [128, 512], BF16, "kn") for _ in range(NBUF)]
            ktk = [sb(st, [128, 4, 128], BF16, "ktk") for _ in range(NBUF)]

            def qk_gen(i, which, h, w, c0, wd_, t0):
                p = pb(); n = 0
                for j in range(3):
                    for kc in range(8):
                        S.mm(p[:, 0:wd_], w[:, j, kc, :], hT[:, kc, c0 + j - 1:c0 + j - 1 + wd_], n == 0, n == 23, reads=[(w, j)], writes=[p])
                        n += 1
                s = s_[i % NBUF]; sq = sq_[i % NBUF]; ri = rin[i % NBUF]; qn = kn[i % NBUF]; kt = ktk[i % NBUF]
                S.act(s[:, 0:wd_], p[:, 0:wd_], AF.Silu, reads=[p], writes=[s])
                S.tt("pool", sq[:, 0:wd_], s[:, 0:wd_], s[:, 0:wd_], ALU.mult, reads=[s], writes=[sq])
                yield
                p2 = pb()
                S.mm(p2[:, 0:wd_], onesb[:], sq[:, 0:wd_], True, True, reads=[sq], writes=[p2])
                S.act(ri[:, 0:wd_], p2[:, 0:wd_], AF.Sqrt, bias=1e-6, reads=[p2], writes=[ri])
                S.op("dve", lambda e: e.reciprocal(ri[:, 0:wd_], ri[:, 0:wd_]), reads=[ri], writes=[ri])
                if which == 0:
                    S.stt("dve", qn[:, 0:wd_], s[:, 0:wd_], 128.0 ** -0.5, ri[:, 0:wd_], ALU.mult, ALU.mult, reads=[s, ri], writes=[qn])
                else:
                    S.tt("dve", qn[:, 0:wd_], s[:, 0:wd_], ri[:, 0:wd_], ALU.mult, reads=[s, ri], writes=[qn])
                dstT = (qT_s if which == 0 else kT_s)[h][:, t0:t0 + wd_]
                S.dma(dstT, qn[:, 0:wd_], reads=[qn])
                if which == 1:
                    yield
                    p3 = pb()
                    na = wd_ // 128
                    for j4 in range(na):
                        S.mm(p3[:, j4 * 128:(j4 + 1) * 128], qn[:, j4 * 128:(j4 + 1) * 128], identb[:], True, True, reads=[qn], writes=[p3])
                    S.copy("act", kt[:, 0:na, :], p3[:, 0:wd_].rearrange("p (a b) -> p a b", b=128), reads=[p3], writes=[kt])
                    S.dma(k_s[t0:t0 + wd_, h * 128:(h + 1) * 128].rearrange("(a p) d -> p a d", p=128), kt[:, 0:na, :], reads=[kt])

            def all_qk():
                i = 0
                wi = 0
                for which in range(2):
                    for h in range(4):
                        w = wB[wi % 2]; wi += 1
                        col = which * 512 + h * 128
                        for j in range(3):
                            S.dma(w[:, j], wB_b[j][:, col:col + 128].rearrange("(kc p) n -> p kc n", p=128), writes=[(w, j)])
                        for (c0, wd_, t0) in BLOCKS:
                            yield qk_gen(i, which, h, w, c0, wd_, t0)
                            i += 1
            run_pipe(all_qk(), 3)
            wV = sb(st, [128, 3, 8, 512], BF16, "wV")
            for j in range(3):
                S.dma(wV[:, j], wB_b[j][:, 1024:1536].rearrange("(kc p) n -> p kc n", p=128), writes=[(wV, j)])
            wG = sb(st, [128, 8, 512], BF16, "wG")
            S.dma(wG[:], evwin_b[:, 3072:3584].rearrange("(kc p) n -> p kc n", p=128), writes=[wG])
            wba = sb(st, [128, 8, 16], BF16, "wba")
            S.dma(wba[:], evwin_b[:, 3584:3600].rearrange("(kc p) n -> p kc n", p=128), writes=[wba])
            vt = [sb(st, [128, 512], BF16, "vt") for _ in range(2)]
            gt = [sb(st, [128, 512], F32, "gt") for _ in range(2)]
            for ti in range(NT):
                c = tcol(ti)
                p = pb(); n = 0
                for j in range(3):
                    for kc in range(8):
                        S.mm(p[:], hT[:, kc, c + j - 1:c + j - 1 + 128], wV[:, j, kc, :], n == 0, n == 23, reads=[(wV, j)], writes=[p])
                        n += 1
                v = vt[ti % 2]
                S.act(v[:], p[:], AF.Silu, reads=[p], writes=[v])
                S.dma(v_s[ti * 128:(ti + 1) * 128, :], v[:], reads=[v])
                p = pb()
                for kc in range(8):
                    S.mm(p[:], hT[:, kc, c:c + 128], wG[:, kc, :], kc == 0, kc == 7, reads=[wG], writes=[p])
                g = gt[ti % 2]
                S.act(g[:], p[:], AF.Silu, reads=[p], writes=[g])
                S.dma(gate_s[ti * 128:(ti + 1) * 128, :], g[:], reads=[g])
                p = pb()
                for kc in range(8):
                    S.mm(p[:, 0:16], hT[:, kc, c:c + 128], wba[:, kc, :], kc == 0, kc == 7, reads=[wba], writes=[p])
                S.copy("dve", BG[:, ti, :], p[:, 0:16], reads=[p], writes=[(BG, ti)])
            allbg = [(BG, ti) for ti in range(NT)]
            smb = sb(st, [128, NT, 8], F32, "smb")
            S.act(BG[:, :, 0:8], BG[:, :, 0:8], AF.Sigmoid, reads=allbg, writes=[(BG, "beta")])
            S.tt("dve", smb[:], BG[:, :, 8:16], dtb[:].unsqueeze(1).to_broadcast([128, NT, 8]), ALU.add, reads=allbg, writes=[smb])
            S.ts("dve", smb[:], smb[:], 30.0, None, ALU.min, reads=[smb], writes=[smb])
            S.act(smb[:], smb[:], AF.Exp, reads=[smb], writes=[smb])
            S.act(smb[:], smb[:], AF.Ln, bias=1.0, reads=[smb], writes=[smb])
            S.tt("dve", BG[:, :, 8:16], smb[:], negexpA[:].unsqueeze(1).to_broadcast([128, NT, 8]), ALU.mult, reads=[smb] + allbg, writes=[(BG, "g")])

        def phase_D(b, BG, catT, st):
            Sst = sb(st, [128, 2, 4, 128], F32, "Sst")
            S.memset("pool", Sst[:], 0.0, writes=[Sst])
            names16 = ["kT", "qT", "ktok", "v", "TG", "A", "AT", "Mb", "MbT", "P", "vb", "kbg"]
            names32 = ["DT", "kd", "u", "wT", "Eg", "qg0", "qg1", "vnew", "osb", "oprev", "gate"]
            slots = []
            for d in range(2):
                sl = {nm: sb(st, [128, 4, 128], BF16, nm) for nm in names16}
                sl.update({nm: sb(st, [128, 4, 128], F32, nm) for nm in names32})
                sl["E"] = sb(st, [128, 16], F32, "E"); sl["bg2"] = sb(st, [128, 4], F32, "bg2")
                sl["ss"] = sb(st, [128, 8], F32, "ss"); sl["junk"] = sb(st, [128, 128], F32, "junk")
                S.memset("pool", sl["qg0"][:], 0.0, writes=[sl["qg0"]]); S.memset("pool", sl["qg1"][:], 0.0, writes=[sl["qg1"]])
                slots.append(sl)
            visited = set()
            identbc = ident[:].unsqueeze(1).to_broadcast([128, 4, 128])
            bnbc = bnorm[:].unsqueeze(1).to_broadcast([128, 4, 128])

            def bc4(ap):
                return ap.unsqueeze(2).to_broadcast([128, 4, 128])

            def f2(t):
                return t[:].rearrange("p h d -> p (h d)")

            def hs(h):
                return slice(h * 128, (h + 1) * 128)

            ringi = [0, 0]

            def item(ti, d, sl):
                def pb():
                    r = PB[4 * d + ringi[d] % 3]
                    ringi[d] += 1
                    return r
                mi, ms, negS, negI = (0, 1, 4, 5) if d == 0 else (2, 3, 6, 7)
                gd = BG[:, ti, 8 + 4 * d:12 + 4 * d]
                bd = BG[:, ti, 4 * d:4 * d + 4]
                rows = slice(ti * 128, (ti + 1) * 128)
                kT, qT, ktok, v, TG, A, AT, P, DT = sl["kT"], sl["qT"], sl["ktok"], sl["v"], sl["TG"], sl["A"], sl["AT"], sl["P"], sl["DT"]
                E = sl["E"]
                S.dma(kT[:], kT_s.rearrange("h d t -> d h t")[:, :, rows], writes=[kT])
                S.dma(qT[:], qT_s.rearrange("h d t -> d h t")[:, :, rows], writes=[qT])
                S.dma(f2(ktok), k_s[rows, :], writes=[ktok])
                S.dma(f2(v), v_s[rows, :], writes=[v])
                yield
                ps_ = pb()
                S.mm(ps_[:, 0:4], MK[:, mi, :], gd, True, True, reads=[(BG, ti)], writes=[ps_])
                S.mm(ps_[:, 4:8], MK[:, ms, :], gd, True, True, reads=[(BG, ti)], writes=[ps_])
                S.mm(ps_[:, 8:12], MK[:, 8, :], gd, True, True, reads=[(BG, ti)], writes=[ps_])
                S.mm(ps_[:, 12:16], MK[:, 9, :], gd, True, True, reads=[(BG, ti)], writes=[ps_])
                S.act(E[:], ps_[:, 0:16], AF.Exp, reads=[ps_], writes=[E])
                S.tt("dve", sl["bg2"][:], bd, E[:, 0:4], ALU.mult, reads=[E, (BG, ti)], writes=[sl["bg2"]])
                for h in range(4):
                    S.ts("dve", TG[:, h, :], MK[:, mi, :], gd[:, h:h + 1], None, ALU.mult, reads=[(BG, ti)], writes=[TG])
                yield
                pKK = pb(); pL = pb()
                for h in range(4):
                    S.mm(pKK[:, hs(h)], kT[:, h, :], kT[:, h, :], True, True, reads=[kT], writes=[pKK])
                for h in range(4):
                    S.mm(pL[:, hs(h)], TG[:, h, :], MKb[:, ms, :], True, False, reads=[TG], writes=[pL])
                    S.mm(pL[:, hs(h)], identb[:], MKb[:, negS, :], False, True, reads=[TG], writes=[pL])
                S.act(f2(A), pL[:], AF.Exp, reads=[pL], writes=[A])
                for h in range(4):
                    S.stt("dve", A[:, h, :], pKK[:, hs(h)], bd[:, h:h + 1], A[:, h, :], ALU.mult, ALU.mult, reads=[pKK, A, (BG, ti)], writes=[A])
                yield
                pAT = pb()
                for h in range(4):
                    S.mm(pAT[:, hs(h)], A[:, h, :], identb[:], True, True, reads=[A], writes=[pAT])
                S.copy("act", f2(AT), pAT[:], reads=[pAT], writes=[AT])
                S.stt("dve", P[:], pAT[:].rearrange("p (h d) -> p h d", h=4), -1.0, identbc, ALU.mult, ALU.add, reads=[pAT], writes=[P])
                pLT = pb(); pQK = pb()
                for h in range(4):
                    S.mm(pLT[:, hs(h)], MKb[:, ms, :], TG[:, h, :], True, False, reads=[TG], writes=[pLT])
                    S.mm(pLT[:, hs(h)], identb[:], MKb[:, negI, :], False, True, reads=[TG], writes=[pLT])
                for h in range(4):
                    S.mm(pQK[:, hs(h)], kT[:, h, :], qT[:, h, :], True, True, reads=[kT, qT], writes=[pQK])
                S.act(f2(DT), pLT[:], AF.Exp, reads=[pLT], writes=[DT])
                S.tt("dve", f2(DT), pQK[:], f2(DT), ALU.mult, reads=[pQK, DT], writes=[DT])
                yield
                N_, NT_ = AT, A
                Y_, YT_ = sl["Mb"], sl["MbT"]
                prevYT = None
                for lev in range(6):
                    pP = None
                    if prevYT is not None:
                        pP = pb()
                        for h in range(4):
                            S.mm(pP[:, hs(h)], prevYT[:, h, :], P[:, h, :], True, True, reads=[prevYT, P], writes=[pP])
                    if lev < 5:
                        last = lev == 4
                        pMT = pb()
                        pM = None if last else pb()
                        for h in range(4):
                            if not last:
                                S.mm(pM[:, hs(h)], NT_[:, h, :], N_[:, h, :], True, True, reads=[N_, NT_], writes=[pM])
                            S.mm(pMT[:, hs(h)], N_[:, h, :], NT_[:, h, :], True, True, reads=[N_, NT_], writes=[pMT])
                        if not last:
                            S.copy("act", f2(Y_), pM[:], reads=[pM], writes=[Y_])
                        S.copy("act" if last else "dve", f2(YT_), pMT[:], reads=[pMT], writes=[YT_])
                    if pP is not None:
                        S.tt("dve", f2(P), f2(P), pP[:], ALU.add, reads=[pP, P], writes=[P])
                    if lev < 5:
                        prevYT = YT_
                        N_, NT_, Y_, YT_ = Y_, YT_, N_, NT_
                    yield
                vb, kbg, kd, u, wT, Eg, vnew = sl["vb"], sl["kbg"], sl["kd"], sl["u"], sl["wT"], sl["Eg"], sl["vnew"]
                S.tt("dve", vb[:], v[:], bc4(bd), ALU.mult, reads=[v, (BG, ti)], writes=[vb])
                S.tt("pool", kbg[:], ktok[:], bc4(sl["bg2"][:]), ALU.mult, reads=[ktok, sl["bg2"]], writes=[kbg])
                S.tt("pool", kd[:], ktok[:], bc4(E[:, 4:8]), ALU.mult, reads=[ktok, E], writes=[kd])
                pu = pb(); pw = pb(); pE = pb()
                for h in range(4):
                    S.mm(pu[:, hs(h)], P[:, h, :], vb[:, h, :], True, True, reads=[P, vb], writes=[pu])
                for h in range(4):
                    S.mm(pw[:, hs(h)], kbg[:, h, :], P[:, h, :], True, True, reads=[P, kbg], writes=[pw])
                for h in range(4):
                    S.mm(pE[:, hs(h)], onesb[:], TG[:, h, :], True, True, reads=[TG], writes=[pE])
                S.copy("act", f2(u), pu[:], reads=[pu], writes=[u])
                S.copy("dve", f2(wT), pw[:], reads=[pw], writes=[wT])
                S.act(f2(Eg), pE[:], AF.Exp, reads=[pE], writes=[Eg])
                S.tt("dve", sl["qg0"][:, :, 0:64], qT[:, :, 0:64], Eg[:, :, 0:64], ALU.mult, reads=[qT, Eg], writes=[sl["qg0"]])
                S.tt("pool", sl["qg1"][:, :, 64:128], qT[:, :, 64:128], Eg[:, :, 64:128], ALU.mult, reads=[qT, Eg], writes=[sl["qg1"]])
                yield
                po = PB[4 * d + 3]
                S.memset("dve", po[:], 0.0, writes=[po])
                for c in ([0, 1] if d == 0 else [1, 0]):
                    Rr = slice(64 * c, 64 * c + 64)
                    pws = pb()
                    for h in range(4):
                        S.mm(pws[:, hs(h)], wT[:, h, :], Sst[:, d, h, :], True, True, reads=[wT, (Sst, d)], writes=[pws])
                    S.tt("dve", f2(vnew)[Rr, :], f2(u)[Rr, :], pws[Rr, :], ALU.subtract, reads=[u, pws], writes=[vnew])
                    qg = sl["qg0"] if c == 0 else sl["qg1"]
                    for h in range(4):
                        S.op("pe", lambda e, h=h, qg=qg: e.matmul(po[:, hs(h)], qg[:, h, :], Sst[:, d, h, :], start=False, stop=False, skip_group_check=True),
                             reads=[qg, (Sst, d)], writes=[po])
                    pS = pb()
                    for h in range(4):
                        S.mm(pS[:, hs(h)], kd[Rr, h, :], vnew[Rr, h, :], True, True, reads=[kd, vnew], writes=[pS])
                    for h in range(4):
                        S.stt("dve", Sst[:, d, h, :], Sst[:, d, h, :], E[:, 8 + 4 * c + h:9 + 4 * c + h], pS[:, hs(h)], ALU.mult, ALU.add,
                              reads=[pS, E, (Sst, d)], writes=[(Sst, d)])
                    yield
                for h in range(4):
                    S.op("pe", lambda e, h=h: e.matmul(po[:, hs(h)], DT[:, h, :], vnew[:, h, :], start=False, stop=False, skip_group_check=True),
                         reads=[DT, vnew], writes=[po])
                osb, oprev, gate = sl["osb"], sl["oprev"], sl["gate"]
                if ti not in visited:
                    visited.add(ti)
                    S.copy("act", f2(osb), po[:], reads=[po], writes=[osb])
                    S.dma(o_s[rows, :], f2(osb), reads=[osb], writes=[("o_s", ti)])
                else:
                    ss = sl["ss"]
                    S.dma(f2(oprev), o_s[rows, :], reads=[("o_s", ti)], writes=[oprev])
                    S.dma(f2(gate), gate_s[rows, :], writes=[gate])
                    S.tt("dve", f2(osb), po[:], f2(oprev), ALU.add, reads=[po, oprev], writes=[osb])
                    S.memset("pool", ss[:], 0.0, writes=[ss])
                    for h in range(4):
                        S.act(sl["junk"][:], osb[:, h, :], AF.Square, accum_out=ss[:, h:h + 1], reads=[osb, ss], writes=[sl["junk"], ss])
                    S.act(ss[:, 4:8], ss[:, 0:4], AF.Sqrt, scale=1.0 / 128, bias=1e-6, reads=[ss], writes=[ss])
                    S.op("dve", lambda e: e.reciprocal(ss[:, 4:8], ss[:, 4:8]), reads=[ss], writes=[ss])
                    S.tt("dve", osb[:], osb[:], bc4(ss[:, 4:8]), ALU.mult, reads=[osb, ss], writes=[osb])
                    S.tt("pool", osb[:], osb[:], gate[:], ALU.mult, reads=[osb, gate], writes=[osb])
                    S.tt("pool", osb[:], osb[:], bnbc, ALU.mult, reads=[osb], writes=[osb])
                    pT = pb()
                    for h in range(4):
                        S.tr(pT[:, hs(h)], osb[:, h, :], ident[:], reads=[osb], writes=[pT])
                    S.copy("act", catT[:, 4:8, tcol(ti):tcol(ti) + 128], pT[:].rearrange("p (h t) -> p h t", h=4), reads=[pT], writes=[(catT, "B", ti)])

            fwd_order = list(range(18))
            bwd_order = [1, 0] + list(range(17, 1, -1))
            for s_i in range(18):
                gens = [item(fwd_order[s_i], 0, slots[0]), item(bwd_order[s_i], 1, slots[1])]
                while gens:
                    for g in list(gens):
                        try:
                            next(g)
                        except StopIteration:
                            gens.remove(g)

        def layer0(b):
            tiles = list(range(18))
            with ExitStack() as stL:
                gates = sb(stL, [128, 18, 16], F32, "gates")
                with ExitStack() as stC:
                    catT = sb(stC, [128, 8, 2307], BF16, "catT")
                    BG = sb(stC, [128, 18, 16], F32, "BG")
                    with ExitStack() as st:
                        hT = sb(st, [128, 8, 2307], BF16, "hT")
                        for c in (0, 257, 2306):
                            S.memset("pool", hT[:, :, c:c + 1], 0.0, writes=[(hT, "pad", c)])
                        if want('A'):
                            with ExitStack() as st2:
                                phase_A(0, b, hT, tiles, st2)
                                S.barrier()
                        if want('B'):
                            with ExitStack() as st2:
                                phase_B(b, hT, catT, st2)
                                S.barrier()
                        if want('C'):
                            with ExitStack() as st2:
                                phase_C(b, hT, BG, st2)
                                S.barrier()
                    if want('D'):
                        with ExitStack() as st:
                            phase_D(b, BG, catT, st)
                            S.barrier()
                    post_E(0, b, catT, tiles, gates)
                post_FG(0, b, tiles, gates)

        LBLK = [(258 + 512 * j, 512, 256 + 512 * j, 512 * j) for j in range(4)]

        def rearr_w(ap):
            return ap.rearrange("(kc p) n -> p kc n", p=128)

        def l1_proj_mla(b, hT, dqn, dkvn, KR, st):
            wdq = sb(st, [128, 8, 384], BF16, "wdq"); S.dma(wdq[:], rearr_w(odwin_b[:, 768:1152]), writes=[wdq])
            wdkv = sb(st, [128, 8, 256], BF16, "wdkv"); S.dma(wdkv[:], rearr_w(odwin_b[:, 1152:1408]), writes=[wdkv])
            wk = sb(st, [128, 8, 96], BF16, "wk"); S.dma(wk[:], rearr_w(wkr_b), writes=[wk])
            wks = sb(st, [128, 8, 96], BF16, "wks"); S.dma(wks[:], rearr_w(wkrsw_b), writes=[wks])
            r32 = sb(st, [96, 2, LT], F32, "r32"); S.dma(r32[:], rope32_d.rearrange("a p t -> p a t"), writes=[r32])
            sq = [sb(st, [128, 512], F32, "sq1") for _ in range(3)]
            rinv = sb(st, [128, 512], F32, "rinv1")
            t1 = sb(st, [96, 512], F32, "t1a"); t2 = sb(st, [96, 512], F32, "t2a")

            def rms_proj(wt, nch, normT, dst, c0, w, d0, inv_n):
                pp = [pb() for _ in range(nch)]
                for c in range(nch):
                    for kc in range(8):
                        S.mm(pp[c][:, 0:w], wt[:, kc, c * 128:(c + 1) * 128], hT[:, kc, c0:c0 + w], kc == 0, kc == 7, reads=[wt], writes=[pp[c]])
                    S.act(sq[c][:, 0:w], pp[c][:, 0:w], AF.Square, reads=[pp[c]], writes=[sq[c]])
                pss = pb()
                for c in range(nch):
                    S.mm(pss[:, 0:w], ones[:], sq[c][:, 0:w], c == 0, c == nch - 1, reads=[sq[c]], writes=[pss])
                S.act(rinv[:, 0:w], pss[:, 0:w], AF.Sqrt, scale=inv_n, bias=1e-6, reads=[pss], writes=[rinv])
                S.op("dve", lambda e: e.reciprocal(rinv[:, 0:w], rinv[:, 0:w]), reads=[rinv], writes=[rinv])
                for c in range(nch):
                    S.stt("dve", dst[:, c, d0:d0 + w], pp[c][:, 0:w], normT[:, c:c + 1], rinv[:, 0:w], ALU.mult, ALU.mult,
                          reads=[pp[c], rinv], writes=[(dst, c, d0)])

            for bi, (c0, w, t0) in enumerate(BLOCKS):
                rms_proj(wdkv, 2, kvnT, dkvn, c0, w, t0, 1.0 / 256)
                pk = pb(); pks = pb()
                for kc in range(8):
                    S.mm(pk[0:96, 0:w], wk[:, kc, :], hT[:, kc, c0:c0 + w], kc == 0, kc == 7, reads=[wk], writes=[pk])
                if bi == 0:
                    S.copy("act", KR[64:96, t0:t0 + w], pk[64:96, 0:w], reads=[pk], writes=[(KR, t0)])
                else:
                    l0 = t0 - 256
                    for kc in range(8):
                        S.mm(pks[0:96, 0:w], wks[:, kc, :], hT[:, kc, c0:c0 + w], kc == 0, kc == 7, reads=[wks], writes=[pks])
                    S.tt("dve", t1[64:96, 0:w], pk[64:96, 0:w], r32[64:96, 0, l0:l0 + w], ALU.mult, reads=[pk, r32], writes=[t1])
                    S.tt("dve", t2[64:96, 0:w], pks[64:96, 0:w], r32[64:96, 1, l0:l0 + w], ALU.mult, reads=[pks, r32], writes=[t2])
                    S.tt("pool", KR[64:96, t0:t0 + w], t1[64:96, 0:w], t2[64:96, 0:w], ALU.add, reads=[t1, t2], writes=[(KR, t0)])
                    rms_proj(wdq, 3, qnT, dqn, c0, w, l0, 1.0 / 384)

        def l1_proj_win(b, hT, CQ, CK, CV, st):
            wq = sb(st, [128, 8, 512], BF16, "wq"); S.dma(wq[:], rearr_w(odwin_b[:, 0:512]), writes=[wq])
            wqs = sb(st, [128, 8, 512], BF16, "wqs"); S.dma(wqs[:], rearr_w(odwinsw_b[:, 0:512]), writes=[wqs])
            wkk = sb(st, [128, 8, 128], BF16, "wkk"); S.dma(wkk[:], rearr_w(odwin_b[:, 512:640]), writes=[wkk])
            wkks = sb(st, [128, 8, 128], BF16, "wkks"); S.dma(wkks[:], rearr_w(odwinsw_b[:, 512:640]), writes=[wkks])
            wv = sb(st, [128, 8, 128], BF16, "wv"); S.dma(wv[:], rearr_w(odwin_b[:, 640:768]), writes=[wv])
            r64 = sb(st, [64, 2, LT], F32, "r64"); S.dma(r64[:], rope64_d.rearrange("a p t -> p a t"), writes=[r64])
            t1 = [sb(st, [64, 512], F32, "t1w") for _ in range(2)]; t2 = [sb(st, [64, 512], F32, "t2w") for _ in range(2)]
            S.memset("pool", CV[:, :, :, 64:65], 1.0, writes=[(CV, "ones")])
            ii = 0
            for bi, (c0, w, t0) in enumerate(BLOCKS):
                l0 = t0 - 256
                for g in range(2):
                    pk = pb(); pks = pb()
                    for kc in range(8):
                        S.mm(pk[0:64, 0:w], wkk[:, kc, g * 64:(g + 1) * 64], hT[:, kc, c0:c0 + w], kc == 0, kc == 7, reads=[wkk], writes=[pk])
                    if bi == 0:
                        S.copy("act", CK[:, g, t0:t0 + w], pk[0:64, 0:w], reads=[pk], writes=[(CK, g, t0)])
                    else:
                        for kc in range(8):
                            S.mm(pks[0:64, 0:w], wkks[:, kc, g * 64:(g + 1) * 64], hT[:, kc, c0:c0 + w], kc == 0, kc == 7, reads=[wkks], writes=[pks])
                        a1 = t1[ii % 2]; a2 = t2[ii % 2]; ii += 1
                        S.tt("dve", a1[:, 0:w], pk[0:64, 0:w], r64[:, 0, l0:l0 + w], ALU.mult, reads=[pk, r64], writes=[a1])
                        S.tt("dve", a2[:, 0:w], pks[0:64, 0:w], r64[:, 1, l0:l0 + w], ALU.mult, reads=[pks, r64], writes=[a2])
                        S.tt("pool", CK[:, g, t0:t0 + w], a1[:, 0:w], a2[:, 0:w], ALU.add, reads=[a1, a2], writes=[(CK, g, t0)])
                for j in range(w // 128):
                    ti = t0 // 128 + j
                    pv = pb()
                    for kc in range(8):
                        S.mm(pv[:, 0:128], hT[:, kc, tcol(ti):tcol(ti) + 128], wv[:, kc, :], kc == 0, kc == 7, reads=[wv], writes=[pv])
                    S.copy("act", CV[:, ti, :, 0:64], pv[:, 0:128].rearrange("p (g d) -> p g d", g=2), reads=[pv], writes=[(CV, ti)])
                if bi > 0:
                    for h in range(8):
                        pq = pb(); pqs = pb()
                        for kc in range(8):
                            S.mm(pq[0:64, 0:w], wq[:, kc, h * 64:(h + 1) * 64], hT[:, kc, c0:c0 + w], kc == 0, kc == 7, reads=[wq], writes=[pq])
                        for kc in range(8):
                            S.mm(pqs[0:64, 0:w], wqs[:, kc, h * 64:(h + 1) * 64], hT[:, kc, c0:c0 + w], kc == 0, kc == 7, reads=[wqs], writes=[pqs])
                        a1 = t1[ii % 2]; a2 = t2[ii % 2]; ii += 1
                        S.tt("dve", a1[:, 0:w], pq[0:64, 0:w], r64[:, 0, l0:l0 + w], ALU.mult, reads=[pq, r64], writes=[a1])
                        S.tt("dve", a2[:, 0:w], pqs[0:64, 0:w], r64[:, 1, l0:l0 + w], ALU.mult, reads=[pqs, r64], writes=[a2])
                        S.tt("pool", CQ[:, h, l0:l0 + w], a1[:, 0:w], a2[:, 0:w], ALU.add, reads=[a1, a2], writes=[(CQ, h, l0)])

        def l1_attn_win(b, CQ, CK, CV, catT, st):
            PT = [sb(st, [128, 512], BF16, "PTw") for _ in range(2)]
            Yw = [sb(st, [128, 512], F32, "Yw") for _ in range(2)]
            den = sb(st, [128, 8], F32, "denw")
            ring = [0]

            def rb():
                r = PB[ring[0] % 6]
                ring[0] += 1
                return r
            seq = []
            for qt in range(16):
                for g in range(2):
                    keys = [(0, None), (1, None)]
                    if qt >= 1:
                        keys.append((qt + 1, 10))
                    keys.append((qt + 2, None))
                    if qt <= 14:
                        keys.append((qt + 3, 11))
                    for ki, (kt, mk) in enumerate(keys):
                        seq.append((qt, g, kt, mk, ki == 0, ki == len(keys) - 1))
            pss = {}

            def issue_S(i):
                qt, g, kt, mk, first, last = seq[i]
                ps = rb()
                S.mm(ps[:, 0:512], CK[:, g, kt * 128:(kt + 1) * 128], CQ[:, 4 * g:4 * g + 4, qt * 128:(qt + 1) * 128], True, True, reads=[], writes=[ps])
                pss[i] = ps
            issue_S(0)
            io = 0
            po = None; po3 = None
            for i, (qt, g, kt, mk, first, last) in enumerate(seq):
                yw = Yw[qt % 2]
                if first:
                    po = PB[6 + io % 2]; io += 1
                    po3 = po[:, 0:260].rearrange("p (a b) -> p a b", a=4)
                    S.memset("dve", po[:, 0:260], 0.0, writes=[po])
                if i + 1 < len(seq):
                    issue_S(i + 1)
                ps = pss.pop(i)
                pt = PT[i % 2]
                S.act(pt[:], ps[:, 0:512], AF.Exp, scale=0.125, reads=[ps], writes=[pt])
                if mk is not None:
                    pt3 = pt[:].rearrange("p (a b) -> p a b", a=4)
                    S.tt("pool", pt3, pt3, MK[:, mk, :].unsqueeze(1).to_broadcast([128, 4, 128]), ALU.mult, reads=[pt], writes=[pt])
                for hh in range(4):
                    S.op("pe", lambda e, hh=hh, pt=pt, kt=kt, po=po, g=g: e.matmul(po[:, hh * 65:(hh + 1) * 65], pt[:, hh * 128:(hh + 1) * 128], CV[:, kt, g, :],
                                                                                 start=False, stop=False, skip_group_check=True), reads=[pt], writes=[po])
                if last:
                    S.tt("dve", den[:, 0:4], po3[:, :, 64], expsink[:, 4 * g:4 * g + 4], ALU.add, reads=[po], writes=[den])
                    S.op("dve", lambda e: e.reciprocal(den[:, 4:8], den[:, 0:4]), reads=[den], writes=[den])
                    S.tt("dve", yw[:, g * 256:(g + 1) * 256].rearrange("p (a b) -> p a b", a=4), po3[:, :, 0:64],
                         den[:, 4:8].unsqueeze(2).to_broadcast([128, 4, 64]), ALU.mult, reads=[po, den], writes=[yw])
                    if g == 1:
                        pT = rb()
                        for j in range(4):
                            S.tr(pT[:, j * 128:(j + 1) * 128], yw[:, j * 128:(j + 1) * 128], ident[:], reads=[yw], writes=[pT])
                        S.copy("act", catT[:, 0:4, qt * 128:(qt + 1) * 128], pT[:, 0:512].rearrange("p (a b) -> p a b", a=4), reads=[pT], writes=[(catT, "w", qt)])

        def l1_up_mla(b, dqn, dkvn, KR, MQ, MKk, MV, st):
            wuq = sb(st, [128, 3, 768], BF16, "wuq"); S.dma(wuq[:], wuq_b.rearrange("(c p) n -> p c n", p=128), writes=[wuq])
            wuqs = sb(st, [128, 3, 768], BF16, "wuqs"); S.dma(wuqs[:], wuqsw_b.rearrange("(c p) n -> p c n", p=128), writes=[wuqs])
            wukv = sb(st, [128, 2, 1024], BF16, "wukv"); S.dma(wukv[:], wukv_b.rearrange("(c p) n -> p c n", p=128), writes=[wukv])
            r32 = sb(st, [96, 2, LT], F32, "r32b"); S.dma(r32[:], rope32_d.rearrange("a p t -> p a t"), writes=[r32])
            t1 = [sb(st, [96, 512], F32, "t1m") for _ in range(2)]; t2 = [sb(st, [96, 512], F32, "t2m") for _ in range(2)]
            S.memset("pool", MV[:, :, :, 64:65], 1.0, writes=[(MV, "ones")])
            ii = 0
            for (c0, w, t0, l0) in LBLK:
                for h in range(8):
                    pq = pb(); pqs = pb()
                    for c in range(3):
                        S.mm(pq[0:96, 0:w], wuq[:, c, h * 96:(h + 1) * 96], dqn[:, c, l0:l0 + w], c == 0, c == 2, reads=[wuq], writes=[pq])
                    for c in range(3):
                        S.mm(pqs[0:96, 0:w], wuqs[:, c, h * 96:(h + 1) * 96], dqn[:, c, l0:l0 + w], c == 0, c == 2, reads=[wuqs], writes=[pqs])
                    S.copy("act", MQ[0:64, h, l0:l0 + w], pq[0:64, 0:w], reads=[pq], writes=[(MQ, h, l0, 0)])
                    a1 = t1[ii % 2]; a2 = t2[ii % 2]; ii += 1
                    S.tt("dve", a1[64:96, 0:w], pq[64:96, 0:w], r32[64:96, 0, l0:l0 + w], ALU.mult, reads=[pq, r32], writes=[a1])
                    S.tt("dve", a2[64:96, 0:w], pqs[64:96, 0:w], r32[64:96, 1, l0:l0 + w], ALU.mult, reads=[pqs, r32], writes=[a2])
                    S.tt("pool", MQ[64:96, h, l0:l0 + w], a1[64:96, 0:w], a2[64:96, 0:w], ALU.add, reads=[a1, a2], writes=[(MQ, h, l0, 1)])
            for (c0, w, t0) in BLOCKS:
                for h in range(8):
                    pk = pb()
                    for c in range(2):
                        S.mm(pk[0:64, 0:w], wukv[:, c, h * 128:h * 128 + 64], dkvn[:, c, t0:t0 + w], c == 0, c == 1, reads=[wukv], writes=[pk])
                    if h % 2 == 0:
                        S.copy("act", MKk[0:64, h, t0:t0 + w], pk[0:64, 0:w], reads=[pk], writes=[(MKk, h, t0, 0)])
                    else:
                        S.copy("dve", MKk[0:64, h, t0:t0 + w], pk[0:64, 0:w], reads=[pk], writes=[(MKk, h, t0, 0)])
                    S.copy("pool", MKk[64:96, h, t0:t0 + w], KR[64:96, t0:t0 + w], reads=[], writes=[(MKk, h, t0, 1)])
                for j in range(w // 128):
                    ti = t0 // 128 + j
                    pv = pb()
                    wv3 = wukv[:].rearrange("p c (h x) -> p c h x", h=8)
                    for c in range(2):
                        S.mm(pv[:, 0:512], dkvn[:, c, ti * 128:(ti + 1) * 128], wv3[:, c, :, 64:128], c == 0, c == 1, reads=[wukv], writes=[pv])
                    S.copy("act", MV[:, ti, :, 0:64], pv[:, 0:512].rearrange("p (h x) -> p h x", h=8), reads=[pv], writes=[(MV, ti)])

        def l1_attn_mla(b, MQ, MKk, MV, catT, st):
            PT = [sb(st, [128, 512], BF16, "PTm") for _ in range(3)]
            Ym = sb(st, [128, 4, 512], F32, "Ym")
            den = sb(st, [128, 8], F32, "denm")
            ring = [0]

            def rb():
                r = PB[ring[0] % 6]
                ring[0] += 1
                return r
            scale = 96.0 ** -0.5
            seq = [(qb, h, kt) for qb in range(4) for h in range(8) for kt in range(18)]
            pss = {}

            def issue_S(i):
                qb, h, kt = seq[i]
                ps = rb()
                S.mm(ps[:, 0:512], MKk[:, h, kt * 128:(kt + 1) * 128], MQ[:, h, qb * 512:(qb + 1) * 512], True, True, reads=[], writes=[ps])
                pss[i] = ps
            issue_S(0)
            io = 0
            po = None; po3 = None
            for i, (qb, h, kt) in enumerate(seq):
                if kt == 0:
                    po = PB[6 + io % 2]; io += 1
                    po3 = po[:, 0:260].rearrange("p (a b) -> p a b", a=4)
                    S.memset("dve", po[:, 0:260], 0.0, writes=[po])
                if i + 1 < len(seq):
                    issue_S(i + 1)
                ps = pss.pop(i)
                pt = PT[i % 3]
                S.act(pt[:], ps[:, 0:512], AF.Exp, scale=scale, reads=[ps], writes=[pt])
                for qs in range(4):
                    S.op("pe", lambda e, qs=qs, pt=pt, kt=kt, po=po, h=h: e.matmul(po[:, qs * 65:(qs + 1) * 65], pt[:, qs * 128:(qs + 1) * 128], MV[:, kt, h, :],
                                                                                  start=False, stop=False, skip_group_check=True), reads=[pt], writes=[po])
                if kt == 17:
                    S.op("dve", lambda e, po3=po3: e.reciprocal(den[:, 0:4], po3[:, :, 64]), reads=[po], writes=[den])
                    S.tt("dve", Ym[:, :, h * 64:(h + 1) * 64], po3[:, :, 0:64], den[:, 0:4].unsqueeze(2).to_broadcast([128, 4, 64]), ALU.mult,
                         reads=[po, den], writes=[(Ym, h)])
                    if h == 7:
                        for qs in range(4):
                            pT = rb()
                            for j in range(4):
                                S.tr(pT[:, j * 128:(j + 1) * 128], Ym[:, qs, j * 128:(j + 1) * 128], ident[:], reads=[(Ym, hh) for hh in range(8)], writes=[pT])
                            qt = qb * 4 + qs
                            S.copy("act", catT[:, 4:8, qt * 128:(qt + 1) * 128], pT[:, 0:512].rearrange("p (a b) -> p a b", a=4), reads=[pT], writes=[(catT, "m", qt)])

        def layer1(b):
            tiles = list(range(2, 18))
            with ExitStack() as stL:
                gates = sb(stL, [128, 16, 16], F32, "gates1")
                with ExitStack() as stC:
                    catT = sb(stC, [128, 8, LT], BF16, "catT1")
                    dqn = sb(stC, [128, 3, LT], BF16, "dqn"); dkvn = sb(stC, [128, 2, T], BF16, "dkvn"); KR = sb(stC, [96, T], BF16, "KR")
                    with ExitStack() as stW:
                        CQ = sb(stW, [64, 8, LT], BF16, "CQ"); CK = sb(stW, [64, 2, T], BF16, "CK"); CV = sb(stW, [128, 18, 2, 65], BF16, "CV")
                        with ExitStack() as st:
                            hT = sb(st, [128, 8, 2307], BF16, "hT1")
                            with ExitStack() as st2:
                                phase_A(1, b, hT, list(range(18)), st2)
                                S.barrier()
                            if want('L1P'):
                              with ExitStack() as st2:
                                l1_proj_mla(b, hT, dqn, dkvn, KR, st2)
                                S.barrier()
                            if want('L1W'):
                              with ExitStack() as st2:
                                l1_proj_win(b, hT, CQ, CK, CV, st2)
                                S.barrier()
                        if want('L1WA'):
                          with ExitStack() as st2:
                            l1_attn_win(b, CQ, CK, CV, catT, st2)
                            S.barrier()
                    with ExitStack() as stM:
                        MQ = sb(stM, [96, 8, LT], BF16, "MQ"); MKk = sb(stM, [96, 8, T], BF16, "MKk"); MV = sb(stM, [128, 18, 8, 65], BF16, "MV")
                        if want('L1U'):
                          with ExitStack() as st2:
                            l1_up_mla(b, dqn, dkvn, KR, MQ, MKk, MV, st2)
                            S.barrier()
                        if want('L1MA'):
                          with ExitStack() as st2:
                            l1_attn_mla(b, MQ, MKk, MV, catT, st2)
                            S.barrier()
                    if dbg:
                        S.dma(cat_dbg, catT[:], reads=[])
                        S.barrier()
                    if want('L1E'):
                        post_E(1, b, catT, tiles, gates)
                if want('L1F'):
                    post_FG(1, b, tiles, gates)


        for b in range(NB):
            layer0(b)
            if not only0 and want('L1A'):
                layer1(b)
        S.finish([])
    return nc


NEGV = -30000.0


def make_consts():
    p = np.arange(128)
    ch = p // 64
    same = ch[:, None] == ch[None, :]
    a = p[:, None]; bb = p[None, :]
    M = np.zeros((14, 128, 128), np.float32)
    M[0] = same & (a <= bb)
    M[1] = same & (a > bb)
    M[2] = same & (a >= bb)
    M[3] = same & (a < bb)
    M[4] = np.where(same & (a > bb), 0.0, NEGV)
    M[5] = np.where(same & (bb >= a), 0.0, NEGV)
    M[6] = np.where(same & (a < bb), 0.0, NEGV)
    M[7] = np.where(same & (bb <= a), 0.0, NEGV)
    M[8] = (a < 64) & (bb >= 0)
    M[9] = (a >= 64) & (bb >= 0)
    M[10] = bb <= a
    M[11] = a <= bb
    t = np.arange(LT)
    rows = (t // 64).astype(np.float32); cols = (t % 64).astype(np.float32)

    def ang(rot):
        nf = rot // 4
        inv = (10000.0 ** (-np.arange(nf, dtype=np.float32) / nf)).astype(np.float32)
        return np.concatenate([rows[:, None] * inv, cols[:, None] * inv], -1).astype(np.float32)
    a64 = ang(64); a32 = ang(32)
    r64 = np.zeros((2, 64, LT), np.float32)
    r64[0] = np.concatenate([np.cos(a64), np.cos(a64)], 1).T
    r64[1] = np.concatenate([-np.sin(a64), np.sin(a64)], 1).T
    r32 = np.zeros((2, 96, LT), np.float32)
    r32[0, :64] = 1.0
    r32[0, 64:] = np.concatenate([np.cos(a32), np.cos(a32)], 1).T
    r32[1, 64:] = np.concatenate([-np.sin(a32), np.sin(a32)], 1).T
    return M, r64, r32


def prep_shared(inp):
    f = lambda a: np.ascontiguousarray(a, dtype=np.float32)
    M, r64, r32 = make_consts()
    w1 = inp["od_w_in"][0]
    w1s = w1.copy()
    for h in range(8):
        b0 = h * 64
        w1s[:, b0:b0 + 32] = w1[:, b0 + 32:b0 + 64]; w1s[:, b0 + 32:b0 + 64] = w1[:, b0:b0 + 32]
    for g in range(2):
        b0 = 512 + g * 64
        w1s[:, b0:b0 + 32] = w1[:, b0 + 32:b0 + 64]; w1s[:, b0 + 32:b0 + 64] = w1[:, b0:b0 + 32]
    wkr = np.zeros((1024, 96), np.float32); wkrs = np.zeros((1024, 96), np.float32)
    wkr[:, 64:96] = w1[:, 1408:1440]
    wkrs[:, 64:80] = w1[:, 1424:1440]; wkrs[:, 80:96] = w1[:, 1408:1424]
    wuq = inp["od_d_wuq"][0]
    wuqs = wuq.copy()
    for h in range(8):
        b0 = h * 96 + 64
        wuqs[:, b0:b0 + 16] = wuq[:, b0 + 16:b0 + 32]; wuqs[:, b0 + 16:b0 + 32] = wuq[:, b0:b0 + 16]
    d = {
        "ada_w": f(inp["ada_w"]), "ada_b": f(inp["ada_b"]),
        "ln_g": f(inp["ln_g"].reshape(4, 1024)), "ln_b": f(inp["ln_b"].reshape(4, 1024)),
        "ev_w_in": f(inp["ev_w_in"][0]),
        "a_convT": f(inp["ev_a_conv"][0].reshape(3, 4, 128).transpose(2, 1, 0)),
        "ev_b_conv": f(inp["ev_b_conv"][0]),
        "alog": f(inp["ev_b_alog"].reshape(1, 8)), "dtbias": f(inp["ev_b_dtbias"].reshape(1, 8)),
        "bnorm": f(inp["ev_b_norm"].reshape(1, 128)), "ev_w_out": f(inp["ev_w_out"][0]),
        "od_w_in": f(w1), "od_w_in_sw": f(w1s), "wkr": f(wkr), "wkr_sw": f(wkrs),
        "sink": f(inp["od_c_sink"].reshape(1, 8)),
        "qnormT": f(inp["od_d_qnorm"][0].reshape(3, 128).T), "kvnormT": f(inp["od_d_kvnorm"][0].reshape(2, 128).T),
        "wuq": f(wuq), "wuq_sw": f(wuqs), "wukv": f(inp["od_d_wukv"][0]), "od_w_out": f(inp["od_w_out"][0]),
        "router_w": f(inp["router_w"]), "router_bias": f(inp["router_bias"].reshape(1, 16)),
        "moe_g": f(inp["moe_w_gate"].reshape(32768, 512)), "moe_u": f(inp["moe_w_up"].reshape(32768, 512)),
        "moe_d": f(inp["moe_w_down"].reshape(16384, 1024)),
        "idn": np.eye(128, dtype=np.float32), "masks": M, "rope64": r64, "rope32": r32,
    }
    return d


def prep_core(inp, shared, b0, NB):
    f = lambda a: np.ascontiguousarray(a, dtype=np.float32)
    cs = np.concatenate([inp["c"][b0:b0 + NB], inp["c_ctx"][None, :]], 0)
    csT = cs.reshape(NB + 1, 8, 128).transpose(2, 1, 0)
    d = dict(shared)
    d["x"] = f(inp["x"][b0:b0 + NB]); d["ctx"] = f(inp["ctx"][b0:b0 + NB]); d["csT"] = f(csT)
    return d


_NC_CACHE = {}


def kernel(**inputs):
    inp = {k: np.asarray(v) for k, v in inputs.items()}
    NBC = 4
    if "nc" not in _NC_CACHE:
        _NC_CACHE["nc"] = build(NBC)
    nc = _NC_CACHE["nc"]
    shared = prep_shared(inp)
    in_maps = [prep_core(inp, shared, NBC * i, NBC) for i in range(8)]
    res = run_bass_kernel_spmd(nc, in_maps, core_ids=list(range(8)))
    return np.ascontiguousarray(np.concatenate([np.asarray(r["out"]) for r in res.results], 0).astype(np.float32))
```

```python
from concourse.bass_utils import run_bass_kernel_spmd
import numpy as np
import concourse.bass as bass
import concourse.mybir as mybir
from contextlib import ExitStack

F32 = mybir.dt.float32
BF16 = mybir.dt.bfloat16
AF = mybir.ActivationFunctionType
ALU = mybir.AluOpType
AX = mybir.AxisListType

N_DMA_SEMS = 40
USE_NOPS = False
SEM_ROLL = 30000


class Tok:
    __slots__ = ("sem", "val", "eng")

    def __init__(self, sem, val, eng):
        self.sem = sem
        self.val = val
        self.eng = eng


class Sched:
    def __init__(self, nc, stack):
        self.nc = nc
        self.stack = stack
        self.cengs = ["pe", "act", "dve", "pool"]
        self.all = ["pe", "act", "dve", "pool", "sp"]
        self.prog = {e: [] for e in self.all}
        self.nsem = 0
        self.sem = {e: self._newsem() for e in self.cengs}
        self.cnt = {e: 0 for e in self.cengs}
        self.waited = {e: {} for e in self.all}
        self.dsem = [self._newsem() for _ in range(N_DMA_SEMS)]
        self.dcnt = [0] * N_DMA_SEMS
        self.drr = 0
        self.res = {}
        self.pend = {e: {} for e in self.all}
        self.excl = set()
        self.old = []

    def _newsem(self):
        self.nsem += 1
        return self.stack.enter_context(self.nc.semaphore("s%d" % self.nsem))

    def _st(self, r):
        if isinstance(r, tuple):
            k = tuple(x if isinstance(x, (str, int)) else id(x) for x in r)
        elif isinstance(r, str):
            k = r
        else:
            k = id(r)
        st = self.res.get(k)
        if st is None:
            st = {"w": None, "r": {}}
            self.res[k] = st
        return st

    def _emit(self, eng, fn, reads, writes, dma):
        if self.excl:
            ex = [r for r in reads if (not isinstance(r, (tuple, str))) and id(r) in self.excl]
            if ex:
                reads = [r for r in reads if not ((not isinstance(r, (tuple, str))) and id(r) in self.excl)]
                writes = list(writes) + ex
        deps = []
        for r in reads:
            st = self._st(r)
            if st["w"] is not None:
                deps.append(st["w"])
        for w in writes:
            st = self._st(w)
            if st["w"] is not None:
                deps.append(st["w"])
            deps.extend(st["r"].values())
        need = {}
        for t in deps:
            if t.eng == eng and eng == "pe" and not dma:
                continue
            cur = need.get(id(t.sem))
            if cur is None or cur[1] < t.val:
                need[id(t.sem)] = (t.sem, t.val)
        if self.pend[eng]:
            for sid, (s_, v_) in self.pend[eng].items():
                cur = need.get(sid)
                if cur is None or cur[1] < v_:
                    need[sid] = (s_, v_)
            self.pend[eng] = {}
        if dma:
            i = self.drr
            self.drr = (self.drr + 1) % N_DMA_SEMS
            prev = self.dcnt[i]
            if prev > 0:
                cur = need.get(id(self.dsem[i]))
                if cur is None or cur[1] < prev:
                    need[id(self.dsem[i])] = (self.dsem[i], prev)
            self.dcnt[i] += 16
            tok = Tok(self.dsem[i], self.dcnt[i], "dma")
            inc = (self.dsem[i], 16)
        else:
            if self.cnt[eng] >= SEM_ROLL:
                self.old.append((self.sem[eng], self.cnt[eng]))
                self.sem[eng] = self._newsem()
                self.cnt[eng] = 0
            self.cnt[eng] += 1
            tok = Tok(self.sem[eng], self.cnt[eng], eng)
            inc = (self.sem[eng], 1)
        waits = []
        wd = self.waited[eng]
        for sid, (s, v) in need.items():
            if wd.get(sid, 0) >= v:
                continue
            wd[sid] = v
            waits.append((s, v))
        for r in reads:
            self._st(r)["r"][id(tok.sem)] = tok
        for w in writes:
            st = self._st(w)
            st["w"] = tok
            st["r"] = {}
        self.prog[eng].append((waits, fn, inc))
        return tok

    def op(self, eng, fn, reads=(), writes=()):
        return self._emit(eng, fn, reads, writes, False)

    def all_tokens(self):
        toks = {}
        for e in self.cengs:
            if self.cnt[e] > 0:
                toks[id(self.sem[e])] = (self.sem[e], self.cnt[e])
        for i in range(N_DMA_SEMS):
            if self.dcnt[i] > 0:
                toks[id(self.dsem[i])] = (self.dsem[i], self.dcnt[i])
        return toks

    def barrier(self):
        toks = self.all_tokens()
        for e in self.all:
            self.pend[e] = dict(toks)
        self.res = {}

    def dma(self, out, in_, reads=(), writes=(), q="sp", **kw):
        return self._emit(q, lambda e: e.dma_start(out=out, in_=in_, **kw), reads, writes, True)

    def mm(self, out, lhsT, rhs, start, stop, reads=(), writes=()):
        return self.op("pe", lambda e: e.matmul(out, lhsT, rhs, start=start, stop=stop), reads, writes)

    def tr(self, out, in_, ident, reads=(), writes=()):
        return self.op("pe", lambda e: e.transpose(out, in_, ident), reads, writes)

    def act(self, out, in_, func, bias=None, scale=None, accum_out=None, reads=(), writes=(), eng="act"):
        kw = {}
        if bias is not None:
            kw["bias"] = bias
        if scale is not None:
            kw["scale"] = scale
        if accum_out is not None:
            kw["accum_out"] = accum_out
        return self.op(eng, lambda e: e.activation(out, in_, func, **kw), reads, writes)

    def tt(self, eng, out, in0, in1, op, reads=(), writes=()):
        return self.op(eng, lambda e: e.tensor_tensor(out, in0, in1, op), reads, writes)

    def ts(self, eng, out, in0, s1, s2, op0, op1=None, reads=(), writes=(), accum_out=None):
        kw = {}
        if accum_out is not None:
            kw["accum_out"] = accum_out
        if op1 is None:
            return self.op(eng, lambda e: e.tensor_scalar(out, in0, s1, None, op0, **kw), reads, writes)
        return self.op(eng, lambda e: e.tensor_scalar(out, in0, s1, s2, op0, op1, **kw), reads, writes)

    def stt(self, eng, out, in0, scalar, in1, op0, op1, reads=(), writes=()):
        return self.op(eng, lambda e: e.scalar_tensor_tensor(out, in0, scalar, in1, op0, op1), reads, writes)

    def copy(self, eng, out, in_, reads=(), writes=()):
        if eng == "act":
            return self.op(eng, lambda e: e.copy(out, in_), reads, writes)
        return self.op(eng, lambda e: e.tensor_copy(out, in_), reads, writes)

    def memset(self, eng, ap, val, writes=()):
        return self.op(eng, lambda e: e.memset(ap, val), (), writes)

    def finish(self, final_tokens):
        nc = self.nc
        prog = self.prog
        engmap = {"pe": "tensor", "act": "scalar", "dve": "vector", "pool": "gpsimd", "sp": "sync"}
        fin = self.all_tokens()
        with nc.Block() as block:
            for ename in self.all:
                entries = prog[ename]
                is_sp = ename == "sp"

                def body(e, entries=entries, is_sp=is_sp):
                    for waits, fn, inc in entries:
                        for wi_, (s, v) in enumerate(waits):
                            e.wait_ge(s, v)
                            if USE_NOPS and wi_ + 1 < len(waits):
                                e.nop(nofuse=True)
                        ins = fn(e)
                        ins.then_inc(inc[0], inc[1])
                    if is_sp:
                        for (s, v) in fin.values():
                            e.wait_ge(s, v)

                getattr(block, engmap[ename])(body)


D = 1024
T = 2304
NT = 18
LT = 2048
ALPHA = (2.0 * 2) ** 0.25
NEG = -30000.0


def tcol(ti):
    return 1 + 128 * ti if ti < 2 else 2 + 128 * ti


BLOCKS = [(1, 256, 0)] + [(258 + 512 * j, 512, 256 + 512 * j) for j in range(4)]


ORDER = ['pw', 'pm', 'A', 'B', 'C', 'D', 'E', 'F', 'G', 'L1A', 'L1P', 'L1W', 'L1WA', 'L1U', 'L1MA', 'L1E', 'L1F', 'L1']


def build(NB, dbg=False, only0=False, stop='L1'):
    def want(nm):
        return ORDER.index(nm) <= ORDER.index(stop)

    nc = bass.Bass("TRN2", target_bir_lowering=False)
    R = NB + 1
    dd = {}

    def din(name, shape, dt=F32):
        dd[name] = nc.dram_tensor(name, list(shape), dt, kind="ExternalInput").ap()
        return dd[name]

    def dscr(name, shape, dt=F32, out=False):
        return nc.dram_tensor(name, list(shape), dt, kind="ExternalOutput" if out else "Internal").ap()

    x_d = din("x", [NB, LT, D]); ctx_d = din("ctx", [NB, 256, D]); csT_d = din("csT", [128, 8, R])
    adaw_d = din("ada_w", [2, D, 6144]); adab_d = din("ada_b", [2, 6144])
    lng_d = din("ln_g", [4, D]); lnb_d = din("ln_b", [4, D])
    evwin_d = din("ev_w_in", [D, 3600]); aconvT_d = din("a_convT", [128, 4, 3]); bconv_d = din("ev_b_conv", [3, 1536])
    alog_d = din("alog", [1, 8]); dtb_d = din("dtbias", [1, 8]); bnorm_d = din("bnorm", [1, 128]); evwout_d = din("ev_w_out", [D, D])
    odwin_d = din("od_w_in", [D, 1440]); odwinsw_d = din("od_w_in_sw", [D, 1440])
    wkr_d = din("wkr", [D, 96]); wkrsw_d = din("wkr_sw", [D, 96])
    sink_d = din("sink", [1, 8]); qnT_d = din("qnormT", [128, 3]); kvnT_d = din("kvnormT", [128, 2])
    wuq_d = din("wuq", [384, 768]); wuqsw_d = din("wuq_sw", [384, 768]); wukv_d = din("wukv", [256, 1024]); odwout_d = din("od_w_out", [D, D])
    rw_d = din("router_w", [D, 16]); rb_d = din("router_bias", [1, 16])
    mg_d = din("moe_g", [32768, 512]); mu_d = din("moe_u", [32768, 512]); md_d = din("moe_d", [16384, 1024])
    idn_d = din("idn", [128, 128]); masks_d = din("masks", [14, 128, 128])
    rope64_d = din("rope64", [2, 64, LT]); rope32_d = din("rope32", [2, 96, LT])
    out_d = dscr("out", [NB, LT, D], F32, out=True)

    evwin_b = dscr("evwin_b", [D, 3600], BF16); wB_b = [dscr("wB%d_b" % j, [D, 1536], BF16) for j in range(3)]
    evwout_b = dscr("evwout_b", [D, D], BF16)
    odwin_b = dscr("odwin_b", [D, 1440], BF16); odwinsw_b = dscr("odwinsw_b", [D, 1440], BF16)
    wkr_b = dscr("wkr_b", [D, 96], BF16); wkrsw_b = dscr("wkrsw_b", [D, 96], BF16)
    wuq_b = dscr("wuq_b", [384, 768], BF16); wuqsw_b = dscr("wuqsw_b", [384, 768], BF16); wukv_b = dscr("wukv_b", [256, 1024], BF16)
    odwout_b = dscr("odwout_b", [D, D], BF16)
    modrow_s = dscr("modrow_s", [2, R, 6144], F32, out=dbg)
    qT_s = dscr("qT_s", [4, 128, T], BF16); kT_s = dscr("kT_s", [4, 128, T], BF16)
    k_s = dscr("k_s", [T, 512], BF16); v_s = dscr("v_s", [T, 512], BF16); gate_s = dscr("gate_s", [T, 512]); o_s = dscr("o_s", [T, 512])
    xa_s = dscr("xa_s", [NB, 2, T, D], F32, out=dbg)
    xb_s = dscr("xb_s", [NB, T, D], F32, out=dbg)
    h2T_s = dscr("h2T_s", [8, 128, T], BF16)
    cat_dbg = dscr("cat_dbg", [128, 8, LT], BF16, out=True) if dbg else None
    ymix_s = dscr("ymix_s", [NB, 2, T, D], F32, out=dbg) if dbg else None

    with ExitStack() as st0:
        S = Sched(nc, st0)
        cnt = [0]

        def sb(stack, shape, dt=F32, name=None):
            cnt[0] += 1
            return stack.enter_context(nc.sbuf_tensor("%s_%d" % (name or "t", cnt[0]), list(shape), dt))

        PB = [st0.enter_context(nc.psum_tensor("pb%d" % i, [128, 512], F32)) for i in range(8)]
        pbi = [0]
        S.excl = set(id(p_) for p_ in PB)

        def pb():
            p = PB[pbi[0] % 8]
            pbi[0] += 1
            return p

        ld_rr = [0]

        def rr3():
            ld_rr[0] += 1
            return ("dve", "pool", "act")[ld_rr[0] % 3]

        ident = sb(st0, [128, 128], name="ident"); S.dma(ident[:], idn_d, writes=[ident])
        ones = sb(st0, [128, 128], name="ones"); S.memset("pool", ones[:], 1.0, writes=[ones])
        MK = sb(st0, [128, 14, 128], name="masks")
        S.dma(MK[:], masks_d.rearrange("m p f -> p m f"), writes=[MK])
        modT = sb(st0, [128, 2, 48, R], name="modT")
        MKb = sb(st0, [128, 14, 128], BF16, name="masksb"); S.copy("dve", MKb[:], MK[:], reads=[MK], writes=[MKb])
        identb = sb(st0, [128, 128], BF16, name="identb"); S.copy("dve", identb[:], ident[:], reads=[ident], writes=[identb])
        onesb = sb(st0, [128, 128], BF16, name="onesb"); S.memset("pool", onesb[:], 1.0, writes=[onesb])
        rwt = sb(st0, [128, 8, 16], name="rw"); S.dma(rwt[:], rw_d.rearrange("(kc p) e -> p kc e", p=128), writes=[rwt])
        rbias = sb(st0, [128, 16], name="rbias"); S.dma(rbias[:], rb_d.to_broadcast([128, 16]), writes=[rbias])
        aconvT = sb(st0, [128, 4, 3], name="aconvT"); S.dma(aconvT[:], aconvT_d, writes=[aconvT])
        negexpA = sb(st0, [128, 8], name="negexpA"); dtb = sb(st0, [128, 8], name="dtb")
        S.dma(negexpA[:], alog_d.to_broadcast([128, 8]), writes=[negexpA]); S.dma(dtb[:], dtb_d.to_broadcast([128, 8]), writes=[dtb])
        S.act(negexpA[:], negexpA[:], AF.Exp, reads=[negexpA], writes=[negexpA])
        S.ts("dve", negexpA[:], negexpA[:], -1.0, None, ALU.mult, reads=[negexpA], writes=[negexpA])
        bnorm = sb(st0, [128, 128], name="bnorm"); S.dma(bnorm[:], bnorm_d.to_broadcast([128, 128]), writes=[bnorm])
        expsink = sb(st0, [128, 8], name="expsink"); S.dma(expsink[:], sink_d.to_broadcast([128, 8]), writes=[expsink])
        S.act(expsink[:], expsink[:], AF.Exp, reads=[expsink], writes=[expsink])
        qnT = sb(st0, [128, 3], name="qnT"); S.dma(qnT[:], qnT_d, writes=[qnT])
        kvnT = sb(st0, [128, 2], name="kvnT"); S.dma(kvnT[:], kvnT_d, writes=[kvnT])

        with ExitStack() as st:
            NSTG = 6
            stg = [sb(st, [128, 4096], F32, "stg") for _ in range(NSTG)]
            stgb = [sb(st, [128, 4096], BF16, "stgb") for _ in range(NSTG)]
            bcv = sb(st, [128, 3, 1536], F32, "bcv")
            for j in range(3):
                S.dma(bcv[:, j, :], bconv_d[j:j + 1, :].to_broadcast([128, 1536]), writes=[(bcv, j)])
            ui = [0]

            def conv(src, dst, rows, cols, scale=None):
                nrc = rows // 128
                G = max(1, min(nrc, 4096 // cols)) if cols <= 4096 else 1
                if scale is not None:
                    G = 1
                while nrc % G:
                    G -= 1
                for r0 in range(0, nrc, G):
                    i = ui[0] % NSTG
                    ui[0] += 1
                    eng = ("dve", "pool", "act")[i % 3]
                    a = stg[i][:, 0:G * cols].rearrange("p (g c) -> p g c", g=G)
                    b = stgb[i][:, 0:G * cols].rearrange("p (g c) -> p g c", g=G)
                    sv = src[r0 * 128:(r0 + G) * 128, :].rearrange("(g p) c -> p g c", p=128)
                    dv = dst[r0 * 128:(r0 + G) * 128, :].rearrange("(g p) c -> p g c", p=128)
                    S.dma(a, sv, writes=[stg[i]])
                    if scale is None:
                        S.copy(eng, b, a, reads=[stg[i]], writes=[stgb[i]])
                    else:
                        e2 = "pool" if eng == "act" else eng
                        S.tt(e2, b[:, 0, :], a[:, 0, :], scale, ALU.mult, reads=[stg[i], (bcv, 0), (bcv, 1), (bcv, 2)], writes=[stgb[i]])
                    S.dma(dv, b, reads=[stgb[i]])

            conv(evwin_d, evwin_b, D, 3600)
            for j in range(3):
                conv(evwin_d[:, 1536:3072], wB_b[j], D, 1536, scale=bcv[:, j, :])
            conv(evwout_d, evwout_b, D, D)
            conv(odwin_d, odwin_b, D, 1440); conv(odwinsw_d, odwinsw_b, D, 1440)
            conv(wkr_d, wkr_b, D, 96); conv(wkrsw_d, wkrsw_b, D, 96)
            conv(wuq_d, wuq_b, 384, 768); conv(wuqsw_d, wuqsw_b, 384, 768); conv(wukv_d, wukv_b, 256, 1024)
            conv(odwout_d, odwout_b, D, D)
            S.barrier()
        with ExitStack() as st:
          if want('pm'):
            csT = sb(st, [128, 8, R], F32, "csT")
            S.dma(csT[:], csT_d, writes=[csT])
            S.act(csT[:], csT[:], AF.Silu, reads=[csT], writes=[csT])
            awt = [sb(st, [128, 8, 1536], F32, "awt") for _ in range(2)]
            modrow = sb(st, [R, 6144], F32, "modrow")
            abr = sb(st, [R, 6144], F32, "abr")
            for l in range(2):
                S.dma(abr[:], adab_d[l:l + 1, :].to_broadcast([R, 6144]), reads=[modrow], writes=[abr])
                for q in range(4):
                    aw = awt[q % 2]
                    S.dma(aw[:], adaw_d[l, :, q * 1536:(q + 1) * 1536].rearrange("(kc p) n -> p kc n", p=128), writes=[aw])
                    for nb_ in range(3):
                        p = pb()
                        c0 = q * 1536 + nb_ * 512
                        for kc in range(8):
                            S.mm(p[0:R, :], csT[:, kc, :], aw[:, kc, nb_ * 512:(nb_ + 1) * 512], kc == 0, kc == 7, reads=[csT, aw], writes=[p])
                        S.tt("dve", modrow[:, c0:c0 + 512], p[0:R, :], abr[:, c0:c0 + 512], ALU.add, reads=[p, abr], writes=[modrow])
                S.dma(modrow_s[l], modrow[:], reads=[modrow])
                p = pb()
                for ch in range(48):
                    S.tr(p[:, ch * R:(ch + 1) * R], modrow[0:R, ch * 128:(ch + 1) * 128], ident[0:R, 0:R], reads=[modrow, ident], writes=[p])
                S.copy("dve", modT[:, l, :, :], p[:, 0:48 * R].rearrange("p (c r) -> p c r", r=R), reads=[p], writes=[modT])
                for c0 in (8, 32):
                    S.ts("dve", modT[:, l, c0:c0 + 8, :], modT[:, l, c0:c0 + 8, :], 1.0, None, ALU.add, reads=[modT], writes=[modT])
            S.barrier()

        def xsrc(layer, b, ti):
            if layer == 0:
                return ctx_d[b, ti * 128:(ti + 1) * 128, :] if ti < 2 else x_d[b, (ti - 2) * 128:(ti - 1) * 128, :]
            return xb_s[b, ti * 128:(ti + 1) * 128, :]

        def phase_A(layer, b, hT, tiles, st):
            xin = [sb(st, [128, D], F32, "xin") for _ in range(2)]
            for n, ti in enumerate(tiles):
                xt = xin[n % 2]
                r = NB if ti < 2 else b
                S.dma(xt[:], xsrc(layer, b, ti), writes=[xt])
                for half in range(2):
                    p = pb()
                    for k4 in range(4):
                        kc = half * 4 + k4
                        S.tr(p[:, k4 * 128:(k4 + 1) * 128], xt[:, kc * 128:(kc + 1) * 128], ident[:], reads=[xt], writes=[p])
                    for k4 in range(4):
                        kc = half * 4 + k4
                        dst = hT[:, kc, tcol(ti):tcol(ti) + 128]
                        if half == 0:
                            S.act(dst, p[:, k4 * 128:(k4 + 1) * 128], AF.Identity, bias=modT[:, layer, kc, r:r + 1],
                                  scale=modT[:, layer, 8 + kc, r:r + 1], reads=[p], writes=[(hT, ti, kc)])
                        else:
                            S.ts("dve", dst, p[:, k4 * 128:(k4 + 1) * 128], modT[:, layer, 8 + kc, r:r + 1], modT[:, layer, kc, r:r + 1],
                                 ALU.mult, ALU.add, reads=[p], writes=[(hT, ti, kc)])

        def layer_norm_tile(st_tiles, rt, lng, lnb, outt):
            stats, ag, sm = st_tiles
            for hf in range(2):
                S.op("dve", lambda e, hf=hf: e.bn_stats(stats[:, hf, :], rt[:, hf * 512:(hf + 1) * 512]), reads=[rt], writes=[stats])
            S.op("dve", lambda e: e.bn_aggr(ag[:], stats[:].rearrange('p a b -> p (a b)')), reads=[stats], writes=[ag])
            S.act(sm[:, 0:1], ag[:, 1:2], AF.Sqrt, bias=1e-5, reads=[ag], writes=[sm])
            S.op("dve", lambda e: e.reciprocal(sm[:, 1:2], sm[:, 0:1]), reads=[sm], writes=[sm])
            S.stt("dve", sm[:, 2:3], ag[:, 0:1], -1.0, sm[:, 1:2], ALU.mult, ALU.mult, reads=[ag, sm], writes=[sm])
            S.act(rt[:], rt[:], AF.Identity, bias=sm[:, 2:3], scale=sm[:, 1:2], reads=[rt, sm], writes=[rt])
            S.tt("pool", rt[:], rt[:], lng[:], ALU.mult, reads=[rt, lng], writes=[rt])
            S.tt("pool", outt[:], rt[:], lnb[:], ALU.add, reads=[rt, lnb], writes=[outt])

        def run_pipe(gens, depth):
            active = []
            it = iter(gens)
            fin = False
            while True:
                while len(active) < depth and not fin:
                    try:
                        active.append(next(it))
                    except StopIteration:
                        fin = True
                if not active:
                    break
                for g_ in list(active):
                    try:
                        next(g_)
                    except StopIteration:
                        active.remove(g_)

        def ln_stats(rt, stats, ag, sm):
            for hf in range(2):
                S.op("dve", lambda e, hf=hf: e.bn_stats(stats[:, hf, :], rt[:, hf * 512:(hf + 1) * 512]), reads=[rt], writes=[stats])
            S.op("dve", lambda e: e.bn_aggr(ag[:], stats[:].rearrange('p a b -> p (a b)')), reads=[stats], writes=[ag])
            S.act(sm[:, 0:1], ag[:, 1:2], AF.Sqrt, bias=1e-5, reads=[ag], writes=[sm])
            S.op("dve", lambda e: e.reciprocal(sm[:, 1:2], sm[:, 0:1]), reads=[sm], writes=[sm])
            S.stt("dve", sm[:, 2:3], ag[:, 0:1], -1.0, sm[:, 1:2], ALU.mult, ALU.mult, reads=[ag, sm], writes=[sm])

        def ln_apply(rt, sm, lng, lnb, outt):
            S.act(rt[:], rt[:], AF.Identity, bias=sm[:, 2:3], scale=sm[:, 1:2], reads=[rt, sm], writes=[rt])
            S.tt("dve", rt[:], rt[:], lng[:], ALU.mult, reads=[rt, lng], writes=[rt])
            S.tt("dve", outt[:], rt[:], lnb[:], ALU.add, reads=[rt, lnb], writes=[outt])

        def phase_E(layer, b, catT, tiles, gates, st):
            nt = len(tiles)
            NBUF = 5
            wo = sb(st, [128, 8, D], BF16, "wo")
            wsrc = evwout_b if layer == 0 else odwout_b
            S.dma(wo[:], wsrc.rearrange("(kc p) n -> p kc n", p=128), writes=[wo])
            lng = sb(st, [128, D], F32, "lng"); lnb = sb(st, [128, D], F32, "lnb")
            S.dma(lng[:], lng_d[2 * layer:2 * layer + 1, :].to_broadcast([128, D]), writes=[lng])
            S.dma(lnb[:], lnb_d[2 * layer:2 * layer + 1, :].to_broadcast([128, D]), writes=[lnb])
            g1 = [sb(st, [128, D], F32, "g1") for _ in range(2)]
            S.dma(g1[0][:], modrow_s[layer, NB:NB + 1, 2048:3072].to_broadcast([128, D]), writes=[g1[0]])
            S.dma(g1[1][:], modrow_s[layer, b:b + 1, 2048:3072].to_broadcast([128, D]), writes=[g1[1]])
            xin = [sb(st, [128, D], F32, "xin") for _ in range(NBUF)]
            rts = [sb(st, [128, D], F32, "rt") for _ in range(NBUF)]
            xns = [sb(st, [128, D], F32, "xn") for _ in range(NBUF)]
            h2f = [sb(st, [128, 8, 128], F32, "h2f") for _ in range(NBUF)]
            h2b = [sb(st, [128, 8, 128], BF16, "h2b") for _ in range(NBUF)]
            statsL = [sb(st, [128, 2, 6], F32, "stats") for _ in range(NBUF)]
            agL = [sb(st, [128, 2], F32, "ag") for _ in range(NBUF)]
            smL = [sb(st, [128, 4], F32, "sm") for _ in range(NBUF)]
            aff = sb(st, [128, nt, 16], F32, "aff")

            def tile_gen(n, ti):
                k = n % NBUF
                xt = xin[k]; rt = rts[k]; xn = xns[k]; hf_ = h2f[k]; hb_ = h2b[k]; stats = statsL[k]; ag = agL[k]; sm = smL[k]
                r = NB if ti < 2 else b
                gt = g1[0] if ti < 2 else g1[1]
                S.dma(xt[:], xsrc(layer, b, ti), writes=[xt])
                c0 = tcol(ti) if layer == 0 else (ti - 2) * 128
                for hf in range(2):
                    p = pb()
                    for kc in range(8):
                        S.mm(p[:], catT[:, kc, c0:c0 + 128], wo[:, kc, hf * 512:(hf + 1) * 512], kc == 0, kc == 7, reads=[wo], writes=[p])
                    if dbg:
                        S.copy("act", rt[:, hf * 512:(hf + 1) * 512], p[:], reads=[p], writes=[rt])
                        S.dma(ymix_s[b, layer, ti * 128:(ti + 1) * 128, hf * 512:(hf + 1) * 512], rt[:, hf * 512:(hf + 1) * 512], reads=[rt])
                    S.tt("dve", rt[:, hf * 512:(hf + 1) * 512], p[:], gt[:, hf * 512:(hf + 1) * 512], ALU.mult, reads=[p, gt], writes=[rt])
                yield
                S.stt("dve", rt[:], xt[:], ALPHA, rt[:], ALU.mult, ALU.add, reads=[xt, rt], writes=[rt])
                ln_stats(rt, stats, ag, sm)
                yield
                ln_apply(rt, sm, lng, lnb, xn)
                S.dma(xa_s[b, layer, ti * 128:(ti + 1) * 128, :], xn[:], reads=[xn])
                yield
                for half in range(2):
                    p = pb()
                    for k4 in range(4):
                        kc = half * 4 + k4
                        S.tr(p[:, k4 * 128:(k4 + 1) * 128], xn[:, kc * 128:(kc + 1) * 128], ident[:], reads=[xn], writes=[p])
                    for k4 in range(4):
                        kc = half * 4 + k4
                        if half == 0:
                            S.act(hf_[:, kc, :], p[:, k4 * 128:(k4 + 1) * 128], AF.Identity, bias=modT[:, layer, 24 + kc, r:r + 1],
                                  scale=modT[:, layer, 32 + kc, r:r + 1], reads=[p], writes=[hf_])
                        else:
                            S.ts("dve", hf_[:, kc, :], p[:, k4 * 128:(k4 + 1) * 128], modT[:, layer, 32 + kc, r:r + 1], modT[:, layer, 24 + kc, r:r + 1],
                                 ALU.mult, ALU.add, reads=[p], writes=[hf_])
                yield
                S.copy("act", hb_[:], hf_[:], reads=[hf_], writes=[hb_])
                S.dma(h2T_s.rearrange("k p t -> p k t")[:, :, n * 128:(n + 1) * 128], hb_[:], reads=[hb_])
                p = pb()
                for kc in range(8):
                    S.mm(p[:, 0:16], hf_[:, kc, :], rwt[:, kc, :], kc == 0, kc == 7, reads=[hf_], writes=[p])
                S.act(aff[:, n, :], p[:, 0:16], AF.Sigmoid, reads=[p], writes=[(aff, n)])

            run_pipe((tile_gen(n, ti) for n, ti in enumerate(tiles)), 4)
            router_batched(aff, gates, nt, st)

        def router_batched(aff, gates, nt, st):
            G4 = nt * 4
            sel = sb(st, [128, nt, 16], F32, "r_sel"); t1 = sb(st, [128, nt, 16], F32, "r_t1"); t2 = sb(st, [128, nt, 16], F32, "r_t2")
            m1 = sb(st, [128, G4], F32, "r_m1"); sec = sb(st, [128, G4], F32, "r_sec"); gs = sb(st, [128, G4], F32, "r_gs")
            gm = sb(st, [128, G4], F32, "r_gm"); tm = sb(st, [128, G4], F32, "r_tm")
            s1 = sb(st, [128, nt], F32, "r_s1"); s2 = sb(st, [128, nt], F32, "r_s2"); den = sb(st, [128, nt], F32, "r_den")
            allaff = [(aff, n) for n in range(nt)]
            RS = "rsres"

            def g4(t):
                return t[:].rearrange("p n (g e) -> p (n g) e", g=4)

            def bc44(t):
                return t[:].unsqueeze(2).to_broadcast([128, G4, 4])

            def bc16(t):
                return t[:].unsqueeze(2).to_broadcast([128, nt, 16])

            def dv(fn, *a, **k):
                return S.op("dve", fn, reads=allaff + [RS], writes=[RS])
            dv(lambda e: e.tensor_tensor(sel[:], aff[:], rbias[:].unsqueeze(1).to_broadcast([128, nt, 16]), ALU.add))
            dv(lambda e: e.tensor_reduce(m1[:], g4(sel), AX.X, ALU.max))
            dv(lambda e: e.tensor_tensor(g4(t1), g4(sel), bc44(m1), ALU.is_lt))
            dv(lambda e: e.tensor_scalar(t2[:], t1[:], 1.0, 1e9, ALU.subtract, ALU.mult))
            dv(lambda e: e.tensor_tensor(t1[:], t1[:], sel[:], ALU.mult))
            dv(lambda e: e.tensor_tensor(t2[:], t2[:], t1[:], ALU.add))
            dv(lambda e: e.tensor_reduce(sec[:], g4(t2), AX.X, ALU.max))
            dv(lambda e: e.tensor_tensor(gs[:], m1[:], sec[:], ALU.add))
            gs3 = gs[:].rearrange("p (n g) -> p n g", g=4)
            dv(lambda e: e.tensor_reduce(s1[:], gs3, AX.X, ALU.max))
            dv(lambda e: e.tensor_tensor(gm[:].rearrange("p (n g) -> p n g", g=4), gs3, s1[:].unsqueeze(2).to_broadcast([128, nt, 4]), ALU.is_ge))
            dv(lambda e: e.tensor_tensor(g4(t1), g4(sel), bc44(gm), ALU.mult))
            dv(lambda e: e.tensor_scalar(tm[:], gm[:], 1.0, 1e9, ALU.subtract, ALU.mult))
            dv(lambda e: e.tensor_tensor(g4(t1), g4(t1), bc44(tm), ALU.add))
            dv(lambda e: e.tensor_reduce(s1[:], t1[:], AX.X, ALU.max))
            dv(lambda e: e.tensor_tensor(t2[:], t1[:], bc16(s1), ALU.is_lt))
            dv(lambda e: e.tensor_tensor(sel[:], t1[:], t2[:], ALU.mult))
            dv(lambda e: e.tensor_scalar(t2[:], t2[:], 1.0, 1e9, ALU.subtract, ALU.mult))
            dv(lambda e: e.tensor_tensor(sel[:], sel[:], t2[:], ALU.add))
            dv(lambda e: e.tensor_reduce(s2[:], sel[:], AX.X, ALU.max))
            dv(lambda e: e.tensor_tensor(t2[:], t1[:], bc16(s2), ALU.is_ge))
            dv(lambda e: e.tensor_tensor(t2[:], t2[:], aff[:], ALU.mult))
            dv(lambda e: e.tensor_reduce(den[:], t2[:], AX.X, ALU.add))
            dv(lambda e: e.reciprocal(den[:], den[:]))
            S.op("dve", lambda e: e.tensor_tensor(gates[:], t2[:], bc16(den), ALU.mult), reads=[RS], writes=[gates])

        def phase_F(layer, b, h2T, gates, ntile, outacc, st):
            wgu = [sb(st, [128, 2, 8, 512], BF16, "wgu") for _ in range(2)]
            wdn = [sb(st, [128, 4, D], BF16, "wdn") for _ in range(2)]
            actT = [sb(st, [128, 4, 512], BF16, "actT") for _ in range(2)]
            sl = [sb(st, [128, 512], F32, "sl") for _ in range(2)]
            stgF = [sb(st, [128, 2048], F32, "stgF") for _ in range(2)]
            ntok = ntile * 128
            blocks = [(c, min(512, ntok - c)) for c in range(0, ntok, 512)]
            pi = [0]

            def pieces(e):
                wg = wgu[e % 2]; wd = wdn[e % 2]
                r0 = (layer * 16 + e) * 1024
                r1 = (layer * 16 + e) * 512
                out = []
                for which, src in ((0, mg_d), (1, mu_d)):
                    for half in range(2):
                        src_ap = src[r0 + half * 512:r0 + (half + 1) * 512, :].rearrange("(kc p) f -> p kc f", p=128)
                        out.append((wg[:, which, half * 4:(half + 1) * 4, :], src_ap, (wg, which, half), 4))
                for half in range(2):
                    src_ap = md_d[r1 + half * 256:r1 + (half + 1) * 256, :].rearrange("(fc p) n -> p fc n", p=128)
                    out.append((wd[:, half * 2:(half + 1) * 2, :], src_ap, (wd, half), 2))
                return out

            def load_cast(pc):
                dst_ap, src_ap, key, G = pc
                sg = stgF[pi[0] % 2]; pi[0] += 1
                sgv = sg[:, 0:2048].rearrange("p (g c) -> p g c", g=G)
                S.dma(sgv, src_ap, writes=[sg])
                S.copy("pool", dst_ap, sgv, reads=[sg], writes=[key])

            def down(at, wd, e, c0, w):
                for j in range(w // 128):
                    n = c0 // 128 + j
                    for hf in range(2):
                        pd = pb()
                        for fc in range(4):
                            S.mm(pd[:], at[:, fc, j * 128:(j + 1) * 128], wd[:, fc, hf * 512:(hf + 1) * 512], fc == 0, fc == 3,
                                 reads=[(wd, fc // 2), (at, 0), (at, 1), (at, 2), (at, 3)], writes=[pd])
                        dst = outacc[:, n, hf * 512:(hf + 1) * 512]
                        if e == 0:
                            S.ts("dve", dst, pd[:], gates[:, n, e:e + 1], None, ALU.mult, reads=[pd], writes=[(outacc, n, hf)])
                        else:
                            S.stt("dve", dst, pd[:], gates[:, n, e:e + 1], dst, ALU.mult, ALU.add, reads=[pd], writes=[(outacc, n, hf)])

            for pc in pieces(0):
                load_cast(pc)
            it = 0
            pending = None
            for e in range(16):
                wg = wgu[e % 2]; wd = wdn[e % 2]
                nxt = pieces(e + 1) if e + 1 < 16 else []
                for bi, (c0, w) in enumerate(blocks):
                    at = actT[it % 2]; it += 1
                    for fc in range(4):
                        pg = pb(); pu = pb()
                        for kc in range(8):
                            S.mm(pg[:, 0:w], wg[:, 0, kc, fc * 128:(fc + 1) * 128], h2T[:, kc, c0:c0 + w], kc == 0, kc == 7, reads=[(wg, 0, kc // 4)], writes=[pg])
                        for kc in range(8):
                            S.mm(pu[:, 0:w], wg[:, 1, kc, fc * 128:(fc + 1) * 128], h2T[:, kc, c0:c0 + w], kc == 0, kc == 7, reads=[(wg, 1, kc // 4)], writes=[pu])
                        s_ = sl[fc % 2]
                        S.act(s_[:, 0:w], pg[:, 0:w], AF.Silu, reads=[pg], writes=[s_])
                        S.tt("dve", at[:, fc, 0:w], s_[:, 0:w], pu[:, 0:w], ALU.mult, reads=[s_, pu], writes=[(at, fc)])
                    if pending is not None:
                        down(*pending)
                    pending = (at, wd, e, c0, w)
                    k = 2 if bi == 0 else 1
                    for _ in range(k):
                        if nxt:
                            load_cast(nxt.pop(0))
                while nxt:
                    load_cast(nxt.pop(0))
            down(*pending)

        def phase_G(layer, b, tiles, outacc, st):
            NBUF = 5
            lng = sb(st, [128, D], F32, "lng2"); lnb = sb(st, [128, D], F32, "lnb2")
            S.dma(lng[:], lng_d[2 * layer + 1:2 * layer + 2, :].to_broadcast([128, D]), writes=[lng])
            S.dma(lnb[:], lnb_d[2 * layer + 1:2 * layer + 2, :].to_broadcast([128, D]), writes=[lnb])
            g2 = [sb(st, [128, D], F32, "g2") for _ in range(2)]
            S.dma(g2[0][:], modrow_s[layer, NB:NB + 1, 5120:6144].to_broadcast([128, D]), writes=[g2[0]])
            S.dma(g2[1][:], modrow_s[layer, b:b + 1, 5120:6144].to_broadcast([128, D]), writes=[g2[1]])
            xin = [sb(st, [128, D], F32, "xin2") for _ in range(NBUF)]
            rts = [sb(st, [128, D], F32, "rt2") for _ in range(NBUF)]
            xns = [sb(st, [128, D], F32, "xn2") for _ in range(NBUF)]
            statsL = [sb(st, [128, 2, 6], F32, "stats2") for _ in range(NBUF)]
            agL = [sb(st, [128, 2], F32, "ag2") for _ in range(NBUF)]
            smL = [sb(st, [128, 4], F32, "sm2") for _ in range(NBUF)]

            def tile_gen(n, ti):
                k = n % NBUF
                xt = xin[k]; rt = rts[k]; xn = xns[k]; stats = statsL[k]; ag = agL[k]; sm = smL[k]
                gt = g2[0] if ti < 2 else g2[1]
                S.dma(xt[:], xa_s[b, layer, ti * 128:(ti + 1) * 128, :], writes=[xt])
                S.tt("dve", rt[:], outacc[:, n, :], gt[:], ALU.mult, reads=[gt, (outacc, n, 0), (outacc, n, 1)], writes=[rt])
                yield
                S.stt("dve", rt[:], xt[:], ALPHA, rt[:], ALU.mult, ALU.add, reads=[xt, rt], writes=[rt])
                ln_stats(rt, stats, ag, sm)
                yield
                ln_apply(rt, sm, lng, lnb, xn)
                if layer == 0:
                    S.dma(xb_s[b, ti * 128:(ti + 1) * 128, :], xn[:], reads=[xn])
                else:
                    S.dma(out_d[b, (ti - 2) * 128:(ti - 1) * 128, :], xn[:], reads=[xn])

            run_pipe((tile_gen(n, ti) for n, ti in enumerate(tiles)), 4)

        def post_E(layer, b, catT, tiles, gates):
            if not want('E'):
                return
            with ExitStack() as st2:
                phase_E(layer, b, catT, tiles, gates, st2)
                S.barrier()

        def post_FG(layer, b, tiles, gates):
            nt = len(tiles)
            if not want('F'):
                return
            with ExitStack() as st:
                outacc = sb(st, [128, nt, D], F32, "outacc")
                with ExitStack() as st2:
                    h2T = sb(st2, [128, 8, nt * 128], BF16, "h2T")
                    for kc in range(8):
                        S.dma(h2T[:, kc, :], h2T_s[kc, :, 0:nt * 128], writes=[h2T])
                    phase_F(layer, b, h2T, gates, nt, outacc, st2)
                    S.barrier()
                if not want('G'):
                    return
                with ExitStack() as st2:
                    phase_G(layer, b, tiles, outacc, st2)
                    S.barrier()

        def phase_B(b, hT, catT, st):
            wA = [sb(st, [128, 3, 8, 128], BF16, "wA") for _ in range(2)]
            u = sb(st, [128, 2307], F32, "u"); p0s = sb(st, [128, 2307], F32, "p0s"); y = sb(st, [128, 2307], F32, "y")
            t1 = [sb(st, [128, 512], F32, "t1") for _ in range(2)]
            for c in (0, 257, 2306):
                S.memset("pool", u[:, c:c + 1], 0.0, writes=[(u, "pad")])
            bi = 0
            for ch in range(4):
                w = wA[ch % 2]
                for which in range(3):
                    cc = which * 512 + ch * 128
                    S.dma(w[:, which], evwin_b[:, cc:cc + 128].rearrange("(kc p) n -> p kc n", p=128), writes=[(w, which)])
                for (c0, wd_, t0) in BLOCKS:
                    pp = [pb(), pb(), pb()]
                    for which in range(3):
                        for kc in range(8):
                            S.mm(pp[which][:, 0:wd_], w[:, which, kc, :], hT[:, kc, c0:c0 + wd_], kc == 0, kc == 7, reads=[(w, which)], writes=[pp[which]])
                    tt_ = t1[bi % 2]; bi += 1
                    S.copy("act", tt_[:, 0:wd_], pp[1][:, 0:wd_], reads=[pp[1]], writes=[tt_])
                    S.tt("dve", u[:, c0:c0 + wd_], tt_[:, 0:wd_], pp[2][:, 0:wd_], ALU.mult, reads=[tt_, pp[2]], writes=[(u, c0)])
                    S.copy("act", p0s[:, c0:c0 + wd_], pp[0][:, 0:wd_], reads=[pp[0]], writes=[(p0s, c0)])
                allu = [(u, c0) for c0, _, _ in BLOCKS] + [(u, "pad")]
                allp = [(p0s, c0) for c0, _, _ in BLOCKS]
                S.ts("dve", y[:, 1:2306], u[:, 1:2306], aconvT[:, ch, 1:2], None, ALU.mult, reads=allu, writes=[y])
                S.stt("dve", y[:, 1:2306], u[:, 0:2305], aconvT[:, ch, 0:1], y[:, 1:2306], ALU.mult, ALU.add, reads=allu + [y], writes=[y])
                S.stt("dve", y[:, 1:2306], u[:, 2:2307], aconvT[:, ch, 2:3], y[:, 1:2306], ALU.mult, ALU.add, reads=allu + [y], writes=[y])
                S.tt("dve", catT[:, ch, 1:2306], y[:, 1:2306], p0s[:, 1:2306], ALU.mult, reads=[y] + allp, writes=[(catT, ch)])

        def phase_C(b, hT, BG, st):
            NBUF = 4
            wB = [sb(st, [128, 3, 8, 128], BF16, "wB") for _ in range(2)]
            s_ = [sb(st, [128, 512], F32, "s_") for _ in range(NBUF)]
            sq_ = [sb(st, [128, 512], BF16, "sq_") for _ in range(NBUF)]
            rin = [sb(st, [128, 512], F32, "rin") for _ in range(NBUF)]
            kn = [sb(st, [128, 512], BF16, "kn") for _ in range(NBUF)]
            ktk = [sb(st, [128, 4, 128], BF16, "ktk") for _ in range(NBUF)]

            def qk_gen(i, which, h, w, c0, wd_, t0):
                p = pb(); n = 0
                for j in range(3):
                    for kc in range(8):
                        S.mm(p[:, 0:wd_], w[:, j, kc, :], hT[:, kc, c0 + j - 1:c0 + j - 1 + wd_], n == 0, n == 23, reads=[(w, j)], writes=[p])
                        n += 1
                s = s_[i % NBUF]; sq = sq_[i % NBUF]; ri = rin[i % NBUF]; qn = kn[i % NBUF]; kt = ktk[i % NBUF]
                S.act(s[:, 0:wd_], p[:, 0:wd_], AF.Silu, reads=[p], writes=[s])
                S.tt("dve", sq[:, 0:wd_], s[:, 0:wd_], s[:, 0:wd_], ALU.mult, reads=[s], writes=[sq])
                yield
                p2 = pb()
                S.mm(p2[:, 0:wd_], onesb[:], sq[:, 0:wd_], True, True, reads=[sq], writes=[p2])
                S.act(ri[:, 0:wd_], p2[:, 0:wd_], AF.Sqrt, bias=1e-6, reads=[p2], writes=[ri])
                S.op("dve", lambda e: e.reciprocal(ri[:, 0:wd_], ri[:, 0:wd_]), reads=[ri], writes=[ri])
                if which == 0:
                    S.stt("dve", qn[:, 0:wd_], s[:, 0:wd_], 128.0 ** -0.5, ri[:, 0:wd_], ALU.mult, ALU.mult, reads=[s, ri], writes=[qn])
                else:
                    S.tt("dve", qn[:, 0:wd_], s[:, 0:wd_], ri[:, 0:wd_], ALU.mult, reads=[s, ri], writes=[qn])
                dstT = (qT_s if which == 0 else kT_s)[h][:, t0:t0 + wd_]
                S.dma(dstT, qn[:, 0:wd_], reads=[qn])
                if which == 1:
                    yield
                    p3 = pb()
                    na = wd_ // 128
                    for j4 in range(na):
                        S.mm(p3[:, j4 * 128:(j4 + 1) * 128], qn[:, j4 * 128:(j4 + 1) * 128], identb[:], True, True, reads=[qn], writes=[p3])
                    S.copy("act", kt[:, 0:na, :], p3[:, 0:wd_].rearrange("p (a b) -> p a b", b=128), reads=[p3], writes=[kt])
                    S.dma(k_s[t0:t0 + wd_, h * 128:(h + 1) * 128].rearrange("(a p) d -> p a d", p=128), kt[:, 0:na, :], reads=[kt])

            def all_qk():
                i = 0
                wi = 0
                for which in range(2):
                    for h in range(4):
                        w = wB[wi % 2]; wi += 1
                        col = which * 512 + h * 128
                        for j in range(3):
                            S.dma(w[:, j], wB_b[j][:, col:col + 128].rearrange("(kc p) n -> p kc n", p=128), writes=[(w, j)])
                        for (c0, wd_, t0) in BLOCKS:
                            yield qk_gen(i, which, h, w, c0, wd_, t0)
                            i += 1
            run_pipe(all_qk(), 3)
            wV = sb(st, [128, 3, 8, 512], BF16, "wV")
            for j in range(3):
                S.dma(wV[:, j], wB_b[j][:, 1024:1536].rearrange("(kc p) n -> p kc n", p=128), writes=[(wV, j)])
            wG = sb(st, [128, 8, 512], BF16, "wG")
            S.dma(wG[:], evwin_b[:, 3072:3584].rearrange("(kc p) n -> p kc n", p=128), writes=[wG])
            wba = sb(st, [128, 8, 16], BF16, "wba")
            S.dma(wba[:], evwin_b[:, 3584:3600].rearrange("(kc p) n -> p kc n", p=128), writes=[wba])
            vt = [sb(st, [128, 512], BF16, "vt") for _ in range(2)]
            gt = [sb(st, [128, 512], F32, "gt") for _ in range(2)]
            for ti in range(NT):
                c = tcol(ti)
                p = pb(); n = 0
                for j in range(3):
                    for kc in range(8):
                        S.mm(p[:], hT[:, kc, c + j - 1:c + j - 1 + 128], wV[:, j, kc, :], n == 0, n == 23, reads=[(wV, j)], writes=[p])
                        n += 1
                v = vt[ti % 2]
                S.act(v[:], p[:], AF.Silu, reads=[p], writes=[v])
                S.dma(v_s[ti * 128:(ti + 1) * 128, :], v[:], reads=[v])
                p = pb()
                for kc in range(8):
                    S.mm(p[:], hT[:, kc, c:c + 128], wG[:, kc, :], kc == 0, kc == 7, reads=[wG], writes=[p])
                g = gt[ti % 2]
                S.act(g[:], p[:], AF.Silu, reads=[p], writes=[g])
                S.dma(gate_s[ti * 128:(ti + 1) * 128, :], g[:], reads=[g])
                p = pb()
                for kc in range(8):
                    S.mm(p[:, 0:16], hT[:, kc, c:c + 128], wba[:, kc, :], kc == 0, kc == 7, reads=[wba], writes=[p])
                S.copy("dve", BG[:, ti, :], p[:, 0:16], reads=[p], writes=[(BG, ti)])
            allbg = [(BG, ti) for ti in range(NT)]
            smb = sb(st, [128, NT, 8], F32, "smb")
            S.act(BG[:, :, 0:8], BG[:, :, 0:8], AF.Sigmoid, reads=allbg, writes=[(BG, "beta")])
            S.tt("dve", smb[:], BG[:, :, 8:16], dtb[:].unsqueeze(1).to_broadcast([128, NT, 8]), ALU.add, reads=allbg, writes=[smb])
            S.ts("dve", smb[:], smb[:], 30.0, None, ALU.min, reads=[smb], writes=[smb])
            S.act(smb[:], smb[:], AF.Exp, reads=[smb], writes=[smb])
            S.act(smb[:], smb[:], AF.Ln, bias=1.0, reads=[smb], writes=[smb])
            S.tt("dve", BG[:, :, 8:16], smb[:], negexpA[:].unsqueeze(1).to_broadcast([128, NT, 8]), ALU.mult, reads=[smb] + allbg, writes=[(BG, "g")])

        def phase_D(b, BG, catT, st):
            Sst = sb(st, [128, 2, 4, 128], F32, "Sst")
            S.memset("pool", Sst[:], 0.0, writes=[Sst])
            Sb = sb(st, [128, 2, 4, 128], BF16, "Sb")
            S.memset("pool", Sb[:], 0.0, writes=[Sb])
            names16 = ["kT", "qT", "ktok", "v", "TG", "A", "AT", "Mb", "MbT", "P", "vb", "kbg", "DT", "kd", "wT", "qg0", "qg1", "vnew"]
            names32 = ["u", "Eg", "osb", "oprev", "gate", "Stmp"]
            slots = []
            for d in range(2):
                sl = {nm: sb(st, [128, 4, 128], BF16, nm) for nm in names16}
                sl.update({nm: sb(st, [128, 4, 128], F32, nm) for nm in names32})
                sl["E"] = sb(st, [128, 16], F32, "E"); sl["bg2"] = sb(st, [128, 4], F32, "bg2"); sl["lnb"] = sb(st, [128, 4], F32, "lnbeta")
                sl["ss"] = sb(st, [128, 8], F32, "ss"); sl["junk"] = sb(st, [128, 128], F32, "junk")
                S.memset("pool", sl["qg0"][:], 0.0, writes=[sl["qg0"]]); S.memset("pool", sl["qg1"][:], 0.0, writes=[sl["qg1"]])
                slots.append(sl)
            visited = set()
            identbc = ident[:].unsqueeze(1).to_broadcast([128, 4, 128])
            bnbc = bnorm[:].unsqueeze(1).to_broadcast([128, 4, 128])

            def bc4(ap):
                return ap.unsqueeze(2).to_broadcast([128, 4, 128])

            def f2(t):
                return t[:].rearrange("p h d -> p (h d)")

            def hs(h):
                return slice(h * 128, (h + 1) * 128)

            ringi = [0, 0]

            def item(ti, d, sl):
                def pb():
                    r = PB[4 * d + ringi[d] % 3]
                    ringi[d] += 1
                    return r
                mi, ms, negS, negI = (0, 1, 4, 5) if d == 0 else (2, 3, 6, 7)
                gd = BG[:, ti, 8 + 4 * d:12 + 4 * d]
                bd = BG[:, ti, 4 * d:4 * d + 4]
                rows = slice(ti * 128, (ti + 1) * 128)
                kT, qT, ktok, v, TG, A, AT, P, DT = sl["kT"], sl["qT"], sl["ktok"], sl["v"], sl["TG"], sl["A"], sl["AT"], sl["P"], sl["DT"]
                E = sl["E"]
                S.dma(kT[:], kT_s.rearrange("h d t -> d h t")[:, :, rows], writes=[kT])
                S.dma(qT[:], qT_s.rearrange("h d t -> d h t")[:, :, rows], writes=[qT])
                S.dma(f2(ktok), k_s[rows, :], writes=[ktok])
                S.dma(f2(v), v_s[rows, :], writes=[v])
                yield
                ps_ = pb()
                S.mm(ps_[:, 0:4], MK[:, mi, :], gd, True, True, reads=[(BG, ti)], writes=[ps_])
                S.mm(ps_[:, 4:8], MK[:, ms, :], gd, True, True, reads=[(BG, ti)], writes=[ps_])
                S.mm(ps_[:, 8:12], MK[:, 8, :], gd, True, True, reads=[(BG, ti)], writes=[ps_])
                S.mm(ps_[:, 12:16], MK[:, 9, :], gd, True, True, reads=[(BG, ti)], writes=[ps_])
                S.act(E[:], ps_[:, 0:16], AF.Exp, reads=[ps_], writes=[E])
                S.tt("dve", sl["bg2"][:], bd, E[:, 0:4], ALU.mult, reads=[E, (BG, ti)], writes=[sl["bg2"]])
                S.act(sl["lnb"][:], bd, AF.Ln, reads=[(BG, ti)], writes=[sl["lnb"]])
                S.tt("dve", TG[:], MK[:, mi, :].unsqueeze(1).to_broadcast([128, 4, 128]), bc4(gd), ALU.mult, reads=[(BG, ti)], writes=[TG])
                yield
                pKK = pb(); pL = pb()
                for h in range(4):
                    S.mm(pKK[:, hs(h)], kT[:, h, :], kT[:, h, :], True, True, reads=[kT], writes=[pKK])
                for h in range(4):
                    S.mm(pL[:, hs(h)], TG[:, h, :], MKb[:, ms, :], True, False, reads=[TG], writes=[pL])
                    S.mm(pL[:, hs(h)], identb[:], MKb[:, negS, :], False, True, reads=[TG], writes=[pL])
                for h in range(4):
                    S.act(A[:, h, :], pL[:, hs(h)], AF.Exp, bias=sl["lnb"][:, h:h + 1], reads=[pL, sl["lnb"]], writes=[A])
                S.tt("dve", f2(A), pKK[:], f2(A), ALU.mult, reads=[pKK, A], writes=[A])
                yield
                pAT = pb()
                for h in range(4):
                    S.mm(pAT[:, hs(h)], A[:, h, :], identb[:], True, True, reads=[A], writes=[pAT])
                S.copy("act", f2(AT), pAT[:], reads=[pAT], writes=[AT])
                S.stt("dve", P[:], pAT[:].rearrange("p (h d) -> p h d", h=4), -1.0, identbc, ALU.mult, ALU.add, reads=[pAT], writes=[P])
                pLT = pb(); pQK = pb()
                for h in range(4):
                    S.mm(pLT[:, hs(h)], MKb[:, ms, :], TG[:, h, :], True, False, reads=[TG], writes=[pLT])
                    S.mm(pLT[:, hs(h)], identb[:], MKb[:, negI, :], False, True, reads=[TG], writes=[pLT])
                for h in range(4):
                    S.mm(pQK[:, hs(h)], kT[:, h, :], qT[:, h, :], True, True, reads=[kT, qT], writes=[pQK])
                S.act(f2(DT), pLT[:], AF.Exp, reads=[pLT], writes=[DT])
                S.tt("dve", f2(DT), pQK[:], f2(DT), ALU.mult, reads=[pQK, DT], writes=[DT])
                yield
                N_, NT_ = AT, A
                Y_, YT_ = sl["Mb"], sl["MbT"]
                prevYT = None
                for lev in range(6):
                    pP = None
                    if prevYT is not None:
                        pP = pb()
                        for h in range(4):
                            S.mm(pP[:, hs(h)], prevYT[:, h, :], P[:, h, :], True, True, reads=[prevYT, P], writes=[pP])
                    if lev < 5:
                        last = lev == 4
                        pMT = pb()
                        pM = None if last else pb()
                        for h in range(4):
                            if not last:
                                S.mm(pM[:, hs(h)], NT_[:, h, :], N_[:, h, :], True, True, reads=[N_, NT_], writes=[pM])
                            S.mm(pMT[:, hs(h)], N_[:, h, :], NT_[:, h, :], True, True, reads=[N_, NT_], writes=[pMT])
                        if not last:
                            S.copy("act", f2(Y_), pM[:], reads=[pM], writes=[Y_])
                        S.copy("act" if (last or lev % 2 == 1) else "dve", f2(YT_), pMT[:], reads=[pMT], writes=[YT_])
                    if pP is not None:
                        S.tt("dve", f2(P), f2(P), pP[:], ALU.add, reads=[pP, P], writes=[P])
                    if lev < 5:
                        prevYT = YT_
                        N_, NT_, Y_, YT_ = Y_, YT_, N_, NT_
                    yield
                vb, kbg, kd, u, wT, Eg, vnew = sl["vb"], sl["kbg"], sl["kd"], sl["u"], sl["wT"], sl["Eg"], sl["vnew"]
                S.tt("dve", vb[:], v[:], bc4(bd), ALU.mult, reads=[v, (BG, ti)], writes=[vb])
                S.tt("pool", kbg[:], ktok[:], bc4(sl["bg2"][:]), ALU.mult, reads=[ktok, sl["bg2"]], writes=[kbg])
                S.tt("pool", kd[:], ktok[:], bc4(E[:, 4:8]), ALU.mult, reads=[ktok, E], writes=[kd])
                pu = pb(); pw = pb(); pE = pb()
                for h in range(4):
                    S.mm(pu[:, hs(h)], P[:, h, :], vb[:, h, :], True, True, reads=[P, vb], writes=[pu])
                for h in range(4):
                    S.mm(pw[:, hs(h)], kbg[:, h, :], P[:, h, :], True, True, reads=[P, kbg], writes=[pw])
                for h in range(4):
                    S.mm(pE[:, hs(h)], onesb[:], TG[:, h, :], True, True, reads=[TG], writes=[pE])
                S.copy("act", f2(u), pu[:], reads=[pu], writes=[u])
                S.copy("dve", f2(wT), pw[:], reads=[pw], writes=[wT])
                S.act(f2(Eg), pE[:], AF.Exp, reads=[pE], writes=[Eg])
                S.tt("dve", sl["qg0"][:, :, 0:64], qT[:, :, 0:64], Eg[:, :, 0:64], ALU.mult, reads=[qT, Eg], writes=[sl["qg0"]])
                S.tt("pool", sl["qg1"][:, :, 64:128], qT[:, :, 64:128], Eg[:, :, 64:128], ALU.mult, reads=[qT, Eg], writes=[sl["qg1"]])
                yield
                po = PB[4 * d + 3]
                S.memset("dve", po[:], 0.0, writes=[po])
                for c in ([0, 1] if d == 0 else [1, 0]):
                    Rr = slice(64 * c, 64 * c + 64)
                    pws = pb()
                    for h in range(4):
                        S.mm(pws[:, hs(h)], wT[:, h, :], Sb[:, d, h, :], True, True, reads=[wT, (Sb, d)], writes=[pws])
                    S.tt("pool", sl["Stmp"][:], Sst[:, d], bc4(E[:, 8 + 4 * c:12 + 4 * c]), ALU.mult, reads=[E, (Sst, d)], writes=[sl["Stmp"]])
                    S.tt("dve", f2(vnew)[Rr, :], f2(u)[Rr, :], pws[Rr, :], ALU.subtract, reads=[u, pws], writes=[vnew])
                    qg = sl["qg0"] if c == 0 else sl["qg1"]
                    for h in range(4):
                        S.op("pe", lambda e, h=h, qg=qg: e.matmul(po[:, hs(h)], qg[:, h, :], Sb[:, d, h, :], start=False, stop=False, skip_group_check=True),
                             reads=[qg, (Sb, d)], writes=[po])
                    pS = pb()
                    for h in range(4):
                        S.mm(pS[:, hs(h)], kd[Rr, h, :], vnew[Rr, h, :], True, True, reads=[kd, vnew], writes=[pS])
                    S.tt("dve", Sb[:, d].rearrange("p h d -> p (h d)"), f2(sl["Stmp"]), pS[:], ALU.add, reads=[pS, sl["Stmp"]], writes=[(Sb, d)])
                    S.tt("dve", Sst[:, d].rearrange("p h d -> p (h d)"), f2(sl["Stmp"]), pS[:], ALU.add, reads=[pS, sl["Stmp"], (Sst, d)], writes=[(Sst, d)])
                    yield
                for h in range(4):
                    S.op("pe", lambda e, h=h: e.matmul(po[:, hs(h)], DT[:, h, :], vnew[:, h, :], start=False, stop=False, skip_group_check=True),
                         reads=[DT, vnew], writes=[po])
                osb, oprev, gate = sl["osb"], sl["oprev"], sl["gate"]
                if ti not in visited:
                    visited.add(ti)
                    S.copy("act", f2(osb), po[:], reads=[po], writes=[osb])
                    S.dma(o_s[rows, :], f2(osb), reads=[osb], writes=[("o_s", ti)])
                else:
                    ss = sl["ss"]
                    S.dma(f2(oprev), o_s[rows, :], reads=[("o_s", ti)], writes=[oprev])
                    S.dma(f2(gate), gate_s[rows, :], writes=[gate])
                    S.tt("dve", f2(osb), po[:], f2(oprev), ALU.add, reads=[po, oprev], writes=[osb])
                    S.memset("pool", ss[:], 0.0, writes=[ss])
                    for h in range(4):
                        S.act(sl["junk"][:], osb[:, h, :], AF.Square, accum_out=ss[:, h:h + 1], reads=[osb, ss], writes=[sl["junk"], ss])
                    S.act(ss[:, 4:8], ss[:, 0:4], AF.Sqrt, scale=1.0 / 128, bias=1e-6, reads=[ss], writes=[ss])
                    S.op("dve", lambda e: e.reciprocal(ss[:, 4:8], ss[:, 4:8]), reads=[ss], writes=[ss])
                    S.tt("dve", osb[:], osb[:], bc4(ss[:, 4:8]), ALU.mult, reads=[osb, ss], writes=[osb])
                    S.tt("pool", osb[:], osb[:], gate[:], ALU.mult, reads=[osb, gate], writes=[osb])
                    S.tt("pool", osb[:], osb[:], bnbc, ALU.mult, reads=[osb], writes=[osb])
                    pT = pb()
                    for h in range(4):
                        S.tr(pT[:, hs(h)], osb[:, h, :], ident[:], reads=[osb], writes=[pT])
                    S.copy("act", catT[:, 4:8, tcol(ti):tcol(ti) + 128], pT[:].rearrange("p (h t) -> p h t", h=4), reads=[pT], writes=[(catT, "B", ti)])

            fwd_order = list(range(18))
            bwd_order = [1, 0] + list(range(17, 1, -1))
            for s_i in range(18):
                gens = [item(fwd_order[s_i], 0, slots[0]), item(bwd_order[s_i], 1, slots[1])]
                while gens:
                    for g in list(gens):
                        try:
                            next(g)
                        except StopIteration:
                            gens.remove(g)

        def layer0(b):
            tiles = list(range(18))
            with ExitStack() as stL:
                gates = sb(stL, [128, 18, 16], F32, "gates")
                with ExitStack() as stC:
                    catT = sb(stC, [128, 8, 2307], BF16, "catT")
                    BG = sb(stC, [128, 18, 16], F32, "BG")
                    with ExitStack() as st:
                        hT = sb(st, [128, 8, 2307], BF16, "hT")
                        for c in (0, 257, 2306):
                            S.memset("pool", hT[:, :, c:c + 1], 0.0, writes=[(hT, "pad", c)])
                        if want('A'):
                            with ExitStack() as st2:
                                phase_A(0, b, hT, tiles, st2)
                                S.barrier()
                        if want('B'):
                            with ExitStack() as st2:
                                phase_B(b, hT, catT, st2)
                                S.barrier()
                        if want('C'):
                            with ExitStack() as st2:
                                phase_C(b, hT, BG, st2)
                                S.barrier()
                    if want('D'):
                        with ExitStack() as st:
                            phase_D(b, BG, catT, st)
                            S.barrier()
                    post_E(0, b, catT, tiles, gates)
                post_FG(0, b, tiles, gates)

        LBLK = [(258 + 512 * j, 512, 256 + 512 * j, 512 * j) for j in range(4)]

        def rearr_w(ap):
            return ap.rearrange("(kc p) n -> p kc n", p=128)

        def l1_proj_mla(b, hT, dqn, dkvn, KR, st):
            wdq = sb(st, [128, 8, 384], BF16, "wdq"); S.dma(wdq[:], rearr_w(odwin_b[:, 768:1152]), writes=[wdq])
            wdkv = sb(st, [128, 8, 256], BF16, "wdkv"); S.dma(wdkv[:], rearr_w(odwin_b[:, 1152:1408]), writes=[wdkv])
            wk = sb(st, [128, 8, 96], BF16, "wk"); S.dma(wk[:], rearr_w(wkr_b), writes=[wk])
            wks = sb(st, [128, 8, 96], BF16, "wks"); S.dma(wks[:], rearr_w(wkrsw_b), writes=[wks])
            r32 = sb(st, [96, 2, LT], F32, "r32"); S.dma(r32[:], rope32_d.rearrange("a p t -> p a t"), writes=[r32])
            sq = [sb(st, [128, 512], F32, "sq1") for _ in range(3)]
            rinv = sb(st, [128, 512], F32, "rinv1")
            t1 = sb(st, [96, 512], F32, "t1a"); t2 = sb(st, [96, 512], F32, "t2a")

            def rms_proj(wt, nch, normT, dst, c0, w, d0, inv_n):
                pp = [pb() for _ in range(nch)]
                for c in range(nch):
                    for kc in range(8):
                        S.mm(pp[c][:, 0:w], wt[:, kc, c * 128:(c + 1) * 128], hT[:, kc, c0:c0 + w], kc == 0, kc == 7, reads=[wt], writes=[pp[c]])
                    S.act(sq[c][:, 0:w], pp[c][:, 0:w], AF.Square, reads=[pp[c]], writes=[sq[c]])
                pss = pb()
                for c in range(nch):
                    S.mm(pss[:, 0:w], ones[:], sq[c][:, 0:w], c == 0, c == nch - 1, reads=[sq[c]], writes=[pss])
                S.act(rinv[:, 0:w], pss[:, 0:w], AF.Sqrt, scale=inv_n, bias=1e-6, reads=[pss], writes=[rinv])
                S.op("dve", lambda e: e.reciprocal(rinv[:, 0:w], rinv[:, 0:w]), reads=[rinv], writes=[rinv])
                for c in range(nch):
                    S.stt("dve", dst[:, c, d0:d0 + w], pp[c][:, 0:w], normT[:, c:c + 1], rinv[:, 0:w], ALU.mult, ALU.mult,
                          reads=[pp[c], rinv], writes=[(dst, c, d0)])

            for bi, (c0, w, t0) in enumerate(BLOCKS):
                rms_proj(wdkv, 2, kvnT, dkvn, c0, w, t0, 1.0 / 256)
                pk = pb(); pks = pb()
                for kc in range(8):
                    S.mm(pk[0:96, 0:w], wk[:, kc, :], hT[:, kc, c0:c0 + w], kc == 0, kc == 7, reads=[wk], writes=[pk])
                if bi == 0:
                    S.copy("act", KR[64:96, t0:t0 + w], pk[64:96, 0:w], reads=[pk], writes=[(KR, t0)])
                else:
                    l0 = t0 - 256
                    for kc in range(8):
                        S.mm(pks[0:96, 0:w], wks[:, kc, :], hT[:, kc, c0:c0 + w], kc == 0, kc == 7, reads=[wks], writes=[pks])
                    S.tt("dve", t1[64:96, 0:w], pk[64:96, 0:w], r32[64:96, 0, l0:l0 + w], ALU.mult, reads=[pk, r32], writes=[t1])
                    S.tt("dve", t2[64:96, 0:w], pks[64:96, 0:w], r32[64:96, 1, l0:l0 + w], ALU.mult, reads=[pks, r32], writes=[t2])
                    S.tt("dve", KR[64:96, t0:t0 + w], t1[64:96, 0:w], t2[64:96, 0:w], ALU.add, reads=[t1, t2], writes=[(KR, t0)])
                    rms_proj(wdq, 3, qnT, dqn, c0, w, l0, 1.0 / 384)

        def l1_proj_win(b, hT, CQ, CK, CV, st):
            wq = sb(st, [128, 8, 512], BF16, "wq"); S.dma(wq[:], rearr_w(odwin_b[:, 0:512]), writes=[wq])
            wqs = sb(st, [128, 8, 512], BF16, "wqs"); S.dma(wqs[:], rearr_w(odwinsw_b[:, 0:512]), writes=[wqs])
            wkk = sb(st, [128, 8, 128], BF16, "wkk"); S.dma(wkk[:], rearr_w(odwin_b[:, 512:640]), writes=[wkk])
            wkks = sb(st, [128, 8, 128], BF16, "wkks"); S.dma(wkks[:], rearr_w(odwinsw_b[:, 512:640]), writes=[wkks])
            wv = sb(st, [128, 8, 128], BF16, "wv"); S.dma(wv[:], rearr_w(odwin_b[:, 640:768]), writes=[wv])
            r64 = sb(st, [64, 2, LT], F32, "r64"); S.dma(r64[:], rope64_d.rearrange("a p t -> p a t"), writes=[r64])
            t1 = [sb(st, [64, 512], F32, "t1w") for _ in range(2)]; t2 = [sb(st, [64, 512], F32, "t2w") for _ in range(2)]
            S.memset("pool", CV[:, :, :, 64:65], 1.0, writes=[(CV, "ones")])
            ii = 0
            for bi, (c0, w, t0) in enumerate(BLOCKS):
                l0 = t0 - 256
                for g in range(2):
                    pk = pb(); pks = pb()
                    for kc in range(8):
                        S.mm(pk[0:64, 0:w], wkk[:, kc, g * 64:(g + 1) * 64], hT[:, kc, c0:c0 + w], kc == 0, kc == 7, reads=[wkk], writes=[pk])
                    if bi == 0:
                        S.copy("act", CK[:, g, t0:t0 + w], pk[0:64, 0:w], reads=[pk], writes=[(CK, g, t0)])
                    else:
                        for kc in range(8):
                            S.mm(pks[0:64, 0:w], wkks[:, kc, g * 64:(g + 1) * 64], hT[:, kc, c0:c0 + w], kc == 0, kc == 7, reads=[wkks], writes=[pks])
                        a1 = t1[ii % 2]; a2 = t2[ii % 2]; ii += 1
                        S.tt("dve", a1[:, 0:w], pk[0:64, 0:w], r64[:, 0, l0:l0 + w], ALU.mult, reads=[pk, r64], writes=[a1])
                        S.tt("dve", a2[:, 0:w], pks[0:64, 0:w], r64[:, 1, l0:l0 + w], ALU.mult, reads=[pks, r64], writes=[a2])
                        S.tt("dve", CK[:, g, t0:t0 + w], a1[:, 0:w], a2[:, 0:w], ALU.add, reads=[a1, a2], writes=[(CK, g, t0)])
                for j in range(w // 128):
                    ti = t0 // 128 + j
                    pv = pb()
                    for kc in range(8):
                        S.mm(pv[:, 0:128], hT[:, kc, tcol(ti):tcol(ti) + 128], wv[:, kc, :], kc == 0, kc == 7, reads=[wv], writes=[pv])
                    S.copy("act", CV[:, ti, :, 0:64], pv[:, 0:128].rearrange("p (g d) -> p g d", g=2), reads=[pv], writes=[(CV, ti)])
                if bi > 0:
                    for h in range(8):
                        pq = pb(); pqs = pb()
                        for kc in range(8):
                            S.mm(pq[0:64, 0:w], wq[:, kc, h * 64:(h + 1) * 64], hT[:, kc, c0:c0 + w], kc == 0, kc == 7, reads=[wq], writes=[pq])
                        for kc in range(8):
                            S.mm(pqs[0:64, 0:w], wqs[:, kc, h * 64:(h + 1) * 64], hT[:, kc, c0:c0 + w], kc == 0, kc == 7, reads=[wqs], writes=[pqs])
                        a1 = t1[ii % 2]; a2 = t2[ii % 2]; ii += 1
                        S.tt("dve", a1[:, 0:w], pq[0:64, 0:w], r64[:, 0, l0:l0 + w], ALU.mult, reads=[pq, r64], writes=[a1])
                        S.tt("dve", a2[:, 0:w], pqs[0:64, 0:w], r64[:, 1, l0:l0 + w], ALU.mult, reads=[pqs, r64], writes=[a2])
                        S.tt("dve", CQ[:, h, l0:l0 + w], a1[:, 0:w], a2[:, 0:w], ALU.add, reads=[a1, a2], writes=[(CQ, h, l0)])

        def l1_attn_win(b, CQ, CK, CV, catT, st):
            PT = [sb(st, [128, 512], BF16, "PTw") for _ in range(2)]
            Yw = [sb(st, [128, 512], F32, "Yw") for _ in range(2)]
            den = sb(st, [128, 8], F32, "denw")
            ring = [0]

            def rb():
                r = PB[ring[0] % 6]
                ring[0] += 1
                return r
            seq = []
            for qt in range(16):
                for g in range(2):
                    keys = [(0, None), (1, None)]
                    if qt >= 1:
                        keys.append((qt + 1, 10))
                    keys.append((qt + 2, None))
                    if qt <= 14:
                        keys.append((qt + 3, 11))
                    for ki, (kt, mk) in enumerate(keys):
                        seq.append((qt, g, kt, mk, ki == 0, ki == len(keys) - 1))
            pss = {}

            def issue_S(i):
                qt, g, kt, mk, first, last = seq[i]
                ps = rb()
                S.mm(ps[:, 0:512], CK[:, g, kt * 128:(kt + 1) * 128], CQ[:, 4 * g:4 * g + 4, qt * 128:(qt + 1) * 128], True, True, reads=[], writes=[ps])
                pss[i] = ps
            issue_S(0)
            io = 0
            po = None; po3 = None
            for i, (qt, g, kt, mk, first, last) in enumerate(seq):
                yw = Yw[qt % 2]
                if first:
                    po = PB[6 + io % 2]; io += 1
                    po3 = po[:, 0:260].rearrange("p (a b) -> p a b", a=4)
                    S.memset("dve", po[:, 0:260], 0.0, writes=[po])
                if i + 1 < len(seq):
                    issue_S(i + 1)
                ps = pss.pop(i)
                pt = PT[i % 2]
                S.act(pt[:], ps[:, 0:512], AF.Exp, scale=0.125, reads=[ps], writes=[pt])
                if mk is not None:
                    pt3 = pt[:].rearrange("p (a b) -> p a b", a=4)
                    S.tt("dve", pt3, pt3, MK[:, mk, :].unsqueeze(1).to_broadcast([128, 4, 128]), ALU.mult, reads=[pt], writes=[pt])
                for hh in range(4):
                    S.op("pe", lambda e, hh=hh, pt=pt, kt=kt, po=po, g=g: e.matmul(po[:, hh * 65:(hh + 1) * 65], pt[:, hh * 128:(hh + 1) * 128], CV[:, kt, g, :],
                                                                                 start=False, stop=False, skip_group_check=True), reads=[pt], writes=[po])
                if last:
                    S.tt("dve", den[:, 0:4], po3[:, :, 64], expsink[:, 4 * g:4 * g + 4], ALU.add, reads=[po], writes=[den])
                    S.op("dve", lambda e: e.reciprocal(den[:, 4:8], den[:, 0:4]), reads=[den], writes=[den])
                    S.tt("dve", yw[:, g * 256:(g + 1) * 256].rearrange("p (a b) -> p a b", a=4), po3[:, :, 0:64],
                         den[:, 4:8].unsqueeze(2).to_broadcast([128, 4, 64]), ALU.mult, reads=[po, den], writes=[yw])
                    if g == 1:
                        pT = rb()
                        for j in range(4):
                            S.tr(pT[:, j * 128:(j + 1) * 128], yw[:, j * 128:(j + 1) * 128], ident[:], reads=[yw], writes=[pT])
                        S.copy("act", catT[:, 0:4, qt * 128:(qt + 1) * 128], pT[:, 0:512].rearrange("p (a b) -> p a b", a=4), reads=[pT], writes=[(catT, "w", qt)])

        def l1_up_mla(b, dqn, dkvn, KR, MQ, MKk, MV, st):
            wuq = sb(st, [128, 3, 768], BF16, "wuq"); S.dma(wuq[:], wuq_b.rearrange("(c p) n -> p c n", p=128), writes=[wuq])
            wuqs = sb(st, [128, 3, 768], BF16, "wuqs"); S.dma(wuqs[:], wuqsw_b.rearrange("(c p) n -> p c n", p=128), writes=[wuqs])
            wukv = sb(st, [128, 2, 1024], BF16, "wukv"); S.dma(wukv[:], wukv_b.rearrange("(c p) n -> p c n", p=128), writes=[wukv])
            r32 = sb(st, [96, 2, LT], F32, "r32b"); S.dma(r32[:], rope32_d.rearrange("a p t -> p a t"), writes=[r32])
            t1 = [sb(st, [96, 512], F32, "t1m") for _ in range(2)]; t2 = [sb(st, [96, 512], F32, "t2m") for _ in range(2)]
            S.memset("pool", MV[:, :, :, 64:65], 1.0, writes=[(MV, "ones")])
            ii = 0
            for (c0, w, t0, l0) in LBLK:
                for h in range(8):
                    pq = pb(); pqs = pb()
                    for c in range(3):
                        S.mm(pq[0:96, 0:w], wuq[:, c, h * 96:(h + 1) * 96], dqn[:, c, l0:l0 + w], c == 0, c == 2, reads=[wuq], writes=[pq])
                    for c in range(3):
                        S.mm(pqs[0:96, 0:w], wuqs[:, c, h * 96:(h + 1) * 96], dqn[:, c, l0:l0 + w], c == 0, c == 2, reads=[wuqs], writes=[pqs])
                    S.copy("act", MQ[0:64, h, l0:l0 + w], pq[0:64, 0:w], reads=[pq], writes=[(MQ, h, l0, 0)])
                    a1 = t1[ii % 2]; a2 = t2[ii % 2]; ii += 1
                    S.tt("dve", a1[64:96, 0:w], pq[64:96, 0:w], r32[64:96, 0, l0:l0 + w], ALU.mult, reads=[pq, r32], writes=[a1])
                    S.tt("dve", a2[64:96, 0:w], pqs[64:96, 0:w], r32[64:96, 1, l0:l0 + w], ALU.mult, reads=[pqs, r32], writes=[a2])
                    S.tt("dve", MQ[64:96, h, l0:l0 + w], a1[64:96, 0:w], a2[64:96, 0:w], ALU.add, reads=[a1, a2], writes=[(MQ, h, l0, 1)])
            for (c0, w, t0) in BLOCKS:
                for h in range(8):
                    pk = pb()
                    for c in range(2):
                        S.mm(pk[0:64, 0:w], wukv[:, c, h * 128:h * 128 + 64], dkvn[:, c, t0:t0 + w], c == 0, c == 1, reads=[wukv], writes=[pk])
                    if h % 2 == 0:
                        S.copy("act", MKk[0:64, h, t0:t0 + w], pk[0:64, 0:w], reads=[pk], writes=[(MKk, h, t0, 0)])
                    else:
                        S.copy("dve", MKk[0:64, h, t0:t0 + w], pk[0:64, 0:w], reads=[pk], writes=[(MKk, h, t0, 0)])
                    S.copy("pool", MKk[64:96, h, t0:t0 + w], KR[64:96, t0:t0 + w], reads=[], writes=[(MKk, h, t0, 1)])
                for j in range(w // 128):
                    ti = t0 // 128 + j
                    pv = pb()
                    wv3 = wukv[:].rearrange("p c (h x) -> p c h x", h=8)
                    for c in range(2):
                        S.mm(pv[:, 0:512], dkvn[:, c, ti * 128:(ti + 1) * 128], wv3[:, c, :, 64:128], c == 0, c == 1, reads=[wukv], writes=[pv])
                    S.copy("act", MV[:, ti, :, 0:64], pv[:, 0:512].rearrange("p (h x) -> p h x", h=8), reads=[pv], writes=[(MV, ti)])

        def l1_attn_mla(b, MQ, MKk, MV, catT, st):
            PT = [sb(st, [128, 512], BF16, "PTm") for _ in range(3)]
            Ym = sb(st, [128, 4, 512], F32, "Ym")
            den = sb(st, [128, 8], F32, "denm")
            ring = [0]

            def rb():
                r = PB[ring[0] % 6]
                ring[0] += 1
                return r
            scale = 96.0 ** -0.5
            seq = [(qb, h, kt) for qb in range(4) for h in range(8) for kt in range(18)]
            pss = {}

            def issue_S(i):
                qb, h, kt = seq[i]
                ps = rb()
                S.mm(ps[:, 0:512], MKk[:, h, kt * 128:(kt + 1) * 128], MQ[:, h, qb * 512:(qb + 1) * 512], True, True, reads=[], writes=[ps])
                pss[i] = ps
            issue_S(0)
            io = 0
            po = None; po3 = None
            for i, (qb, h, kt) in enumerate(seq):
                if kt == 0:
                    po = PB[6 + io % 2]; io += 1
                    po3 = po[:, 0:260].rearrange("p (a b) -> p a b", a=4)
                    S.memset("dve", po[:, 0:260], 0.0, writes=[po])
                if i + 1 < len(seq):
                    issue_S(i + 1)
                ps = pss.pop(i)
                pt = PT[i % 3]
                S.act(pt[:], ps[:, 0:512], AF.Exp, scale=scale, reads=[ps], writes=[pt])
                for qs in range(4):
                    S.op("pe", lambda e, qs=qs, pt=pt, kt=kt, po=po, h=h: e.matmul(po[:, qs * 65:(qs + 1) * 65], pt[:, qs * 128:(qs + 1) * 128], MV[:, kt, h, :],
                                                                                  start=False, stop=False, skip_group_check=True), reads=[pt], writes=[po])
                if kt == 17:
                    S.op("dve", lambda e, po3=po3: e.reciprocal(den[:, 0:4], po3[:, :, 64]), reads=[po], writes=[den])
                    S.tt("dve", Ym[:, :, h * 64:(h + 1) * 64], po3[:, :, 0:64], den[:, 0:4].unsqueeze(2).to_broadcast([128, 4, 64]), ALU.mult,
                         reads=[po, den], writes=[(Ym, h)])
                    if h == 7:
                        for qs in range(4):
                            pT = rb()
                            for j in range(4):
                                S.tr(pT[:, j * 128:(j + 1) * 128], Ym[:, qs, j * 128:(j + 1) * 128], ident[:], reads=[(Ym, hh) for hh in range(8)], writes=[pT])
                            qt = qb * 4 + qs
                            S.copy("act", catT[:, 4:8, qt * 128:(qt + 1) * 128], pT[:, 0:512].rearrange("p (a b) -> p a b", a=4), reads=[pT], writes=[(catT, "m", qt)])

        def layer1(b):
            tiles = list(range(2, 18))
            with ExitStack() as stL:
                gates = sb(stL, [128, 16, 16], F32, "gates1")
                with ExitStack() as stC:
                    catT = sb(stC, [128, 8, LT], BF16, "catT1")
                    dqn = sb(stC, [128, 3, LT], BF16, "dqn"); dkvn = sb(stC, [128, 2, T], BF16, "dkvn"); KR = sb(stC, [96, T], BF16, "KR")
                    with ExitStack() as stW:
                        CQ = sb(stW, [64, 8, LT], BF16, "CQ"); CK = sb(stW, [64, 2, T], BF16, "CK"); CV = sb(stW, [128, 18, 2, 65], BF16, "CV")
                        with ExitStack() as st:
                            hT = sb(st, [128, 8, 2307], BF16, "hT1")
                            with ExitStack() as st2:
                                phase_A(1, b, hT, list(range(18)), st2)
                                S.barrier()
                            if want('L1P'):
                              with ExitStack() as st2:
                                l1_proj_mla(b, hT, dqn, dkvn, KR, st2)
                                S.barrier()
                            if want('L1W'):
                              with ExitStack() as st2:
                                l1_proj_win(b, hT, CQ, CK, CV, st2)
                                S.barrier()
                        if want('L1WA'):
                          with ExitStack() as st2:
                            l1_attn_win(b, CQ, CK, CV, catT, st2)
                            S.barrier()
                    with ExitStack() as stM:
                        MQ = sb(stM, [96, 8, LT], BF16, "MQ"); MKk = sb(stM, [96, 8, T], BF16, "MKk"); MV = sb(stM, [128, 18, 8, 65], BF16, "MV")
                        if want('L1U'):
                          with ExitStack() as st2:
                            l1_up_mla(b, dqn, dkvn, KR, MQ, MKk, MV, st2)
                            S.barrier()
                        if want('L1MA'):
                          with ExitStack() as st2:
                            l1_attn_mla(b, MQ, MKk, MV, catT, st2)
                            S.barrier()
                    if dbg:
                        S.dma(cat_dbg, catT[:], reads=[])
                        S.barrier()
                    if want('L1E'):
                        post_E(1, b, catT, tiles, gates)
                if want('L1F'):
                    post_FG(1, b, tiles, gates)


        for b in range(NB):
            layer0(b)
            if not only0 and want('L1A'):
                layer1(b)
        S.finish([])
    return nc


NEGV = -30000.0


def make_consts():
    p = np.arange(128)
    ch = p // 64
    same = ch[:, None] == ch[None, :]
    a = p[:, None]; bb = p[None, :]
    M = np.zeros((14, 128, 128), np.float32)
    M[0] = same & (a <= bb)
    M[1] = same & (a > bb)
    M[2] = same & (a >= bb)
    M[3] = same & (a < bb)
    M[4] = np.where(same & (a > bb), 0.0, NEGV)
    M[5] = np.where(same & (bb >= a), 0.0, NEGV)
    M[6] = np.where(same & (a < bb), 0.0, NEGV)
    M[7] = np.where(same & (bb <= a), 0.0, NEGV)
    M[8] = (a < 64) & (bb >= 0)
    M[9] = (a >= 64) & (bb >= 0)
    M[10] = bb <= a
    M[11] = a <= bb
    t = np.arange(LT)
    rows = (t // 64).astype(np.float32); cols = (t % 64).astype(np.float32)

    def ang(rot):
        nf = rot // 4
        inv = (10000.0 ** (-np.arange(nf, dtype=np.float32) / nf)).astype(np.float32)
        return np.concatenate([rows[:, None] * inv, cols[:, None] * inv], -1).astype(np.float32)
    a64 = ang(64); a32 = ang(32)
    r64 = np.zeros((2, 64, LT), np.float32)
    r64[0] = np.concatenate([np.cos(a64), np.cos(a64)], 1).T
    r64[1] = np.concatenate([-np.sin(a64), np.sin(a64)], 1).T
    r32 = np.zeros((2, 96, LT), np.float32)
    r32[0, :64] = 1.0
    r32[0, 64:] = np.concatenate([np.cos(a32), np.cos(a32)], 1).T
    r32[1, 64:] = np.concatenate([-np.sin(a32), np.sin(a32)], 1).T
    return M, r64, r32


def prep_shared(inp):
    f = lambda a: np.ascontiguousarray(a, dtype=np.float32)
    M, r64, r32 = make_consts()
    w1 = inp["od_w_in"][0]
    w1s = w1.copy()
    for h in range(8):
        b0 = h * 64
        w1s[:, b0:b0 + 32] = w1[:, b0 + 32:b0 + 64]; w1s[:, b0 + 32:b0 + 64] = w1[:, b0:b0 + 32]
    for g in range(2):
        b0 = 512 + g * 64
        w1s[:, b0:b0 + 32] = w1[:, b0 + 32:b0 + 64]; w1s[:, b0 + 32:b0 + 64] = w1[:, b0:b0 + 32]
    wkr = np.zeros((1024, 96), np.float32); wkrs = np.zeros((1024, 96), np.float32)
    wkr[:, 64:96] = w1[:, 1408:1440]
    wkrs[:, 64:80] = w1[:, 1424:1440]; wkrs[:, 80:96] = w1[:, 1408:1424]
    wuq = inp["od_d_wuq"][0]
    wuqs = wuq.copy()
    for h in range(8):
        b0 = h * 96 + 64
        wuqs[:, b0:b0 + 16] = wuq[:, b0 + 16:b0 + 32]; wuqs[:, b0 + 16:b0 + 32] = wuq[:, b0:b0 + 16]
    d = {
        "ada_w": f(inp["ada_w"]), "ada_b": f(inp["ada_b"]),
        "ln_g": f(inp["ln_g"].reshape(4, 1024)), "ln_b": f(inp["ln_b"].reshape(4, 1024)),
        "ev_w_in": f(inp["ev_w_in"][0]),
        "a_convT": f(inp["ev_a_conv"][0].reshape(3, 4, 128).transpose(2, 1, 0)),
        "ev_b_conv": f(inp["ev_b_conv"][0]),
        "alog": f(inp["ev_b_alog"].reshape(1, 8)), "dtbias": f(inp["ev_b_dtbias"].reshape(1, 8)),
        "bnorm": f(inp["ev_b_norm"].reshape(1, 128)), "ev_w_out": f(inp["ev_w_out"][0]),
        "od_w_in": f(w1), "od_w_in_sw": f(w1s), "wkr": f(wkr), "wkr_sw": f(wkrs),
        "sink": f(inp["od_c_sink"].reshape(1, 8)),
        "qnormT": f(inp["od_d_qnorm"][0].reshape(3, 128).T), "kvnormT": f(inp["od_d_kvnorm"][0].reshape(2, 128).T),
        "wuq": f(wuq), "wuq_sw": f(wuqs), "wukv": f(inp["od_d_wukv"][0]), "od_w_out": f(inp["od_w_out"][0]),
        "router_w": f(inp["router_w"]), "router_bias": f(inp["router_bias"].reshape(1, 16)),
        "moe_g": f(inp["moe_w_gate"].reshape(32768, 512)), "moe_u": f(inp["moe_w_up"].reshape(32768, 512)),
        "moe_d": f(inp["moe_w_down"].reshape(16384, 1024)),
        "idn": np.eye(128, dtype=np.float32), "masks": M, "rope64": r64, "rope32": r32,
    }
    return d


def prep_core(inp, shared, b0, NB):
    f = lambda a: np.ascontiguousarray(a, dtype=np.float32)
    cs = np.concatenate([inp["c"][b0:b0 + NB], inp["c_ctx"][None, :]], 0)
    csT = cs.reshape(NB + 1, 8, 128).transpose(2, 1, 0)
    d = dict(shared)
    d["x"] = f(inp["x"][b0:b0 + NB]); d["ctx"] = f(inp["ctx"][b0:b0 + NB]); d["csT"] = f(csT)
    return d


_NC_CACHE = {}


def kernel(**inputs):
    inp = {k: np.asarray(v) for k, v in inputs.items()}
    NBC = 4
    if "nc" not in _NC_CACHE:
        _NC_CACHE["nc"] = build(NBC)
    nc = _NC_CACHE["nc"]
    shared = prep_shared(inp)
    in_maps = [prep_core(inp, shared, NBC * i, NBC) for i in range(8)]
    res = run_bass_kernel_spmd(nc, in_maps, core_ids=list(range(8)))
    return np.ascontiguousarray(np.concatenate([np.asarray(r["out"]) for r in res.results], 0).astype(np.float32))
```

```python
from concourse.bass_utils import run_bass_kernel_spmd
import numpy as np
import concourse.bass as bass
import concourse.mybir as mybir
from contextlib import ExitStack

F32 = mybir.dt.float32
BF16 = mybir.dt.bfloat16
AF = mybir.ActivationFunctionType
ALU = mybir.AluOpType
AX = mybir.AxisListType

N_DMA_SEMS = 40
USE_NOPS = False
SEM_ROLL = 30000


class Tok:
    __slots__ = ("sem", "val", "eng")

    def __init__(self, sem, val, eng):
        self.sem = sem
        self.val = val
        self.eng = eng


class Sched:
    def __init__(self, nc, stack):
        self.nc = nc
        self.stack = stack
        self.cengs = ["pe", "act", "dve", "pool"]
        self.all = ["pe", "act", "dve", "pool", "sp"]
        self.prog = {e: [] for e in self.all}
        self.nsem = 0
        self.sem = {e: self._newsem() for e in self.cengs}
        self.cnt = {e: 0 for e in self.cengs}
        self.waited = {e: {} for e in self.all}
        self.dsem = [self._newsem() for _ in range(N_DMA_SEMS)]
        self.dcnt = [0] * N_DMA_SEMS
        self.drr = 0
        self.res = {}
        self.pend = {e: {} for e in self.all}
        self.excl = set()
        self.old = []

    def _newsem(self):
        self.nsem += 1
        return self.stack.enter_context(self.nc.semaphore("s%d" % self.nsem))

    def _st(self, r):
        if isinstance(r, tuple):
            k = tuple(x if isinstance(x, (str, int)) else id(x) for x in r)
        elif isinstance(r, str):
            k = r
        else:
            k = id(r)
        st = self.res.get(k)
        if st is None:
            st = {"w": None, "r": {}}
            self.res[k] = st
        return st

    def _emit(self, eng, fn, reads, writes, dma):
        if self.excl:
            ex = [r for r in reads if (not isinstance(r, (tuple, str))) and id(r) in self.excl]
            if ex:
                reads = [r for r in reads if not ((not isinstance(r, (tuple, str))) and id(r) in self.excl)]
                writes = list(writes) + ex
        deps = []
        for r in reads:
            st = self._st(r)
            if st["w"] is not None:
                deps.append(st["w"])
        for w in writes:
            st = self._st(w)
            if st["w"] is not None:
                deps.append(st["w"])
            deps.extend(st["r"].values())
        need = {}
        for t in deps:
            if t.eng == eng and eng == "pe" and not dma:
                continue
            cur = need.get(id(t.sem))
            if cur is None or cur[1] < t.val:
                need[id(t.sem)] = (t.sem, t.val)
        if self.pend[eng]:
            for sid, (s_, v_) in self.pend[eng].items():
                cur = need.get(sid)
                if cur is None or cur[1] < v_:
                    need[sid] = (s_, v_)
            self.pend[eng] = {}
        if dma:
            i = self.drr
            self.drr = (self.drr + 1) % N_DMA_SEMS
            prev = self.dcnt[i]
            if prev > 0:
                cur = need.get(id(self.dsem[i]))
                if cur is None or cur[1] < prev:
                    need[id(self.dsem[i])] = (self.dsem[i], prev)
            self.dcnt[i] += 16
            tok = Tok(self.dsem[i], self.dcnt[i], "dma")
            inc = (self.dsem[i], 16)
        else:
            if self.cnt[eng] >= SEM_ROLL:
                self.old.append((self.sem[eng], self.cnt[eng]))
                self.sem[eng] = self._newsem()
                self.cnt[eng] = 0
            self.cnt[eng] += 1
            tok = Tok(self.sem[eng], self.cnt[eng], eng)
            inc = (self.sem[eng], 1)
        waits = []
        wd = self.waited[eng]
        for sid, (s, v) in need.items():
            if wd.get(sid, 0) >= v:
                continue
            wd[sid] = v
            waits.append((s, v))
        for r in reads:
            self._st(r)["r"][id(tok.sem)] = tok
        for w in writes:
            st = self._st(w)
            st["w"] = tok
            st["r"] = {}
        self.prog[eng].append((waits, fn, inc))
        return tok

    def op(self, eng, fn, reads=(), writes=()):
        return self._emit(eng, fn, reads, writes, False)

    def all_tokens(self):
        toks = {}
        for e in self.cengs:
            if self.cnt[e] > 0:
                toks[id(self.sem[e])] = (self.sem[e], self.cnt[e])
        for i in range(N_DMA_SEMS):
            if self.dcnt[i] > 0:
                toks[id(self.dsem[i])] = (self.dsem[i], self.dcnt[i])
        return toks

    def barrier(self):
        toks = self.all_tokens()
        for e in self.all:
            self.pend[e] = dict(toks)
        self.res = {}

    def dma(self, out, in_, reads=(), writes=(), q="sp", **kw):
        return self._emit(q, lambda e: e.dma_start(out=out, in_=in_, **kw), reads, writes, True)

    def mm(self, out, lhsT, rhs, start, stop, reads=(), writes=()):
        return self.op("pe", lambda e: e.matmul(out, lhsT, rhs, start=start, stop=stop), reads, writes)

    def tr(self, out, in_, ident, reads=(), writes=()):
        return self.op("pe", lambda e: e.transpose(out, in_, ident), reads, writes)

    def act(self, out, in_, func, bias=None, scale=None, accum_out=None, reads=(), writes=(), eng="act"):
        kw = {}
        if bias is not None:
            kw["bias"] = bias
        if scale is not None:
            kw["scale"] = scale
        if accum_out is not None:
            kw["accum_out"] = accum_out
        return self.op(eng, lambda e: e.activation(out, in_, func, **kw), reads, writes)

    def tt(self, eng, out, in0, in1, op, reads=(), writes=()):
        return self.op(eng, lambda e: e.tensor_tensor(out, in0, in1, op), reads, writes)

    def ts(self, eng, out, in0, s1, s2, op0, op1=None, reads=(), writes=(), accum_out=None):
        kw = {}
        if accum_out is not None:
            kw["accum_out"] = accum_out
        if op1 is None:
            return self.op(eng, lambda e: e.tensor_scalar(out, in0, s1, None, op0, **kw), reads, writes)
        return self.op(eng, lambda e: e.tensor_scalar(out, in0, s1, s2, op0, op1, **kw), reads, writes)

    def stt(self, eng, out, in0, scalar, in1, op0, op1, reads=(), writes=()):
        return self.op(eng, lambda e: e.scalar_tensor_tensor(out, in0, scalar, in1, op0, op1), reads, writes)

    def copy(self, eng, out, in_, reads=(), writes=()):
        if eng == "act":
            return self.op(eng, lambda e: e.copy(out, in_), reads, writes)
        return self.op(eng, lambda e: e.tensor_copy(out, in_), reads, writes)

    def memset(self, eng, ap, val, writes=()):
        return self.op(eng, lambda e: e.memset(ap, val), (), writes)

    def finish(self, final_tokens):
        nc = self.nc
        prog = self.prog
        engmap = {"pe": "tensor", "act": "scalar", "dve": "vector", "pool": "gpsimd", "sp": "sync"}
        fin = self.all_tokens()
        with nc.Block() as block:
            for ename in self.all:
                entries = prog[ename]
                is_sp = ename == "sp"

                def body(e, entries=entries, is_sp=is_sp):
                    for waits, fn, inc in entries:
                        for wi_, (s, v) in enumerate(waits):
                            e.wait_ge(s, v)
                            if USE_NOPS and wi_ + 1 < len(waits):
                                e.nop(nofuse=True)
                        ins = fn(e)
                        ins.then_inc(inc[0], inc[1])
                    if is_sp:
                        for (s, v) in fin.values():
                            e.wait_ge(s, v)

                getattr(block, engmap[ename])(body)


D = 1024
T = 2304
NT = 18
LT = 2048
ALPHA = (2.0 * 2) ** 0.25
NEG = -30000.0


def tcol(ti):
    return 1 + 128 * ti if ti < 2 else 2 + 128 * ti


BLOCKS = [(1, 256, 0)] + [(258 + 512 * j, 512, 256 + 512 * j) for j in range(4)]


ORDER = ['pw', 'pm', 'A', 'B', 'C', 'D', 'E', 'F', 'G', 'L1A', 'L1P', 'L1W', 'L1WA', 'L1U', 'L1MA', 'L1E', 'L1F', 'L1']


def build(NB, dbg=False, only0=False, stop='L1'):
    def want(nm):
        return ORDER.index(nm) <= ORDER.index(stop)

    nc = bass.Bass("TRN2", target_bir_lowering=False)
    R = NB + 1
    dd = {}

    def din(name, shape, dt=F32):
        dd[name] = nc.dram_tensor(name, list(shape), dt, kind="ExternalInput").ap()
        return dd[name]

    def dscr(name, shape, dt=F32, out=False):
        return nc.dram_tensor(name, list(shape), dt, kind="ExternalOutput" if out else "Internal").ap()

    x_d = din("x", [NB, LT, D]); ctx_d = din("ctx", [NB, 256, D]); csT_d = din("csT", [128, 8, R])
    adaw_d = din("ada_w", [2, D, 6144]); adab_d = din("ada_b", [2, 6144])
    lng_d = din("ln_g", [4, D]); lnb_d = din("ln_b", [4, D])
    evwin_d = din("ev_w_in", [D, 3600]); aconvT_d = din("a_convT", [128, 4, 3]); bconv_d = din("ev_b_conv", [3, 1536])
    alog_d = din("alog", [1, 8]); dtb_d = din("dtbias", [1, 8]); bnorm_d = din("bnorm", [1, 128]); evwout_d = din("ev_w_out", [D, D])
    odwin_d = din("od_w_in", [D, 1440]); odwinsw_d = din("od_w_in_sw", [D, 1440])
    wkr_d = din("wkr", [D, 96]); wkrsw_d = din("wkr_sw", [D, 96])
    sink_d = din("sink", [1, 8]); qnT_d = din("qnormT", [128, 3]); kvnT_d = din("kvnormT", [128, 2])
    wuq_d = din("wuq", [384, 768]); wuqsw_d = din("wuq_sw", [384, 768]); wukv_d = din("wukv", [256, 1024]); odwout_d = din("od_w_out", [D, D])
    rw_d = din("router_w", [D, 16]); rb_d = din("router_bias", [1, 16])
    mg_d = din("moe_g", [32768, 512]); mu_d = din("moe_u", [32768, 512]); md_d = din("moe_d", [16384, 1024])
    idn_d = din("idn", [128, 128]); masks_d = din("masks", [14, 128, 128])
    rope64_d = din("rope64", [2, 64, LT]); rope32_d = din("rope32", [2, 96, LT])
    out_d = dscr("out", [NB, LT, D], F32, out=True)

    evwin_b = dscr("evwin_b", [D, 3600], BF16); wB_b = [dscr("wB%d_b" % j, [D, 1536], BF16) for j in range(3)]
    evwout_b = dscr("evwout_b", [D, D], BF16)
    odwin_b = dscr("odwin_b", [D, 1440], BF16); odwinsw_b = dscr("odwinsw_b", [D, 1440], BF16)
    wkr_b = dscr("wkr_b", [D, 96], BF16); wkrsw_b = dscr("wkrsw_b", [D, 96], BF16)
    wuq_b = dscr("wuq_b", [384, 768], BF16); wuqsw_b = dscr("wuqsw_b", [384, 768], BF16); wukv_b = dscr("wukv_b", [256, 1024], BF16)
    odwout_b = dscr("odwout_b", [D, D], BF16)
    modrow_s = dscr("modrow_s", [2, R, 6144], F32, out=dbg)
    qT_s = dscr("qT_s", [4, 128, T], BF16); kT_s = dscr("kT_s", [4, 128, T], BF16)
    k_s = dscr("k_s", [T, 512], BF16); v_s = dscr("v_s", [T, 512], BF16); gate_s = dscr("gate_s", [T, 512]); o_s = dscr("o_s", [T, 512])
    xa_s = dscr("xa_s", [NB, 2, T, D], F32, out=dbg)
    xb_s = dscr("xb_s", [NB, T, D], F32, out=dbg)
    h2T_s = dscr("h2T_s", [8, 128, T], BF16)
    cat_dbg = dscr("cat_dbg", [128, 8, LT], BF16, out=True) if dbg else None
    ymix_s = dscr("ymix_s", [NB, 2, T, D], F32, out=dbg) if dbg else None

    with ExitStack() as st0:
        S = Sched(nc, st0)
        cnt = [0]

        def sb(stack, shape, dt=F32, name=None):
            cnt[0] += 1
            return stack.enter_context(nc.sbuf_tensor("%s_%d" % (name or "t", cnt[0]), list(shape), dt))

        P2 = [st0.enter_context(nc.psum_tensor("pp%d" % i, [128, 1024], F32)) for i in range(4)]
        PB = [P2[i // 2][:, (i % 2) * 512:(i % 2 + 1) * 512] for i in range(8)]
        pbi = [0]
        S.excl = set(id(p_) for p_ in PB)

        def pb():
            p = PB[pbi[0] % 8]
            pbi[0] += 1
            return p

        ld_rr = [0]

        def rr3():
            ld_rr[0] += 1
            return ("dve", "pool", "act")[ld_rr[0] % 3]

        ident = sb(st0, [128, 128], name="ident"); S.dma(ident[:], idn_d, writes=[ident])
        ones = sb(st0, [128, 128], name="ones"); S.memset("pool", ones[:], 1.0, writes=[ones])
        MK = sb(st0, [128, 14, 128], name="masks")
        S.dma(MK[:], masks_d.rearrange("m p f -> p m f"), writes=[MK])
        modT = sb(st0, [128, 2, 48, R], name="modT")
        MKb = sb(st0, [128, 14, 128], BF16, name="masksb"); S.copy("dve", MKb[:], MK[:], reads=[MK], writes=[MKb])
        identb = sb(st0, [128, 128], BF16, name="identb"); S.copy("dve", identb[:], ident[:], reads=[ident], writes=[identb])
        onesb = sb(st0, [128, 128], BF16, name="onesb"); S.memset("pool", onesb[:], 1.0, writes=[onesb])
        rwt = sb(st0, [128, 8, 16], name="rw"); S.dma(rwt[:], rw_d.rearrange("(kc p) e -> p kc e", p=128), writes=[rwt])
        rbias = sb(st0, [128, 16], name="rbias"); S.dma(rbias[:], rb_d.to_broadcast([128, 16]), writes=[rbias])
        aconvT = sb(st0, [128, 4, 3], name="aconvT"); S.dma(aconvT[:], aconvT_d, writes=[aconvT])
        negexpA = sb(st0, [128, 8], name="negexpA"); dtb = sb(st0, [128, 8], name="dtb")
        S.dma(negexpA[:], alog_d.to_broadcast([128, 8]), writes=[negexpA]); S.dma(dtb[:], dtb_d.to_broadcast([128, 8]), writes=[dtb])
        S.act(negexpA[:], negexpA[:], AF.Exp, reads=[negexpA], writes=[negexpA])
        S.ts("dve", negexpA[:], negexpA[:], -1.0, None, ALU.mult, reads=[negexpA], writes=[negexpA])
        bnorm = sb(st0, [128, 128], name="bnorm"); S.dma(bnorm[:], bnorm_d.to_broadcast([128, 128]), writes=[bnorm])
        expsink = sb(st0, [128, 8], name="expsink"); S.dma(expsink[:], sink_d.to_broadcast([128, 8]), writes=[expsink])
        S.act(expsink[:], expsink[:], AF.Exp, reads=[expsink], writes=[expsink])
        qnT = sb(st0, [128, 3], name="qnT"); S.dma(qnT[:], qnT_d, writes=[qnT])
        kvnT = sb(st0, [128, 2], name="kvnT"); S.dma(kvnT[:], kvnT_d, writes=[kvnT])

        with ExitStack() as st:
            NSTG = 6
            stg = [sb(st, [128, 4096], F32, "stg") for _ in range(NSTG)]
            stgb = [sb(st, [128, 4096], BF16, "stgb") for _ in range(NSTG)]
            bcv = sb(st, [128, 3, 1536], F32, "bcv")
            for j in range(3):
                S.dma(bcv[:, j, :], bconv_d[j:j + 1, :].to_broadcast([128, 1536]), writes=[(bcv, j)])
            ui = [0]

            def conv(src, dst, rows, cols, scale=None):
                nrc = rows // 128
                G = max(1, min(nrc, 4096 // cols)) if cols <= 4096 else 1
                if scale is not None:
                    G = 1
                while nrc % G:
                    G -= 1
                for r0 in range(0, nrc, G):
                    i = ui[0] % NSTG
                    ui[0] += 1
                    eng = ("dve", "pool", "act")[i % 3]
                    a = stg[i][:, 0:G * cols].rearrange("p (g c) -> p g c", g=G)
                    b = stgb[i][:, 0:G * cols].rearrange("p (g c) -> p g c", g=G)
                    sv = src[r0 * 128:(r0 + G) * 128, :].rearrange("(g p) c -> p g c", p=128)
                    dv = dst[r0 * 128:(r0 + G) * 128, :].rearrange("(g p) c -> p g c", p=128)
                    S.dma(a, sv, writes=[stg[i]])
                    if scale is None:
                        S.copy(eng, b, a, reads=[stg[i]], writes=[stgb[i]])
                    else:
                        e2 = "pool" if eng == "act" else eng
                        S.tt(e2, b[:, 0, :], a[:, 0, :], scale, ALU.mult, reads=[stg[i], (bcv, 0), (bcv, 1), (bcv, 2)], writes=[stgb[i]])
                    S.dma(dv, b, reads=[stgb[i]])

            conv(evwin_d, evwin_b, D, 3600)
            for j in range(3):
                conv(evwin_d[:, 1536:3072], wB_b[j], D, 1536, scale=bcv[:, j, :])
            conv(evwout_d, evwout_b, D, D)
            conv(odwin_d, odwin_b, D, 1440); conv(odwinsw_d, odwinsw_b, D, 1440)
            conv(wkr_d, wkr_b, D, 96); conv(wkrsw_d, wkrsw_b, D, 96)
            conv(wuq_d, wuq_b, 384, 768); conv(wuqsw_d, wuqsw_b, 384, 768); conv(wukv_d, wukv_b, 256, 1024)
            conv(odwout_d, odwout_b, D, D)
            S.barrier()
        with ExitStack() as st:
          if want('pm'):
            csT = sb(st, [128, 8, R], F32, "csT")
            S.dma(csT[:], csT_d, writes=[csT])
            S.act(csT[:], csT[:], AF.Silu, reads=[csT], writes=[csT])
            awt = [sb(st, [128, 8, 1536], F32, "awt") for _ in range(2)]
            modrow = sb(st, [R, 6144], F32, "modrow")
            abr = sb(st, [R, 6144], F32, "abr")
            for l in range(2):
                S.dma(abr[:], adab_d[l:l + 1, :].to_broadcast([R, 6144]), reads=[modrow], writes=[abr])
                for q in range(4):
                    aw = awt[q % 2]
                    S.dma(aw[:], adaw_d[l, :, q * 1536:(q + 1) * 1536].rearrange("(kc p) n -> p kc n", p=128), writes=[aw])
                    for nb_ in range(3):
                        p = pb()
                        c0 = q * 1536 + nb_ * 512
                        for kc in range(8):
                            S.mm(p[0:R, :], csT[:, kc, :], aw[:, kc, nb_ * 512:(nb_ + 1) * 512], kc == 0, kc == 7, reads=[csT, aw], writes=[p])
                        S.tt("dve", modrow[:, c0:c0 + 512], p[0:R, :], abr[:, c0:c0 + 512], ALU.add, reads=[p, abr], writes=[modrow])
                S.dma(modrow_s[l], modrow[:], reads=[modrow])
                p = pb()
                for ch in range(48):
                    S.tr(p[:, ch * R:(ch + 1) * R], modrow[0:R, ch * 128:(ch + 1) * 128], ident[0:R, 0:R], reads=[modrow, ident], writes=[p])
                S.copy("dve", modT[:, l, :, :], p[:, 0:48 * R].rearrange("p (c r) -> p c r", r=R), reads=[p], writes=[modT])
                for c0 in (8, 32):
                    S.ts("dve", modT[:, l, c0:c0 + 8, :], modT[:, l, c0:c0 + 8, :], 1.0, None, ALU.add, reads=[modT], writes=[modT])
            S.barrier()

        def xsrc(layer, b, ti):
            if layer == 0:
                return ctx_d[b, ti * 128:(ti + 1) * 128, :] if ti < 2 else x_d[b, (ti - 2) * 128:(ti - 1) * 128, :]
            return xb_s[b, ti * 128:(ti + 1) * 128, :]

        def phase_A(layer, b, hT, tiles, st):
            xin = [sb(st, [128, D], F32, "xin") for _ in range(2)]
            for n, ti in enumerate(tiles):
                xt = xin[n % 2]
                r = NB if ti < 2 else b
                S.dma(xt[:], xsrc(layer, b, ti), writes=[xt])
                for half in range(2):
                    p = pb()
                    for k4 in range(4):
                        kc = half * 4 + k4
                        S.tr(p[:, k4 * 128:(k4 + 1) * 128], xt[:, kc * 128:(kc + 1) * 128], ident[:], reads=[xt], writes=[p])
                    for k4 in range(4):
                        kc = half * 4 + k4
                        dst = hT[:, kc, tcol(ti):tcol(ti) + 128]
                        if half == 0:
                            S.act(dst, p[:, k4 * 128:(k4 + 1) * 128], AF.Identity, bias=modT[:, layer, kc, r:r + 1],
                                  scale=modT[:, layer, 8 + kc, r:r + 1], reads=[p], writes=[(hT, ti, kc)])
                        else:
                            S.ts("dve", dst, p[:, k4 * 128:(k4 + 1) * 128], modT[:, layer, 8 + kc, r:r + 1], modT[:, layer, kc, r:r + 1],
                                 ALU.mult, ALU.add, reads=[p], writes=[(hT, ti, kc)])

        def layer_norm_tile(st_tiles, rt, lng, lnb, outt):
            stats, ag, sm = st_tiles
            for hf in range(2):
                S.op("dve", lambda e, hf=hf: e.bn_stats(stats[:, hf, :], rt[:, hf * 512:(hf + 1) * 512]), reads=[rt], writes=[stats])
            S.op("dve", lambda e: e.bn_aggr(ag[:], stats[:].rearrange('p a b -> p (a b)')), reads=[stats], writes=[ag])
            S.act(sm[:, 0:1], ag[:, 1:2], AF.Sqrt, bias=1e-5, reads=[ag], writes=[sm])
            S.op("dve", lambda e: e.reciprocal(sm[:, 1:2], sm[:, 0:1]), reads=[sm], writes=[sm])
            S.stt("dve", sm[:, 2:3], ag[:, 0:1], -1.0, sm[:, 1:2], ALU.mult, ALU.mult, reads=[ag, sm], writes=[sm])
            S.act(rt[:], rt[:], AF.Identity, bias=sm[:, 2:3], scale=sm[:, 1:2], reads=[rt, sm], writes=[rt])
            S.tt("pool", rt[:], rt[:], lng[:], ALU.mult, reads=[rt, lng], writes=[rt])
            S.tt("pool", outt[:], rt[:], lnb[:], ALU.add, reads=[rt, lnb], writes=[outt])

        def run_pipe(gens, depth):
            active = []
            it = iter(gens)
            fin = False
            while True:
                while len(active) < depth and not fin:
                    try:
                        active.append(next(it))
                    except StopIteration:
                        fin = True
                if not active:
                    break
                for g_ in list(active):
                    try:
                        next(g_)
                    except StopIteration:
                        active.remove(g_)

        def ln_stats(rt, stats, ag, sm):
            for hf in range(2):
                S.op("dve", lambda e, hf=hf: e.bn_stats(stats[:, hf, :], rt[:, hf * 512:(hf + 1) * 512]), reads=[rt], writes=[stats])
            S.op("dve", lambda e: e.bn_aggr(ag[:], stats[:].rearrange('p a b -> p (a b)')), reads=[stats], writes=[ag])
            S.act(sm[:, 0:1], ag[:, 1:2], AF.Sqrt, bias=1e-5, reads=[ag], writes=[sm])
            S.op("dve", lambda e: e.reciprocal(sm[:, 1:2], sm[:, 0:1]), reads=[sm], writes=[sm])
            S.stt("dve", sm[:, 2:3], ag[:, 0:1], -1.0, sm[:, 1:2], ALU.mult, ALU.mult, reads=[ag, sm], writes=[sm])

        def ln_apply(rt, sm, lng, lnb, outt):
            S.act(rt[:], rt[:], AF.Identity, bias=sm[:, 2:3], scale=sm[:, 1:2], reads=[rt, sm], writes=[rt])
            S.tt("dve", rt[:], rt[:], lng[:], ALU.mult, reads=[rt, lng], writes=[rt])
            S.tt("dve", outt[:], rt[:], lnb[:], ALU.add, reads=[rt, lnb], writes=[outt])

        def phase_E(layer, b, catT, tiles, gates, st):
            nt = len(tiles)
            NBUF = 5
            wo = sb(st, [128, 8, D], BF16, "wo")
            wsrc = evwout_b if layer == 0 else odwout_b
            S.dma(wo[:], wsrc.rearrange("(kc p) n -> p kc n", p=128), writes=[wo])
            lng = sb(st, [128, D], F32, "lng"); lnb = sb(st, [128, D], F32, "lnb")
            S.dma(lng[:], lng_d[2 * layer:2 * layer + 1, :].to_broadcast([128, D]), writes=[lng])
            S.dma(lnb[:], lnb_d[2 * layer:2 * layer + 1, :].to_broadcast([128, D]), writes=[lnb])
            g1 = [sb(st, [128, D], F32, "g1") for _ in range(2)]
            S.dma(g1[0][:], modrow_s[layer, NB:NB + 1, 2048:3072].to_broadcast([128, D]), writes=[g1[0]])
            S.dma(g1[1][:], modrow_s[layer, b:b + 1, 2048:3072].to_broadcast([128, D]), writes=[g1[1]])
            xin = [sb(st, [128, D], F32, "xin") for _ in range(NBUF)]
            rts = [sb(st, [128, D], F32, "rt") for _ in range(NBUF)]
            xns = [sb(st, [128, D], F32, "xn") for _ in range(NBUF)]
            h2f = [sb(st, [128, 8, 128], F32, "h2f") for _ in range(NBUF)]
            h2b = [sb(st, [128, 8, 128], BF16, "h2b") for _ in range(NBUF)]
            statsL = [sb(st, [128, 2, 6], F32, "stats") for _ in range(NBUF)]
            agL = [sb(st, [128, 2], F32, "ag") for _ in range(NBUF)]
            smL = [sb(st, [128, 4], F32, "sm") for _ in range(NBUF)]
            aff = sb(st, [128, nt, 16], F32, "aff")

            def tile_gen(n, ti):
                k = n % NBUF
                xt = xin[k]; rt = rts[k]; xn = xns[k]; hf_ = h2f[k]; hb_ = h2b[k]; stats = statsL[k]; ag = agL[k]; sm = smL[k]
                r = NB if ti < 2 else b
                gt = g1[0] if ti < 2 else g1[1]
                S.dma(xt[:], xsrc(layer, b, ti), writes=[xt])
                c0 = tcol(ti) if layer == 0 else (ti - 2) * 128
                for hf in range(2):
                    p = pb()
                    for kc in range(8):
                        S.mm(p[:], catT[:, kc, c0:c0 + 128], wo[:, kc, hf * 512:(hf + 1) * 512], kc == 0, kc == 7, reads=[wo], writes=[p])
                    if dbg:
                        S.copy("act", rt[:, hf * 512:(hf + 1) * 512], p[:], reads=[p], writes=[rt])
                        S.dma(ymix_s[b, layer, ti * 128:(ti + 1) * 128, hf * 512:(hf + 1) * 512], rt[:, hf * 512:(hf + 1) * 512], reads=[rt])
                    S.tt("dve", rt[:, hf * 512:(hf + 1) * 512], p[:], gt[:, hf * 512:(hf + 1) * 512], ALU.mult, reads=[p, gt], writes=[rt])
                yield
                S.stt("dve", rt[:], xt[:], ALPHA, rt[:], ALU.mult, ALU.add, reads=[xt, rt], writes=[rt])
                ln_stats(rt, stats, ag, sm)
                yield
                ln_apply(rt, sm, lng, lnb, xn)
                S.dma(xa_s[b, layer, ti * 128:(ti + 1) * 128, :], xn[:], reads=[xn])
                yield
                for half in range(2):
                    p = pb()
                    for k4 in range(4):
                        kc = half * 4 + k4
                        S.tr(p[:, k4 * 128:(k4 + 1) * 128], xn[:, kc * 128:(kc + 1) * 128], ident[:], reads=[xn], writes=[p])
                    for k4 in range(4):
                        kc = half * 4 + k4
                        if half == 0:
                            S.act(hf_[:, kc, :], p[:, k4 * 128:(k4 + 1) * 128], AF.Identity, bias=modT[:, layer, 24 + kc, r:r + 1],
                                  scale=modT[:, layer, 32 + kc, r:r + 1], reads=[p], writes=[hf_])
                        else:
                            S.ts("dve", hf_[:, kc, :], p[:, k4 * 128:(k4 + 1) * 128], modT[:, layer, 32 + kc, r:r + 1], modT[:, layer, 24 + kc, r:r + 1],
                                 ALU.mult, ALU.add, reads=[p], writes=[hf_])
                yield
                S.copy("act", hb_[:], hf_[:], reads=[hf_], writes=[hb_])
                S.dma(h2T_s.rearrange("k p t -> p k t")[:, :, n * 128:(n + 1) * 128], hb_[:], reads=[hb_])
                p = pb()
                for kc in range(8):
                    S.mm(p[:, 0:16], hf_[:, kc, :], rwt[:, kc, :], kc == 0, kc == 7, reads=[hf_], writes=[p])
                S.act(aff[:, n, :], p[:, 0:16], AF.Sigmoid, reads=[p], writes=[(aff, n)])

            run_pipe((tile_gen(n, ti) for n, ti in enumerate(tiles)), 4)
            router_batched(aff, gates, nt, st)

        def router_batched(aff, gates, nt, st):
            G4 = nt * 4
            sel = sb(st, [128, nt, 16], F32, "r_sel"); t1 = sb(st, [128, nt, 16], F32, "r_t1"); t2 = sb(st, [128, nt, 16], F32, "r_t2")
            m1 = sb(st, [128, G4], F32, "r_m1"); sec = sb(st, [128, G4], F32, "r_sec"); gs = sb(st, [128, G4], F32, "r_gs")
            gm = sb(st, [128, G4], F32, "r_gm"); tm = sb(st, [128, G4], F32, "r_tm")
            s1 = sb(st, [128, nt], F32, "r_s1"); s2 = sb(st, [128, nt], F32, "r_s2"); den = sb(st, [128, nt], F32, "r_den")
            allaff = [(aff, n) for n in range(nt)]
            RS = "rsres"

            def g4(t):
                return t[:].rearrange("p n (g e) -> p (n g) e", g=4)

            def bc44(t):
                return t[:].unsqueeze(2).to_broadcast([128, G4, 4])

            def bc16(t):
                return t[:].unsqueeze(2).to_broadcast([128, nt, 16])

            def dv(fn, *a, **k):
                return S.op("dve", fn, reads=allaff + [RS], writes=[RS])
            dv(lambda e: e.tensor_tensor(sel[:], aff[:], rbias[:].unsqueeze(1).to_broadcast([128, nt, 16]), ALU.add))
            dv(lambda e: e.tensor_reduce(m1[:], g4(sel), AX.X, ALU.max))
            dv(lambda e: e.tensor_tensor(g4(t1), g4(sel), bc44(m1), ALU.is_lt))
            dv(lambda e: e.tensor_scalar(t2[:], t1[:], 1.0, 1e9, ALU.subtract, ALU.mult))
            dv(lambda e: e.tensor_tensor(t1[:], t1[:], sel[:], ALU.mult))
            dv(lambda e: e.tensor_tensor(t2[:], t2[:], t1[:], ALU.add))
            dv(lambda e: e.tensor_reduce(sec[:], g4(t2), AX.X, ALU.max))
            dv(lambda e: e.tensor_tensor(gs[:], m1[:], sec[:], ALU.add))
            gs3 = gs[:].rearrange("p (n g) -> p n g", g=4)
            dv(lambda e: e.tensor_reduce(s1[:], gs3, AX.X, ALU.max))
            dv(lambda e: e.tensor_tensor(gm[:].rearrange("p (n g) -> p n g", g=4), gs3, s1[:].unsqueeze(2).to_broadcast([128, nt, 4]), ALU.is_ge))
            dv(lambda e: e.tensor_tensor(g4(t1), g4(sel), bc44(gm), ALU.mult))
            dv(lambda e: e.tensor_scalar(tm[:], gm[:], 1.0, 1e9, ALU.subtract, ALU.mult))
            dv(lambda e: e.tensor_tensor(g4(t1), g4(t1), bc44(tm), ALU.add))
            dv(lambda e: e.tensor_reduce(s1[:], t1[:], AX.X, ALU.max))
            dv(lambda e: e.tensor_tensor(t2[:], t1[:], bc16(s1), ALU.is_lt))
            dv(lambda e: e.tensor_tensor(sel[:], t1[:], t2[:], ALU.mult))
            dv(lambda e: e.tensor_scalar(t2[:], t2[:], 1.0, 1e9, ALU.subtract, ALU.mult))
            dv(lambda e: e.tensor_tensor(sel[:], sel[:], t2[:], ALU.add))
            dv(lambda e: e.tensor_reduce(s2[:], sel[:], AX.X, ALU.max))
            dv(lambda e: e.tensor_tensor(t2[:], t1[:], bc16(s2), ALU.is_ge))
            dv(lambda e: e.tensor_tensor(t2[:], t2[:], aff[:], ALU.mult))
            dv(lambda e: e.tensor_reduce(den[:], t2[:], AX.X, ALU.add))
            dv(lambda e: e.reciprocal(den[:], den[:]))
            S.op("dve", lambda e: e.tensor_tensor(gates[:], t2[:], bc16(den), ALU.mult), reads=[RS], writes=[gates])

        def phase_F(layer, b, h2T, gates, ntile, outacc, st):
            wgu = [sb(st, [128, 2, 8, 512], BF16, "wgu") for _ in range(2)]
            wdn = [sb(st, [128, 4, D], BF16, "wdn") for _ in range(2)]
            actT = [sb(st, [128, 4, 512], BF16, "actT") for _ in range(2)]
            sl = [sb(st, [128, 512], F32, "sl") for _ in range(2)]
            stgF = [sb(st, [128, 2048], F32, "stgF") for _ in range(2)]
            ntok = ntile * 128
            blocks = [(c, min(512, ntok - c)) for c in range(0, ntok, 512)]
            pi = [0]

            def pieces(e):
                wg = wgu[e % 2]; wd = wdn[e % 2]
                r0 = (layer * 16 + e) * 1024
                r1 = (layer * 16 + e) * 512
                out = []
                for which, src in ((0, mg_d), (1, mu_d)):
                    for half in range(2):
                        src_ap = src[r0 + half * 512:r0 + (half + 1) * 512, :].rearrange("(kc p) f -> p kc f", p=128)
                        out.append((wg[:, which, half * 4:(half + 1) * 4, :], src_ap, (wg, which, half), 4))
                for half in range(2):
                    src_ap = md_d[r1 + half * 256:r1 + (half + 1) * 256, :].rearrange("(fc p) n -> p fc n", p=128)
                    out.append((wd[:, half * 2:(half + 1) * 2, :], src_ap, (wd, half), 2))
                return out

            def load_cast(pc):
                dst_ap, src_ap, key, G = pc
                sg = stgF[pi[0] % 2]; pi[0] += 1
                sgv = sg[:, 0:2048].rearrange("p (g c) -> p g c", g=G)
                S.dma(sgv, src_ap, writes=[sg])
                S.copy("pool", dst_ap, sgv, reads=[sg], writes=[key])

            def down(at, wd, e, c0, w):
                for j in range(w // 128):
                    n = c0 // 128 + j
                    for hf in range(2):
                        pd = pb()
                        for fc in range(4):
                            S.mm(pd[:], at[:, fc, j * 128:(j + 1) * 128], wd[:, fc, hf * 512:(hf + 1) * 512], fc == 0, fc == 3,
                                 reads=[(wd, fc // 2), (at, 0), (at, 1), (at, 2), (at, 3)], writes=[pd])
                        dst = outacc[:, n, hf * 512:(hf + 1) * 512]
                        if e == 0:
                            S.ts("dve", dst, pd[:], gates[:, n, e:e + 1], None, ALU.mult, reads=[pd], writes=[(outacc, n, hf)])
                        else:
                            S.stt("dve", dst, pd[:], gates[:, n, e:e + 1], dst, ALU.mult, ALU.add, reads=[pd], writes=[(outacc, n, hf)])

            for pc in pieces(0):
                load_cast(pc)
            it = 0
            pending = None
            for e in range(16):
                wg = wgu[e % 2]; wd = wdn[e % 2]
                nxt = pieces(e + 1) if e + 1 < 16 else []
                for bi, (c0, w) in enumerate(blocks):
                    at = actT[it % 2]; it += 1
                    for fc in range(4):
                        pg = pb(); pu = pb()
                        for kc in range(8):
                            S.mm(pg[:, 0:w], wg[:, 0, kc, fc * 128:(fc + 1) * 128], h2T[:, kc, c0:c0 + w], kc == 0, kc == 7, reads=[(wg, 0, kc // 4)], writes=[pg])
                        for kc in range(8):
                            S.mm(pu[:, 0:w], wg[:, 1, kc, fc * 128:(fc + 1) * 128], h2T[:, kc, c0:c0 + w], kc == 0, kc == 7, reads=[(wg, 1, kc // 4)], writes=[pu])
                        s_ = sl[fc % 2]
                        S.act(s_[:, 0:w], pg[:, 0:w], AF.Silu, reads=[pg], writes=[s_])
                        S.tt("dve", at[:, fc, 0:w], s_[:, 0:w], pu[:, 0:w], ALU.mult, reads=[s_, pu], writes=[(at, fc)])
                    if pending is not None:
                        down(*pending)
                    pending = (at, wd, e, c0, w)
                    k = 2 if bi == 0 else 1
                    for _ in range(k):
                        if nxt:
                            load_cast(nxt.pop(0))
                while nxt:
                    load_cast(nxt.pop(0))
            down(*pending)

        def phase_G(layer, b, tiles, outacc, st):
            NBUF = 5
            lng = sb(st, [128, D], F32, "lng2"); lnb = sb(st, [128, D], F32, "lnb2")
            S.dma(lng[:], lng_d[2 * layer + 1:2 * layer + 2, :].to_broadcast([128, D]), writes=[lng])
            S.dma(lnb[:], lnb_d[2 * layer + 1:2 * layer + 2, :].to_broadcast([128, D]), writes=[lnb])
            g2 = [sb(st, [128, D], F32, "g2") for _ in range(2)]
            S.dma(g2[0][:], modrow_s[layer, NB:NB + 1, 5120:6144].to_broadcast([128, D]), writes=[g2[0]])
            S.dma(g2[1][:], modrow_s[layer, b:b + 1, 5120:6144].to_broadcast([128, D]), writes=[g2[1]])
            xin = [sb(st, [128, D], F32, "xin2") for _ in range(NBUF)]
            rts = [sb(st, [128, D], F32, "rt2") for _ in range(NBUF)]
            xns = [sb(st, [128, D], F32, "xn2") for _ in range(NBUF)]
            statsL = [sb(st, [128, 2, 6], F32, "stats2") for _ in range(NBUF)]
            agL = [sb(st, [128, 2], F32, "ag2") for _ in range(NBUF)]
            smL = [sb(st, [128, 4], F32, "sm2") for _ in range(NBUF)]

            def tile_gen(n, ti):
                k = n % NBUF
                xt = xin[k]; rt = rts[k]; xn = xns[k]; stats = statsL[k]; ag = agL[k]; sm = smL[k]
                gt = g2[0] if ti < 2 else g2[1]
                S.dma(xt[:], xa_s[b, layer, ti * 128:(ti + 1) * 128, :], writes=[xt])
                S.tt("dve", rt[:], outacc[:, n, :], gt[:], ALU.mult, reads=[gt, (outacc, n, 0), (outacc, n, 1)], writes=[rt])
                yield
                S.stt("dve", rt[:], xt[:], ALPHA, rt[:], ALU.mult, ALU.add, reads=[xt, rt], writes=[rt])
                ln_stats(rt, stats, ag, sm)
                yield
                ln_apply(rt, sm, lng, lnb, xn)
                if layer == 0:
                    S.dma(xb_s[b, ti * 128:(ti + 1) * 128, :], xn[:], reads=[xn])
                else:
                    S.dma(out_d[b, (ti - 2) * 128:(ti - 1) * 128, :], xn[:], reads=[xn])

            run_pipe((tile_gen(n, ti) for n, ti in enumerate(tiles)), 4)

        def post_E(layer, b, catT, tiles, gates):
            if not want('E'):
                return
            with ExitStack() as st2:
                phase_E(layer, b, catT, tiles, gates, st2)
                S.barrier()

        def post_FG(layer, b, tiles, gates):
            nt = len(tiles)
            if not want('F'):
                return
            with ExitStack() as st:
                outacc = sb(st, [128, nt, D], F32, "outacc")
                with ExitStack() as st2:
                    h2T = sb(st2, [128, 8, nt * 128], BF16, "h2T")
                    for kc in range(8):
                        S.dma(h2T[:, kc, :], h2T_s[kc, :, 0:nt * 128], writes=[h2T])
                    phase_F(layer, b, h2T, gates, nt, outacc, st2)
                    S.barrier()
                if not want('G'):
                    return
                with ExitStack() as st2:
                    phase_G(layer, b, tiles, outacc, st2)
                    S.barrier()

        def phase_B(b, hT, catT, st):
            wA = [sb(st, [128, 3, 8, 128], BF16, "wA") for _ in range(2)]
            u = sb(st, [128, 2307], F32, "u"); p0s = sb(st, [128, 2307], F32, "p0s"); y = sb(st, [128, 2307], F32, "y")
            t1 = [sb(st, [128, 512], F32, "t1") for _ in range(2)]
            for c in (0, 257, 2306):
                S.memset("pool", u[:, c:c + 1], 0.0, writes=[(u, "pad")])
            bi = 0
            for ch in range(4):
                w = wA[ch % 2]
                for which in range(3):
                    cc = which * 512 + ch * 128
                    S.dma(w[:, which], evwin_b[:, cc:cc + 128].rearrange("(kc p) n -> p kc n", p=128), writes=[(w, which)])
                for (c0, wd_, t0) in BLOCKS:
                    pp = [pb(), pb(), pb()]
                    for which in range(3):
                        for kc in range(8):
                            S.mm(pp[which][:, 0:wd_], w[:, which, kc, :], hT[:, kc, c0:c0 + wd_], kc == 0, kc == 7, reads=[(w, which)], writes=[pp[which]])
                    tt_ = t1[bi % 2]; bi += 1
                    S.copy("act", tt_[:, 0:wd_], pp[1][:, 0:wd_], reads=[pp[1]], writes=[tt_])
                    S.tt("dve", u[:, c0:c0 + wd_], tt_[:, 0:wd_], pp[2][:, 0:wd_], ALU.mult, reads=[tt_, pp[2]], writes=[(u, c0)])
                    S.copy("act", p0s[:, c0:c0 + wd_], pp[0][:, 0:wd_], reads=[pp[0]], writes=[(p0s, c0)])
                allu = [(u, c0) for c0, _, _ in BLOCKS] + [(u, "pad")]
                allp = [(p0s, c0) for c0, _, _ in BLOCKS]
                S.ts("dve", y[:, 1:2306], u[:, 1:2306], aconvT[:, ch, 1:2], None, ALU.mult, reads=allu, writes=[y])
                S.stt("dve", y[:, 1:2306], u[:, 0:2305], aconvT[:, ch, 0:1], y[:, 1:2306], ALU.mult, ALU.add, reads=allu + [y], writes=[y])
                S.stt("dve", y[:, 1:2306], u[:, 2:2307], aconvT[:, ch, 2:3], y[:, 1:2306], ALU.mult, ALU.add, reads=allu + [y], writes=[y])
                S.tt("dve", catT[:, ch, 1:2306], y[:, 1:2306], p0s[:, 1:2306], ALU.mult, reads=[y] + allp, writes=[(catT, ch)])

        def phase_C(b, hT, BG, st):
            NBUF = 4
            wB = [sb(st, [128, 3, 8, 128], BF16, "wB") for _ in range(2)]
            s_ = [sb(st, [128, 512], F32, "s_") for _ in range(NBUF)]
            sq_ = [sb(st, [128, 512], BF16, "sq_") for _ in range(NBUF)]
            rin = [sb(st, [128, 512], F32, "rin") for _ in range(NBUF)]
            kn = [sb(st, [128, 512], BF16, "kn") for _ in range(NBUF)]
            ktk = [sb(st, [128, 4, 128], BF16, "ktk") for _ in range(NBUF)]

            def qk_gen(i, which, h, w, c0, wd_, t0):
                p = pb(); n = 0
                for j in range(3):
                    for kc in range(8):
                        S.mm(p[:, 0:wd_], w[:, j, kc, :], hT[:, kc, c0 + j - 1:c0 + j - 1 + wd_], n == 0, n == 23, reads=[(w, j)], writes=[p])
                        n += 1
                s = s_[i % NBUF]; sq = sq_[i % NBUF]; ri = rin[i % NBUF]; qn = kn[i % NBUF]; kt = ktk[i % NBUF]
                S.act(s[:, 0:wd_], p[:, 0:wd_], AF.Silu, reads=[p], writes=[s])
                S.tt("dve", sq[:, 0:wd_], s[:, 0:wd_], s[:, 0:wd_], ALU.mult, reads=[s], writes=[sq])
                yield
                p2 = pb()
                S.mm(p2[:, 0:wd_], onesb[:], sq[:, 0:wd_], True, True, reads=[sq], writes=[p2])
                S.act(ri[:, 0:wd_], p2[:, 0:wd_], AF.Sqrt, bias=1e-6, reads=[p2], writes=[ri])
                S.op("dve", lambda e: e.reciprocal(ri[:, 0:wd_], ri[:, 0:wd_]), reads=[ri], writes=[ri])
                if which == 0:
                    S.stt("dve", qn[:, 0:wd_], s[:, 0:wd_], 128.0 ** -0.5, ri[:, 0:wd_], ALU.mult, ALU.mult, reads=[s, ri], writes=[qn])
                else:
                    S.tt("dve", qn[:, 0:wd_], s[:, 0:wd_], ri[:, 0:wd_], ALU.mult, reads=[s, ri], writes=[qn])
                dstT = (qT_s if which == 0 else kT_s)[h][:, t0:t0 + wd_]
                S.dma(dstT, qn[:, 0:wd_], reads=[qn])
                if which == 1:
                    yield
                    p3 = pb()
                    na = wd_ // 128
                    for j4 in range(na):
                        S.mm(p3[:, j4 * 128:(j4 + 1) * 128], qn[:, j4 * 128:(j4 + 1) * 128], identb[:], True, True, reads=[qn], writes=[p3])
                    S.copy("act", kt[:, 0:na, :], p3[:, 0:wd_].rearrange("p (a b) -> p a b", b=128), reads=[p3], writes=[kt])
                    S.dma(k_s[t0:t0 + wd_, h * 128:(h + 1) * 128].rearrange("(a p) d -> p a d", p=128), kt[:, 0:na, :], reads=[kt])

            def all_qk():
                i = 0
                wi = 0
                for which in range(2):
                    for h in range(4):
                        w = wB[wi % 2]; wi += 1
                        col = which * 512 + h * 128
                        for j in range(3):
                            S.dma(w[:, j], wB_b[j][:, col:col + 128].rearrange("(kc p) n -> p kc n", p=128), writes=[(w, j)])
                        for (c0, wd_, t0) in BLOCKS:
                            yield qk_gen(i, which, h, w, c0, wd_, t0)
                            i += 1
            run_pipe(all_qk(), 3)
            wV = sb(st, [128, 3, 8, 512], BF16, "wV")
            for j in range(3):
                S.dma(wV[:, j], wB_b[j][:, 1024:1536].rearrange("(kc p) n -> p kc n", p=128), writes=[(wV, j)])
            wG = sb(st, [128, 8, 512], BF16, "wG")
            S.dma(wG[:], evwin_b[:, 3072:3584].rearrange("(kc p) n -> p kc n", p=128), writes=[wG])
            wba = sb(st, [128, 8, 16], BF16, "wba")
            S.dma(wba[:], evwin_b[:, 3584:3600].rearrange("(kc p) n -> p kc n", p=128), writes=[wba])
            vt = [sb(st, [128, 512], BF16, "vt") for _ in range(2)]
            gt = [sb(st, [128, 512], F32, "gt") for _ in range(2)]
            for ti in range(NT):
                c = tcol(ti)
                p = pb(); n = 0
                for j in range(3):
                    for kc in range(8):
                        S.mm(p[:], hT[:, kc, c + j - 1:c + j - 1 + 128], wV[:, j, kc, :], n == 0, n == 23, reads=[(wV, j)], writes=[p])
                        n += 1
                v = vt[ti % 2]
                S.act(v[:], p[:], AF.Silu, reads=[p], writes=[v])
                S.dma(v_s[ti * 128:(ti + 1) * 128, :], v[:], reads=[v])
                p = pb()
                for kc in range(8):
                    S.mm(p[:], hT[:, kc, c:c + 128], wG[:, kc, :], kc == 0, kc == 7, reads=[wG], writes=[p])
                g = gt[ti % 2]
                S.act(g[:], p[:], AF.Silu, reads=[p], writes=[g])
                S.dma(gate_s[ti * 128:(ti + 1) * 128, :], g[:], reads=[g])
                p = pb()
                for kc in range(8):
                    S.mm(p[:, 0:16], hT[:, kc, c:c + 128], wba[:, kc, :], kc == 0, kc == 7, reads=[wba], writes=[p])
                S.copy("dve", BG[:, ti, :], p[:, 0:16], reads=[p], writes=[(BG, ti)])
            allbg = [(BG, ti) for ti in range(NT)]
            smb = sb(st, [128, NT, 8], F32, "smb")
            S.act(BG[:, :, 0:8], BG[:, :, 0:8], AF.Sigmoid, reads=allbg, writes=[(BG, "beta")])
            S.tt("dve", smb[:], BG[:, :, 8:16], dtb[:].unsqueeze(1).to_broadcast([128, NT, 8]), ALU.add, reads=allbg, writes=[smb])
            S.ts("dve", smb[:], smb[:], 30.0, None, ALU.min, reads=[smb], writes=[smb])
            S.act(smb[:], smb[:], AF.Exp, reads=[smb], writes=[smb])
            S.act(smb[:], smb[:], AF.Ln, bias=1.0, reads=[smb], writes=[smb])
            S.tt("dve", BG[:, :, 8:16], smb[:], negexpA[:].unsqueeze(1).to_broadcast([128, NT, 8]), ALU.mult, reads=[smb] + allbg, writes=[(BG, "g")])

        def phase_D(b, BG, catT, st):
            Sst = sb(st, [128, 2, 4, 128], F32, "Sst")
            S.memset("pool", Sst[:], 0.0, writes=[Sst])
            Sb = sb(st, [128, 2, 4, 128], BF16, "Sb")
            S.memset("pool", Sb[:], 0.0, writes=[Sb])
            names16 = ["kT", "qT", "ktok", "v", "TG", "A", "AT", "Mb", "MbT", "P", "vb", "kbg", "DT", "kd", "wT", "qg0", "qg1", "vnew"]
            names32 = ["u", "Eg", "osb", "oprev", "gate", "Stmp"]
            slots = []
            for d in range(2):
                sl = {nm: sb(st, [128, 4, 128], BF16, nm) for nm in names16}
                sl.update({nm: sb(st, [128, 4, 128], F32, nm) for nm in names32})
                sl["E"] = sb(st, [128, 16], F32, "E"); sl["bg2"] = sb(st, [128, 4], F32, "bg2"); sl["lnb"] = sb(st, [128, 4], F32, "lnbeta")
                sl["ss"] = sb(st, [128, 8], F32, "ss"); sl["junk"] = sb(st, [128, 128], F32, "junk")
                S.memset("pool", sl["qg0"][:], 0.0, writes=[sl["qg0"]]); S.memset("pool", sl["qg1"][:], 0.0, writes=[sl["qg1"]])
                slots.append(sl)
            visited = set()
            identbc = ident[:].unsqueeze(1).to_broadcast([128, 4, 128])
            bnbc = bnorm[:].unsqueeze(1).to_broadcast([128, 4, 128])

            def bc4(ap):
                return ap.unsqueeze(2).to_broadcast([128, 4, 128])

            def f2(t):
                return t[:].rearrange("p h d -> p (h d)")

            def hs(h):
                return slice(h * 128, (h + 1) * 128)

            ringi = [0, 0]

            def item(ti, d, sl):
                def pb():
                    r = PB[4 * d + ringi[d] % 3]
                    ringi[d] += 1
                    return r
                mi, ms, negS, negI = (0, 1, 4, 5) if d == 0 else (2, 3, 6, 7)
                gd = BG[:, ti, 8 + 4 * d:12 + 4 * d]
                bd = BG[:, ti, 4 * d:4 * d + 4]
                rows = slice(ti * 128, (ti + 1) * 128)
                kT, qT, ktok, v, TG, A, AT, P, DT = sl["kT"], sl["qT"], sl["ktok"], sl["v"], sl["TG"], sl["A"], sl["AT"], sl["P"], sl["DT"]
                E = sl["E"]
                S.dma(kT[:], kT_s.rearrange("h d t -> d h t")[:, :, rows], writes=[kT])
                S.dma(qT[:], qT_s.rearrange("h d t -> d h t")[:, :, rows], writes=[qT])
                S.dma(f2(ktok), k_s[rows, :], writes=[ktok])
                S.dma(f2(v), v_s[rows, :], writes=[v])
                ps_ = pb()
                S.mm(ps_[:, 0:4], MK[:, mi, :], gd, True, True, reads=[(BG, ti)], writes=[ps_])
                S.mm(ps_[:, 4:8], MK[:, ms, :], gd, True, True, reads=[(BG, ti)], writes=[ps_])
                S.mm(ps_[:, 8:12], MK[:, 8, :], gd, True, True, reads=[(BG, ti)], writes=[ps_])
                S.mm(ps_[:, 12:16], MK[:, 9, :], gd, True, True, reads=[(BG, ti)], writes=[ps_])
                S.act(E[:], ps_[:, 0:16], AF.Exp, reads=[ps_], writes=[E])
                S.tt("dve", sl["bg2"][:], bd, E[:, 0:4], ALU.mult, reads=[E, (BG, ti)], writes=[sl["bg2"]])
                S.act(sl["lnb"][:], bd, AF.Ln, reads=[(BG, ti)], writes=[sl["lnb"]])
                S.tt("dve", TG[:], MK[:, mi, :].unsqueeze(1).to_broadcast([128, 4, 128]), bc4(gd), ALU.mult, reads=[(BG, ti)], writes=[TG])
                yield
                pKK = pb(); pL = pb()
                for h in range(4):
                    S.mm(pKK[:, hs(h)], kT[:, h, :], kT[:, h, :], True, True, reads=[kT], writes=[pKK])
                for h in range(4):
                    S.mm(pL[:, hs(h)], TG[:, h, :], MKb[:, ms, :], True, False, reads=[TG], writes=[pL])
                    S.mm(pL[:, hs(h)], identb[:], MKb[:, negS, :], False, True, reads=[TG], writes=[pL])
                for h in range(4):
                    S.act(A[:, h, :], pL[:, hs(h)], AF.Exp, bias=sl["lnb"][:, h:h + 1], reads=[pL, sl["lnb"]], writes=[A])
                S.tt("dve", f2(A), pKK[:], f2(A), ALU.mult, reads=[pKK, A], writes=[A])
                yield
                pAT = pb()
                for h in range(4):
                    S.mm(pAT[:, hs(h)], A[:, h, :], identb[:], True, True, reads=[A], writes=[pAT])
                S.copy("act", f2(AT), pAT[:], reads=[pAT], writes=[AT])
                S.stt("dve", P[:], pAT[:].rearrange("p (h d) -> p h d", h=4), -1.0, identbc, ALU.mult, ALU.add, reads=[pAT], writes=[P])
                pLT = pb(); pQK = pb()
                for h in range(4):
                    S.mm(pLT[:, hs(h)], MKb[:, ms, :], TG[:, h, :], True, False, reads=[TG], writes=[pLT])
                    S.mm(pLT[:, hs(h)], identb[:], MKb[:, negI, :], False, True, reads=[TG], writes=[pLT])
                for h in range(4):
                    S.mm(pQK[:, hs(h)], kT[:, h, :], qT[:, h, :], True, True, reads=[kT, qT], writes=[pQK])
                S.act(f2(DT), pLT[:], AF.Exp, reads=[pLT], writes=[DT])
                S.tt("dve", f2(DT), pQK[:], f2(DT), ALU.mult, reads=[pQK, DT], writes=[DT])
                yield
                N_, NT_ = AT, A
                Y_, YT_ = sl["Mb"], sl["MbT"]
                prevYT = None
                for lev in range(6):
                    pP = None
                    if prevYT is not None:
                        pP = pb()
                        for h in range(4):
                            S.mm(pP[:, hs(h)], prevYT[:, h, :], P[:, h, :], True, True, reads=[prevYT, P], writes=[pP])
                    if lev < 5:
                        last = lev == 4
                        pMT = pb()
                        pM = None if last else pb()
                        for h in range(4):
                            if not last:
                                S.mm(pM[:, hs(h)], NT_[:, h, :], N_[:, h, :], True, True, reads=[N_, NT_], writes=[pM])
                            S.mm(pMT[:, hs(h)], N_[:, h, :], NT_[:, h, :], True, True, reads=[N_, NT_], writes=[pMT])
                        if not last:
                            S.copy("act", f2(Y_), pM[:], reads=[pM], writes=[Y_])
                        S.copy("act" if (last or lev % 2 == 1) else "dve", f2(YT_), pMT[:], reads=[pMT], writes=[YT_])
                    if pP is not None:
                        S.tt("dve", f2(P), f2(P), pP[:], ALU.add, reads=[pP, P], writes=[P])
                    if lev < 5:
                        prevYT = YT_
                        N_, NT_, Y_, YT_ = Y_, YT_, N_, NT_
                    yield
                vb, kbg, kd, u, wT, Eg, vnew = sl["vb"], sl["kbg"], sl["kd"], sl["u"], sl["wT"], sl["Eg"], sl["vnew"]
                S.tt("dve", vb[:], v[:], bc4(bd), ALU.mult, reads=[v, (BG, ti)], writes=[vb])
                S.tt("pool", kbg[:], ktok[:], bc4(sl["bg2"][:]), ALU.mult, reads=[ktok, sl["bg2"]], writes=[kbg])
                S.tt("pool", kd[:], ktok[:], bc4(E[:, 4:8]), ALU.mult, reads=[ktok, E], writes=[kd])
                pu = pb(); pw = pb(); pE = pb()
                for h in range(4):
                    S.mm(pu[:, hs(h)], P[:, h, :], vb[:, h, :], True, True, reads=[P, vb], writes=[pu])
                for h in range(4):
                    S.mm(pw[:, hs(h)], kbg[:, h, :], P[:, h, :], True, True, reads=[P, kbg], writes=[pw])
                for h in range(4):
                    S.mm(pE[:, hs(h)], onesb[:], TG[:, h, :], True, True, reads=[TG], writes=[pE])
                S.copy("act", f2(u), pu[:], reads=[pu], writes=[u])
                S.copy("dve", f2(wT), pw[:], reads=[pw], writes=[wT])
                S.act(f2(Eg), pE[:], AF.Exp, reads=[pE], writes=[Eg])
                S.tt("dve", sl["qg0"][:, :, 0:64], qT[:, :, 0:64], Eg[:, :, 0:64], ALU.mult, reads=[qT, Eg], writes=[sl["qg0"]])
                S.tt("pool", sl["qg1"][:, :, 64:128], qT[:, :, 64:128], Eg[:, :, 64:128], ALU.mult, reads=[qT, Eg], writes=[sl["qg1"]])
                yield
                po = PB[4 * d + 3]
                S.memset("dve", po[:], 0.0, writes=[po])
                for c in ([0, 1] if d == 0 else [1, 0]):
                    Rr = slice(64 * c, 64 * c + 64)
                    pws = pb()
                    for h in range(4):
                        S.mm(pws[:, hs(h)], wT[:, h, :], Sb[:, d, h, :], True, True, reads=[wT, (Sb, d)], writes=[pws])
                    S.tt("pool", sl["Stmp"][:], Sst[:, d], bc4(E[:, 8 + 4 * c:12 + 4 * c]), ALU.mult, reads=[E, (Sst, d)], writes=[sl["Stmp"]])
                    S.tt("dve", f2(vnew)[Rr, :], f2(u)[Rr, :], pws[Rr, :], ALU.subtract, reads=[u, pws], writes=[vnew])
                    qg = sl["qg0"] if c == 0 else sl["qg1"]
                    for h in range(4):
                        S.op("pe", lambda e, h=h, qg=qg: e.matmul(po[:, hs(h)], qg[:, h, :], Sb[:, d, h, :], start=False, stop=False, skip_group_check=True),
                             reads=[qg, (Sb, d)], writes=[po])
                    pS = pb()
                    for h in range(4):
                        S.mm(pS[:, hs(h)], kd[Rr, h, :], vnew[Rr, h, :], True, True, reads=[kd, vnew], writes=[pS])
                    S.tt("dve", Sb[:, d].rearrange("p h d -> p (h d)"), f2(sl["Stmp"]), pS[:], ALU.add, reads=[pS, sl["Stmp"]], writes=[(Sb, d)])
                    S.tt("dve", Sst[:, d].rearrange("p h d -> p (h d)"), f2(sl["Stmp"]), pS[:], ALU.add, reads=[pS, sl["Stmp"], (Sst, d)], writes=[(Sst, d)])
                    yield
                for h in range(4):
                    S.op("pe", lambda e, h=h: e.matmul(po[:, hs(h)], DT[:, h, :], vnew[:, h, :], start=False, stop=False, skip_group_check=True),
                         reads=[DT, vnew], writes=[po])
                osb, oprev, gate = sl["osb"], sl["oprev"], sl["gate"]
                if ti not in visited:
                    visited.add(ti)
                    S.copy("act", f2(osb), po[:], reads=[po], writes=[osb])
                    S.dma(o_s[rows, :], f2(osb), reads=[osb], writes=[("o_s", ti)])
                else:
                    ss = sl["ss"]
                    S.dma(f2(oprev), o_s[rows, :], reads=[("o_s", ti)], writes=[oprev])
                    S.dma(f2(gate), gate_s[rows, :], writes=[gate])
                    S.tt("dve", f2(osb), po[:], f2(oprev), ALU.add, reads=[po, oprev], writes=[osb])
                    S.memset("pool", ss[:], 0.0, writes=[ss])
                    for h in range(4):
                        S.act(sl["junk"][:], osb[:, h, :], AF.Square, accum_out=ss[:, h:h + 1], reads=[osb, ss], writes=[sl["junk"], ss])
                    S.act(ss[:, 4:8], ss[:, 0:4], AF.Sqrt, scale=1.0 / 128, bias=1e-6, reads=[ss], writes=[ss])
                    S.op("dve", lambda e: e.reciprocal(ss[:, 4:8], ss[:, 4:8]), reads=[ss], writes=[ss])
                    S.tt("dve", osb[:], osb[:], bc4(ss[:, 4:8]), ALU.mult, reads=[osb, ss], writes=[osb])
                    S.tt("pool", osb[:], osb[:], gate[:], ALU.mult, reads=[osb, gate], writes=[osb])
                    S.tt("pool", osb[:], osb[:], bnbc, ALU.mult, reads=[osb], writes=[osb])
                    pT = pb()
                    for h in range(4):
                        S.tr(pT[:, hs(h)], osb[:, h, :], ident[:], reads=[osb], writes=[pT])
                    S.copy("act", catT[:, 4:8, tcol(ti):tcol(ti) + 128], pT[:].rearrange("p (h t) -> p h t", h=4), reads=[pT], writes=[(catT, "B", ti)])

            fwd_order = list(range(18))
            bwd_order = [1, 0] + list(range(17, 1, -1))
            for s_i in range(18):
                gens = [item(fwd_order[s_i], 0, slots[0]), item(bwd_order[s_i], 1, slots[1])]
                while gens:
                    for g in list(gens):
                        try:
                            next(g)
                        except StopIteration:
                            gens.remove(g)

        def layer0(b):
            tiles = list(range(18))
            with ExitStack() as stL:
                gates = sb(stL, [128, 18, 16], F32, "gates")
                with ExitStack() as stC:
                    catT = sb(stC, [128, 8, 2307], BF16, "catT")
                    BG = sb(stC, [128, 18, 16], F32, "BG")
                    with ExitStack() as st:
                        hT = sb(st, [128, 8, 2307], BF16, "hT")
                        for c in (0, 257, 2306):
                            S.memset("pool", hT[:, :, c:c + 1], 0.0, writes=[(hT, "pad", c)])
                        if want('A'):
                            with ExitStack() as st2:
                                phase_A(0, b, hT, tiles, st2)
                                S.barrier()
                        if want('B'):
                            with ExitStack() as st2:
                                phase_B(b, hT, catT, st2)
                                S.barrier()
                        if want('C'):
                            with ExitStack() as st2:
                                phase_C(b, hT, BG, st2)
                                S.barrier()
                    if want('D'):
                        with ExitStack() as st:
                            phase_D(b, BG, catT, st)
                            S.barrier()
                    post_E(0, b, catT, tiles, gates)
                post_FG(0, b, tiles, gates)

        LBLK = [(258 + 512 * j, 512, 256 + 512 * j, 512 * j) for j in range(4)]

        def rearr_w(ap):
            return ap.rearrange("(kc p) n -> p kc n", p=128)

        def l1_proj_mla(b, hT, dqn, dkvn, KR, st):
            wdq = sb(st, [128, 8, 384], BF16, "wdq"); S.dma(wdq[:], rearr_w(odwin_b[:, 768:1152]), writes=[wdq])
            wdkv = sb(st, [128, 8, 256], BF16, "wdkv"); S.dma(wdkv[:], rearr_w(odwin_b[:, 1152:1408]), writes=[wdkv])
            wk = sb(st, [128, 8, 96], BF16, "wk"); S.dma(wk[:], rearr_w(wkr_b), writes=[wk])
            wks = sb(st, [128, 8, 96], BF16, "wks"); S.dma(wks[:], rearr_w(wkrsw_b), writes=[wks])
            r32 = sb(st, [96, 2, LT], F32, "r32"); S.dma(r32[:], rope32_d.rearrange("a p t -> p a t"), writes=[r32])
            sq = [sb(st, [128, 512], F32, "sq1") for _ in range(3)]
            rinv = sb(st, [128, 512], F32, "rinv1")
            t1 = sb(st, [96, 512], F32, "t1a"); t2 = sb(st, [96, 512], F32, "t2a")

            def rms_proj(wt, nch, normT, dst, c0, w, d0, inv_n):
                pp = [pb() for _ in range(nch)]
                for c in range(nch):
                    for kc in range(8):
                        S.mm(pp[c][:, 0:w], wt[:, kc, c * 128:(c + 1) * 128], hT[:, kc, c0:c0 + w], kc == 0, kc == 7, reads=[wt], writes=[pp[c]])
                    S.act(sq[c][:, 0:w], pp[c][:, 0:w], AF.Square, reads=[pp[c]], writes=[sq[c]])
                pss = pb()
                for c in range(nch):
                    S.mm(pss[:, 0:w], ones[:], sq[c][:, 0:w], c == 0, c == nch - 1, reads=[sq[c]], writes=[pss])
                S.act(rinv[:, 0:w], pss[:, 0:w], AF.Sqrt, scale=inv_n, bias=1e-6, reads=[pss], writes=[rinv])
                S.op("dve", lambda e: e.reciprocal(rinv[:, 0:w], rinv[:, 0:w]), reads=[rinv], writes=[rinv])
                for c in range(nch):
                    S.stt("dve", dst[:, c, d0:d0 + w], pp[c][:, 0:w], normT[:, c:c + 1], rinv[:, 0:w], ALU.mult, ALU.mult,
                          reads=[pp[c], rinv], writes=[(dst, c, d0)])

            for bi, (c0, w, t0) in enumerate(BLOCKS):
                rms_proj(wdkv, 2, kvnT, dkvn, c0, w, t0, 1.0 / 256)
                pk = pb(); pks = pb()
                for kc in range(8):
                    S.mm(pk[0:96, 0:w], wk[:, kc, :], hT[:, kc, c0:c0 + w], kc == 0, kc == 7, reads=[wk], writes=[pk])
                if bi == 0:
                    S.copy("act", KR[64:96, t0:t0 + w], pk[64:96, 0:w], reads=[pk], writes=[(KR, t0)])
                else:
                    l0 = t0 - 256
                    for kc in range(8):
                        S.mm(pks[0:96, 0:w], wks[:, kc, :], hT[:, kc, c0:c0 + w], kc == 0, kc == 7, reads=[wks], writes=[pks])
                    S.tt("dve", t1[64:96, 0:w], pk[64:96, 0:w], r32[64:96, 0, l0:l0 + w], ALU.mult, reads=[pk, r32], writes=[t1])
                    S.tt("dve", t2[64:96, 0:w], pks[64:96, 0:w], r32[64:96, 1, l0:l0 + w], ALU.mult, reads=[pks, r32], writes=[t2])
                    S.tt("dve", KR[64:96, t0:t0 + w], t1[64:96, 0:w], t2[64:96, 0:w], ALU.add, reads=[t1, t2], writes=[(KR, t0)])
                    rms_proj(wdq, 3, qnT, dqn, c0, w, l0, 1.0 / 384)

        def l1_proj_win(b, hT, CQ, CK, CV, st):
            wq = sb(st, [128, 8, 512], BF16, "wq"); S.dma(wq[:], rearr_w(odwin_b[:, 0:512]), writes=[wq])
            wqs = sb(st, [128, 8, 512], BF16, "wqs"); S.dma(wqs[:], rearr_w(odwinsw_b[:, 0:512]), writes=[wqs])
            wkk = sb(st, [128, 8, 128], BF16, "wkk"); S.dma(wkk[:], rearr_w(odwin_b[:, 512:640]), writes=[wkk])
            wkks = sb(st, [128, 8, 128], BF16, "wkks"); S.dma(wkks[:], rearr_w(odwinsw_b[:, 512:640]), writes=[wkks])
            wv = sb(st, [128, 8, 128], BF16, "wv"); S.dma(wv[:], rearr_w(odwin_b[:, 640:768]), writes=[wv])
            r64 = sb(st, [64, 2, LT], F32, "r64"); S.dma(r64[:], rope64_d.rearrange("a p t -> p a t"), writes=[r64])
            t1 = [sb(st, [64, 512], F32, "t1w") for _ in range(2)]; t2 = [sb(st, [64, 512], F32, "t2w") for _ in range(2)]
            S.memset("pool", CV[:, :, :, 64:65], 1.0, writes=[(CV, "ones")])
            ii = 0
            for bi, (c0, w, t0) in enumerate(BLOCKS):
                l0 = t0 - 256
                for g in range(2):
                    pk = pb(); pks = pb()
                    for kc in range(8):
                        S.mm(pk[0:64, 0:w], wkk[:, kc, g * 64:(g + 1) * 64], hT[:, kc, c0:c0 + w], kc == 0, kc == 7, reads=[wkk], writes=[pk])
                    if bi == 0:
                        S.copy("act", CK[:, g, t0:t0 + w], pk[0:64, 0:w], reads=[pk], writes=[(CK, g, t0)])
                    else:
                        for kc in range(8):
                            S.mm(pks[0:64, 0:w], wkks[:, kc, g * 64:(g + 1) * 64], hT[:, kc, c0:c0 + w], kc == 0, kc == 7, reads=[wkks], writes=[pks])
                        a1 = t1[ii % 2]; a2 = t2[ii % 2]; ii += 1
                        S.tt("dve", a1[:, 0:w], pk[0:64, 0:w], r64[:, 0, l0:l0 + w], ALU.mult, reads=[pk, r64], writes=[a1])
                        S.tt("dve", a2[:, 0:w], pks[0:64, 0:w], r64[:, 1, l0:l0 + w], ALU.mult, reads=[pks, r64], writes=[a2])
                        S.tt("dve", CK[:, g, t0:t0 + w], a1[:, 0:w], a2[:, 0:w], ALU.add, reads=[a1, a2], writes=[(CK, g, t0)])
                for j in range(w // 128):
                    ti = t0 // 128 + j
                    pv = pb()
                    for kc in range(8):
                        S.mm(pv[:, 0:128], hT[:, kc, tcol(ti):tcol(ti) + 128], wv[:, kc, :], kc == 0, kc == 7, reads=[wv], writes=[pv])
                    S.copy("act", CV[:, ti, :, 0:64], pv[:, 0:128].rearrange("p (g d) -> p g d", g=2), reads=[pv], writes=[(CV, ti)])
                if bi > 0:
                    for h in range(8):
                        pq = pb(); pqs = pb()
                        for kc in range(8):
                            S.mm(pq[0:64, 0:w], wq[:, kc, h * 64:(h + 1) * 64], hT[:, kc, c0:c0 + w], kc == 0, kc == 7, reads=[wq], writes=[pq])
                        for kc in range(8):
                            S.mm(pqs[0:64, 0:w], wqs[:, kc, h * 64:(h + 1) * 64], hT[:, kc, c0:c0 + w], kc == 0, kc == 7, reads=[wqs], writes=[pqs])
                        a1 = t1[ii % 2]; a2 = t2[ii % 2]; ii += 1
                        S.tt("dve", a1[:, 0:w], pq[0:64, 0:w], r64[:, 0, l0:l0 + w], ALU.mult, reads=[pq, r64], writes=[a1])
                        S.tt("dve", a2[:, 0:w], pqs[0:64, 0:w], r64[:, 1, l0:l0 + w], ALU.mult, reads=[pqs, r64], writes=[a2])
                        S.tt("dve", CQ[:, h, l0:l0 + w], a1[:, 0:w], a2[:, 0:w], ALU.add, reads=[a1, a2], writes=[(CQ, h, l0)])

        def l1_attn_win(b, CQ, CK, CV, catT, st):
            PT = [sb(st, [128, 512], BF16, "PTw") for _ in range(2)]
            Yw = [sb(st, [128, 512], F32, "Yw") for _ in range(2)]
            den = sb(st, [128, 8], F32, "denw")
            ring = [0]

            def rb():
                r = PB[ring[0] % 6]
                ring[0] += 1
                return r
            seq = []
            for qt in range(16):
                for g in range(2):
                    keys = [(0, None), (1, None)]
                    if qt >= 1:
                        keys.append((qt + 1, 10))
                    keys.append((qt + 2, None))
                    if qt <= 14:
                        keys.append((qt + 3, 11))
                    for ki, (kt, mk) in enumerate(keys):
                        seq.append((qt, g, kt, mk, ki == 0, ki == len(keys) - 1))
            pss = {}

            def issue_S(i):
                qt, g, kt, mk, first, last = seq[i]
                ps = rb()
                S.mm(ps[:, 0:512], CK[:, g, kt * 128:(kt + 1) * 128], CQ[:, 4 * g:4 * g + 4, qt * 128:(qt + 1) * 128], True, True, reads=[], writes=[ps])
                pss[i] = ps
            issue_S(0)
            io = 0
            po = None; po3 = None
            for i, (qt, g, kt, mk, first, last) in enumerate(seq):
                yw = Yw[qt % 2]
                if first:
                    po = PB[6 + io % 2]; io += 1
                    po3 = po[:, 0:260].rearrange("p (a b) -> p a b", a=4)
                    S.memset("dve", po[:, 0:260], 0.0, writes=[po])
                if i + 1 < len(seq):
                    issue_S(i + 1)
                ps = pss.pop(i)
                pt = PT[i % 2]
                S.act(pt[:], ps[:, 0:512], AF.Exp, scale=0.125, reads=[ps], writes=[pt])
                if mk is not None:
                    pt3 = pt[:].rearrange("p (a b) -> p a b", a=4)
                    S.tt("dve", pt3, pt3, MK[:, mk, :].unsqueeze(1).to_broadcast([128, 4, 128]), ALU.mult, reads=[pt], writes=[pt])
                for hh in range(4):
                    S.op("pe", lambda e, hh=hh, pt=pt, kt=kt, po=po, g=g: e.matmul(po[:, hh * 65:(hh + 1) * 65], pt[:, hh * 128:(hh + 1) * 128], CV[:, kt, g, :],
                                                                                 start=False, stop=False, skip_group_check=True), reads=[pt], writes=[po])
                if last:
                    S.tt("dve", den[:, 0:4], po3[:, :, 64], expsink[:, 4 * g:4 * g + 4], ALU.add, reads=[po], writes=[den])
                    S.op("dve", lambda e: e.reciprocal(den[:, 4:8], den[:, 0:4]), reads=[den], writes=[den])
                    S.tt("dve", yw[:, g * 256:(g + 1) * 256].rearrange("p (a b) -> p a b", a=4), po3[:, :, 0:64],
                         den[:, 4:8].unsqueeze(2).to_broadcast([128, 4, 64]), ALU.mult, reads=[po, den], writes=[yw])
                    if g == 1:
                        pT = rb()
                        for j in range(4):
                            S.tr(pT[:, j * 128:(j + 1) * 128], yw[:, j * 128:(j + 1) * 128], ident[:], reads=[yw], writes=[pT])
                        S.copy("act", catT[:, 0:4, qt * 128:(qt + 1) * 128], pT[:, 0:512].rearrange("p (a b) -> p a b", a=4), reads=[pT], writes=[(catT, "w", qt)])

        def l1_up_mla(b, dqn, dkvn, KR, MQ, MKk, MV, st):
            wuq = sb(st, [128, 3, 768], BF16, "wuq"); S.dma(wuq[:], wuq_b.rearrange("(c p) n -> p c n", p=128), writes=[wuq])
            wuqs = sb(st, [128, 3, 768], BF16, "wuqs"); S.dma(wuqs[:], wuqsw_b.rearrange("(c p) n -> p c n", p=128), writes=[wuqs])
            wukv = sb(st, [128, 2, 1024], BF16, "wukv"); S.dma(wukv[:], wukv_b.rearrange("(c p) n -> p c n", p=128), writes=[wukv])
            r32 = sb(st, [96, 2, LT], F32, "r32b"); S.dma(r32[:], rope32_d.rearrange("a p t -> p a t"), writes=[r32])
            t1 = [sb(st, [96, 512], F32, "t1m") for _ in range(2)]; t2 = [sb(st, [96, 512], F32, "t2m") for _ in range(2)]
            S.memset("pool", MV[:, :, :, 64:65], 1.0, writes=[(MV, "ones")])
            ii = 0
            for (c0, w, t0, l0) in LBLK:
                for h in range(8):
                    pq = pb(); pqs = pb()
                    for c in range(3):
                        S.mm(pq[0:96, 0:w], wuq[:, c, h * 96:(h + 1) * 96], dqn[:, c, l0:l0 + w], c == 0, c == 2, reads=[wuq], writes=[pq])
                    for c in range(3):
                        S.mm(pqs[0:96, 0:w], wuqs[:, c, h * 96:(h + 1) * 96], dqn[:, c, l0:l0 + w], c == 0, c == 2, reads=[wuqs], writes=[pqs])
                    S.copy("act", MQ[0:64, h, l0:l0 + w], pq[0:64, 0:w], reads=[pq], writes=[(MQ, h, l0, 0)])
                    a1 = t1[ii % 2]; a2 = t2[ii % 2]; ii += 1
                    S.tt("dve", a1[64:96, 0:w], pq[64:96, 0:w], r32[64:96, 0, l0:l0 + w], ALU.mult, reads=[pq, r32], writes=[a1])
                    S.tt("dve", a2[64:96, 0:w], pqs[64:96, 0:w], r32[64:96, 1, l0:l0 + w], ALU.mult, reads=[pqs, r32], writes=[a2])
                    S.tt("dve", MQ[64:96, h, l0:l0 + w], a1[64:96, 0:w], a2[64:96, 0:w], ALU.add, reads=[a1, a2], writes=[(MQ, h, l0, 1)])
            for (c0, w, t0) in BLOCKS:
                for h in range(8):
                    pk = pb()
                    for c in range(2):
                        S.mm(pk[0:64, 0:w], wukv[:, c, h * 128:h * 128 + 64], dkvn[:, c, t0:t0 + w], c == 0, c == 1, reads=[wukv], writes=[pk])
                    if h % 2 == 0:
                        S.copy("act", MKk[0:64, h, t0:t0 + w], pk[0:64, 0:w], reads=[pk], writes=[(MKk, h, t0, 0)])
                    else:
                        S.copy("dve", MKk[0:64, h, t0:t0 + w], pk[0:64, 0:w], reads=[pk], writes=[(MKk, h, t0, 0)])
                    S.copy("pool", MKk[64:96, h, t0:t0 + w], KR[64:96, t0:t0 + w], reads=[], writes=[(MKk, h, t0, 1)])
                for j in range(w // 128):
                    ti = t0 // 128 + j
                    pv = pb()
                    wv3 = wukv[:].rearrange("p c (h x) -> p c h x", h=8)
                    for c in range(2):
                        S.mm(pv[:, 0:512], dkvn[:, c, ti * 128:(ti + 1) * 128], wv3[:, c, :, 64:128], c == 0, c == 1, reads=[wukv], writes=[pv])
                    S.copy("act", MV[:, ti, :, 0:64], pv[:, 0:512].rearrange("p (h x) -> p h x", h=8), reads=[pv], writes=[(MV, ti)])

        def l1_attn_mla(b, MQ, MKk, MV, catT, st):
            PT = [sb(st, [128, 1024], BF16, "PTm") for _ in range(3)]
            Ym = sb(st, [128, 4, 512], F32, "Ym")
            den = sb(st, [128, 8], F32, "denm")
            ring = [0]

            def rb():
                r = PB[ring[0] % 6]
                ring[0] += 1
                return r
            scale = 96.0 ** -0.5
            seq = [(qb, h, kp) for qb in range(4) for h in range(8) for kp in range(9)]
            pss = {}
            r2 = [0]

            def issue_S(i):
                qb, h, kp = seq[i]
                j = r2[0] % 3
                r2[0] += 1
                for half in range(2):
                    kt = 2 * kp + half
                    ps = PB[2 * j + half]
                    S.mm(ps[:, 0:512], MKk[:, h, kt * 128:(kt + 1) * 128], MQ[:, h, qb * 512:(qb + 1) * 512], True, True, reads=[], writes=[ps])
                pss[i] = j
            issue_S(0)
            io = 0
            po = None; po3 = None
            for i, (qb, h, kp) in enumerate(seq):
                if kp == 0:
                    po = PB[6 + io % 2]; io += 1
                    po3 = po[:, 0:260].rearrange("p (a b) -> p a b", a=4)
                    S.memset("dve", po[:, 0:260], 0.0, writes=[po])
                defer = (h == 7 and kp == 8)
                if i + 1 < len(seq) and not defer:
                    issue_S(i + 1)
                j = pss.pop(i)
                pt = PT[i % 3]
                S.act(pt[:], P2[j][:, 0:1024], AF.Exp, scale=scale, reads=[PB[2 * j], PB[2 * j + 1]], writes=[pt])
                for half in range(2):
                    kt = 2 * kp + half
                    for qs in range(4):
                        S.op("pe", lambda e, qs=qs, pt=pt, kt=kt, po=po, h=h, half=half: e.matmul(po[:, qs * 65:(qs + 1) * 65], pt[:, half * 512 + qs * 128:half * 512 + (qs + 1) * 128], MV[:, kt, h, :],
                                                                                                 start=False, stop=False, skip_group_check=True), reads=[pt], writes=[po])
                if kp == 8:
                    S.op("dve", lambda e, po3=po3: e.reciprocal(den[:, 0:4], po3[:, :, 64]), reads=[po], writes=[den])
                    S.tt("dve", Ym[:, :, h * 64:(h + 1) * 64], po3[:, :, 0:64], den[:, 0:4].unsqueeze(2).to_broadcast([128, 4, 64]), ALU.mult,
                         reads=[po, den], writes=[(Ym, h)])
                    if h == 7:
                        for qs in range(4):
                            pT = PB[2 * (r2[0] % 3)]
                            r2[0] += 1
                            for jj in range(4):
                                S.tr(pT[:, jj * 128:(jj + 1) * 128], Ym[:, qs, jj * 128:(jj + 1) * 128], ident[:], reads=[(Ym, hh) for hh in range(8)], writes=[pT])
                            qt = qb * 4 + qs
                            S.copy("act", catT[:, 4:8, qt * 128:(qt + 1) * 128], pT[:, 0:512].rearrange("p (a b) -> p a b", a=4), reads=[pT], writes=[(catT, "m", qt)])
                if defer and i + 1 < len(seq):
                    issue_S(i + 1)

        def layer1(b):
            tiles = list(range(2, 18))
            with ExitStack() as stL:
                gates = sb(stL, [128, 16, 16], F32, "gates1")
                with ExitStack() as stC:
                    catT = sb(stC, [128, 8, LT], BF16, "catT1")
                    dqn = sb(stC, [128, 3, LT], BF16, "dqn"); dkvn = sb(stC, [128, 2, T], BF16, "dkvn"); KR = sb(stC, [96, T], BF16, "KR")
                    with ExitStack() as stW:
                        CQ = sb(stW, [64, 8, LT], BF16, "CQ"); CK = sb(stW, [64, 2, T], BF16, "CK"); CV = sb(stW, [128, 18, 2, 65], BF16, "CV")
                        with ExitStack() as st:
                            hT = sb(st, [128, 8, 2307], BF16, "hT1")
                            with ExitStack() as st2:
                                phase_A(1, b, hT, list(range(18)), st2)
                                S.barrier()
                            if want('L1P'):
                              with ExitStack() as st2:
                                l1_proj_mla(b, hT, dqn, dkvn, KR, st2)
                                S.barrier()
                            if want('L1W'):
                              with ExitStack() as st2:
                                l1_proj_win(b, hT, CQ, CK, CV, st2)
                                S.barrier()
                        if want('L1WA'):
                          with ExitStack() as st2:
                            l1_attn_win(b, CQ, CK, CV, catT, st2)
                            S.barrier()
                    with ExitStack() as stM:
                        MQ = sb(stM, [96, 8, LT], BF16, "MQ"); MKk = sb(stM, [96, 8, T], BF16, "MKk"); MV = sb(stM, [128, 18, 8, 65], BF16, "MV")
                        if want('L1U'):
                          with ExitStack() as st2:
                            l1_up_mla(b, dqn, dkvn, KR, MQ, MKk, MV, st2)
                            S.barrier()
                        if want('L1MA'):
                          with ExitStack() as st2:
                            l1_attn_mla(b, MQ, MKk, MV, catT, st2)
                            S.barrier()
                    if dbg:
                        S.dma(cat_dbg, catT[:], reads=[])
                        S.barrier()
                    if want('L1E'):
                        post_E(1, b, catT, tiles, gates)
                if want('L1F'):
                    post_FG(1, b, tiles, gates)


        for b in range(NB):
            layer0(b)
            if not only0 and want('L1A'):
                layer1(b)
        S.finish([])
    return nc


NEGV = -30000.0


def make_consts():
    p = np.arange(128)
    ch = p // 64
    same = ch[:, None] == ch[None, :]
    a = p[:, None]; bb = p[None, :]
    M = np.zeros((14, 128, 128), np.float32)
    M[0] = same & (a <= bb)
    M[1] = same & (a > bb)
    M[2] = same & (a >= bb)
    M[3] = same & (a < bb)
    M[4] = np.where(same & (a > bb), 0.0, NEGV)
    M[5] = np.where(same & (bb >= a), 0.0, NEGV)
    M[6] = np.where(same & (a < bb), 0.0, NEGV)
    M[7] = np.where(same & (bb <= a), 0.0, NEGV)
    M[8] = (a < 64) & (bb >= 0)
    M[9] = (a >= 64) & (bb >= 0)
    M[10] = bb <= a
    M[11] = a <= bb
    t = np.arange(LT)
    rows = (t // 64).astype(np.float32); cols = (t % 64).astype(np.float32)

    def ang(rot):
        nf = rot // 4
        inv = (10000.0 ** (-np.arange(nf, dtype=np.float32) / nf)).astype(np.float32)
        return np.concatenate([rows[:, None] * inv, cols[:, None] * inv], -1).astype(np.float32)
    a64 = ang(64); a32 = ang(32)
    r64 = np.zeros((2, 64, LT), np.float32)
    r64[0] = np.concatenate([np.cos(a64), np.cos(a64)], 1).T
    r64[1] = np.concatenate([-np.sin(a64), np.sin(a64)], 1).T
    r32 = np.zeros((2, 96, LT), np.float32)
    r32[0, :64] = 1.0
    r32[0, 64:] = np.concatenate([np.cos(a32), np.cos(a32)], 1).T
    r32[1, 64:] = np.concatenate([-np.sin(a32), np.sin(a32)], 1).T
    return M, r64, r32


def prep_shared(inp):
    f = lambda a: np.ascontiguousarray(a, dtype=np.float32)
    M, r64, r32 = make_consts()
    w1 = inp["od_w_in"][0]
    w1s = w1.copy()
    for h in range(8):
        b0 = h * 64
        w1s[:, b0:b0 + 32] = w1[:, b0 + 32:b0 + 64]; w1s[:, b0 + 32:b0 + 64] = w1[:, b0:b0 + 32]
    for g in range(2):
        b0 = 512 + g * 64
        w1s[:, b0:b0 + 32] = w1[:, b0 + 32:b0 + 64]; w1s[:, b0 + 32:b0 + 64] = w1[:, b0:b0 + 32]
    wkr = np.zeros((1024, 96), np.float32); wkrs = np.zeros((1024, 96), np.float32)
    wkr[:, 64:96] = w1[:, 1408:1440]
    wkrs[:, 64:80] = w1[:, 1424:1440]; wkrs[:, 80:96] = w1[:, 1408:1424]
    wuq = inp["od_d_wuq"][0]
    wuqs = wuq.copy()
    for h in range(8):
        b0 = h * 96 + 64
        wuqs[:, b0:b0 + 16] = wuq[:, b0 + 16:b0 + 32]; wuqs[:, b0 + 16:b0 + 32] = wuq[:, b0:b0 + 16]
    d = {
        "ada_w": f(inp["ada_w"]), "ada_b": f(inp["ada_b"]),
        "ln_g": f(inp["ln_g"].reshape(4, 1024)), "ln_b": f(inp["ln_b"].reshape(4, 1024)),
        "ev_w_in": f(inp["ev_w_in"][0]),
        "a_convT": f(inp["ev_a_conv"][0].reshape(3, 4, 128).transpose(2, 1, 0)),
        "ev_b_conv": f(inp["ev_b_conv"][0]),
        "alog": f(inp["ev_b_alog"].reshape(1, 8)), "dtbias": f(inp["ev_b_dtbias"].reshape(1, 8)),
        "bnorm": f(inp["ev_b_norm"].reshape(1, 128)), "ev_w_out": f(inp["ev_w_out"][0]),
        "od_w_in": f(w1), "od_w_in_sw": f(w1s), "wkr": f(wkr), "wkr_sw": f(wkrs),
        "sink": f(inp["od_c_sink"].reshape(1, 8)),
        "qnormT": f(inp["od_d_qnorm"][0].reshape(3, 128).T), "kvnormT": f(inp["od_d_kvnorm"][0].reshape(2, 128).T),
        "wuq": f(wuq), "wuq_sw": f(wuqs), "wukv": f(inp["od_d_wukv"][0]), "od_w_out": f(inp["od_w_out"][0]),
        "router_w": f(inp["router_w"]), "router_bias": f(inp["router_bias"].reshape(1, 16)),
        "moe_g": f(inp["moe_w_gate"].reshape(32768, 512)), "moe_u": f(inp["moe_w_up"].reshape(32768, 512)),
        "moe_d": f(inp["moe_w_down"].reshape(16384, 1024)),
        "idn": np.eye(128, dtype=np.float32), "masks": M, "rope64": r64, "rope32": r32,
    }
    return d


def prep_core(inp, shared, b0, NB):
    f = lambda a: np.ascontiguousarray(a, dtype=np.float32)
    cs = np.concatenate([inp["c"][b0:b0 + NB], inp["c_ctx"][None, :]], 0)
    csT = cs.reshape(NB + 1, 8, 128).transpose(2, 1, 0)
    d = dict(shared)
    d["x"] = f(inp["x"][b0:b0 + NB]); d["ctx"] = f(inp["ctx"][b0:b0 + NB]); d["csT"] = f(csT)
    return d


_NC_CACHE = {}


def kernel(**inputs):
    inp = {k: np.asarray(v) for k, v in inputs.items()}
    NBC = 4
    if "nc" not in _NC_CACHE:
        _NC_CACHE["nc"] = build(NBC)
    nc = _NC_CACHE["nc"]
    shared = prep_shared(inp)
    in_maps = [prep_core(inp, shared, NBC * i, NBC) for i in range(8)]
    res = run_bass_kernel_spmd(nc, in_maps, core_ids=list(range(8)))
    return np.ascontiguousarray(np.concatenate([np.asarray(r["out"]) for r in res.results], 0).astype(np.float32))
```

```python
from concourse.bass_utils import run_bass_kernel_spmd
import numpy as np
import concourse.bass as bass
import concourse.mybir as mybir
from contextlib import ExitStack

F32 = mybir.dt.float32
BF16 = mybir.dt.bfloat16
AF = mybir.ActivationFunctionType
ALU = mybir.AluOpType
AX = mybir.AxisListType

N_DMA_SEMS = 40
USE_NOPS = False
SEM_ROLL = 30000


class Tok:
    __slots__ = ("sem", "val", "eng")

    def __init__(self, sem, val, eng):
        self.sem = sem
        self.val = val
        self.eng = eng


class Sched:
    def __init__(self, nc, stack):
        self.nc = nc
        self.stack = stack
        self.cengs = ["pe", "act", "dve", "pool"]
        self.all = ["pe", "act", "dve", "pool", "sp"]
        self.prog = {e: [] for e in self.all}
        self.nsem = 0
        self.sem = {e: self._newsem() for e in self.cengs}
        self.cnt = {e: 0 for e in self.cengs}
        self.waited = {e: {} for e in self.all}
        self.dsem = [self._newsem() for _ in range(N_DMA_SEMS)]
        self.dcnt = [0] * N_DMA_SEMS
        self.drr = 0
        self.res = {}
        self.pend = {e: {} for e in self.all}
        self.excl = set()
        self.old = []

    def _newsem(self):
        self.nsem += 1
        return self.stack.enter_context(self.nc.semaphore("s%d" % self.nsem))

    def _st(self, r):
        if isinstance(r, tuple):
            k = tuple(x if isinstance(x, (str, int)) else id(x) for x in r)
        elif isinstance(r, str):
            k = r
        else:
            k = id(r)
        st = self.res.get(k)
        if st is None:
            st = {"w": None, "r": {}}
            self.res[k] = st
        return st

    def _emit(self, eng, fn, reads, writes, dma):
        if self.excl:
            ex = [r for r in reads if (not isinstance(r, (tuple, str))) and id(r) in self.excl]
            if ex:
                reads = [r for r in reads if not ((not isinstance(r, (tuple, str))) and id(r) in self.excl)]
                writes = list(writes) + ex
        deps = []
        for r in reads:
            st = self._st(r)
            if st["w"] is not None:
                deps.append(st["w"])
        for w in writes:
            st = self._st(w)
            if st["w"] is not None:
                deps.append(st["w"])
            deps.extend(st["r"].values())
        need = {}
        for t in deps:
            if t.eng == eng and eng == "pe" and not dma:
                continue
            cur = need.get(id(t.sem))
            if cur is None or cur[1] < t.val:
                need[id(t.sem)] = (t.sem, t.val)
        if self.pend[eng]:
            for sid, (s_, v_) in self.pend[eng].items():
                cur = need.get(sid)
                if cur is None or cur[1] < v_:
                    need[sid] = (s_, v_)
            self.pend[eng] = {}
        if dma:
            i = self.drr
            self.drr = (self.drr + 1) % N_DMA_SEMS
            prev = self.dcnt[i]
            if prev > 0:
                cur = need.get(id(self.dsem[i]))
                if cur is None or cur[1] < prev:
                    need[id(self.dsem[i])] = (self.dsem[i], prev)
            self.dcnt[i] += 16
            tok = Tok(self.dsem[i], self.dcnt[i], "dma")
            inc = (self.dsem[i], 16)
        else:
            if self.cnt[eng] >= SEM_ROLL:
                self.old.append((self.sem[eng], self.cnt[eng]))
                self.sem[eng] = self._newsem()
                self.cnt[eng] = 0
            self.cnt[eng] += 1
            tok = Tok(self.sem[eng], self.cnt[eng], eng)
            inc = (self.sem[eng], 1)
        waits = []
        wd = self.waited[eng]
        for sid, (s, v) in need.items():
            if wd.get(sid, 0) >= v:
                continue
            wd[sid] = v
            waits.append((s, v))
        for r in reads:
            self._st(r)["r"][id(tok.sem)] = tok
        for w in writes:
            st = self._st(w)
            st["w"] = tok
            st["r"] = {}
        self.prog[eng].append((waits, fn, inc))
        return tok

    def op(self, eng, fn, reads=(), writes=()):
        return self._emit(eng, fn, reads, writes, False)

    def all_tokens(self):
        toks = {}
        for e in self.cengs:
            if self.cnt[e] > 0:
                toks[id(self.sem[e])] = (self.sem[e], self.cnt[e])
        for i in range(N_DMA_SEMS):
            if self.dcnt[i] > 0:
                toks[id(self.dsem[i])] = (self.dsem[i], self.dcnt[i])
        return toks

    def barrier(self):
        toks = self.all_tokens()
        for e in self.all:
            self.pend[e] = dict(toks)
        self.res = {}

    def dma(self, out, in_, reads=(), writes=(), q="sp", **kw):
        return self._emit(q, lambda e: e.dma_start(out=out, in_=in_, **kw), reads, writes, True)

    def mm(self, out, lhsT, rhs, start, stop, reads=(), writes=()):
        return self.op("pe", lambda e: e.matmul(out, lhsT, rhs, start=start, stop=stop), reads, writes)

    def tr(self, out, in_, ident, reads=(), writes=()):
        return self.op("pe", lambda e: e.transpose(out, in_, ident), reads, writes)

    def act(self, out, in_, func, bias=None, scale=None, accum_out=None, reads=(), writes=(), eng="act"):
        kw = {}
        if bias is not None:
            kw["bias"] = bias
        if scale is not None:
            kw["scale"] = scale
        if accum_out is not None:
            kw["accum_out"] = accum_out
        return self.op(eng, lambda e: e.activation(out, in_, func, **kw), reads, writes)

    def tt(self, eng, out, in0, in1, op, reads=(), writes=()):
        return self.op(eng, lambda e: e.tensor_tensor(out, in0, in1, op), reads, writes)

    def ts(self, eng, out, in0, s1, s2, op0, op1=None, reads=(), writes=(), accum_out=None):
        kw = {}
        if accum_out is not None:
            kw["accum_out"] = accum_out
        if op1 is None:
            return self.op(eng, lambda e: e.tensor_scalar(out, in0, s1, None, op0, **kw), reads, writes)
        return self.op(eng, lambda e: e.tensor_scalar(out, in0, s1, s2, op0, op1, **kw), reads, writes)

    def stt(self, eng, out, in0, scalar, in1, op0, op1, reads=(), writes=()):
        return self.op(eng, lambda e: e.scalar_tensor_tensor(out, in0, scalar, in1, op0, op1), reads, writes)

    def copy(self, eng, out, in_, reads=(), writes=()):
        if eng == "act":
            return self.op(eng, lambda e: e.copy(out, in_), reads, writes)
        return self.op(eng, lambda e: e.tensor_copy(out, in_), reads, writes)

    def memset(self, eng, ap, val, writes=()):
        return self.op(eng, lambda e: e.memset(ap, val), (), writes)

    def finish(self, final_tokens):
        nc = self.nc
        prog = self.prog
        engmap = {"pe": "tensor", "act": "scalar", "dve": "vector", "pool": "gpsimd", "sp": "sync"}
        fin = self.all_tokens()
        with nc.Block() as block:
            for ename in self.all:
                entries = prog[ename]
                is_sp = ename == "sp"

                def body(e, entries=entries, is_sp=is_sp):
                    for waits, fn, inc in entries:
                        for wi_, (s, v) in enumerate(waits):
                            e.wait_ge(s, v)
                            if USE_NOPS and wi_ + 1 < len(waits):
                                e.nop(nofuse=True)
                        ins = fn(e)
                        ins.then_inc(inc[0], inc[1])
                    if is_sp:
                        for (s, v) in fin.values():
                            e.wait_ge(s, v)

                getattr(block, engmap[ename])(body)


D = 1024
T = 2304
NT = 18
LT = 2048
ALPHA = (2.0 * 2) ** 0.25
NEG = -30000.0


def tcol(ti):
    return 1 + 128 * ti if ti < 2 else 2 + 128 * ti


BLOCKS = [(1, 256, 0)] + [(258 + 512 * j, 512, 256 + 512 * j) for j in range(4)]


ORDER = ['pw', 'pm', 'A', 'B', 'C', 'D', 'E', 'F', 'G', 'L1A', 'L1P', 'L1W', 'L1WA', 'L1U', 'L1MA', 'L1E', 'L1F', 'L1']


def build(NB, dbg=False, only0=False, stop='L1'):
    def want(nm):
        return ORDER.index(nm) <= ORDER.index(stop)

    nc = bass.Bass("TRN2", target_bir_lowering=False)
    R = NB + 1
    dd = {}

    def din(name, shape, dt=F32):
        dd[name] = nc.dram_tensor(name, list(shape), dt, kind="ExternalInput").ap()
        return dd[name]

    def dscr(name, shape, dt=F32, out=False):
        return nc.dram_tensor(name, list(shape), dt, kind="ExternalOutput" if out else "Internal").ap()

    x_d = din("x", [NB, LT, D]); ctx_d = din("ctx", [NB, 256, D]); csT_d = din("csT", [128, 8, R])
    adaw_d = din("ada_w", [2, D, 6144]); adab_d = din("ada_b", [2, 6144])
    lng_d = din("ln_g", [4, D]); lnb_d = din("ln_b", [4, D])
    evwin_d = din("ev_w_in", [D, 3600]); aconvT_d = din("a_convT", [128, 4, 3]); bconv_d = din("ev_b_conv", [3, 1536])
    alog_d = din("alog", [1, 8]); dtb_d = din("dtbias", [1, 8]); bnorm_d = din("bnorm", [1, 128]); evwout_d = din("ev_w_out", [D, D])
    odwin_d = din("od_w_in", [D, 1440]); odwinsw_d = din("od_w_in_sw", [D, 1440])
    wkr_d = din("wkr", [D, 96]); wkrsw_d = din("wkr_sw", [D, 96])
    sink_d = din("sink", [1, 8]); qnT_d = din("qnormT", [128, 3]); kvnT_d = din("kvnormT", [128, 2])
    wuq_d = din("wuq", [384, 768]); wuqsw_d = din("wuq_sw", [384, 768]); wukv_d = din("wukv", [256, 1024]); odwout_d = din("od_w_out", [D, D])
    rw_d = din("router_w", [D, 16]); rb_d = din("router_bias", [1, 16])
    mg_d = din("moe_g", [32768, 512]); mu_d = din("moe_u", [32768, 512]); md_d = din("moe_d", [16384, 1024])
    idn_d = din("idn", [128, 128]); masks_d = din("masks", [14, 128, 128])
    rope64_d = din("rope64", [2, 64, LT]); rope32_d = din("rope32", [2, 96, LT])
    out_d = dscr("out", [NB, LT, D], F32, out=True)

    evwin_b = dscr("evwin_b", [D, 3600], BF16); wB_b = [dscr("wB%d_b" % j, [D, 1536], BF16) for j in range(3)]
    evwout_b = dscr("evwout_b", [D, D], BF16)
    odwin_b = dscr("odwin_b", [D, 1440], BF16); odwinsw_b = dscr("odwinsw_b", [D, 1440], BF16)
    wkr_b = dscr("wkr_b", [D, 96], BF16); wkrsw_b = dscr("wkrsw_b", [D, 96], BF16)
    wuq_b = dscr("wuq_b", [384, 768], BF16); wuqsw_b = dscr("wuqsw_b", [384, 768], BF16); wukv_b = dscr("wukv_b", [256, 1024], BF16)
    odwout_b = dscr("odwout_b", [D, D], BF16)
    modrow_s = dscr("modrow_s", [2, R, 6144], F32, out=dbg)
    qT_s = dscr("qT_s", [4, 128, T], BF16); kT_s = dscr("kT_s", [4, 128, T], BF16)
    k_s = dscr("k_s", [T, 512], BF16); v_s = dscr("v_s", [T, 512], BF16); gate_s = dscr("gate_s", [T, 512]); o_s = dscr("o_s", [T, 512])
    xa_s = dscr("xa_s", [NB, 2, T, D], F32, out=dbg)
    xb_s = dscr("xb_s", [NB, T, D], F32, out=dbg)
    h2T_s = dscr("h2T_s", [8, 128, T], BF16)
    cat_dbg = dscr("cat_dbg", [128, 8, LT], BF16, out=True) if dbg else None
    ymix_s = dscr("ymix_s", [NB, 2, T, D], F32, out=dbg) if dbg else None

    with ExitStack() as st0:
        S = Sched(nc, st0)
        cnt = [0]

        def sb(stack, shape, dt=F32, name=None):
            cnt[0] += 1
            return stack.enter_context(nc.sbuf_tensor("%s_%d" % (name or "t", cnt[0]), list(shape), dt))

        P2 = [st0.enter_context(nc.psum_tensor("pp%d" % i, [128, 1024], F32)) for i in range(4)]
        PB = [P2[i // 2][:, (i % 2) * 512:(i % 2 + 1) * 512] for i in range(8)]
        pbi = [0]
        S.excl = set(id(p_) for p_ in PB)

        def pb():
            p = PB[pbi[0] % 8]
            pbi[0] += 1
            return p

        ld_rr = [0]

        def rr3():
            ld_rr[0] += 1
            return ("dve", "pool", "act")[ld_rr[0] % 3]

        ident = sb(st0, [128, 128], name="ident"); S.dma(ident[:], idn_d, writes=[ident])
        ones = sb(st0, [128, 128], name="ones"); S.memset("pool", ones[:], 1.0, writes=[ones])
        MK = sb(st0, [128, 14, 128], name="masks")
        S.dma(MK[:], masks_d.rearrange("m p f -> p m f"), writes=[MK])
        modT = sb(st0, [128, 2, 48, R], name="modT")
        MKb = sb(st0, [128, 14, 128], BF16, name="masksb"); S.copy("dve", MKb[:], MK[:], reads=[MK], writes=[MKb])
        identb = sb(st0, [128, 128], BF16, name="identb"); S.copy("dve", identb[:], ident[:], reads=[ident], writes=[identb])
        onesb = sb(st0, [128, 128], BF16, name="onesb"); S.memset("pool", onesb[:], 1.0, writes=[onesb])
        rwt = sb(st0, [128, 8, 16], name="rw"); S.dma(rwt[:], rw_d.rearrange("(kc p) e -> p kc e", p=128), writes=[rwt])
        rbias = sb(st0, [128, 16], name="rbias"); S.dma(rbias[:], rb_d.to_broadcast([128, 16]), writes=[rbias])
        aconvT = sb(st0, [128, 4, 3], name="aconvT"); S.dma(aconvT[:], aconvT_d, writes=[aconvT])
        negexpA = sb(st0, [128, 8], name="negexpA"); dtb = sb(st0, [128, 8], name="dtb")
        S.dma(negexpA[:], alog_d.to_broadcast([128, 8]), writes=[negexpA]); S.dma(dtb[:], dtb_d.to_broadcast([128, 8]), writes=[dtb])
        S.act(negexpA[:], negexpA[:], AF.Exp, reads=[negexpA], writes=[negexpA])
        S.ts("dve", negexpA[:], negexpA[:], -1.0, None, ALU.mult, reads=[negexpA], writes=[negexpA])
        bnorm = sb(st0, [128, 128], name="bnorm"); S.dma(bnorm[:], bnorm_d.to_broadcast([128, 128]), writes=[bnorm])
        expsink = sb(st0, [128, 8], name="expsink"); S.dma(expsink[:], sink_d.to_broadcast([128, 8]), writes=[expsink])
        S.act(expsink[:], expsink[:], AF.Exp, reads=[expsink], writes=[expsink])
        qnT = sb(st0, [128, 3], name="qnT"); S.dma(qnT[:], qnT_d, writes=[qnT])
        kvnT = sb(st0, [128, 2], name="kvnT"); S.dma(kvnT[:], kvnT_d, writes=[kvnT])

        with ExitStack() as st:
            NSTG = 6
            stg = [sb(st, [128, 4096], F32, "stg") for _ in range(NSTG)]
            stgb = [sb(st, [128, 4096], BF16, "stgb") for _ in range(NSTG)]
            bcv = sb(st, [128, 3, 1536], F32, "bcv")
            for j in range(3):
                S.dma(bcv[:, j, :], bconv_d[j:j + 1, :].to_broadcast([128, 1536]), writes=[(bcv, j)])
            ui = [0]

            def conv(src, dst, rows, cols, scale=None):
                nrc = rows // 128
                G = max(1, min(nrc, 4096 // cols)) if cols <= 4096 else 1
                if scale is not None:
                    G = 1
                while nrc % G:
                    G -= 1
                for r0 in range(0, nrc, G):
                    i = ui[0] % NSTG
                    ui[0] += 1
                    eng = ("dve", "pool", "act")[i % 3]
                    a = stg[i][:, 0:G * cols].rearrange("p (g c) -> p g c", g=G)
                    b = stgb[i][:, 0:G * cols].rearrange("p (g c) -> p g c", g=G)
                    sv = src[r0 * 128:(r0 + G) * 128, :].rearrange("(g p) c -> p g c", p=128)
                    dv = dst[r0 * 128:(r0 + G) * 128, :].rearrange("(g p) c -> p g c", p=128)
                    S.dma(a, sv, writes=[stg[i]])
                    if scale is None:
                        S.copy(eng, b, a, reads=[stg[i]], writes=[stgb[i]])
                    else:
                        e2 = "pool" if eng == "act" else eng
                        S.tt(e2, b[:, 0, :], a[:, 0, :], scale, ALU.mult, reads=[stg[i], (bcv, 0), (bcv, 1), (bcv, 2)], writes=[stgb[i]])
                    S.dma(dv, b, reads=[stgb[i]])

            conv(evwin_d, evwin_b, D, 3600)
            for j in range(3):
                conv(evwin_d[:, 1536:3072], wB_b[j], D, 1536, scale=bcv[:, j, :])
            conv(evwout_d, evwout_b, D, D)
            conv(odwin_d, odwin_b, D, 1440); conv(odwinsw_d, odwinsw_b, D, 1440)
            conv(wkr_d, wkr_b, D, 96); conv(wkrsw_d, wkrsw_b, D, 96)
            conv(wuq_d, wuq_b, 384, 768); conv(wuqsw_d, wuqsw_b, 384, 768); conv(wukv_d, wukv_b, 256, 1024)
            conv(odwout_d, odwout_b, D, D)
            S.barrier()
        with ExitStack() as st:
          if want('pm'):
            csT = sb(st, [128, 8, R], F32, "csT")
            S.dma(csT[:], csT_d, writes=[csT])
            S.act(csT[:], csT[:], AF.Silu, reads=[csT], writes=[csT])
            awt = [sb(st, [128, 8, 1536], F32, "awt") for _ in range(2)]
            modrow = sb(st, [R, 6144], F32, "modrow")
            abr = sb(st, [R, 6144], F32, "abr")
            for l in range(2):
                S.dma(abr[:], adab_d[l:l + 1, :].to_broadcast([R, 6144]), reads=[modrow], writes=[abr])
                for q in range(4):
                    aw = awt[q % 2]
                    S.dma(aw[:], adaw_d[l, :, q * 1536:(q + 1) * 1536].rearrange("(kc p) n -> p kc n", p=128), writes=[aw])
                    for nb_ in range(3):
                        p = pb()
                        c0 = q * 1536 + nb_ * 512
                        for kc in range(8):
                            S.mm(p[0:R, :], csT[:, kc, :], aw[:, kc, nb_ * 512:(nb_ + 1) * 512], kc == 0, kc == 7, reads=[csT, aw], writes=[p])
                        S.tt("dve", modrow[:, c0:c0 + 512], p[0:R, :], abr[:, c0:c0 + 512], ALU.add, reads=[p, abr], writes=[modrow])
                S.dma(modrow_s[l], modrow[:], reads=[modrow])
                p = pb()
                for ch in range(48):
                    S.tr(p[:, ch * R:(ch + 1) * R], modrow[0:R, ch * 128:(ch + 1) * 128], ident[0:R, 0:R], reads=[modrow, ident], writes=[p])
                S.copy("dve", modT[:, l, :, :], p[:, 0:48 * R].rearrange("p (c r) -> p c r", r=R), reads=[p], writes=[modT])
                for c0 in (8, 32):
                    S.ts("dve", modT[:, l, c0:c0 + 8, :], modT[:, l, c0:c0 + 8, :], 1.0, None, ALU.add, reads=[modT], writes=[modT])
            S.barrier()

        def xsrc(layer, b, ti):
            if layer == 0:
                return ctx_d[b, ti * 128:(ti + 1) * 128, :] if ti < 2 else x_d[b, (ti - 2) * 128:(ti - 1) * 128, :]
            return xb_s[b, ti * 128:(ti + 1) * 128, :]

        def phase_A(layer, b, hT, tiles, st):
            xin = [sb(st, [128, D], F32, "xin") for _ in range(2)]
            for n, ti in enumerate(tiles):
                xt = xin[n % 2]
                r = NB if ti < 2 else b
                S.dma(xt[:], xsrc(layer, b, ti), writes=[xt])
                for half in range(2):
                    p = pb()
                    for k4 in range(4):
                        kc = half * 4 + k4
                        S.tr(p[:, k4 * 128:(k4 + 1) * 128], xt[:, kc * 128:(kc + 1) * 128], ident[:], reads=[xt], writes=[p])
                    for k4 in range(4):
                        kc = half * 4 + k4
                        dst = hT[:, kc, tcol(ti):tcol(ti) + 128]
                        if half == 0:
                            S.act(dst, p[:, k4 * 128:(k4 + 1) * 128], AF.Identity, bias=modT[:, layer, kc, r:r + 1],
                                  scale=modT[:, layer, 8 + kc, r:r + 1], reads=[p], writes=[(hT, ti, kc)])
                        else:
                            S.ts("dve", dst, p[:, k4 * 128:(k4 + 1) * 128], modT[:, layer, 8 + kc, r:r + 1], modT[:, layer, kc, r:r + 1],
                                 ALU.mult, ALU.add, reads=[p], writes=[(hT, ti, kc)])

        def layer_norm_tile(st_tiles, rt, lng, lnb, outt):
            stats, ag, sm = st_tiles
            for hf in range(2):
                S.op("dve", lambda e, hf=hf: e.bn_stats(stats[:, hf, :], rt[:, hf * 512:(hf + 1) * 512]), reads=[rt], writes=[stats])
            S.op("dve", lambda e: e.bn_aggr(ag[:], stats[:].rearrange('p a b -> p (a b)')), reads=[stats], writes=[ag])
            S.act(sm[:, 0:1], ag[:, 1:2], AF.Sqrt, bias=1e-5, reads=[ag], writes=[sm])
            S.op("dve", lambda e: e.reciprocal(sm[:, 1:2], sm[:, 0:1]), reads=[sm], writes=[sm])
            S.stt("dve", sm[:, 2:3], ag[:, 0:1], -1.0, sm[:, 1:2], ALU.mult, ALU.mult, reads=[ag, sm], writes=[sm])
            S.act(rt[:], rt[:], AF.Identity, bias=sm[:, 2:3], scale=sm[:, 1:2], reads=[rt, sm], writes=[rt])
            S.tt("pool", rt[:], rt[:], lng[:], ALU.mult, reads=[rt, lng], writes=[rt])
            S.tt("pool", outt[:], rt[:], lnb[:], ALU.add, reads=[rt, lnb], writes=[outt])

        def run_pipe(gens, depth):
            active = []
            it = iter(gens)
            fin = False
            while True:
                while len(active) < depth and not fin:
                    try:
                        active.append(next(it))
                    except StopIteration:
                        fin = True
                if not active:
                    break
                for g_ in list(active):
                    try:
                        next(g_)
                    except StopIteration:
                        active.remove(g_)

        def ln_stats(rt, stats, ag, sm):
            for hf in range(2):
                S.op("dve", lambda e, hf=hf: e.bn_stats(stats[:, hf, :], rt[:, hf * 512:(hf + 1) * 512]), reads=[rt], writes=[stats])
            S.op("dve", lambda e: e.bn_aggr(ag[:], stats[:].rearrange('p a b -> p (a b)')), reads=[stats], writes=[ag])
            S.act(sm[:, 0:1], ag[:, 1:2], AF.Sqrt, bias=1e-5, reads=[ag], writes=[sm])
            S.op("dve", lambda e: e.reciprocal(sm[:, 1:2], sm[:, 0:1]), reads=[sm], writes=[sm])
            S.stt("dve", sm[:, 2:3], ag[:, 0:1], -1.0, sm[:, 1:2], ALU.mult, ALU.mult, reads=[ag, sm], writes=[sm])

        def ln_apply(rt, sm, lng, lnb, outt):
            S.act(rt[:], rt[:], AF.Identity, bias=sm[:, 2:3], scale=sm[:, 1:2], reads=[rt, sm], writes=[rt])
            S.tt("dve", rt[:], rt[:], lng[:], ALU.mult, reads=[rt, lng], writes=[rt])
            S.tt("dve", outt[:], rt[:], lnb[:], ALU.add, reads=[rt, lnb], writes=[outt])

        def phase_E(layer, b, catT, tiles, gates, st):
            nt = len(tiles)
            NBUF = 5
            wo = sb(st, [128, 8, D], BF16, "wo")
            wsrc = evwout_b if layer == 0 else odwout_b
            S.dma(wo[:], wsrc.rearrange("(kc p) n -> p kc n", p=128), writes=[wo])
            lng = sb(st, [128, D], F32, "lng"); lnb = sb(st, [128, D], F32, "lnb")
            S.dma(lng[:], lng_d[2 * layer:2 * layer + 1, :].to_broadcast([128, D]), writes=[lng])
            S.dma(lnb[:], lnb_d[2 * layer:2 * layer + 1, :].to_broadcast([128, D]), writes=[lnb])
            g1 = [sb(st, [128, D], F32, "g1") for _ in range(2)]
            S.dma(g1[0][:], modrow_s[layer, NB:NB + 1, 2048:3072].to_broadcast([128, D]), writes=[g1[0]])
            S.dma(g1[1][:], modrow_s[layer, b:b + 1, 2048:3072].to_broadcast([128, D]), writes=[g1[1]])
            xin = [sb(st, [128, D], F32, "xin") for _ in range(NBUF)]
            rts = [sb(st, [128, D], F32, "rt") for _ in range(NBUF)]
            xns = [sb(st, [128, D], F32, "xn") for _ in range(NBUF)]
            h2f = [sb(st, [128, 8, 128], F32, "h2f") for _ in range(NBUF)]
            h2b = [sb(st, [128, 8, 128], BF16, "h2b") for _ in range(NBUF)]
            statsL = [sb(st, [128, 2, 6], F32, "stats") for _ in range(NBUF)]
            agL = [sb(st, [128, 2], F32, "ag") for _ in range(NBUF)]
            smL = [sb(st, [128, 4], F32, "sm") for _ in range(NBUF)]
            aff = sb(st, [128, nt, 16], F32, "aff")

            def tile_gen(n, ti):
                k = n % NBUF
                xt = xin[k]; rt = rts[k]; xn = xns[k]; hf_ = h2f[k]; hb_ = h2b[k]; stats = statsL[k]; ag = agL[k]; sm = smL[k]
                r = NB if ti < 2 else b
                gt = g1[0] if ti < 2 else g1[1]
                S.dma(xt[:], xsrc(layer, b, ti), writes=[xt])
                c0 = tcol(ti) if layer == 0 else (ti - 2) * 128
                for hf in range(2):
                    p = pb()
                    for kc in range(8):
                        S.mm(p[:], catT[:, kc, c0:c0 + 128], wo[:, kc, hf * 512:(hf + 1) * 512], kc == 0, kc == 7, reads=[wo], writes=[p])
                    if dbg:
                        S.copy("act", rt[:, hf * 512:(hf + 1) * 512], p[:], reads=[p], writes=[rt])
                        S.dma(ymix_s[b, layer, ti * 128:(ti + 1) * 128, hf * 512:(hf + 1) * 512], rt[:, hf * 512:(hf + 1) * 512], reads=[rt])
                    S.tt("dve", rt[:, hf * 512:(hf + 1) * 512], p[:], gt[:, hf * 512:(hf + 1) * 512], ALU.mult, reads=[p, gt], writes=[rt])
                yield
                S.stt("dve", rt[:], xt[:], ALPHA, rt[:], ALU.mult, ALU.add, reads=[xt, rt], writes=[rt])
                ln_stats(rt, stats, ag, sm)
                yield
                ln_apply(rt, sm, lng, lnb, xn)
                S.dma(xa_s[b, layer, ti * 128:(ti + 1) * 128, :], xn[:], reads=[xn])
                yield
                for half in range(2):
                    p = pb()
                    for k4 in range(4):
                        kc = half * 4 + k4
                        S.tr(p[:, k4 * 128:(k4 + 1) * 128], xn[:, kc * 128:(kc + 1) * 128], ident[:], reads=[xn], writes=[p])
                    for k4 in range(4):
                        kc = half * 4 + k4
                        if half == 0:
                            S.act(hf_[:, kc, :], p[:, k4 * 128:(k4 + 1) * 128], AF.Identity, bias=modT[:, layer, 24 + kc, r:r + 1],
                                  scale=modT[:, layer, 32 + kc, r:r + 1], reads=[p], writes=[hf_])
                        else:
                            S.ts("dve", hf_[:, kc, :], p[:, k4 * 128:(k4 + 1) * 128], modT[:, layer, 32 + kc, r:r + 1], modT[:, layer, 24 + kc, r:r + 1],
                                 ALU.mult, ALU.add, reads=[p], writes=[hf_])
                yield
                S.copy("act", hb_[:], hf_[:], reads=[hf_], writes=[hb_])
                S.dma(h2T_s.rearrange("k p t -> p k t")[:, :, n * 128:(n + 1) * 128], hb_[:], reads=[hb_])
                p = pb()
                for kc in range(8):
                    S.mm(p[:, 0:16], hf_[:, kc, :], rwt[:, kc, :], kc == 0, kc == 7, reads=[hf_], writes=[p])
                S.act(aff[:, n, :], p[:, 0:16], AF.Sigmoid, reads=[p], writes=[(aff, n)])

            run_pipe((tile_gen(n, ti) for n, ti in enumerate(tiles)), 4)
            router_batched(aff, gates, nt, st)

        def router_batched(aff, gates, nt, st):
            G4 = nt * 4
            sel = sb(st, [128, nt, 16], F32, "r_sel"); t1 = sb(st, [128, nt, 16], F32, "r_t1"); t2 = sb(st, [128, nt, 16], F32, "r_t2")
            m1 = sb(st, [128, G4], F32, "r_m1"); sec = sb(st, [128, G4], F32, "r_sec"); gs = sb(st, [128, G4], F32, "r_gs")
            gm = sb(st, [128, G4], F32, "r_gm"); tm = sb(st, [128, G4], F32, "r_tm")
            s1 = sb(st, [128, nt], F32, "r_s1"); s2 = sb(st, [128, nt], F32, "r_s2"); den = sb(st, [128, nt], F32, "r_den")
            allaff = [(aff, n) for n in range(nt)]
            RS = "rsres"

            def g4(t):
                return t[:].rearrange("p n (g e) -> p (n g) e", g=4)

            def bc44(t):
                return t[:].unsqueeze(2).to_broadcast([128, G4, 4])

            def bc16(t):
                return t[:].unsqueeze(2).to_broadcast([128, nt, 16])

            def dv(fn, *a, **k):
                return S.op("dve", fn, reads=allaff + [RS], writes=[RS])
            dv(lambda e: e.tensor_tensor(sel[:], aff[:], rbias[:].unsqueeze(1).to_broadcast([128, nt, 16]), ALU.add))
            dv(lambda e: e.tensor_reduce(m1[:], g4(sel), AX.X, ALU.max))
            dv(lambda e: e.tensor_tensor(g4(t1), g4(sel), bc44(m1), ALU.is_lt))
            dv(lambda e: e.tensor_scalar(t2[:], t1[:], 1.0, 1e9, ALU.subtract, ALU.mult))
            dv(lambda e: e.tensor_tensor(t1[:], t1[:], sel[:], ALU.mult))
            dv(lambda e: e.tensor_tensor(t2[:], t2[:], t1[:], ALU.add))
            dv(lambda e: e.tensor_reduce(sec[:], g4(t2), AX.X, ALU.max))
            dv(lambda e: e.tensor_tensor(gs[:], m1[:], sec[:], ALU.add))
            gs3 = gs[:].rearrange("p (n g) -> p n g", g=4)
            dv(lambda e: e.tensor_reduce(s1[:], gs3, AX.X, ALU.max))
            dv(lambda e: e.tensor_tensor(gm[:].rearrange("p (n g) -> p n g", g=4), gs3, s1[:].unsqueeze(2).to_broadcast([128, nt, 4]), ALU.is_ge))
            dv(lambda e: e.tensor_tensor(g4(t1), g4(sel), bc44(gm), ALU.mult))
            dv(lambda e: e.tensor_scalar(tm[:], gm[:], 1.0, 1e9, ALU.subtract, ALU.mult))
            dv(lambda e: e.tensor_tensor(g4(t1), g4(t1), bc44(tm), ALU.add))
            dv(lambda e: e.tensor_reduce(s1[:], t1[:], AX.X, ALU.max))
            dv(lambda e: e.tensor_tensor(t2[:], t1[:], bc16(s1), ALU.is_lt))
            dv(lambda e: e.tensor_tensor(sel[:], t1[:], t2[:], ALU.mult))
            dv(lambda e: e.tensor_scalar(t2[:], t2[:], 1.0, 1e9, ALU.subtract, ALU.mult))
            dv(lambda e: e.tensor_tensor(sel[:], sel[:], t2[:], ALU.add))
            dv(lambda e: e.tensor_reduce(s2[:], sel[:], AX.X, ALU.max))
            dv(lambda e: e.tensor_tensor(t2[:], t1[:], bc16(s2), ALU.is_ge))
            dv(lambda e: e.tensor_tensor(t2[:], t2[:], aff[:], ALU.mult))
            dv(lambda e: e.tensor_reduce(den[:], t2[:], AX.X, ALU.add))
            dv(lambda e: e.reciprocal(den[:], den[:]))
            S.op("dve", lambda e: e.tensor_tensor(gates[:], t2[:], bc16(den), ALU.mult), reads=[RS], writes=[gates])

        def phase_F(layer, b, h2T, gates, ntile, outacc, st):
            wgu = [sb(st, [128, 2, 8, 512], BF16, "wgu") for _ in range(2)]
            wdn = [sb(st, [128, 4, D], BF16, "wdn") for _ in range(2)]
            actT = [sb(st, [128, 4, 512], BF16, "actT") for _ in range(2)]
            sl = [sb(st, [128, 512], F32, "sl") for _ in range(2)]
            stgF = [sb(st, [128, 2048], F32, "stgF") for _ in range(2)]
            ntok = ntile * 128
            blocks = [(c, min(512, ntok - c)) for c in range(0, ntok, 512)]
            pi = [0]

            def pieces(e):
                wg = wgu[e % 2]; wd = wdn[e % 2]
                r0 = (layer * 16 + e) * 1024
                r1 = (layer * 16 + e) * 512
                out = []
                for which, src in ((0, mg_d), (1, mu_d)):
                    for half in range(2):
                        src_ap = src[r0 + half * 512:r0 + (half + 1) * 512, :].rearrange("(kc p) f -> p kc f", p=128)
                        out.append((wg[:, which, half * 4:(half + 1) * 4, :], src_ap, (wg, which, half), 4))
                for half in range(2):
                    src_ap = md_d[r1 + half * 256:r1 + (half + 1) * 256, :].rearrange("(fc p) n -> p fc n", p=128)
                    out.append((wd[:, half * 2:(half + 1) * 2, :], src_ap, (wd, half), 2))
                return out

            def load_cast(pc):
                dst_ap, src_ap, key, G = pc
                sg = stgF[pi[0] % 2]; pi[0] += 1
                sgv = sg[:, 0:2048].rearrange("p (g c) -> p g c", g=G)
                S.dma(sgv, src_ap, writes=[sg])
                S.copy("pool", dst_ap, sgv, reads=[sg], writes=[key])

            def down(at, wd, e, c0, w):
                for j in range(w // 128):
                    n = c0 // 128 + j
                    for hf in range(2):
                        pd = pb()
                        for fc in range(4):
                            S.mm(pd[:], at[:, fc, j * 128:(j + 1) * 128], wd[:, fc, hf * 512:(hf + 1) * 512], fc == 0, fc == 3,
                                 reads=[(wd, fc // 2), (at, 0), (at, 1), (at, 2), (at, 3)], writes=[pd])
                        dst = outacc[:, n, hf * 512:(hf + 1) * 512]
                        if e == 0:
                            S.ts("dve", dst, pd[:], gates[:, n, e:e + 1], None, ALU.mult, reads=[pd], writes=[(outacc, n, hf)])
                        else:
                            S.stt("dve", dst, pd[:], gates[:, n, e:e + 1], dst, ALU.mult, ALU.add, reads=[pd], writes=[(outacc, n, hf)])

            for pc in pieces(0):
                load_cast(pc)
            it = 0
            pending = None
            for e in range(16):
                wg = wgu[e % 2]; wd = wdn[e % 2]
                nxt = pieces(e + 1) if e + 1 < 16 else []
                for bi, (c0, w) in enumerate(blocks):
                    at = actT[it % 2]; it += 1
                    for fc in range(4):
                        pg = pb(); pu = pb()
                        for kc in range(8):
                            S.mm(pg[:, 0:w], wg[:, 0, kc, fc * 128:(fc + 1) * 128], h2T[:, kc, c0:c0 + w], kc == 0, kc == 7, reads=[(wg, 0, kc // 4)], writes=[pg])
                        for kc in range(8):
                            S.mm(pu[:, 0:w], wg[:, 1, kc, fc * 128:(fc + 1) * 128], h2T[:, kc, c0:c0 + w], kc == 0, kc == 7, reads=[(wg, 1, kc // 4)], writes=[pu])
                        s_ = sl[fc % 2]
                        S.act(s_[:, 0:w], pg[:, 0:w], AF.Silu, reads=[pg], writes=[s_])
                        S.tt("dve", at[:, fc, 0:w], s_[:, 0:w], pu[:, 0:w], ALU.mult, reads=[s_, pu], writes=[(at, fc)])
                    if pending is not None:
                        down(*pending)
                    pending = (at, wd, e, c0, w)
                    k = 2 if bi == 0 else 1
                    for _ in range(k):
                        if nxt:
                            load_cast(nxt.pop(0))
                while nxt:
                    load_cast(nxt.pop(0))
            down(*pending)

        def phase_G(layer, b, tiles, outacc, st):
            NBUF = 5
            lng = sb(st, [128, D], F32, "lng2"); lnb = sb(st, [128, D], F32, "lnb2")
            S.dma(lng[:], lng_d[2 * layer + 1:2 * layer + 2, :].to_broadcast([128, D]), writes=[lng])
            S.dma(lnb[:], lnb_d[2 * layer + 1:2 * layer + 2, :].to_broadcast([128, D]), writes=[lnb])
            g2 = [sb(st, [128, D], F32, "g2") for _ in range(2)]
            S.dma(g2[0][:], modrow_s[layer, NB:NB + 1, 5120:6144].to_broadcast([128, D]), writes=[g2[0]])
            S.dma(g2[1][:], modrow_s[layer, b:b + 1, 5120:6144].to_broadcast([128, D]), writes=[g2[1]])
            xin = [sb(st, [128, D], F32, "xin2") for _ in range(NBUF)]
            rts = [sb(st, [128, D], F32, "rt2") for _ in range(NBUF)]
            xns = [sb(st, [128, D], F32, "xn2") for _ in range(NBUF)]
            statsL = [sb(st, [128, 2, 6], F32, "stats2") for _ in range(NBUF)]
            agL = [sb(st, [128, 2], F32, "ag2") for _ in range(NBUF)]
            smL = [sb(st, [128, 4], F32, "sm2") for _ in range(NBUF)]

            def tile_gen(n, ti):
                k = n % NBUF
                xt = xin[k]; rt = rts[k]; xn = xns[k]; stats = statsL[k]; ag = agL[k]; sm = smL[k]
                gt = g2[0] if ti < 2 else g2[1]
                S.dma(xt[:], xa_s[b, layer, ti * 128:(ti + 1) * 128, :], writes=[xt])
                S.tt("dve", rt[:], outacc[:, n, :], gt[:], ALU.mult, reads=[gt, (outacc, n, 0), (outacc, n, 1)], writes=[rt])
                yield
                S.stt("dve", rt[:], xt[:], ALPHA, rt[:], ALU.mult, ALU.add, reads=[xt, rt], writes=[rt])
                ln_stats(rt, stats, ag, sm)
                yield
                ln_apply(rt, sm, lng, lnb, xn)
                if layer == 0:
                    S.dma(xb_s[b, ti * 128:(ti + 1) * 128, :], xn[:], reads=[xn])
                else:
                    S.dma(out_d[b, (ti - 2) * 128:(ti - 1) * 128, :], xn[:], reads=[xn])

            run_pipe((tile_gen(n, ti) for n, ti in enumerate(tiles)), 4)

        def post_E(layer, b, catT, tiles, gates):
            if not want('E'):
                return
            with ExitStack() as st2:
                phase_E(layer, b, catT, tiles, gates, st2)
                S.barrier()

        def post_FG(layer, b, tiles, gates):
            nt = len(tiles)
            if not want('F'):
                return
            with ExitStack() as st:
                outacc = sb(st, [128, nt, D], F32, "outacc")
                with ExitStack() as st2:
                    h2T = sb(st2, [128, 8, nt * 128], BF16, "h2T")
                    for kc in range(8):
                        S.dma(h2T[:, kc, :], h2T_s[kc, :, 0:nt * 128], writes=[h2T])
                    phase_F(layer, b, h2T, gates, nt, outacc, st2)
                    S.barrier()
                if not want('G'):
                    return
                with ExitStack() as st2:
                    phase_G(layer, b, tiles, outacc, st2)
                    S.barrier()

        def phase_B(b, hT, catT, st):
            wA = [sb(st, [128, 3, 8, 128], BF16, "wA") for _ in range(2)]
            u = sb(st, [128, 2307], F32, "u"); p0s = sb(st, [128, 2307], F32, "p0s"); y = sb(st, [128, 2307], F32, "y")
            t1 = [sb(st, [128, 512], F32, "t1") for _ in range(2)]
            for c in (0, 257, 2306):
                S.memset("pool", u[:, c:c + 1], 0.0, writes=[(u, "pad")])
                S.memset("pool", p0s[:, c:c + 1], 0.0, writes=[(p0s, "pad")])
            bi = 0
            for ch in range(4):
                w = wA[ch % 2]
                for which in range(3):
                    cc = which * 512 + ch * 128
                    S.dma(w[:, which], evwin_b[:, cc:cc + 128].rearrange("(kc p) n -> p kc n", p=128), writes=[(w, which)])
                for (c0, wd_, t0) in BLOCKS:
                    pp = [pb(), pb(), pb()]
                    for which in range(3):
                        for kc in range(8):
                            S.mm(pp[which][:, 0:wd_], w[:, which, kc, :], hT[:, kc, c0:c0 + wd_], kc == 0, kc == 7, reads=[(w, which)], writes=[pp[which]])
                    tt_ = t1[bi % 2]; bi += 1
                    S.copy("act", tt_[:, 0:wd_], pp[1][:, 0:wd_], reads=[pp[1]], writes=[tt_])
                    S.tt("dve", u[:, c0:c0 + wd_], tt_[:, 0:wd_], pp[2][:, 0:wd_], ALU.mult, reads=[tt_, pp[2]], writes=[(u, c0)])
                    S.copy("act", p0s[:, c0:c0 + wd_], pp[0][:, 0:wd_], reads=[pp[0]], writes=[(p0s, c0)])
                allu = [(u, c0) for c0, _, _ in BLOCKS] + [(u, "pad")]
                allp = [(p0s, c0) for c0, _, _ in BLOCKS] + [(p0s, "pad")]
                S.ts("dve", y[:, 1:2306], u[:, 1:2306], aconvT[:, ch, 1:2], None, ALU.mult, reads=allu, writes=[y])
                S.stt("dve", y[:, 1:2306], u[:, 0:2305], aconvT[:, ch, 0:1], y[:, 1:2306], ALU.mult, ALU.add, reads=allu + [y], writes=[y])
                S.stt("dve", y[:, 1:2306], u[:, 2:2307], aconvT[:, ch, 2:3], y[:, 1:2306], ALU.mult, ALU.add, reads=allu + [y], writes=[y])
                S.tt("dve", catT[:, ch, 1:2306], y[:, 1:2306], p0s[:, 1:2306], ALU.mult, reads=[y] + allp, writes=[(catT, ch)])

        def phase_C(b, hT, BG, st):
            NBUF = 4
            wB = [sb(st, [128, 3, 8, 128], BF16, "wB") for _ in range(2)]
            s_ = [sb(st, [128, 512], F32, "s_") for _ in range(NBUF)]
            sq_ = [sb(st, [128, 512], BF16, "sq_") for _ in range(NBUF)]
            rin = [sb(st, [128, 512], F32, "rin") for _ in range(NBUF)]
            kn = [sb(st, [128, 512], BF16, "kn") for _ in range(NBUF)]
            ktk = [sb(st, [128, 4, 128], BF16, "ktk") for _ in range(NBUF)]

            def qk_gen(i, which, h, w, c0, wd_, t0):
                p = pb(); n = 0
                for j in range(3):
                    for kc in range(8):
                        S.mm(p[:, 0:wd_], w[:, j, kc, :], hT[:, kc, c0 + j - 1:c0 + j - 1 + wd_], n == 0, n == 23, reads=[(w, j)], writes=[p])
                        n += 1
                s = s_[i % NBUF]; sq = sq_[i % NBUF]; ri = rin[i % NBUF]; qn = kn[i % NBUF]; kt = ktk[i % NBUF]
                S.act(s[:, 0:wd_], p[:, 0:wd_], AF.Silu, reads=[p], writes=[s])
                S.tt("dve", sq[:, 0:wd_], s[:, 0:wd_], s[:, 0:wd_], ALU.mult, reads=[s], writes=[sq])
                yield
                p2 = pb()
                S.mm(p2[:, 0:wd_], onesb[:], sq[:, 0:wd_], True, True, reads=[sq], writes=[p2])
                S.act(ri[:, 0:wd_], p2[:, 0:wd_], AF.Sqrt, bias=1e-6, reads=[p2], writes=[ri])
                S.op("dve", lambda e: e.reciprocal(ri[:, 0:wd_], ri[:, 0:wd_]), reads=[ri], writes=[ri])
                if which == 0:
                    S.stt("dve", qn[:, 0:wd_], s[:, 0:wd_], 128.0 ** -0.5, ri[:, 0:wd_], ALU.mult, ALU.mult, reads=[s, ri], writes=[qn])
                else:
                    S.tt("dve", qn[:, 0:wd_], s[:, 0:wd_], ri[:, 0:wd_], ALU.mult, reads=[s, ri], writes=[qn])
                dstT = (qT_s if which == 0 else kT_s)[h][:, t0:t0 + wd_]
                S.dma(dstT, qn[:, 0:wd_], reads=[qn])
                if which == 1:
                    yield
                    p3 = pb()
                    na = wd_ // 128
                    for j4 in range(na):
                        S.mm(p3[:, j4 * 128:(j4 + 1) * 128], qn[:, j4 * 128:(j4 + 1) * 128], identb[:], True, True, reads=[qn], writes=[p3])
                    S.copy("act", kt[:, 0:na, :], p3[:, 0:wd_].rearrange("p (a b) -> p a b", b=128), reads=[p3], writes=[kt])
                    S.dma(k_s[t0:t0 + wd_, h * 128:(h + 1) * 128].rearrange("(a p) d -> p a d", p=128), kt[:, 0:na, :], reads=[kt])

            def all_qk():
                i = 0
                wi = 0
                for which in range(2):
                    for h in range(4):
                        w = wB[wi % 2]; wi += 1
                        col = which * 512 + h * 128
                        for j in range(3):
                            S.dma(w[:, j], wB_b[j][:, col:col + 128].rearrange("(kc p) n -> p kc n", p=128), writes=[(w, j)])
                        for (c0, wd_, t0) in BLOCKS:
                            yield qk_gen(i, which, h, w, c0, wd_, t0)
                            i += 1
            run_pipe(all_qk(), 3)
            wV = sb(st, [128, 3, 8, 512], BF16, "wV")
            for j in range(3):
                S.dma(wV[:, j], wB_b[j][:, 1024:1536].rearrange("(kc p) n -> p kc n", p=128), writes=[(wV, j)])
            wG = sb(st, [128, 8, 512], BF16, "wG")
            S.dma(wG[:], evwin_b[:, 3072:3584].rearrange("(kc p) n -> p kc n", p=128), writes=[wG])
            wba = sb(st, [128, 8, 16], BF16, "wba")
            S.dma(wba[:], evwin_b[:, 3584:3600].rearrange("(kc p) n -> p kc n", p=128), writes=[wba])
            vt = [sb(st, [128, 512], BF16, "vt") for _ in range(2)]
            gt = [sb(st, [128, 512], F32, "gt") for _ in range(2)]
            for ti in range(NT):
                c = tcol(ti)
                p = pb(); n = 0
                for j in range(3):
                    for kc in range(8):
                        S.mm(p[:], hT[:, kc, c + j - 1:c + j - 1 + 128], wV[:, j, kc, :], n == 0, n == 23, reads=[(wV, j)], writes=[p])
                        n += 1
                v = vt[ti % 2]
                S.act(v[:], p[:], AF.Silu, reads=[p], writes=[v])
                S.dma(v_s[ti * 128:(ti + 1) * 128, :], v[:], reads=[v])
                p = pb()
                for kc in range(8):
                    S.mm(p[:], hT[:, kc, c:c + 128], wG[:, kc, :], kc == 0, kc == 7, reads=[wG], writes=[p])
                g = gt[ti % 2]
                S.act(g[:], p[:], AF.Silu, reads=[p], writes=[g])
                S.dma(gate_s[ti * 128:(ti + 1) * 128, :], g[:], reads=[g])
                p = pb()
                for kc in range(8):
                    S.mm(p[:, 0:16], hT[:, kc, c:c + 128], wba[:, kc, :], kc == 0, kc == 7, reads=[wba], writes=[p])
                S.copy("dve", BG[:, ti, :], p[:, 0:16], reads=[p], writes=[(BG, ti)])
            allbg = [(BG, ti) for ti in range(NT)]
            smb = sb(st, [128, NT, 8], F32, "smb")
            S.act(BG[:, :, 0:8], BG[:, :, 0:8], AF.Sigmoid, reads=allbg, writes=[(BG, "beta")])
            S.tt("dve", smb[:], BG[:, :, 8:16], dtb[:].unsqueeze(1).to_broadcast([128, NT, 8]), ALU.add, reads=allbg, writes=[smb])
            S.ts("dve", smb[:], smb[:], 30.0, None, ALU.min, reads=[smb], writes=[smb])
            S.act(smb[:], smb[:], AF.Exp, reads=[smb], writes=[smb])
            S.act(smb[:], smb[:], AF.Ln, bias=1.0, reads=[smb], writes=[smb])
            S.tt("dve", BG[:, :, 8:16], smb[:], negexpA[:].unsqueeze(1).to_broadcast([128, NT, 8]), ALU.mult, reads=[smb] + allbg, writes=[(BG, "g")])

        def phase_D(b, BG, catT, st):
            Sst = sb(st, [128, 2, 4, 128], F32, "Sst")
            S.memset("pool", Sst[:], 0.0, writes=[Sst])
            Sb = sb(st, [128, 2, 4, 128], BF16, "Sb")
            S.memset("pool", Sb[:], 0.0, writes=[Sb])
            names16 = ["kT", "qT", "ktok", "v", "TG", "A", "AT", "Mb", "MbT", "P", "vb", "kbg", "DT", "kd", "wT", "qg0", "qg1", "vnew"]
            names32 = ["u", "Eg", "osb", "oprev", "gate", "Stmp"]
            slots = []
            for d in range(4):
                sl = {nm: sb(st, [128, 4, 128], BF16, nm) for nm in names16}
                sl.update({nm: sb(st, [128, 4, 128], F32, nm) for nm in names32})
                sl["E"] = sb(st, [128, 16], F32, "E"); sl["bg2"] = sb(st, [128, 4], F32, "bg2"); sl["lnb"] = sb(st, [128, 4], F32, "lnbeta")
                sl["ss"] = sb(st, [128, 8], F32, "ss"); sl["junk"] = sb(st, [128, 128], F32, "junk")
                S.memset("pool", sl["qg0"][:], 0.0, writes=[sl["qg0"]]); S.memset("pool", sl["qg1"][:], 0.0, writes=[sl["qg1"]])
                slots.append(sl)
            visited = set()
            identbc = ident[:].unsqueeze(1).to_broadcast([128, 4, 128])
            bnbc = bnorm[:].unsqueeze(1).to_broadcast([128, 4, 128])

            def bc4(ap):
                return ap.unsqueeze(2).to_broadcast([128, 4, 128])

            def f2(t):
                return t[:].rearrange("p h d -> p (h d)")

            def hs(h):
                return slice(h * 128, (h + 1) * 128)

            ringi = [0, 0]

            def item(ti, d, sl):
                def pb():
                    r = PB[4 * d + ringi[d] % 3]
                    ringi[d] += 1
                    return r
                mi, ms, negS, negI = (0, 1, 4, 5) if d == 0 else (2, 3, 6, 7)
                gd = BG[:, ti, 8 + 4 * d:12 + 4 * d]
                bd = BG[:, ti, 4 * d:4 * d + 4]
                rows = slice(ti * 128, (ti + 1) * 128)
                kT, qT, ktok, v, TG, A, AT, P, DT = sl["kT"], sl["qT"], sl["ktok"], sl["v"], sl["TG"], sl["A"], sl["AT"], sl["P"], sl["DT"]
                E = sl["E"]
                S.dma(kT[:], kT_s.rearrange("h d t -> d h t")[:, :, rows], writes=[kT])
                S.dma(qT[:], qT_s.rearrange("h d t -> d h t")[:, :, rows], writes=[qT])
                S.dma(f2(ktok), k_s[rows, :], writes=[ktok])
                S.dma(f2(v), v_s[rows, :], writes=[v])
                ps_ = pb()
                S.mm(ps_[:, 0:4], MK[:, mi, :], gd, True, True, reads=[(BG, ti)], writes=[ps_])
                S.mm(ps_[:, 4:8], MK[:, ms, :], gd, True, True, reads=[(BG, ti)], writes=[ps_])
                S.mm(ps_[:, 8:12], MK[:, 8, :], gd, True, True, reads=[(BG, ti)], writes=[ps_])
                S.mm(ps_[:, 12:16], MK[:, 9, :], gd, True, True, reads=[(BG, ti)], writes=[ps_])
                S.act(E[:], ps_[:, 0:16], AF.Exp, reads=[ps_], writes=[E])
                S.tt("dve", sl["bg2"][:], bd, E[:, 0:4], ALU.mult, reads=[E, (BG, ti)], writes=[sl["bg2"]])
                S.act(sl["lnb"][:], bd, AF.Ln, reads=[(BG, ti)], writes=[sl["lnb"]])
                S.tt("pool", TG[:], MK[:, mi, :].unsqueeze(1).to_broadcast([128, 4, 128]), bc4(gd), ALU.mult, reads=[(BG, ti)], writes=[TG])
                yield
                pKK = pb(); pL = pb()
                for h in range(4):
                    S.mm(pKK[:, hs(h)], kT[:, h, :], kT[:, h, :], True, True, reads=[kT], writes=[pKK])
                for h in range(4):
                    S.mm(pL[:, hs(h)], TG[:, h, :], MKb[:, ms, :], True, False, reads=[TG], writes=[pL])
                    S.mm(pL[:, hs(h)], identb[:], MKb[:, negS, :], False, True, reads=[TG], writes=[pL])
                for h in range(4):
                    S.act(A[:, h, :], pL[:, hs(h)], AF.Exp, bias=sl["lnb"][:, h:h + 1], reads=[pL, sl["lnb"]], writes=[A])
                S.tt("dve", f2(A), pKK[:], f2(A), ALU.mult, reads=[pKK, A], writes=[A])
                yield
                pAT = pb()
                for h in range(4):
                    S.mm(pAT[:, hs(h)], A[:, h, :], identb[:], True, True, reads=[A], writes=[pAT])
                S.copy("act", f2(AT), pAT[:], reads=[pAT], writes=[AT])
                S.stt("dve", P[:], pAT[:].rearrange("p (h d) -> p h d", h=4), -1.0, identbc, ALU.mult, ALU.add, reads=[pAT], writes=[P])
                pLT = pb(); pQK = pb()
                for h in range(4):
                    S.mm(pLT[:, hs(h)], MKb[:, ms, :], TG[:, h, :], True, False, reads=[TG], writes=[pLT])
                    S.mm(pLT[:, hs(h)], identb[:], MKb[:, negI, :], False, True, reads=[TG], writes=[pLT])
                for h in range(4):
                    S.mm(pQK[:, hs(h)], kT[:, h, :], qT[:, h, :], True, True, reads=[kT, qT], writes=[pQK])
                S.act(f2(DT), pLT[:], AF.Exp, reads=[pLT], writes=[DT])
                S.tt("dve", f2(DT), pQK[:], f2(DT), ALU.mult, reads=[pQK, DT], writes=[DT])
                yield
                N_, NT_ = AT, A
                Y_, YT_ = sl["Mb"], sl["MbT"]
                prevYT = None
                for lev in range(6):
                    pP = None
                    if prevYT is not None:
                        pP = pb()
                        for h in range(4):
                            S.mm(pP[:, hs(h)], prevYT[:, h, :], P[:, h, :], True, True, reads=[prevYT, P], writes=[pP])
                    if lev < 5:
                        last = lev == 4
                        pMT = pb()
                        pM = None if last else pb()
                        for h in range(4):
                            if not last:
                                S.mm(pM[:, hs(h)], NT_[:, h, :], N_[:, h, :], True, True, reads=[N_, NT_], writes=[pM])
                            S.mm(pMT[:, hs(h)], N_[:, h, :], NT_[:, h, :], True, True, reads=[N_, NT_], writes=[pMT])
                        if not last:
                            S.copy("act", f2(Y_), pM[:], reads=[pM], writes=[Y_])
                        S.copy("act", f2(YT_), pMT[:], reads=[pMT], writes=[YT_])
                    if pP is not None:
                        S.tt("dve", f2(P), f2(P), pP[:], ALU.add, reads=[pP, P], writes=[P])
                    if lev < 5:
                        prevYT = YT_
                        N_, NT_, Y_, YT_ = Y_, YT_, N_, NT_
                    yield
                vb, kbg, kd, u, wT, Eg, vnew = sl["vb"], sl["kbg"], sl["kd"], sl["u"], sl["wT"], sl["Eg"], sl["vnew"]
                S.tt("pool", vb[:], v[:], bc4(bd), ALU.mult, reads=[v, (BG, ti)], writes=[vb])
                S.tt("pool", kbg[:], ktok[:], bc4(sl["bg2"][:]), ALU.mult, reads=[ktok, sl["bg2"]], writes=[kbg])
                S.tt("pool", kd[:], ktok[:], bc4(E[:, 4:8]), ALU.mult, reads=[ktok, E], writes=[kd])
                pu = pb(); pw = pb(); pE = pb()
                for h in range(4):
                    S.mm(pu[:, hs(h)], P[:, h, :], vb[:, h, :], True, True, reads=[P, vb], writes=[pu])
                for h in range(4):
                    S.mm(pw[:, hs(h)], kbg[:, h, :], P[:, h, :], True, True, reads=[P, kbg], writes=[pw])
                for h in range(4):
                    S.mm(pE[:, hs(h)], onesb[:], TG[:, h, :], True, True, reads=[TG], writes=[pE])
                S.copy("act", f2(u), pu[:], reads=[pu], writes=[u])
                S.copy("act", f2(wT), pw[:], reads=[pw], writes=[wT])
                S.act(f2(Eg), pE[:], AF.Exp, reads=[pE], writes=[Eg])
                S.tt("dve", sl["qg0"][:, :, 0:64], qT[:, :, 0:64], Eg[:, :, 0:64], ALU.mult, reads=[qT, Eg], writes=[sl["qg0"]])
                S.tt("pool", sl["qg1"][:, :, 64:128], qT[:, :, 64:128], Eg[:, :, 64:128], ALU.mult, reads=[qT, Eg], writes=[sl["qg1"]])
                yield
                po = PB[4 * d + 3]
                S.memset("dve", po[:], 0.0, writes=[po])
                for c in ([0, 1] if d == 0 else [1, 0]):
                    Rr = slice(64 * c, 64 * c + 64)
                    pws = pb()
                    for h in range(4):
                        S.mm(pws[:, hs(h)], wT[:, h, :], Sb[:, d, h, :], True, True, reads=[wT, (Sb, d)], writes=[pws])
                    S.tt("pool", sl["Stmp"][:], Sst[:, d], bc4(E[:, 8 + 4 * c:12 + 4 * c]), ALU.mult, reads=[E, (Sst, d)], writes=[sl["Stmp"]])
                    S.tt("dve", f2(vnew)[Rr, :], f2(u)[Rr, :], pws[Rr, :], ALU.subtract, reads=[u, pws], writes=[vnew])
                    qg = sl["qg0"] if c == 0 else sl["qg1"]
                    for h in range(4):
                        S.op("pe", lambda e, h=h, qg=qg: e.matmul(po[:, hs(h)], qg[:, h, :], Sb[:, d, h, :], start=False, stop=False, skip_group_check=True),
                             reads=[qg, (Sb, d)], writes=[po])
                    pS = pb()
                    for h in range(4):
                        S.mm(pS[:, hs(h)], kd[Rr, h, :], vnew[Rr, h, :], True, True, reads=[kd, vnew], writes=[pS])
                    S.tt("dve", Sb[:, d].rearrange("p h d -> p (h d)"), f2(sl["Stmp"]), pS[:], ALU.add, reads=[pS, sl["Stmp"]], writes=[(Sb, d)])
                    S.tt("dve", Sst[:, d].rearrange("p h d -> p (h d)"), f2(sl["Stmp"]), pS[:], ALU.add, reads=[pS, sl["Stmp"], (Sst, d)], writes=[(Sst, d)])
                    yield
                for h in range(4):
                    S.op("pe", lambda e, h=h: e.matmul(po[:, hs(h)], DT[:, h, :], vnew[:, h, :], start=False, stop=False, skip_group_check=True),
                         reads=[DT, vnew], writes=[po])
                osb, oprev, gate = sl["osb"], sl["oprev"], sl["gate"]
                if ti not in visited:
                    visited.add(ti)
                    S.copy("act", f2(osb), po[:], reads=[po], writes=[osb])
                    S.dma(o_s[rows, :], f2(osb), reads=[osb], writes=[("o_s", ti)])
                else:
                    ss = sl["ss"]
                    S.dma(f2(oprev), o_s[rows, :], reads=[("o_s", ti)], writes=[oprev])
                    S.dma(f2(gate), gate_s[rows, :], writes=[gate])
                    S.tt("dve", f2(osb), po[:], f2(oprev), ALU.add, reads=[po, oprev], writes=[osb])
                    S.memset("pool", ss[:], 0.0, writes=[ss])
                    for h in range(4):
                        S.act(sl["junk"][:], osb[:, h, :], AF.Square, accum_out=ss[:, h:h + 1], reads=[osb, ss], writes=[sl["junk"], ss])
                    S.act(ss[:, 4:8], ss[:, 0:4], AF.Sqrt, scale=1.0 / 128, bias=1e-6, reads=[ss], writes=[ss])
                    S.op("dve", lambda e: e.reciprocal(ss[:, 4:8], ss[:, 4:8]), reads=[ss], writes=[ss])
                    S.tt("dve", osb[:], osb[:], bc4(ss[:, 4:8]), ALU.mult, reads=[osb, ss], writes=[osb])
                    S.tt("pool", osb[:], osb[:], gate[:], ALU.mult, reads=[osb, gate], writes=[osb])
                    S.tt("pool", osb[:], osb[:], bnbc, ALU.mult, reads=[osb], writes=[osb])
                    pT = pb()
                    for h in range(4):
                        S.tr(pT[:, hs(h)], osb[:, h, :], ident[:], reads=[osb], writes=[pT])
                    S.copy("act", catT[:, 4:8, tcol(ti):tcol(ti) + 128], pT[:].rearrange("p (h t) -> p h t", h=4), reads=[pT], writes=[(catT, "B", ti)])

            fwd_order = list(range(18))
            bwd_order = [1, 0] + list(range(17, 1, -1))
            orders = [fwd_order, bwd_order]
            LAG = 7
            active = {0: [], 1: []}
            nexti = {0: 0, 1: 0}
            while True:
                for d in range(2):
                    if nexti[d] < 18 and (not active[d] or (len(active[d]) == 1 and active[d][0][1] >= LAG)):
                        s_i = nexti[d]
                        nexti[d] += 1
                        active[d].append([item(orders[d][s_i], d, slots[2 * d + s_i % 2]), 0])
                if not active[0] and not active[1]:
                    break
                for d in range(2):
                    for ent in list(active[d]):
                        try:
                            next(ent[0])
                            ent[1] += 1
                        except StopIteration:
                            active[d].remove(ent)

        def layer0(b):
            tiles = list(range(18))
            with ExitStack() as stL:
                gates = sb(stL, [128, 18, 16], F32, "gates")
                with ExitStack() as stC:
                    catT = sb(stC, [128, 8, 2307], BF16, "catT")
                    BG = sb(stC, [128, 18, 16], F32, "BG")
                    with ExitStack() as st:
                        hT = sb(st, [128, 8, 2307], BF16, "hT")
                        for c in (0, 257, 2306):
                            S.memset("pool", hT[:, :, c:c + 1], 0.0, writes=[(hT, "pad", c)])
                        if want('A'):
                            with ExitStack() as st2:
                                phase_A(0, b, hT, tiles, st2)
                                S.barrier()
                        if want('B'):
                            with ExitStack() as st2:
                                phase_B(b, hT, catT, st2)
                                S.barrier()
                        if want('C'):
                            with ExitStack() as st2:
                                phase_C(b, hT, BG, st2)
                                S.barrier()
                    if want('D'):
                        with ExitStack() as st:
                            phase_D(b, BG, catT, st)
                            S.barrier()
                    post_E(0, b, catT, tiles, gates)
                post_FG(0, b, tiles, gates)

        LBLK = [(258 + 512 * j, 512, 256 + 512 * j, 512 * j) for j in range(4)]

        def rearr_w(ap):
            return ap.rearrange("(kc p) n -> p kc n", p=128)

        def l1_proj_mla(b, hT, dqn, dkvn, KR, st):
            wdq = sb(st, [128, 8, 384], BF16, "wdq"); S.dma(wdq[:], rearr_w(odwin_b[:, 768:1152]), writes=[wdq])
            wdkv = sb(st, [128, 8, 256], BF16, "wdkv"); S.dma(wdkv[:], rearr_w(odwin_b[:, 1152:1408]), writes=[wdkv])
            wk = sb(st, [128, 8, 96], BF16, "wk"); S.dma(wk[:], rearr_w(wkr_b), writes=[wk])
            wks = sb(st, [128, 8, 96], BF16, "wks"); S.dma(wks[:], rearr_w(wkrsw_b), writes=[wks])
            r32 = sb(st, [96, 2, LT], F32, "r32"); S.dma(r32[:], rope32_d.rearrange("a p t -> p a t"), writes=[r32])
            sq = [sb(st, [128, 512], F32, "sq1") for _ in range(3)]
            rinv = sb(st, [128, 512], F32, "rinv1")
            t1 = sb(st, [96, 512], F32, "t1a"); t2 = sb(st, [96, 512], F32, "t2a")

            def rms_proj(wt, nch, normT, dst, c0, w, d0, inv_n):
                pp = [pb() for _ in range(nch)]
                for c in range(nch):
                    for kc in range(8):
                        S.mm(pp[c][:, 0:w], wt[:, kc, c * 128:(c + 1) * 128], hT[:, kc, c0:c0 + w], kc == 0, kc == 7, reads=[wt], writes=[pp[c]])
                    S.act(sq[c][:, 0:w], pp[c][:, 0:w], AF.Square, reads=[pp[c]], writes=[sq[c]])
                pss = pb()
                for c in range(nch):
                    S.mm(pss[:, 0:w], ones[:], sq[c][:, 0:w], c == 0, c == nch - 1, reads=[sq[c]], writes=[pss])
                S.act(rinv[:, 0:w], pss[:, 0:w], AF.Sqrt, scale=inv_n, bias=1e-6, reads=[pss], writes=[rinv])
                S.op("dve", lambda e: e.reciprocal(rinv[:, 0:w], rinv[:, 0:w]), reads=[rinv], writes=[rinv])
                for c in range(nch):
                    S.stt("dve", dst[:, c, d0:d0 + w], pp[c][:, 0:w], normT[:, c:c + 1], rinv[:, 0:w], ALU.mult, ALU.mult,
                          reads=[pp[c], rinv], writes=[(dst, c, d0)])

            for bi, (c0, w, t0) in enumerate(BLOCKS):
                rms_proj(wdkv, 2, kvnT, dkvn, c0, w, t0, 1.0 / 256)
                pk = pb(); pks = pb()
                for kc in range(8):
                    S.mm(pk[0:96, 0:w], wk[:, kc, :], hT[:, kc, c0:c0 + w], kc == 0, kc == 7, reads=[wk], writes=[pk])
                if bi == 0:
                    S.copy("act", KR[64:96, t0:t0 + w], pk[64:96, 0:w], reads=[pk], writes=[(KR, t0)])
                else:
                    l0 = t0 - 256
                    for kc in range(8):
                        S.mm(pks[0:96, 0:w], wks[:, kc, :], hT[:, kc, c0:c0 + w], kc == 0, kc == 7, reads=[wks], writes=[pks])
                    S.tt("dve", t1[64:96, 0:w], pk[64:96, 0:w], r32[64:96, 0, l0:l0 + w], ALU.mult, reads=[pk, r32], writes=[t1])
                    S.tt("dve", t2[64:96, 0:w], pks[64:96, 0:w], r32[64:96, 1, l0:l0 + w], ALU.mult, reads=[pks, r32], writes=[t2])
                    S.tt("dve", KR[64:96, t0:t0 + w], t1[64:96, 0:w], t2[64:96, 0:w], ALU.add, reads=[t1, t2], writes=[(KR, t0)])
                    rms_proj(wdq, 3, qnT, dqn, c0, w, l0, 1.0 / 384)

        def l1_proj_win(b, hT, CQ, CK, CV, st):
            wq = sb(st, [128, 8, 512], BF16, "wq"); S.dma(wq[:], rearr_w(odwin_b[:, 0:512]), writes=[wq])
            wqs = sb(st, [128, 8, 512], BF16, "wqs"); S.dma(wqs[:], rearr_w(odwinsw_b[:, 0:512]), writes=[wqs])
            wkk = sb(st, [128, 8, 128], BF16, "wkk"); S.dma(wkk[:], rearr_w(odwin_b[:, 512:640]), writes=[wkk])
            wkks = sb(st, [128, 8, 128], BF16, "wkks"); S.dma(wkks[:], rearr_w(odwinsw_b[:, 512:640]), writes=[wkks])
            wv = sb(st, [128, 8, 128], BF16, "wv"); S.dma(wv[:], rearr_w(odwin_b[:, 640:768]), writes=[wv])
            r64 = sb(st, [64, 2, LT], F32, "r64"); S.dma(r64[:], rope64_d.rearrange("a p t -> p a t"), writes=[r64])
            t1 = [sb(st, [64, 512], F32, "t1w") for _ in range(2)]; t2 = [sb(st, [64, 512], F32, "t2w") for _ in range(2)]
            S.memset("pool", CV[:, :, :, 64:65], 1.0, writes=[(CV, "ones")])
            ii = 0
            for bi, (c0, w, t0) in enumerate(BLOCKS):
                l0 = t0 - 256
                for g in range(2):
                    pk = pb(); pks = pb()
                    for kc in range(8):
                        S.mm(pk[0:64, 0:w], wkk[:, kc, g * 64:(g + 1) * 64], hT[:, kc, c0:c0 + w], kc == 0, kc == 7, reads=[wkk], writes=[pk])
                    if bi == 0:
                        S.copy("act", CK[:, g, t0:t0 + w], pk[0:64, 0:w], reads=[pk], writes=[(CK, g, t0)])
                    else:
                        for kc in range(8):
                            S.mm(pks[0:64, 0:w], wkks[:, kc, g * 64:(g + 1) * 64], hT[:, kc, c0:c0 + w], kc == 0, kc == 7, reads=[wkks], writes=[pks])
                        a1 = t1[ii % 2]; a2 = t2[ii % 2]; ii += 1
                        S.tt("dve", a1[:, 0:w], pk[0:64, 0:w], r64[:, 0, l0:l0 + w], ALU.mult, reads=[pk, r64], writes=[a1])
                        S.tt("dve", a2[:, 0:w], pks[0:64, 0:w], r64[:, 1, l0:l0 + w], ALU.mult, reads=[pks, r64], writes=[a2])
                        S.tt("dve", CK[:, g, t0:t0 + w], a1[:, 0:w], a2[:, 0:w], ALU.add, reads=[a1, a2], writes=[(CK, g, t0)])
                for j in range(w // 128):
                    ti = t0 // 128 + j
                    pv = pb()
                    for kc in range(8):
                        S.mm(pv[:, 0:128], hT[:, kc, tcol(ti):tcol(ti) + 128], wv[:, kc, :], kc == 0, kc == 7, reads=[wv], writes=[pv])
                    S.copy("act", CV[:, ti, :, 0:64], pv[:, 0:128].rearrange("p (g d) -> p g d", g=2), reads=[pv], writes=[(CV, ti)])
                if bi > 0:
                    for h in range(8):
                        pq = pb(); pqs = pb()
                        for kc in range(8):
                            S.mm(pq[0:64, 0:w], wq[:, kc, h * 64:(h + 1) * 64], hT[:, kc, c0:c0 + w], kc == 0, kc == 7, reads=[wq], writes=[pq])
                        for kc in range(8):
                            S.mm(pqs[0:64, 0:w], wqs[:, kc, h * 64:(h + 1) * 64], hT[:, kc, c0:c0 + w], kc == 0, kc == 7, reads=[wqs], writes=[pqs])
                        a1 = t1[ii % 2]; a2 = t2[ii % 2]; ii += 1
                        S.tt("dve", a1[:, 0:w], pq[0:64, 0:w], r64[:, 0, l0:l0 + w], ALU.mult, reads=[pq, r64], writes=[a1])
                        S.tt("dve", a2[:, 0:w], pqs[0:64, 0:w], r64[:, 1, l0:l0 + w], ALU.mult, reads=[pqs, r64], writes=[a2])
                        S.tt("dve", CQ[:, h, l0:l0 + w], a1[:, 0:w], a2[:, 0:w], ALU.add, reads=[a1, a2], writes=[(CQ, h, l0)])

        def l1_attn_win(b, CQ, CK, CV, catT, st):
            PT = [sb(st, [128, 512], BF16, "PTw") for _ in range(2)]
            Yw = [sb(st, [128, 512], F32, "Yw") for _ in range(2)]
            den = sb(st, [128, 8], F32, "denw")
            ring = [0]

            def rb():
                r = PB[ring[0] % 6]
                ring[0] += 1
                return r
            seq = []
            for qt in range(16):
                for g in range(2):
                    keys = [(0, None), (1, None)]
                    if qt >= 1:
                        keys.append((qt + 1, 10))
                    keys.append((qt + 2, None))
                    if qt <= 14:
                        keys.append((qt + 3, 11))
                    for ki, (kt, mk) in enumerate(keys):
                        seq.append((qt, g, kt, mk, ki == 0, ki == len(keys) - 1))
            pss = {}

            def issue_S(i):
                qt, g, kt, mk, first, last = seq[i]
                ps = rb()
                S.mm(ps[:, 0:512], CK[:, g, kt * 128:(kt + 1) * 128], CQ[:, 4 * g:4 * g + 4, qt * 128:(qt + 1) * 128], True, True, reads=[], writes=[ps])
                pss[i] = ps
            issue_S(0)
            io = 0
            po = None; po3 = None
            for i, (qt, g, kt, mk, first, last) in enumerate(seq):
                yw = Yw[qt % 2]
                if first:
                    po = PB[6 + io % 2]; io += 1
                    po3 = po[:, 0:260].rearrange("p (a b) -> p a b", a=4)
                    S.memset("dve", po[:, 0:260], 0.0, writes=[po])
                if i + 1 < len(seq):
                    issue_S(i + 1)
                ps = pss.pop(i)
                pt = PT[i % 2]
                S.act(pt[:], ps[:, 0:512], AF.Exp, scale=0.125, reads=[ps], writes=[pt])
                if mk is not None:
                    pt3 = pt[:].rearrange("p (a b) -> p a b", a=4)
                    S.tt("dve", pt3, pt3, MK[:, mk, :].unsqueeze(1).to_broadcast([128, 4, 128]), ALU.mult, reads=[pt], writes=[pt])
                for hh in range(4):
                    S.op("pe", lambda e, hh=hh, pt=pt, kt=kt, po=po, g=g: e.matmul(po[:, hh * 65:(hh + 1) * 65], pt[:, hh * 128:(hh + 1) * 128], CV[:, kt, g, :],
                                                                                 start=False, stop=False, skip_group_check=True), reads=[pt], writes=[po])
                if last:
                    S.tt("dve", den[:, 0:4], po3[:, :, 64], expsink[:, 4 * g:4 * g + 4], ALU.add, reads=[po], writes=[den])
                    S.op("dve", lambda e: e.reciprocal(den[:, 4:8], den[:, 0:4]), reads=[den], writes=[den])
                    S.tt("dve", yw[:, g * 256:(g + 1) * 256].rearrange("p (a b) -> p a b", a=4), po3[:, :, 0:64],
                         den[:, 4:8].unsqueeze(2).to_broadcast([128, 4, 64]), ALU.mult, reads=[po, den], writes=[yw])
                    if g == 1:
                        pT = rb()
                        for j in range(4):
                            S.tr(pT[:, j * 128:(j + 1) * 128], yw[:, j * 128:(j + 1) * 128], ident[:], reads=[yw], writes=[pT])
                        S.copy("act", catT[:, 0:4, qt * 128:(qt + 1) * 128], pT[:, 0:512].rearrange("p (a b) -> p a b", a=4), reads=[pT], writes=[(catT, "w", qt)])

        def l1_up_mla(b, dqn, dkvn, KR, MQ, MKk, MV, st):
            wuq = sb(st, [128, 3, 768], BF16, "wuq"); S.dma(wuq[:], wuq_b.rearrange("(c p) n -> p c n", p=128), writes=[wuq])
            wuqs = sb(st, [128, 3, 768], BF16, "wuqs"); S.dma(wuqs[:], wuqsw_b.rearrange("(c p) n -> p c n", p=128), writes=[wuqs])
            wukv = sb(st, [128, 2, 1024], BF16, "wukv"); S.dma(wukv[:], wukv_b.rearrange("(c p) n -> p c n", p=128), writes=[wukv])
            r32 = sb(st, [96, 2, LT], F32, "r32b"); S.dma(r32[:], rope32_d.rearrange("a p t -> p a t"), writes=[r32])
            t1 = [sb(st, [96, 512], F32, "t1m") for _ in range(2)]; t2 = [sb(st, [96, 512], F32, "t2m") for _ in range(2)]
            S.memset("pool", MV[:, :, :, 64:65], 1.0, writes=[(MV, "ones")])
            ii = 0
            for (c0, w, t0, l0) in LBLK:
                for h in range(8):
                    pq = pb(); pqs = pb()
                    for c in range(3):
                        S.mm(pq[0:96, 0:w], wuq[:, c, h * 96:(h + 1) * 96], dqn[:, c, l0:l0 + w], c == 0, c == 2, reads=[wuq], writes=[pq])
                    for c in range(3):
                        S.mm(pqs[0:96, 0:w], wuqs[:, c, h * 96:(h + 1) * 96], dqn[:, c, l0:l0 + w], c == 0, c == 2, reads=[wuqs], writes=[pqs])
                    S.copy("act", MQ[0:64, h, l0:l0 + w], pq[0:64, 0:w], reads=[pq], writes=[(MQ, h, l0, 0)])
                    a1 = t1[ii % 2]; a2 = t2[ii % 2]; ii += 1
                    S.tt("dve", a1[64:96, 0:w], pq[64:96, 0:w], r32[64:96, 0, l0:l0 + w], ALU.mult, reads=[pq, r32], writes=[a1])
                    S.tt("dve", a2[64:96, 0:w], pqs[64:96, 0:w], r32[64:96, 1, l0:l0 + w], ALU.mult, reads=[pqs, r32], writes=[a2])
                    S.tt("dve", MQ[64:96, h, l0:l0 + w], a1[64:96, 0:w], a2[64:96, 0:w], ALU.add, reads=[a1, a2], writes=[(MQ, h, l0, 1)])
            for (c0, w, t0) in BLOCKS:
                for h in range(8):
                    pk = pb()
                    for c in range(2):
                        S.mm(pk[0:64, 0:w], wukv[:, c, h * 128:h * 128 + 64], dkvn[:, c, t0:t0 + w], c == 0, c == 1, reads=[wukv], writes=[pk])
                    if h % 2 == 0:
                        S.copy("act", MKk[0:64, h, t0:t0 + w], pk[0:64, 0:w], reads=[pk], writes=[(MKk, h, t0, 0)])
                    else:
                        S.copy("dve", MKk[0:64, h, t0:t0 + w], pk[0:64, 0:w], reads=[pk], writes=[(MKk, h, t0, 0)])
                    S.copy("pool", MKk[64:96, h, t0:t0 + w], KR[64:96, t0:t0 + w], reads=[], writes=[(MKk, h, t0, 1)])
                for j in range(w // 128):
                    ti = t0 // 128 + j
                    pv = pb()
                    wv3 = wukv[:].rearrange("p c (h x) -> p c h x", h=8)
                    for c in range(2):
                        S.mm(pv[:, 0:512], dkvn[:, c, ti * 128:(ti + 1) * 128], wv3[:, c, :, 64:128], c == 0, c == 1, reads=[wukv], writes=[pv])
                    S.copy("act", MV[:, ti, :, 0:64], pv[:, 0:512].rearrange("p (h x) -> p h x", h=8), reads=[pv], writes=[(MV, ti)])

        def l1_attn_mla(b, MQ, MKk, MV, catT, st):
            PT = [sb(st, [128, 1024], BF16, "PTm") for _ in range(3)]
            Ym = sb(st, [128, 4, 512], F32, "Ym")
            den = sb(st, [128, 8], F32, "denm")
            ring = [0]

            def rb():
                r = PB[ring[0] % 6]
                ring[0] += 1
                return r
            scale = 96.0 ** -0.5
            seq = [(qb, h, kp) for qb in range(4) for h in range(8) for kp in range(9)]
            pss = {}
            r2 = [0]

            def issue_S(i):
                qb, h, kp = seq[i]
                j = r2[0] % 3
                r2[0] += 1
                for half in range(2):
                    kt = 2 * kp + half
                    ps = PB[2 * j + half]
                    S.mm(ps[:, 0:512], MKk[:, h, kt * 128:(kt + 1) * 128], MQ[:, h, qb * 512:(qb + 1) * 512], True, True, reads=[], writes=[ps])
                pss[i] = j
            issue_S(0)
            io = 0
            po = None; po3 = None
            for i, (qb, h, kp) in enumerate(seq):
                if kp == 0:
                    po = PB[6 + io % 2]; io += 1
                    po3 = po[:, 0:260].rearrange("p (a b) -> p a b", a=4)
                    S.memset("dve", po[:, 0:260], 0.0, writes=[po])
                defer = (h == 7 and kp == 8)
                if i + 1 < len(seq) and not defer:
                    issue_S(i + 1)
                j = pss.pop(i)
                pt = PT[i % 3]
                S.act(pt[:], P2[j][:, 0:1024], AF.Exp, scale=scale, reads=[PB[2 * j], PB[2 * j + 1]], writes=[pt])
                for half in range(2):
                    kt = 2 * kp + half
                    for qs in range(4):
                        S.op("pe", lambda e, qs=qs, pt=pt, kt=kt, po=po, h=h, half=half: e.matmul(po[:, qs * 65:(qs + 1) * 65], pt[:, half * 512 + qs * 128:half * 512 + (qs + 1) * 128], MV[:, kt, h, :],
                                                                                                 start=False, stop=False, skip_group_check=True), reads=[pt], writes=[po])
                if kp == 8:
                    S.op("dve", lambda e, po3=po3: e.reciprocal(den[:, 0:4], po3[:, :, 64]), reads=[po], writes=[den])
                    S.tt("dve", Ym[:, :, h * 64:(h + 1) * 64], po3[:, :, 0:64], den[:, 0:4].unsqueeze(2).to_broadcast([128, 4, 64]), ALU.mult,
                         reads=[po, den], writes=[(Ym, h)])
                    if h == 7:
                        for qs in range(4):
                            pT = PB[2 * (r2[0] % 3)]
                            r2[0] += 1
                            for jj in range(4):
                                S.tr(pT[:, jj * 128:(jj + 1) * 128], Ym[:, qs, jj * 128:(jj + 1) * 128], ident[:], reads=[(Ym, hh) for hh in range(8)], writes=[pT])
                            qt = qb * 4 + qs
                            S.copy("act", catT[:, 4:8, qt * 128:(qt + 1) * 128], pT[:, 0:512].rearrange("p (a b) -> p a b", a=4), reads=[pT], writes=[(catT, "m", qt)])
                if defer and i + 1 < len(seq):
                    issue_S(i + 1)

        def layer1(b):
            tiles = list(range(2, 18))
            with ExitStack() as stL:
                gates = sb(stL, [128, 16, 16], F32, "gates1")
                with ExitStack() as stC:
                    catT = sb(stC, [128, 8, LT], BF16, "catT1")
                    dqn = sb(stC, [128, 3, LT], BF16, "dqn"); dkvn = sb(stC, [128, 2, T], BF16, "dkvn"); KR = sb(stC, [96, T], BF16, "KR")
                    with ExitStack() as stW:
                        CQ = sb(stW, [64, 8, LT], BF16, "CQ"); CK = sb(stW, [64, 2, T], BF16, "CK"); CV = sb(stW, [128, 18, 2, 65], BF16, "CV")
                        with ExitStack() as st:
                            hT = sb(st, [128, 8, 2307], BF16, "hT1")
                            with ExitStack() as st2:
                                phase_A(1, b, hT, list(range(18)), st2)
                                S.barrier()
                            if want('L1P'):
                              with ExitStack() as st2:
                                l1_proj_mla(b, hT, dqn, dkvn, KR, st2)
                                S.barrier()
                            if want('L1W'):
                              with ExitStack() as st2:
                                l1_proj_win(b, hT, CQ, CK, CV, st2)
                                S.barrier()
                        if want('L1WA'):
                          with ExitStack() as st2:
                            l1_attn_win(b, CQ, CK, CV, catT, st2)
                            S.barrier()
                    with ExitStack() as stM:
                        MQ = sb(stM, [96, 8, LT], BF16, "MQ"); MKk = sb(stM, [96, 8, T], BF16, "MKk"); MV = sb(stM, [128, 18, 8, 65], BF16, "MV")
                        if want('L1U'):
                          with ExitStack() as st2:
                            l1_up_mla(b, dqn, dkvn, KR, MQ, MKk, MV, st2)
                            S.barrier()
                        if want('L1MA'):
                          with ExitStack() as st2:
                            l1_attn_mla(b, MQ, MKk, MV, catT, st2)
                            S.barrier()
                    if dbg:
                        S.dma(cat_dbg, catT[:], reads=[])
                        S.barrier()
                    if want('L1E'):
                        post_E(1, b, catT, tiles, gates)
                if want('L1F'):
                    post_FG(1, b, tiles, gates)


        for b in range(NB):
            layer0(b)
            if not only0 and want('L1A'):
                layer1(b)
        S.finish([])
    return nc


NEGV = -30000.0


def make_consts():
    p = np.arange(128)
    ch = p // 64
    same = ch[:, None] == ch[None, :]
    a = p[:, None]; bb = p[None, :]
    M = np.zeros((14, 128, 128), np.float32)
    M[0] = same & (a <= bb)
    M[1] = same & (a > bb)
    M[2] = same & (a >= bb)
    M[3] = same & (a < bb)
    M[4] = np.where(same & (a > bb), 0.0, NEGV)
    M[5] = np.where(same & (bb >= a), 0.0, NEGV)
    M[6] = np.where(same & (a < bb), 0.0, NEGV)
    M[7] = np.where(same & (bb <= a), 0.0, NEGV)
    M[8] = (a < 64) & (bb >= 0)
    M[9] = (a >= 64) & (bb >= 0)
    M[10] = bb <= a
    M[11] = a <= bb
    t = np.arange(LT)
    rows = (t // 64).astype(np.float32); cols = (t % 64).astype(np.float32)

    def ang(rot):
        nf = rot // 4
        inv = (10000.0 ** (-np.arange(nf, dtype=np.float32) / nf)).astype(np.float32)
        return np.concatenate([rows[:, None] * inv, cols[:, None] * inv], -1).astype(np.float32)
    a64 = ang(64); a32 = ang(32)
    r64 = np.zeros((2, 64, LT), np.float32)
    r64[0] = np.concatenate([np.cos(a64), np.cos(a64)], 1).T
    r64[1] = np.concatenate([-np.sin(a64), np.sin(a64)], 1).T
    r32 = np.zeros((2, 96, LT), np.float32)
    r32[0, :64] = 1.0
    r32[0, 64:] = np.concatenate([np.cos(a32), np.cos(a32)], 1).T
    r32[1, 64:] = np.concatenate([-np.sin(a32), np.sin(a32)], 1).T
    return M, r64, r32


def prep_shared(inp):
    f = lambda a: np.ascontiguousarray(a, dtype=np.float32)
    M, r64, r32 = make_consts()
    w1 = inp["od_w_in"][0]
    w1s = w1.copy()
    for h in range(8):
        b0 = h * 64
        w1s[:, b0:b0 + 32] = w1[:, b0 + 32:b0 + 64]; w1s[:, b0 + 32:b0 + 64] = w1[:, b0:b0 + 32]
    for g in range(2):
        b0 = 512 + g * 64
        w1s[:, b0:b0 + 32] = w1[:, b0 + 32:b0 + 64]; w1s[:, b0 + 32:b0 + 64] = w1[:, b0:b0 + 32]
    wkr = np.zeros((1024, 96), np.float32); wkrs = np.zeros((1024, 96), np.float32)
    wkr[:, 64:96] = w1[:, 1408:1440]
    wkrs[:, 64:80] = w1[:, 1424:1440]; wkrs[:, 80:96] = w1[:, 1408:1424]
    wuq = inp["od_d_wuq"][0]
    wuqs = wuq.copy()
    for h in range(8):
        b0 = h * 96 + 64
        wuqs[:, b0:b0 + 16] = wuq[:, b0 + 16:b0 + 32]; wuqs[:, b0 + 16:b0 + 32] = wuq[:, b0:b0 + 16]
    d = {
        "ada_w": f(inp["ada_w"]), "ada_b": f(inp["ada_b"]),
        "ln_g": f(inp["ln_g"].reshape(4, 1024)), "ln_b": f(inp["ln_b"].reshape(4, 1024)),
        "ev_w_in": f(inp["ev_w_in"][0]),
        "a_convT": f(inp["ev_a_conv"][0].reshape(3, 4, 128).transpose(2, 1, 0)),
        "ev_b_conv": f(inp["ev_b_conv"][0]),
        "alog": f(inp["ev_b_alog"].reshape(1, 8)), "dtbias": f(inp["ev_b_dtbias"].reshape(1, 8)),
        "bnorm": f(inp["ev_b_norm"].reshape(1, 128)), "ev_w_out": f(inp["ev_w_out"][0]),
        "od_w_in": f(w1), "od_w_in_sw": f(w1s), "wkr": f(wkr), "wkr_sw": f(wkrs),
        "sink": f(inp["od_c_sink"].reshape(1, 8)),
        "qnormT": f(inp["od_d_qnorm"][0].reshape(3, 128).T), "kvnormT": f(inp["od_d_kvnorm"][0].reshape(2, 128).T),
        "wuq": f(wuq), "wuq_sw": f(wuqs), "wukv": f(inp["od_d_wukv"][0]), "od_w_out": f(inp["od_w_out"][0]),
        "router_w": f(inp["router_w"]), "router_bias": f(inp["router_bias"].reshape(1, 16)),
        "moe_g": f(inp["moe_w_gate"].reshape(32768, 512)), "moe_u": f(inp["moe_w_up"].reshape(32768, 512)),
        "moe_d": f(inp["moe_w_down"].reshape(16384, 1024)),
        "idn": np.eye(128, dtype=np.float32), "masks": M, "rope64": r64, "rope32": r32,
    }
    return d


def prep_core(inp, shared, b0, NB):
    f = lambda a: np.ascontiguousarray(a, dtype=np.float32)
    cs = np.concatenate([inp["c"][b0:b0 + NB], inp["c_ctx"][None, :]], 0)
    csT = cs.reshape(NB + 1, 8, 128).transpose(2, 1, 0)
    d = dict(shared)
    d["x"] = f(inp["x"][b0:b0 + NB]); d["ctx"] = f(inp["ctx"][b0:b0 + NB]); d["csT"] = f(csT)
    return d


_NC_CACHE = {}


def kernel(**inputs):
    inp = {k: np.asarray(v) for k, v in inputs.items()}
    NBC = 4
    if "nc" not in _NC_CACHE:
        _NC_CACHE["nc"] = build(NBC)
    nc = _NC_CACHE["nc"]
    shared = prep_shared(inp)
    in_maps = [prep_core(inp, shared, NBC * i, NBC) for i in range(8)]
    res = run_bass_kernel_spmd(nc, in_maps, core_ids=list(range(8)))
    return np.ascontiguousarray(np.concatenate([np.asarray(r["out"]) for r in res.results], 0).astype(np.float32))
```

```python
from concourse.bass_utils import run_bass_kernel_spmd
import numpy as np
import concourse.bass as bass
import concourse.mybir as mybir
from contextlib import ExitStack

F32 = mybir.dt.float32
BF16 = mybir.dt.bfloat16
AF = mybir.ActivationFunctionType
ALU = mybir.AluOpType
AX = mybir.AxisListType

N_DMA_SEMS = 40
USE_NOPS = False
SEM_ROLL = 30000


class Tok:
    __slots__ = ("sem", "val", "eng")

    def __init__(self, sem, val, eng):
        self.sem = sem
        self.val = val
        self.eng = eng


class Sched:
    def __init__(self, nc, stack):
        self.nc = nc
        self.stack = stack
        self.cengs = ["pe", "act", "dve", "pool"]
        self.all = ["pe", "act", "dve", "pool", "sp"]
        self.prog = {e: [] for e in self.all}
        self.nsem = 0
        self.sem = {e: self._newsem() for e in self.cengs}
        self.cnt = {e: 0 for e in self.cengs}
        self.waited = {e: {} for e in self.all}
        self.dsem = [self._newsem() for _ in range(N_DMA_SEMS)]
        self.dcnt = [0] * N_DMA_SEMS
        self.drr = 0
        self.res = {}
        self.pend = {e: {} for e in self.all}
        self.excl = set()
        self.old = []

    def _newsem(self):
        self.nsem += 1
        return self.stack.enter_context(self.nc.semaphore("s%d" % self.nsem))

    def _st(self, r):
        if isinstance(r, tuple):
            k = tuple(x if isinstance(x, (str, int)) else id(x) for x in r)
        elif isinstance(r, str):
            k = r
        else:
            k = id(r)
        st = self.res.get(k)
        if st is None:
            st = {"w": None, "r": {}}
            self.res[k] = st
        return st

    def _emit(self, eng, fn, reads, writes, dma):
        if self.excl:
            ex = [r for r in reads if (not isinstance(r, (tuple, str))) and id(r) in self.excl]
            if ex:
                reads = [r for r in reads if not ((not isinstance(r, (tuple, str))) and id(r) in self.excl)]
                writes = list(writes) + ex
        deps = []
        for r in reads:
            st = self._st(r)
            if st["w"] is not None:
                deps.append(st["w"])
        for w in writes:
            st = self._st(w)
            if st["w"] is not None:
                deps.append(st["w"])
            deps.extend(st["r"].values())
        need = {}
        for t in deps:
            if t.eng == eng and eng == "pe" and not dma:
                continue
            cur = need.get(id(t.sem))
            if cur is None or cur[1] < t.val:
                need[id(t.sem)] = (t.sem, t.val)
        if self.pend[eng]:
            for sid, (s_, v_) in self.pend[eng].items():
                cur = need.get(sid)
                if cur is None or cur[1] < v_:
                    need[sid] = (s_, v_)
            self.pend[eng] = {}
        if dma:
            i = self.drr
            self.drr = (self.drr + 1) % N_DMA_SEMS
            prev = self.dcnt[i]
            if prev > 0:
                cur = need.get(id(self.dsem[i]))
                if cur is None or cur[1] < prev:
                    need[id(self.dsem[i])] = (self.dsem[i], prev)
            self.dcnt[i] += 16
            tok = Tok(self.dsem[i], self.dcnt[i], "dma")
            inc = (self.dsem[i], 16)
        else:
            if self.cnt[eng] >= SEM_ROLL:
                self.old.append((self.sem[eng], self.cnt[eng]))
                self.sem[eng] = self._newsem()
                self.cnt[eng] = 0
            self.cnt[eng] += 1
            tok = Tok(self.sem[eng], self.cnt[eng], eng)
            inc = (self.sem[eng], 1)
        waits = []
        wd = self.waited[eng]
        for sid, (s, v) in need.items():
            if wd.get(sid, 0) >= v:
                continue
            wd[sid] = v
            waits.append((s, v))
        for r in reads:
            self._st(r)["r"][id(tok.sem)] = tok
        for w in writes:
            st = self._st(w)
            st["w"] = tok
            st["r"] = {}
        self.prog[eng].append((waits, fn, inc))
        return tok

    def op(self, eng, fn, reads=(), writes=()):
        return self._emit(eng, fn, reads, writes, False)

    def all_tokens(self):
        toks = {}
        for e in self.cengs:
            if self.cnt[e] > 0:
                toks[id(self.sem[e])] = (self.sem[e], self.cnt[e])
        for i in range(N_DMA_SEMS):
            if self.dcnt[i] > 0:
                toks[id(self.dsem[i])] = (self.dsem[i], self.dcnt[i])
        return toks

    def barrier(self):
        toks = self.all_tokens()
        for e in self.all:
            self.pend[e] = dict(toks)
        self.res = {}

    def dma(self, out, in_, reads=(), writes=(), q="sp", **kw):
        return self._emit(q, lambda e: e.dma_start(out=out, in_=in_, **kw), reads, writes, True)

    def mm(self, out, lhsT, rhs, start, stop, reads=(), writes=()):
        return self.op("pe", lambda e: e.matmul(out, lhsT, rhs, start=start, stop=stop), reads, writes)

    def tr(self, out, in_, ident, reads=(), writes=()):
        return self.op("pe", lambda e: e.transpose(out, in_, ident), reads, writes)

    def act(self, out, in_, func, bias=None, scale=None, accum_out=None, reads=(), writes=(), eng="act"):
        kw = {}
        if bias is not None:
            kw["bias"] = bias
        if scale is not None:
            kw["scale"] = scale
        if accum_out is not None:
            kw["accum_out"] = accum_out
        return self.op(eng, lambda e: e.activation(out, in_, func, **kw), reads, writes)

    def tt(self, eng, out, in0, in1, op, reads=(), writes=()):
        return self.op(eng, lambda e: e.tensor_tensor(out, in0, in1, op), reads, writes)

    def ts(self, eng, out, in0, s1, s2, op0, op1=None, reads=(), writes=(), accum_out=None):
        kw = {}
        if accum_out is not None:
            kw["accum_out"] = accum_out
        if op1 is None:
            return self.op(eng, lambda e: e.tensor_scalar(out, in0, s1, None, op0, **kw), reads, writes)
        return self.op(eng, lambda e: e.tensor_scalar(out, in0, s1, s2, op0, op1, **kw), reads, writes)

    def stt(self, eng, out, in0, scalar, in1, op0, op1, reads=(), writes=()):
        return self.op(eng, lambda e: e.scalar_tensor_tensor(out, in0, scalar, in1, op0, op1), reads, writes)

    def copy(self, eng, out, in_, reads=(), writes=()):
        if eng == "act":
            return self.op(eng, lambda e: e.copy(out, in_), reads, writes)
        return self.op(eng, lambda e: e.tensor_copy(out, in_), reads, writes)

    def memset(self, eng, ap, val, writes=()):
        return self.op(eng, lambda e: e.memset(ap, val), (), writes)

    def finish(self, final_tokens):
        nc = self.nc
        prog = self.prog
        engmap = {"pe": "tensor", "act": "scalar", "dve": "vector", "pool": "gpsimd", "sp": "sync"}
        fin = self.all_tokens()
        with nc.Block() as block:
            for ename in self.all:
                entries = prog[ename]
                is_sp = ename == "sp"

                def body(e, entries=entries, is_sp=is_sp):
                    for waits, fn, inc in entries:
                        for wi_, (s, v) in enumerate(waits):
                            e.wait_ge(s, v)
                            if USE_NOPS and wi_ + 1 < len(waits):
                                e.nop(nofuse=True)
                        ins = fn(e)
                        ins.then_inc(inc[0], inc[1])
                    if is_sp:
                        for (s, v) in fin.values():
                            e.wait_ge(s, v)

                getattr(block, engmap[ename])(body)


D = 1024
T = 2304
NT = 18
LT = 2048
ALPHA = (2.0 * 2) ** 0.25
NEG = -30000.0


def tcol(ti):
    return 1 + 128 * ti if ti < 2 else 2 + 128 * ti


BLOCKS = [(1, 256, 0)] + [(258 + 512 * j, 512, 256 + 512 * j) for j in range(4)]


ORDER = ['pw', 'pm', 'A', 'B', 'C', 'D', 'E', 'F', 'G', 'L1A', 'L1P', 'L1W', 'L1WA', 'L1U', 'L1MA', 'L1E', 'L1F', 'L1']


def build(NB, dbg=False, only0=False, stop='L1'):
    def want(nm):
        return ORDER.index(nm) <= ORDER.index(stop)

    nc = bass.Bass("TRN2", target_bir_lowering=False)
    R = NB + 1
    dd = {}

    def din(name, shape, dt=F32):
        dd[name] = nc.dram_tensor(name, list(shape), dt, kind="ExternalInput").ap()
        return dd[name]

    def dscr(name, shape, dt=F32, out=False):
        return nc.dram_tensor(name, list(shape), dt, kind="ExternalOutput" if out else "Internal").ap()

    x_d = din("x", [NB, LT, D]); ctx_d = din("ctx", [NB, 256, D]); csT_d = din("csT", [128, 8, R])
    adaw_d = din("ada_w", [2, D, 6144]); adab_d = din("ada_b", [2, 6144])
    lng_d = din("ln_g", [4, D]); lnb_d = din("ln_b", [4, D])
    evwin_d = din("ev_w_in", [D, 3600]); aconvT_d = din("a_convT", [128, 4, 3]); bconv_d = din("ev_b_conv", [3, 1536])
    alog_d = din("alog", [1, 8]); dtb_d = din("dtbias", [1, 8]); bnorm_d = din("bnorm", [1, 128]); evwout_d = din("ev_w_out", [D, D])
    odwin_d = din("od_w_in", [D, 1440]); odwinsw_d = din("od_w_in_sw", [D, 1440])
    wkr_d = din("wkr", [D, 96]); wkrsw_d = din("wkr_sw", [D, 96])
    sink_d = din("sink", [1, 8]); qnT_d = din("qnormT", [128, 3]); kvnT_d = din("kvnormT", [128, 2])
    wuq_d = din("wuq", [384, 768]); wuqsw_d = din("wuq_sw", [384, 768]); wukv_d = din("wukv", [256, 1024]); odwout_d = din("od_w_out", [D, D])
    rw_d = din("router_w", [D, 16]); rb_d = din("router_bias", [1, 16])
    mg_d = din("moe_g", [32768, 512]); mu_d = din("moe_u", [32768, 512]); md_d = din("moe_d", [16384, 1024])
    idn_d = din("idn", [128, 128]); masks_d = din("masks", [14, 128, 128])
    rope64_d = din("rope64", [2, 64, LT]); rope32_d = din("rope32", [2, 96, LT])
    out_d = dscr("out", [NB, LT, D], F32, out=True)

    evwin_b = dscr("evwin_b", [D, 3600], BF16); wB_b = [dscr("wB%d_b" % j, [D, 1536], BF16) for j in range(3)]
    evwout_b = dscr("evwout_b", [D, D], BF16)
    odwin_b = dscr("odwin_b", [D, 1440], BF16); odwinsw_b = dscr("odwinsw_b", [D, 1440], BF16)
    wkr_b = dscr("wkr_b", [D, 96], BF16); wkrsw_b = dscr("wkrsw_b", [D, 96], BF16)
    wuq_b = dscr("wuq_b", [384, 768], BF16); wuqsw_b = dscr("wuqsw_b", [384, 768], BF16); wukv_b = dscr("wukv_b", [256, 1024], BF16)
    odwout_b = dscr("odwout_b", [D, D], BF16)
    modrow_s = dscr("modrow_s", [2, R, 6144], F32, out=dbg)
    qT_s = dscr("qT_s", [4, 128, T], BF16); kT_s = dscr("kT_s", [4, 128, T], BF16)
    k_s = dscr("k_s", [T, 512], BF16); v_s = dscr("v_s", [T, 512], BF16); gate_s = dscr("gate_s", [T, 512]); o_s = dscr("o_s", [T, 512])
    xa_s = dscr("xa_s", [NB, 2, T, D], F32, out=dbg)
    xb_s = dscr("xb_s", [NB, T, D], F32, out=dbg)
    h2T_s = dscr("h2T_s", [8, 128, T], BF16)
    cat_dbg = dscr("cat_dbg", [128, 8, LT], BF16, out=True) if dbg else None
    ymix_s = dscr("ymix_s", [NB, 2, T, D], F32, out=dbg) if dbg else None

    with ExitStack() as st0:
        S = Sched(nc, st0)
        cnt = [0]

        def sb(stack, shape, dt=F32, name=None):
            cnt[0] += 1
            return stack.enter_context(nc.sbuf_tensor("%s_%d" % (name or "t", cnt[0]), list(shape), dt))

        P2 = [st0.enter_context(nc.psum_tensor("pp%d" % i, [128, 1024], F32)) for i in range(4)]
        PB = [P2[i // 2][:, (i % 2) * 512:(i % 2 + 1) * 512] for i in range(8)]
        pbi = [0]
        S.excl = set(id(p_) for p_ in PB)

        def pb():
            p = PB[pbi[0] % 8]
            pbi[0] += 1
            return p

        ld_rr = [0]

        def rr3():
            ld_rr[0] += 1
            return ("dve", "pool", "act")[ld_rr[0] % 3]

        ident = sb(st0, [128, 128], name="ident"); S.dma(ident[:], idn_d, writes=[ident])
        ones = sb(st0, [128, 128], name="ones"); S.memset("pool", ones[:], 1.0, writes=[ones])
        MK = sb(st0, [128, 14, 128], name="masks")
        S.dma(MK[:], masks_d.rearrange("m p f -> p m f"), writes=[MK])
        modT = sb(st0, [128, 2, 48, R], name="modT")
        MKb = sb(st0, [128, 14, 128], BF16, name="masksb"); S.copy("dve", MKb[:], MK[:], reads=[MK], writes=[MKb])
        identb = sb(st0, [128, 128], BF16, name="identb"); S.copy("dve", identb[:], ident[:], reads=[ident], writes=[identb])
        onesb = sb(st0, [128, 128], BF16, name="onesb"); S.memset("pool", onesb[:], 1.0, writes=[onesb])
        rwt = sb(st0, [128, 8, 16], name="rw"); S.dma(rwt[:], rw_d.rearrange("(kc p) e -> p kc e", p=128), writes=[rwt])
        rbias = sb(st0, [128, 16], name="rbias"); S.dma(rbias[:], rb_d.to_broadcast([128, 16]), writes=[rbias])
        aconvT = sb(st0, [128, 4, 3], name="aconvT"); S.dma(aconvT[:], aconvT_d, writes=[aconvT])
        negexpA = sb(st0, [128, 8], name="negexpA"); dtb = sb(st0, [128, 8], name="dtb")
        S.dma(negexpA[:], alog_d.to_broadcast([128, 8]), writes=[negexpA]); S.dma(dtb[:], dtb_d.to_broadcast([128, 8]), writes=[dtb])
        S.act(negexpA[:], negexpA[:], AF.Exp, reads=[negexpA], writes=[negexpA])
        S.ts("dve", negexpA[:], negexpA[:], -1.0, None, ALU.mult, reads=[negexpA], writes=[negexpA])
        bnorm = sb(st0, [128, 128], name="bnorm"); S.dma(bnorm[:], bnorm_d.to_broadcast([128, 128]), writes=[bnorm])
        expsink = sb(st0, [128, 8], name="expsink"); S.dma(expsink[:], sink_d.to_broadcast([128, 8]), writes=[expsink])
        S.act(expsink[:], expsink[:], AF.Exp, reads=[expsink], writes=[expsink])
        qnT = sb(st0, [128, 3], name="qnT"); S.dma(qnT[:], qnT_d, writes=[qnT])
        kvnT = sb(st0, [128, 2], name="kvnT"); S.dma(kvnT[:], kvnT_d, writes=[kvnT])

        with ExitStack() as st:
            NSTG = 6
            stg = [sb(st, [128, 4096], F32, "stg") for _ in range(NSTG)]
            stgb = [sb(st, [128, 4096], BF16, "stgb") for _ in range(NSTG)]
            bcv = sb(st, [128, 3, 1536], F32, "bcv")
            for j in range(3):
                S.dma(bcv[:, j, :], bconv_d[j:j + 1, :].to_broadcast([128, 1536]), writes=[(bcv, j)])
            ui = [0]

            def conv(src, dst, rows, cols, scale=None):
                nrc = rows // 128
                G = max(1, min(nrc, 4096 // cols)) if cols <= 4096 else 1
                if scale is not None:
                    G = 1
                while nrc % G:
                    G -= 1
                for r0 in range(0, nrc, G):
                    i = ui[0] % NSTG
                    ui[0] += 1
                    eng = ("dve", "pool", "act")[i % 3]
                    a = stg[i][:, 0:G * cols].rearrange("p (g c) -> p g c", g=G)
                    b = stgb[i][:, 0:G * cols].rearrange("p (g c) -> p g c", g=G)
                    sv = src[r0 * 128:(r0 + G) * 128, :].rearrange("(g p) c -> p g c", p=128)
                    dv = dst[r0 * 128:(r0 + G) * 128, :].rearrange("(g p) c -> p g c", p=128)
                    S.dma(a, sv, writes=[stg[i]])
                    if scale is None:
                        S.copy(eng, b, a, reads=[stg[i]], writes=[stgb[i]])
                    else:
                        e2 = "pool" if eng == "act" else eng
                        S.tt(e2, b[:, 0, :], a[:, 0, :], scale, ALU.mult, reads=[stg[i], (bcv, 0), (bcv, 1), (bcv, 2)], writes=[stgb[i]])
                    S.dma(dv, b, reads=[stgb[i]])

            conv(evwin_d, evwin_b, D, 3600)
            for j in range(3):
                conv(evwin_d[:, 1536:3072], wB_b[j], D, 1536, scale=bcv[:, j, :])
            conv(evwout_d, evwout_b, D, D)
            conv(odwin_d, odwin_b, D, 1440); conv(odwinsw_d, odwinsw_b, D, 1440)
            conv(wkr_d, wkr_b, D, 96); conv(wkrsw_d, wkrsw_b, D, 96)
            conv(wuq_d, wuq_b, 384, 768); conv(wuqsw_d, wuqsw_b, 384, 768); conv(wukv_d, wukv_b, 256, 1024)
            conv(odwout_d, odwout_b, D, D)
            S.barrier()
        with ExitStack() as st:
          if want('pm'):
            csT = sb(st, [128, 8, R], F32, "csT")
            S.dma(csT[:], csT_d, writes=[csT])
            S.act(csT[:], csT[:], AF.Silu, reads=[csT], writes=[csT])
            awt = [sb(st, [128, 8, 1536], F32, "awt") for _ in range(2)]
            modrow = sb(st, [R, 6144], F32, "modrow")
            abr = sb(st, [R, 6144], F32, "abr")
            for l in range(2):
                S.dma(abr[:], adab_d[l:l + 1, :].to_broadcast([R, 6144]), reads=[modrow], writes=[abr])
                for q in range(4):
                    aw = awt[q % 2]
                    S.dma(aw[:], adaw_d[l, :, q * 1536:(q + 1) * 1536].rearrange("(kc p) n -> p kc n", p=128), writes=[aw])
                    for nb_ in range(3):
                        p = pb()
                        c0 = q * 1536 + nb_ * 512
                        for kc in range(8):
                            S.mm(p[0:R, :], csT[:, kc, :], aw[:, kc, nb_ * 512:(nb_ + 1) * 512], kc == 0, kc == 7, reads=[csT, aw], writes=[p])
                        S.tt("dve", modrow[:, c0:c0 + 512], p[0:R, :], abr[:, c0:c0 + 512], ALU.add, reads=[p, abr], writes=[modrow])
                S.dma(modrow_s[l], modrow[:], reads=[modrow])
                p = pb()
                for ch in range(48):
                    S.tr(p[:, ch * R:(ch + 1) * R], modrow[0:R, ch * 128:(ch + 1) * 128], ident[0:R, 0:R], reads=[modrow, ident], writes=[p])
                S.copy("dve", modT[:, l, :, :], p[:, 0:48 * R].rearrange("p (c r) -> p c r", r=R), reads=[p], writes=[modT])
                for c0 in (8, 32):
                    S.ts("dve", modT[:, l, c0:c0 + 8, :], modT[:, l, c0:c0 + 8, :], 1.0, None, ALU.add, reads=[modT], writes=[modT])
            S.barrier()

        def xsrc(layer, b, ti):
            if layer == 0:
                return ctx_d[b, ti * 128:(ti + 1) * 128, :] if ti < 2 else x_d[b, (ti - 2) * 128:(ti - 1) * 128, :]
            return xb_s[b, ti * 128:(ti + 1) * 128, :]

        def phase_A(layer, b, hT, tiles, st):
            xin = [sb(st, [128, D], F32, "xin") for _ in range(2)]
            for n, ti in enumerate(tiles):
                xt = xin[n % 2]
                r = NB if ti < 2 else b
                S.dma(xt[:], xsrc(layer, b, ti), writes=[xt])
                for half in range(2):
                    p = pb()
                    for k4 in range(4):
                        kc = half * 4 + k4
                        S.tr(p[:, k4 * 128:(k4 + 1) * 128], xt[:, kc * 128:(kc + 1) * 128], ident[:], reads=[xt], writes=[p])
                    for k4 in range(4):
                        kc = half * 4 + k4
                        dst = hT[:, kc, tcol(ti):tcol(ti) + 128]
                        if half == 0:
                            S.act(dst, p[:, k4 * 128:(k4 + 1) * 128], AF.Identity, bias=modT[:, layer, kc, r:r + 1],
                                  scale=modT[:, layer, 8 + kc, r:r + 1], reads=[p], writes=[(hT, ti, kc)])
                        else:
                            S.ts("dve", dst, p[:, k4 * 128:(k4 + 1) * 128], modT[:, layer, 8 + kc, r:r + 1], modT[:, layer, kc, r:r + 1],
                                 ALU.mult, ALU.add, reads=[p], writes=[(hT, ti, kc)])

        def layer_norm_tile(st_tiles, rt, lng, lnb, outt):
            stats, ag, sm = st_tiles
            for hf in range(2):
                S.op("dve", lambda e, hf=hf: e.bn_stats(stats[:, hf, :], rt[:, hf * 512:(hf + 1) * 512]), reads=[rt], writes=[stats])
            S.op("dve", lambda e: e.bn_aggr(ag[:], stats[:].rearrange('p a b -> p (a b)')), reads=[stats], writes=[ag])
            S.act(sm[:, 0:1], ag[:, 1:2], AF.Sqrt, bias=1e-5, reads=[ag], writes=[sm])
            S.op("dve", lambda e: e.reciprocal(sm[:, 1:2], sm[:, 0:1]), reads=[sm], writes=[sm])
            S.stt("dve", sm[:, 2:3], ag[:, 0:1], -1.0, sm[:, 1:2], ALU.mult, ALU.mult, reads=[ag, sm], writes=[sm])
            S.act(rt[:], rt[:], AF.Identity, bias=sm[:, 2:3], scale=sm[:, 1:2], reads=[rt, sm], writes=[rt])
            S.tt("pool", rt[:], rt[:], lng[:], ALU.mult, reads=[rt, lng], writes=[rt])
            S.tt("pool", outt[:], rt[:], lnb[:], ALU.add, reads=[rt, lnb], writes=[outt])

        def run_pipe(gens, depth):
            active = []
            it = iter(gens)
            fin = False
            while True:
                while len(active) < depth and not fin:
                    try:
                        active.append(next(it))
                    except StopIteration:
                        fin = True
                if not active:
                    break
                for g_ in list(active):
                    try:
                        next(g_)
                    except StopIteration:
                        active.remove(g_)

        def ln_stats(rt, stats, ag, sm):
            for hf in range(2):
                S.op("dve", lambda e, hf=hf: e.bn_stats(stats[:, hf, :], rt[:, hf * 512:(hf + 1) * 512]), reads=[rt], writes=[stats])
            S.op("dve", lambda e: e.bn_aggr(ag[:], stats[:].rearrange('p a b -> p (a b)')), reads=[stats], writes=[ag])
            S.act(sm[:, 0:1], ag[:, 1:2], AF.Sqrt, bias=1e-5, reads=[ag], writes=[sm])
            S.op("dve", lambda e: e.reciprocal(sm[:, 1:2], sm[:, 0:1]), reads=[sm], writes=[sm])
            S.stt("dve", sm[:, 2:3], ag[:, 0:1], -1.0, sm[:, 1:2], ALU.mult, ALU.mult, reads=[ag, sm], writes=[sm])

        def ln_apply(rt, sm, lng, lnb, outt):
            S.act(rt[:], rt[:], AF.Identity, bias=sm[:, 2:3], scale=sm[:, 1:2], reads=[rt, sm], writes=[rt])
            S.tt("dve", rt[:], rt[:], lng[:], ALU.mult, reads=[rt, lng], writes=[rt])
            S.tt("dve", outt[:], rt[:], lnb[:], ALU.add, reads=[rt, lnb], writes=[outt])

        def phase_E(layer, b, catT, tiles, gates, st):
            nt = len(tiles)
            NBUF = 5
            wo = sb(st, [128, 8, D], BF16, "wo")
            wsrc = evwout_b if layer == 0 else odwout_b
            S.dma(wo[:], wsrc.rearrange("(kc p) n -> p kc n", p=128), writes=[wo])
            lng = sb(st, [128, D], F32, "lng"); lnb = sb(st, [128, D], F32, "lnb")
            S.dma(lng[:], lng_d[2 * layer:2 * layer + 1, :].to_broadcast([128, D]), writes=[lng])
            S.dma(lnb[:], lnb_d[2 * layer:2 * layer + 1, :].to_broadcast([128, D]), writes=[lnb])
            g1 = [sb(st, [128, D], F32, "g1") for _ in range(2)]
            S.dma(g1[0][:], modrow_s[layer, NB:NB + 1, 2048:3072].to_broadcast([128, D]), writes=[g1[0]])
            S.dma(g1[1][:], modrow_s[layer, b:b + 1, 2048:3072].to_broadcast([128, D]), writes=[g1[1]])
            xin = [sb(st, [128, D], F32, "xin") for _ in range(NBUF)]
            rts = [sb(st, [128, D], F32, "rt") for _ in range(NBUF)]
            xns = [sb(st, [128, D], F32, "xn") for _ in range(NBUF)]
            h2f = [sb(st, [128, 8, 128], F32, "h2f") for _ in range(NBUF)]
            h2b = [sb(st, [128, 8, 128], BF16, "h2b") for _ in range(NBUF)]
            statsL = [sb(st, [128, 2, 6], F32, "stats") for _ in range(NBUF)]
            agL = [sb(st, [128, 2], F32, "ag") for _ in range(NBUF)]
            smL = [sb(st, [128, 4], F32, "sm") for _ in range(NBUF)]
            aff = sb(st, [128, nt, 16], F32, "aff")

            def tile_gen(n, ti):
                k = n % NBUF
                xt = xin[k]; rt = rts[k]; xn = xns[k]; hf_ = h2f[k]; hb_ = h2b[k]; stats = statsL[k]; ag = agL[k]; sm = smL[k]
                r = NB if ti < 2 else b
                gt = g1[0] if ti < 2 else g1[1]
                S.dma(xt[:], xsrc(layer, b, ti), writes=[xt])
                c0 = tcol(ti) if layer == 0 else (ti - 2) * 128
                for hf in range(2):
                    p = pb()
                    for kc in range(8):
                        S.mm(p[:], catT[:, kc, c0:c0 + 128], wo[:, kc, hf * 512:(hf + 1) * 512], kc == 0, kc == 7, reads=[wo], writes=[p])
                    if dbg:
                        S.copy("act", rt[:, hf * 512:(hf + 1) * 512], p[:], reads=[p], writes=[rt])
                        S.dma(ymix_s[b, layer, ti * 128:(ti + 1) * 128, hf * 512:(hf + 1) * 512], rt[:, hf * 512:(hf + 1) * 512], reads=[rt])
                    S.tt("dve", rt[:, hf * 512:(hf + 1) * 512], p[:], gt[:, hf * 512:(hf + 1) * 512], ALU.mult, reads=[p, gt], writes=[rt])
                yield
                S.stt("dve", rt[:], xt[:], ALPHA, rt[:], ALU.mult, ALU.add, reads=[xt, rt], writes=[rt])
                ln_stats(rt, stats, ag, sm)
                yield
                ln_apply(rt, sm, lng, lnb, xn)
                S.dma(xa_s[b, layer, ti * 128:(ti + 1) * 128, :], xn[:], reads=[xn])
                yield
                for half in range(2):
                    p = pb()
                    for k4 in range(4):
                        kc = half * 4 + k4
                        S.tr(p[:, k4 * 128:(k4 + 1) * 128], xn[:, kc * 128:(kc + 1) * 128], ident[:], reads=[xn], writes=[p])
                    for k4 in range(4):
                        kc = half * 4 + k4
                        if half == 0:
                            S.act(hf_[:, kc, :], p[:, k4 * 128:(k4 + 1) * 128], AF.Identity, bias=modT[:, layer, 24 + kc, r:r + 1],
                                  scale=modT[:, layer, 32 + kc, r:r + 1], reads=[p], writes=[hf_])
                        else:
                            S.ts("dve", hf_[:, kc, :], p[:, k4 * 128:(k4 + 1) * 128], modT[:, layer, 32 + kc, r:r + 1], modT[:, layer, 24 + kc, r:r + 1],
                                 ALU.mult, ALU.add, reads=[p], writes=[hf_])
                yield
                S.copy("act", hb_[:], hf_[:], reads=[hf_], writes=[hb_])
                S.dma(h2T_s.rearrange("k p t -> p k t")[:, :, n * 128:(n + 1) * 128], hb_[:], reads=[hb_])
                p = pb()
                for kc in range(8):
                    S.mm(p[:, 0:16], hf_[:, kc, :], rwt[:, kc, :], kc == 0, kc == 7, reads=[hf_], writes=[p])
                S.act(aff[:, n, :], p[:, 0:16], AF.Sigmoid, reads=[p], writes=[(aff, n)])

            run_pipe((tile_gen(n, ti) for n, ti in enumerate(tiles)), 4)
            router_batched(aff, gates, nt, st)

        def router_batched(aff, gates, nt, st):
            G4 = nt * 4
            sel = sb(st, [128, nt, 16], F32, "r_sel"); t1 = sb(st, [128, nt, 16], F32, "r_t1"); t2 = sb(st, [128, nt, 16], F32, "r_t2")
            m1 = sb(st, [128, G4], F32, "r_m1"); sec = sb(st, [128, G4], F32, "r_sec"); gs = sb(st, [128, G4], F32, "r_gs")
            gm = sb(st, [128, G4], F32, "r_gm"); tm = sb(st, [128, G4], F32, "r_tm")
            s1 = sb(st, [128, nt], F32, "r_s1"); s2 = sb(st, [128, nt], F32, "r_s2"); den = sb(st, [128, nt], F32, "r_den")
            allaff = [(aff, n) for n in range(nt)]
            RS = "rsres"

            def g4(t):
                return t[:].rearrange("p n (g e) -> p (n g) e", g=4)

            def bc44(t):
                return t[:].unsqueeze(2).to_broadcast([128, G4, 4])

            def bc16(t):
                return t[:].unsqueeze(2).to_broadcast([128, nt, 16])

            def dv(fn, *a, **k):
                return S.op("dve", fn, reads=allaff + [RS], writes=[RS])
            dv(lambda e: e.tensor_tensor(sel[:], aff[:], rbias[:].unsqueeze(1).to_broadcast([128, nt, 16]), ALU.add))
            dv(lambda e: e.tensor_reduce(m1[:], g4(sel), AX.X, ALU.max))
            dv(lambda e: e.tensor_tensor(g4(t1), g4(sel), bc44(m1), ALU.is_lt))
            dv(lambda e: e.tensor_scalar(t2[:], t1[:], 1.0, 1e9, ALU.subtract, ALU.mult))
            dv(lambda e: e.tensor_tensor(t1[:], t1[:], sel[:], ALU.mult))
            dv(lambda e: e.tensor_tensor(t2[:], t2[:], t1[:], ALU.add))
            dv(lambda e: e.tensor_reduce(sec[:], g4(t2), AX.X, ALU.max))
            dv(lambda e: e.tensor_tensor(gs[:], m1[:], sec[:], ALU.add))
            gs3 = gs[:].rearrange("p (n g) -> p n g", g=4)
            dv(lambda e: e.tensor_reduce(s1[:], gs3, AX.X, ALU.max))
            dv(lambda e: e.tensor_tensor(gm[:].rearrange("p (n g) -> p n g", g=4), gs3, s1[:].unsqueeze(2).to_broadcast([128, nt, 4]), ALU.is_ge))
            dv(lambda e: e.tensor_tensor(g4(t1), g4(sel), bc44(gm), ALU.mult))
            dv(lambda e: e.tensor_scalar(tm[:], gm[:], 1.0, 1e9, ALU.subtract, ALU.mult))
            dv(lambda e: e.tensor_tensor(g4(t1), g4(t1), bc44(tm), ALU.add))
            dv(lambda e: e.tensor_reduce(s1[:], t1[:], AX.X, ALU.max))
            dv(lambda e: e.tensor_tensor(t2[:], t1[:], bc16(s1), ALU.is_lt))
            dv(lambda e: e.tensor_tensor(sel[:], t1[:], t2[:], ALU.mult))
            dv(lambda e: e.tensor_scalar(t2[:], t2[:], 1.0, 1e9, ALU.subtract, ALU.mult))
            dv(lambda e: e.tensor_tensor(sel[:], sel[:], t2[:], ALU.add))
            dv(lambda e: e.tensor_reduce(s2[:], sel[:], AX.X, ALU.max))
            dv(lambda e: e.tensor_tensor(t2[:], t1[:], bc16(s2), ALU.is_ge))
            dv(lambda e: e.tensor_tensor(t2[:], t2[:], aff[:], ALU.mult))
            dv(lambda e: e.tensor_reduce(den[:], t2[:], AX.X, ALU.add))
            dv(lambda e: e.reciprocal(den[:], den[:]))
            S.op("dve", lambda e: e.tensor_tensor(gates[:], t2[:], bc16(den), ALU.mult), reads=[RS], writes=[gates])

        def phase_F(layer, b, h2T, gates, ntile, outacc, st):
            wgu = [sb(st, [128, 2, 8, 512], BF16, "wgu") for _ in range(2)]
            wdn = [sb(st, [128, 4, D], BF16, "wdn") for _ in range(2)]
            actT = [sb(st, [128, 4, 512], BF16, "actT") for _ in range(2)]
            sl = [sb(st, [128, 512], F32, "sl") for _ in range(2)]
            stgF = [sb(st, [128, 2048], F32, "stgF") for _ in range(2)]
            ntok = ntile * 128
            blocks = [(c, min(512, ntok - c)) for c in range(0, ntok, 512)]
            pi = [0]

            def pieces(e):
                wg = wgu[e % 2]; wd = wdn[e % 2]
                r0 = (layer * 16 + e) * 1024
                r1 = (layer * 16 + e) * 512
                out = []
                for which, src in ((0, mg_d), (1, mu_d)):
                    for half in range(2):
                        src_ap = src[r0 + half * 512:r0 + (half + 1) * 512, :].rearrange("(kc p) f -> p kc f", p=128)
                        out.append((wg[:, which, half * 4:(half + 1) * 4, :], src_ap, (wg, which, half), 4))
                for half in range(2):
                    src_ap = md_d[r1 + half * 256:r1 + (half + 1) * 256, :].rearrange("(fc p) n -> p fc n", p=128)
                    out.append((wd[:, half * 2:(half + 1) * 2, :], src_ap, (wd, half), 2))
                return out

            def load_cast(pc, eng="pool"):
                dst_ap, src_ap, key, G = pc
                sg = stgF[pi[0] % 2]; pi[0] += 1
                sgv = sg[:, 0:2048].rearrange("p (g c) -> p g c", g=G)
                S.dma(sgv, src_ap, writes=[sg])
                S.copy(eng, dst_ap, sgv, reads=[sg], writes=[key])

            def down(at, wd, e, c0, w):
                for j in range(w // 128):
                    n = c0 // 128 + j
                    for hf in range(2):
                        pd = pb()
                        for fc in range(4):
                            S.mm(pd[:], at[:, fc, j * 128:(j + 1) * 128], wd[:, fc, hf * 512:(hf + 1) * 512], fc == 0, fc == 3,
                                 reads=[(wd, fc // 2), (at, 0), (at, 1), (at, 2), (at, 3)], writes=[pd])
                        dst = outacc[:, n, hf * 512:(hf + 1) * 512]
                        if e == 0:
                            S.ts("dve", dst, pd[:], gates[:, n, e:e + 1], None, ALU.mult, reads=[pd], writes=[(outacc, n, hf)])
                        else:
                            S.stt("dve", dst, pd[:], gates[:, n, e:e + 1], dst, ALU.mult, ALU.add, reads=[pd], writes=[(outacc, n, hf)])

            for k_, pc in enumerate(pieces(0)):
                load_cast(pc, ("dve", "act", "pool")[k_ % 3])
            it = 0
            pending = None
            for e in range(16):
                wg = wgu[e % 2]; wd = wdn[e % 2]
                nxt = pieces(e + 1) if e + 1 < 16 else []
                for bi, (c0, w) in enumerate(blocks):
                    at = actT[it % 2]; it += 1
                    for fc in range(4):
                        pg = pb(); pu = pb()
                        for kc in range(8):
                            S.mm(pg[:, 0:w], wg[:, 0, kc, fc * 128:(fc + 1) * 128], h2T[:, kc, c0:c0 + w], kc == 0, kc == 7, reads=[(wg, 0, kc // 4)], writes=[pg])
                        for kc in range(8):
                            S.mm(pu[:, 0:w], wg[:, 1, kc, fc * 128:(fc + 1) * 128], h2T[:, kc, c0:c0 + w], kc == 0, kc == 7, reads=[(wg, 1, kc // 4)], writes=[pu])
                        s_ = sl[fc % 2]
                        S.act(s_[:, 0:w], pg[:, 0:w], AF.Silu, reads=[pg], writes=[s_])
                        S.tt("dve", at[:, fc, 0:w], s_[:, 0:w], pu[:, 0:w], ALU.mult, reads=[s_, pu], writes=[(at, fc)])
                    if pending is not None:
                        down(*pending)
                    pending = (at, wd, e, c0, w)
                    k = 2 if bi == 0 else 1
                    for _ in range(k):
                        if nxt:
                            load_cast(nxt.pop(0))
                while nxt:
                    load_cast(nxt.pop(0))
            down(*pending)

        def phase_G(layer, b, tiles, outacc, st):
            NBUF = 5
            lng = sb(st, [128, D], F32, "lng2"); lnb = sb(st, [128, D], F32, "lnb2")
            S.dma(lng[:], lng_d[2 * layer + 1:2 * layer + 2, :].to_broadcast([128, D]), writes=[lng])
            S.dma(lnb[:], lnb_d[2 * layer + 1:2 * layer + 2, :].to_broadcast([128, D]), writes=[lnb])
            g2 = [sb(st, [128, D], F32, "g2") for _ in range(2)]
            S.dma(g2[0][:], modrow_s[layer, NB:NB + 1, 5120:6144].to_broadcast([128, D]), writes=[g2[0]])
            S.dma(g2[1][:], modrow_s[layer, b:b + 1, 5120:6144].to_broadcast([128, D]), writes=[g2[1]])
            xin = [sb(st, [128, D], F32, "xin2") for _ in range(NBUF)]
            rts = [sb(st, [128, D], F32, "rt2") for _ in range(NBUF)]
            xns = [sb(st, [128, D], F32, "xn2") for _ in range(NBUF)]
            statsL = [sb(st, [128, 2, 6], F32, "stats2") for _ in range(NBUF)]
            agL = [sb(st, [128, 2], F32, "ag2") for _ in range(NBUF)]
            smL = [sb(st, [128, 4], F32, "sm2") for _ in range(NBUF)]

            def tile_gen(n, ti):
                k = n % NBUF
                xt = xin[k]; rt = rts[k]; xn = xns[k]; stats = statsL[k]; ag = agL[k]; sm = smL[k]
                gt = g2[0] if ti < 2 else g2[1]
                S.dma(xt[:], xa_s[b, layer, ti * 128:(ti + 1) * 128, :], writes=[xt])
                S.tt("dve", rt[:], outacc[:, n, :], gt[:], ALU.mult, reads=[gt, (outacc, n, 0), (outacc, n, 1)], writes=[rt])
                yield
                S.stt("dve", rt[:], xt[:], ALPHA, rt[:], ALU.mult, ALU.add, reads=[xt, rt], writes=[rt])
                ln_stats(rt, stats, ag, sm)
                yield
                ln_apply(rt, sm, lng, lnb, xn)
                if layer == 0:
                    S.dma(xb_s[b, ti * 128:(ti + 1) * 128, :], xn[:], reads=[xn])
                else:
                    S.dma(out_d[b, (ti - 2) * 128:(ti - 1) * 128, :], xn[:], reads=[xn])

            run_pipe((tile_gen(n, ti) for n, ti in enumerate(tiles)), 4)

        def post_E(layer, b, catT, tiles, gates):
            if not want('E'):
                return
            with ExitStack() as st2:
                phase_E(layer, b, catT, tiles, gates, st2)
                S.barrier()

        def post_FG(layer, b, tiles, gates):
            nt = len(tiles)
            if not want('F'):
                return
            with ExitStack() as st:
                outacc = sb(st, [128, nt, D], F32, "outacc")
                with ExitStack() as st2:
                    h2T = sb(st2, [128, 8, nt * 128], BF16, "h2T")
                    for kc in range(8):
                        S.dma(h2T[:, kc, :], h2T_s[kc, :, 0:nt * 128], writes=[h2T])
                    phase_F(layer, b, h2T, gates, nt, outacc, st2)
                    S.barrier()
                if not want('G'):
                    return
                with ExitStack() as st2:
                    phase_G(layer, b, tiles, outacc, st2)
                    S.barrier()

        def phase_B(b, hT, catT, st):
            wA = [sb(st, [128, 3, 8, 128], BF16, "wA") for _ in range(2)]
            u = sb(st, [128, 2307], F32, "u"); p0s = sb(st, [128, 2307], F32, "p0s"); y = sb(st, [128, 2307], F32, "y")
            t1 = [sb(st, [128, 512], F32, "t1") for _ in range(2)]
            for c in (0, 257, 2306):
                S.memset("pool", u[:, c:c + 1], 0.0, writes=[(u, "pad")])
                S.memset("pool", p0s[:, c:c + 1], 0.0, writes=[(p0s, "pad")])
            bi = 0
            for ch in range(4):
                w = wA[ch % 2]
                for which in range(3):
                    cc = which * 512 + ch * 128
                    S.dma(w[:, which], evwin_b[:, cc:cc + 128].rearrange("(kc p) n -> p kc n", p=128), writes=[(w, which)])
                for (c0, wd_, t0) in BLOCKS:
                    pp = [pb(), pb(), pb()]
                    for which in range(3):
                        for kc in range(8):
                            S.mm(pp[which][:, 0:wd_], w[:, which, kc, :], hT[:, kc, c0:c0 + wd_], kc == 0, kc == 7, reads=[(w, which)], writes=[pp[which]])
                    tt_ = t1[bi % 2]; bi += 1
                    S.copy("act", tt_[:, 0:wd_], pp[1][:, 0:wd_], reads=[pp[1]], writes=[tt_])
                    S.tt("dve", u[:, c0:c0 + wd_], tt_[:, 0:wd_], pp[2][:, 0:wd_], ALU.mult, reads=[tt_, pp[2]], writes=[(u, c0)])
                    S.copy("act", p0s[:, c0:c0 + wd_], pp[0][:, 0:wd_], reads=[pp[0]], writes=[(p0s, c0)])
                allu = [(u, c0) for c0, _, _ in BLOCKS] + [(u, "pad")]
                allp = [(p0s, c0) for c0, _, _ in BLOCKS] + [(p0s, "pad")]
                S.ts("dve", y[:, 1:2306], u[:, 1:2306], aconvT[:, ch, 1:2], None, ALU.mult, reads=allu, writes=[y])
                S.stt("dve", y[:, 1:2306], u[:, 0:2305], aconvT[:, ch, 0:1], y[:, 1:2306], ALU.mult, ALU.add, reads=allu + [y], writes=[y])
                S.stt("dve", y[:, 1:2306], u[:, 2:2307], aconvT[:, ch, 2:3], y[:, 1:2306], ALU.mult, ALU.add, reads=allu + [y], writes=[y])
                S.tt("dve", catT[:, ch, 1:2306], y[:, 1:2306], p0s[:, 1:2306], ALU.mult, reads=[y] + allp, writes=[(catT, ch)])

        def phase_C(b, hT, BG, st):
            NBUF = 4
            wB = [sb(st, [128, 3, 8, 128], BF16, "wB") for _ in range(2)]
            s_ = [sb(st, [128, 512], F32, "s_") for _ in range(NBUF)]
            sq_ = [sb(st, [128, 512], BF16, "sq_") for _ in range(NBUF)]
            rin = [sb(st, [128, 512], F32, "rin") for _ in range(NBUF)]
            kn = [sb(st, [128, 512], BF16, "kn") for _ in range(NBUF)]
            ktk = [sb(st, [128, 4, 128], BF16, "ktk") for _ in range(NBUF)]

            def qk_gen(i, which, h, w, c0, wd_, t0):
                p = pb(); n = 0
                for j in range(3):
                    for kc in range(8):
                        S.mm(p[:, 0:wd_], w[:, j, kc, :], hT[:, kc, c0 + j - 1:c0 + j - 1 + wd_], n == 0, n == 23, reads=[(w, j)], writes=[p])
                        n += 1
                s = s_[i % NBUF]; sq = sq_[i % NBUF]; ri = rin[i % NBUF]; qn = kn[i % NBUF]; kt = ktk[i % NBUF]
                S.act(s[:, 0:wd_], p[:, 0:wd_], AF.Silu, reads=[p], writes=[s])
                S.tt("dve", sq[:, 0:wd_], s[:, 0:wd_], s[:, 0:wd_], ALU.mult, reads=[s], writes=[sq])
                yield
                p2 = pb()
                S.mm(p2[:, 0:wd_], onesb[:], sq[:, 0:wd_], True, True, reads=[sq], writes=[p2])
                S.act(ri[:, 0:wd_], p2[:, 0:wd_], AF.Sqrt, bias=1e-6, reads=[p2], writes=[ri])
                S.op("dve", lambda e: e.reciprocal(ri[:, 0:wd_], ri[:, 0:wd_]), reads=[ri], writes=[ri])
                if which == 0:
                    S.stt("dve", qn[:, 0:wd_], s[:, 0:wd_], 128.0 ** -0.5, ri[:, 0:wd_], ALU.mult, ALU.mult, reads=[s, ri], writes=[qn])
                else:
                    S.tt("dve", qn[:, 0:wd_], s[:, 0:wd_], ri[:, 0:wd_], ALU.mult, reads=[s, ri], writes=[qn])
                dstT = (qT_s if which == 0 else kT_s)[h][:, t0:t0 + wd_]
                S.dma(dstT, qn[:, 0:wd_], reads=[qn])
                if which == 1:
                    yield
                    p3 = pb()
                    na = wd_ // 128
                    for j4 in range(na):
                        S.mm(p3[:, j4 * 128:(j4 + 1) * 128], qn[:, j4 * 128:(j4 + 1) * 128], identb[:], True, True, reads=[qn], writes=[p3])
                    S.copy("act", kt[:, 0:na, :], p3[:, 0:wd_].rearrange("p (a b) -> p a b", b=128), reads=[p3], writes=[kt])
                    S.dma(k_s[t0:t0 + wd_, h * 128:(h + 1) * 128].rearrange("(a p) d -> p a d", p=128), kt[:, 0:na, :], reads=[kt])

            def all_qk():
                i = 0
                wi = 0
                for which in range(2):
                    for h in range(4):
                        w = wB[wi % 2]; wi += 1
                        col = which * 512 + h * 128
                        for j in range(3):
                            S.dma(w[:, j], wB_b[j][:, col:col + 128].rearrange("(kc p) n -> p kc n", p=128), writes=[(w, j)])
                        for (c0, wd_, t0) in BLOCKS:
                            yield qk_gen(i, which, h, w, c0, wd_, t0)
                            i += 1
            run_pipe(all_qk(), 3)
            wV = sb(st, [128, 3, 8, 512], BF16, "wV")
            for j in range(3):
                S.dma(wV[:, j], wB_b[j][:, 1024:1536].rearrange("(kc p) n -> p kc n", p=128), writes=[(wV, j)])
            wG = sb(st, [128, 8, 512], BF16, "wG")
            S.dma(wG[:], evwin_b[:, 3072:3584].rearrange("(kc p) n -> p kc n", p=128), writes=[wG])
            wba = sb(st, [128, 8, 16], BF16, "wba")
            S.dma(wba[:], evwin_b[:, 3584:3600].rearrange("(kc p) n -> p kc n", p=128), writes=[wba])
            vt = [sb(st, [128, 512], BF16, "vt") for _ in range(2)]
            gt = [sb(st, [128, 512], F32, "gt") for _ in range(2)]
            for ti in range(NT):
                c = tcol(ti)
                p = pb(); n = 0
                for j in range(3):
                    for kc in range(8):
                        S.mm(p[:], hT[:, kc, c + j - 1:c + j - 1 + 128], wV[:, j, kc, :], n == 0, n == 23, reads=[(wV, j)], writes=[p])
                        n += 1
                v = vt[ti % 2]
                S.act(v[:], p[:], AF.Silu, reads=[p], writes=[v])
                S.dma(v_s[ti * 128:(ti + 1) * 128, :], v[:], reads=[v])
                p = pb()
                for kc in range(8):
                    S.mm(p[:], hT[:, kc, c:c + 128], wG[:, kc, :], kc == 0, kc == 7, reads=[wG], writes=[p])
                g = gt[ti % 2]
                S.act(g[:], p[:], AF.Silu, reads=[p], writes=[g])
                S.dma(gate_s[ti * 128:(ti + 1) * 128, :], g[:], reads=[g])
                p = pb()
                for kc in range(8):
                    S.mm(p[:, 0:16], hT[:, kc, c:c + 128], wba[:, kc, :], kc == 0, kc == 7, reads=[wba], writes=[p])
                S.copy("dve", BG[:, ti, :], p[:, 0:16], reads=[p], writes=[(BG, ti)])
            allbg = [(BG, ti) for ti in range(NT)]
            smb = sb(st, [128, NT, 8], F32, "smb")
            S.act(BG[:, :, 0:8], BG[:, :, 0:8], AF.Sigmoid, reads=allbg, writes=[(BG, "beta")])
            S.tt("dve", smb[:], BG[:, :, 8:16], dtb[:].unsqueeze(1).to_broadcast([128, NT, 8]), ALU.add, reads=allbg, writes=[smb])
            S.ts("dve", smb[:], smb[:], 30.0, None, ALU.min, reads=[smb], writes=[smb])
            S.act(smb[:], smb[:], AF.Exp, reads=[smb], writes=[smb])
            S.act(smb[:], smb[:], AF.Ln, bias=1.0, reads=[smb], writes=[smb])
            S.tt("dve", BG[:, :, 8:16], smb[:], negexpA[:].unsqueeze(1).to_broadcast([128, NT, 8]), ALU.mult, reads=[smb] + allbg, writes=[(BG, "g")])

        def phase_D(b, BG, catT, st):
            Sst = sb(st, [128, 2, 4, 128], F32, "Sst")
            S.memset("pool", Sst[:], 0.0, writes=[Sst])
            Sb = sb(st, [128, 2, 4, 128], BF16, "Sb")
            S.memset("pool", Sb[:], 0.0, writes=[Sb])
            names16 = ["kT", "qT", "ktok", "v", "TG", "A", "AT", "Mb", "MbT", "P", "vb", "kbg", "DT", "kd", "wT", "qg0", "qg1", "vnew"]
            names32 = ["u", "Eg", "osb", "oprev", "gate", "Stmp"]
            slots = []
            for d in range(4):
                sl = {nm: sb(st, [128, 4, 128], BF16, nm) for nm in names16}
                sl.update({nm: sb(st, [128, 4, 128], F32, nm) for nm in names32})
                sl["E"] = sb(st, [128, 16], F32, "E"); sl["bg2"] = sb(st, [128, 4], F32, "bg2"); sl["lnb"] = sb(st, [128, 4], F32, "lnbeta")
                sl["ss"] = sb(st, [128, 8], F32, "ss"); sl["junk"] = sb(st, [128, 128], F32, "junk")
                S.memset("pool", sl["qg0"][:], 0.0, writes=[sl["qg0"]]); S.memset("pool", sl["qg1"][:], 0.0, writes=[sl["qg1"]])
                slots.append(sl)
            visited = set()
            identbc = ident[:].unsqueeze(1).to_broadcast([128, 4, 128])
            bnbc = bnorm[:].unsqueeze(1).to_broadcast([128, 4, 128])

            def bc4(ap):
                return ap.unsqueeze(2).to_broadcast([128, 4, 128])

            def f2(t):
                return t[:].rearrange("p h d -> p (h d)")

            def hs(h):
                return slice(h * 128, (h + 1) * 128)

            ringi = [0, 0]

            def item(ti, d, sl):
                def pb():
                    r = PB[4 * d + ringi[d] % 3]
                    ringi[d] += 1
                    return r
                mi, ms, negS, negI = (0, 1, 4, 5) if d == 0 else (2, 3, 6, 7)
                gd = BG[:, ti, 8 + 4 * d:12 + 4 * d]
                bd = BG[:, ti, 4 * d:4 * d + 4]
                rows = slice(ti * 128, (ti + 1) * 128)
                kT, qT, ktok, v, TG, A, AT, P, DT = sl["kT"], sl["qT"], sl["ktok"], sl["v"], sl["TG"], sl["A"], sl["AT"], sl["P"], sl["DT"]
                E = sl["E"]
                S.dma(kT[:], kT_s.rearrange("h d t -> d h t")[:, :, rows], writes=[kT])
                S.dma(qT[:], qT_s.rearrange("h d t -> d h t")[:, :, rows], writes=[qT])
                S.dma(f2(ktok), k_s[rows, :], writes=[ktok])
                S.dma(f2(v), v_s[rows, :], writes=[v])
                ps_ = pb()
                S.mm(ps_[:, 0:4], MK[:, mi, :], gd, True, True, reads=[(BG, ti)], writes=[ps_])
                S.mm(ps_[:, 4:8], MK[:, ms, :], gd, True, True, reads=[(BG, ti)], writes=[ps_])
                S.mm(ps_[:, 8:12], MK[:, 8, :], gd, True, True, reads=[(BG, ti)], writes=[ps_])
                S.mm(ps_[:, 12:16], MK[:, 9, :], gd, True, True, reads=[(BG, ti)], writes=[ps_])
                S.act(E[:], ps_[:, 0:16], AF.Exp, reads=[ps_], writes=[E])
                S.tt("dve", sl["bg2"][:], bd, E[:, 0:4], ALU.mult, reads=[E, (BG, ti)], writes=[sl["bg2"]])
                S.act(sl["lnb"][:], bd, AF.Ln, reads=[(BG, ti)], writes=[sl["lnb"]])
                S.tt("pool", TG[:], MK[:, mi, :].unsqueeze(1).to_broadcast([128, 4, 128]), bc4(gd), ALU.mult, reads=[(BG, ti)], writes=[TG])
                yield
                pKK = pb(); pL = pb()
                for h in range(4):
                    S.mm(pKK[:, hs(h)], kT[:, h, :], kT[:, h, :], True, True, reads=[kT], writes=[pKK])
                for h in range(4):
                    S.mm(pL[:, hs(h)], TG[:, h, :], MKb[:, ms, :], True, False, reads=[TG], writes=[pL])
                    S.mm(pL[:, hs(h)], identb[:], MKb[:, negS, :], False, True, reads=[TG], writes=[pL])
                for h in range(4):
                    S.act(A[:, h, :], pL[:, hs(h)], AF.Exp, bias=sl["lnb"][:, h:h + 1], reads=[pL, sl["lnb"]], writes=[A])
                S.tt("dve", f2(A), pKK[:], f2(A), ALU.mult, reads=[pKK, A], writes=[A])
                yield
                pAT = pb()
                for h in range(4):
                    S.mm(pAT[:, hs(h)], A[:, h, :], identb[:], True, True, reads=[A], writes=[pAT])
                S.copy("act", f2(AT), pAT[:], reads=[pAT], writes=[AT])
                S.stt("dve", P[:], pAT[:].rearrange("p (h d) -> p h d", h=4), -1.0, identbc, ALU.mult, ALU.add, reads=[pAT], writes=[P])
                pLT = pb(); pQK = pb()
                for h in range(4):
                    S.mm(pLT[:, hs(h)], MKb[:, ms, :], TG[:, h, :], True, False, reads=[TG], writes=[pLT])
                    S.mm(pLT[:, hs(h)], identb[:], MKb[:, negI, :], False, True, reads=[TG], writes=[pLT])
                for h in range(4):
                    S.mm(pQK[:, hs(h)], kT[:, h, :], qT[:, h, :], True, True, reads=[kT, qT], writes=[pQK])
                S.act(f2(DT), pLT[:], AF.Exp, reads=[pLT], writes=[DT])
                S.tt("dve", f2(DT), pQK[:], f2(DT), ALU.mult, reads=[pQK, DT], writes=[DT])
                yield
                N_, NT_ = AT, A
                Y_, YT_ = sl["Mb"], sl["MbT"]
                prevYT = None
                for lev in range(6):
                    pP = None
                    if prevYT is not None:
                        pP = pb()
                        for h in range(4):
                            S.mm(pP[:, hs(h)], prevYT[:, h, :], P[:, h, :], True, True, reads=[prevYT, P], writes=[pP])
                    if lev < 5:
                        last = lev == 4
                        pMT = pb()
                        pM = None if last else pb()
                        for h in range(4):
                            if not last:
                                S.mm(pM[:, hs(h)], NT_[:, h, :], N_[:, h, :], True, True, reads=[N_, NT_], writes=[pM])
                            S.mm(pMT[:, hs(h)], N_[:, h, :], NT_[:, h, :], True, True, reads=[N_, NT_], writes=[pMT])
                        if not last:
                            S.copy("act", f2(Y_), pM[:], reads=[pM], writes=[Y_])
                        S.copy("act", f2(YT_), pMT[:], reads=[pMT], writes=[YT_])
                    if pP is not None:
                        S.tt("dve", f2(P), f2(P), pP[:], ALU.add, reads=[pP, P], writes=[P])
                    if lev < 5:
                        prevYT = YT_
                        N_, NT_, Y_, YT_ = Y_, YT_, N_, NT_
                    yield
                vb, kbg, kd, u, wT, Eg, vnew = sl["vb"], sl["kbg"], sl["kd"], sl["u"], sl["wT"], sl["Eg"], sl["vnew"]
                S.tt("pool", vb[:], v[:], bc4(bd), ALU.mult, reads=[v, (BG, ti)], writes=[vb])
                S.tt("pool", kbg[:], ktok[:], bc4(sl["bg2"][:]), ALU.mult, reads=[ktok, sl["bg2"]], writes=[kbg])
                S.tt("pool", kd[:], ktok[:], bc4(E[:, 4:8]), ALU.mult, reads=[ktok, E], writes=[kd])
                pu = pb(); pw = pb(); pE = pb()
                for h in range(4):
                    S.mm(pu[:, hs(h)], P[:, h, :], vb[:, h, :], True, True, reads=[P, vb], writes=[pu])
                for h in range(4):
                    S.mm(pw[:, hs(h)], kbg[:, h, :], P[:, h, :], True, True, reads=[P, kbg], writes=[pw])
                for h in range(4):
                    S.mm(pE[:, hs(h)], onesb[:], TG[:, h, :], True, True, reads=[TG], writes=[pE])
                S.copy("act", f2(u), pu[:], reads=[pu], writes=[u])
                S.copy("act", f2(wT), pw[:], reads=[pw], writes=[wT])
                S.act(f2(Eg), pE[:], AF.Exp, reads=[pE], writes=[Eg])
                S.tt("dve", sl["qg0"][:, :, 0:64], qT[:, :, 0:64], Eg[:, :, 0:64], ALU.mult, reads=[qT, Eg], writes=[sl["qg0"]])
                S.tt("pool", sl["qg1"][:, :, 64:128], qT[:, :, 64:128], Eg[:, :, 64:128], ALU.mult, reads=[qT, Eg], writes=[sl["qg1"]])
                yield
                po = PB[4 * d + 3]
                S.memset("dve", po[:], 0.0, writes=[po])
                for c in ([0, 1] if d == 0 else [1, 0]):
                    Rr = slice(64 * c, 64 * c + 64)
                    pws = pb()
                    for h in range(4):
                        S.mm(pws[:, hs(h)], wT[:, h, :], Sb[:, d, h, :], True, True, reads=[wT, (Sb, d)], writes=[pws])
                    S.tt("pool", sl["Stmp"][:], Sst[:, d], bc4(E[:, 8 + 4 * c:12 + 4 * c]), ALU.mult, reads=[E, (Sst, d)], writes=[sl["Stmp"]])
                    S.tt("dve", f2(vnew)[Rr, :], f2(u)[Rr, :], pws[Rr, :], ALU.subtract, reads=[u, pws], writes=[vnew])
                    qg = sl["qg0"] if c == 0 else sl["qg1"]
                    for h in range(4):
                        S.op("pe", lambda e, h=h, qg=qg: e.matmul(po[:, hs(h)], qg[:, h, :], Sb[:, d, h, :], start=False, stop=False, skip_group_check=True),
                             reads=[qg, (Sb, d)], writes=[po])
                    pS = pb()
                    for h in range(4):
                        S.mm(pS[:, hs(h)], kd[Rr, h, :], vnew[Rr, h, :], True, True, reads=[kd, vnew], writes=[pS])
                    S.tt("dve", Sb[:, d].rearrange("p h d -> p (h d)"), f2(sl["Stmp"]), pS[:], ALU.add, reads=[pS, sl["Stmp"]], writes=[(Sb, d)])
                    S.tt("dve", Sst[:, d].rearrange("p h d -> p (h d)"), f2(sl["Stmp"]), pS[:], ALU.add, reads=[pS, sl["Stmp"], (Sst, d)], writes=[(Sst, d)])
                    yield
                for h in range(4):
                    S.op("pe", lambda e, h=h: e.matmul(po[:, hs(h)], DT[:, h, :], vnew[:, h, :], start=False, stop=False, skip_group_check=True),
                         reads=[DT, vnew], writes=[po])
                osb, oprev, gate = sl["osb"], sl["oprev"], sl["gate"]
                if ti not in visited:
                    visited.add(ti)
                    S.copy("act", f2(osb), po[:], reads=[po], writes=[osb])
                    S.dma(o_s[rows, :], f2(osb), reads=[osb], writes=[("o_s", ti)])
                else:
                    ss = sl["ss"]
                    S.dma(f2(oprev), o_s[rows, :], reads=[("o_s", ti)], writes=[oprev])
                    S.dma(f2(gate), gate_s[rows, :], writes=[gate])
                    S.tt("dve", f2(osb), po[:], f2(oprev), ALU.add, reads=[po, oprev], writes=[osb])
                    S.memset("pool", ss[:], 0.0, writes=[ss])
                    for h in range(4):
                        S.act(sl["junk"][:], osb[:, h, :], AF.Square, accum_out=ss[:, h:h + 1], reads=[osb, ss], writes=[sl["junk"], ss])
                    S.act(ss[:, 4:8], ss[:, 0:4], AF.Sqrt, scale=1.0 / 128, bias=1e-6, reads=[ss], writes=[ss])
                    S.op("dve", lambda e: e.reciprocal(ss[:, 4:8], ss[:, 4:8]), reads=[ss], writes=[ss])
                    S.tt("dve", osb[:], osb[:], bc4(ss[:, 4:8]), ALU.mult, reads=[osb, ss], writes=[osb])
                    S.tt("pool", osb[:], osb[:], gate[:], ALU.mult, reads=[osb, gate], writes=[osb])
                    S.tt("pool", osb[:], osb[:], bnbc, ALU.mult, reads=[osb], writes=[osb])
                    pT = pb()
                    for h in range(4):
                        S.tr(pT[:, hs(h)], osb[:, h, :], ident[:], reads=[osb], writes=[pT])
                    S.copy("act", catT[:, 4:8, tcol(ti):tcol(ti) + 128], pT[:].rearrange("p (h t) -> p h t", h=4), reads=[pT], writes=[(catT, "B", ti)])

            fwd_order = list(range(18))
            bwd_order = [1, 0] + list(range(17, 1, -1))
            orders = [fwd_order, bwd_order]
            LAG = 7
            active = {0: [], 1: []}
            nexti = {0: 0, 1: 0}
            while True:
                for d in range(2):
                    if nexti[d] < 18 and (not active[d] or (len(active[d]) == 1 and active[d][0][1] >= LAG)):
                        s_i = nexti[d]
                        nexti[d] += 1
                        active[d].append([item(orders[d][s_i], d, slots[2 * d + s_i % 2]), 0])
                if not active[0] and not active[1]:
                    break
                for d in range(2):
                    for ent in list(active[d]):
                        try:
                            next(ent[0])
                            ent[1] += 1
                        except StopIteration:
                            active[d].remove(ent)

        def layer0(b):
            tiles = list(range(18))
            with ExitStack() as stL:
                gates = sb(stL, [128, 18, 16], F32, "gates")
                with ExitStack() as stC:
                    catT = sb(stC, [128, 8, 2307], BF16, "catT")
                    BG = sb(stC, [128, 18, 16], F32, "BG")
                    with ExitStack() as st:
                        hT = sb(st, [128, 8, 2307], BF16, "hT")
                        for c in (0, 257, 2306):
                            S.memset("pool", hT[:, :, c:c + 1], 0.0, writes=[(hT, "pad", c)])
                        if want('A'):
                            with ExitStack() as st2:
                                phase_A(0, b, hT, tiles, st2)
                                S.barrier()
                        if want('B'):
                            with ExitStack() as st2:
                                phase_B(b, hT, catT, st2)
                                S.barrier()
                        if want('C'):
                            with ExitStack() as st2:
                                phase_C(b, hT, BG, st2)
                                S.barrier()
                    if want('D'):
                        with ExitStack() as st:
                            phase_D(b, BG, catT, st)
                            S.barrier()
                    post_E(0, b, catT, tiles, gates)
                post_FG(0, b, tiles, gates)

        LBLK = [(258 + 512 * j, 512, 256 + 512 * j, 512 * j) for j in range(4)]

        def rearr_w(ap):
            return ap.rearrange("(kc p) n -> p kc n", p=128)

        def l1_proj_mla(b, hT, dqn, dkvn, KR, st):
            wdq = sb(st, [128, 8, 384], BF16, "wdq"); S.dma(wdq[:], rearr_w(odwin_b[:, 768:1152]), writes=[wdq])
            wdkv = sb(st, [128, 8, 256], BF16, "wdkv"); S.dma(wdkv[:], rearr_w(odwin_b[:, 1152:1408]), writes=[wdkv])
            wk = sb(st, [128, 8, 96], BF16, "wk"); S.dma(wk[:], rearr_w(wkr_b), writes=[wk])
            wks = sb(st, [128, 8, 96], BF16, "wks"); S.dma(wks[:], rearr_w(wkrsw_b), writes=[wks])
            r32 = sb(st, [96, 2, LT], F32, "r32"); S.dma(r32[:], rope32_d.rearrange("a p t -> p a t"), writes=[r32])
            sq = [sb(st, [128, 512], F32, "sq1") for _ in range(3)]
            rinv = sb(st, [128, 512], F32, "rinv1")
            t1 = sb(st, [96, 512], F32, "t1a"); t2 = sb(st, [96, 512], F32, "t2a")

            def rms_proj(wt, nch, normT, dst, c0, w, d0, inv_n):
                pp = [pb() for _ in range(nch)]
                for c in range(nch):
                    for kc in range(8):
                        S.mm(pp[c][:, 0:w], wt[:, kc, c * 128:(c + 1) * 128], hT[:, kc, c0:c0 + w], kc == 0, kc == 7, reads=[wt], writes=[pp[c]])
                    S.act(sq[c][:, 0:w], pp[c][:, 0:w], AF.Square, reads=[pp[c]], writes=[sq[c]])
                pss = pb()
                for c in range(nch):
                    S.mm(pss[:, 0:w], ones[:], sq[c][:, 0:w], c == 0, c == nch - 1, reads=[sq[c]], writes=[pss])
                S.act(rinv[:, 0:w], pss[:, 0:w], AF.Sqrt, scale=inv_n, bias=1e-6, reads=[pss], writes=[rinv])
                S.op("dve", lambda e: e.reciprocal(rinv[:, 0:w], rinv[:, 0:w]), reads=[rinv], writes=[rinv])
                for c in range(nch):
                    S.stt("dve", dst[:, c, d0:d0 + w], pp[c][:, 0:w], normT[:, c:c + 1], rinv[:, 0:w], ALU.mult, ALU.mult,
                          reads=[pp[c], rinv], writes=[(dst, c, d0)])

            for bi, (c0, w, t0) in enumerate(BLOCKS):
                rms_proj(wdkv, 2, kvnT, dkvn, c0, w, t0, 1.0 / 256)
                pk = pb(); pks = pb()
                for kc in range(8):
                    S.mm(pk[0:96, 0:w], wk[:, kc, :], hT[:, kc, c0:c0 + w], kc == 0, kc == 7, reads=[wk], writes=[pk])
                if bi == 0:
                    S.copy("act", KR[64:96, t0:t0 + w], pk[64:96, 0:w], reads=[pk], writes=[(KR, t0)])
                else:
                    l0 = t0 - 256
                    for kc in range(8):
                        S.mm(pks[0:96, 0:w], wks[:, kc, :], hT[:, kc, c0:c0 + w], kc == 0, kc == 7, reads=[wks], writes=[pks])
                    S.tt("dve", t1[64:96, 0:w], pk[64:96, 0:w], r32[64:96, 0, l0:l0 + w], ALU.mult, reads=[pk, r32], writes=[t1])
                    S.tt("dve", t2[64:96, 0:w], pks[64:96, 0:w], r32[64:96, 1, l0:l0 + w], ALU.mult, reads=[pks, r32], writes=[t2])
                    S.tt("dve", KR[64:96, t0:t0 + w], t1[64:96, 0:w], t2[64:96, 0:w], ALU.add, reads=[t1, t2], writes=[(KR, t0)])
                    rms_proj(wdq, 3, qnT, dqn, c0, w, l0, 1.0 / 384)

        def l1_proj_win(b, hT, CQ, CK, CV, st):
            wq = sb(st, [128, 8, 512], BF16, "wq"); S.dma(wq[:], rearr_w(odwin_b[:, 0:512]), writes=[wq])
            wqs = sb(st, [128, 8, 512], BF16, "wqs"); S.dma(wqs[:], rearr_w(odwinsw_b[:, 0:512]), writes=[wqs])
            wkk = sb(st, [128, 8, 128], BF16, "wkk"); S.dma(wkk[:], rearr_w(odwin_b[:, 512:640]), writes=[wkk])
            wkks = sb(st, [128, 8, 128], BF16, "wkks"); S.dma(wkks[:], rearr_w(odwinsw_b[:, 512:640]), writes=[wkks])
            wv = sb(st, [128, 8, 128], BF16, "wv"); S.dma(wv[:], rearr_w(odwin_b[:, 640:768]), writes=[wv])
            r64 = sb(st, [64, 2, LT], F32, "r64"); S.dma(r64[:], rope64_d.rearrange("a p t -> p a t"), writes=[r64])
            t1 = [sb(st, [64, 512], F32, "t1w") for _ in range(2)]; t2 = [sb(st, [64, 512], F32, "t2w") for _ in range(2)]
            S.memset("pool", CV[:, :, :, 64:65], 1.0, writes=[(CV, "ones")])
            ii = 0
            for bi, (c0, w, t0) in enumerate(BLOCKS):
                l0 = t0 - 256
                for g in range(2):
                    pk = pb(); pks = pb()
                    for kc in range(8):
                        S.mm(pk[0:64, 0:w], wkk[:, kc, g * 64:(g + 1) * 64], hT[:, kc, c0:c0 + w], kc == 0, kc == 7, reads=[wkk], writes=[pk])
                    if bi == 0:
                        S.copy("act", CK[:, g, t0:t0 + w], pk[0:64, 0:w], reads=[pk], writes=[(CK, g, t0)])
                    else:
                        for kc in range(8):
                            S.mm(pks[0:64, 0:w], wkks[:, kc, g * 64:(g + 1) * 64], hT[:, kc, c0:c0 + w], kc == 0, kc == 7, reads=[wkks], writes=[pks])
                        a1 = t1[ii % 2]; a2 = t2[ii % 2]; ii += 1
                        S.tt("dve", a1[:, 0:w], pk[0:64, 0:w], r64[:, 0, l0:l0 + w], ALU.mult, reads=[pk, r64], writes=[a1])
                        S.tt("dve", a2[:, 0:w], pks[0:64, 0:w], r64[:, 1, l0:l0 + w], ALU.mult, reads=[pks, r64], writes=[a2])
                        S.tt("dve", CK[:, g, t0:t0 + w], a1[:, 0:w], a2[:, 0:w], ALU.add, reads=[a1, a2], writes=[(CK, g, t0)])
                for j in range(w // 128):
                    ti = t0 // 128 + j
                    pv = pb()
                    for kc in range(8):
                        S.mm(pv[:, 0:128], hT[:, kc, tcol(ti):tcol(ti) + 128], wv[:, kc, :], kc == 0, kc == 7, reads=[wv], writes=[pv])
                    S.copy("act", CV[:, ti, :, 0:64], pv[:, 0:128].rearrange("p (g d) -> p g d", g=2), reads=[pv], writes=[(CV, ti)])
                if bi > 0:
                    for h in range(8):
                        pq = pb(); pqs = pb()
                        for kc in range(8):
                            S.mm(pq[0:64, 0:w], wq[:, kc, h * 64:(h + 1) * 64], hT[:, kc, c0:c0 + w], kc == 0, kc == 7, reads=[wq], writes=[pq])
                        for kc in range(8):
                            S.mm(pqs[0:64, 0:w], wqs[:, kc, h * 64:(h + 1) * 64], hT[:, kc, c0:c0 + w], kc == 0, kc == 7, reads=[wqs], writes=[pqs])
                        a1 = t1[ii % 2]; a2 = t2[ii % 2]; ii += 1
                        S.tt("dve", a1[:, 0:w], pq[0:64, 0:w], r64[:, 0, l0:l0 + w], ALU.mult, reads=[pq, r64], writes=[a1])
                        S.tt("dve", a2[:, 0:w], pqs[0:64, 0:w], r64[:, 1, l0:l0 + w], ALU.mult, reads=[pqs, r64], writes=[a2])
                        S.tt("dve", CQ[:, h, l0:l0 + w], a1[:, 0:w], a2[:, 0:w], ALU.add, reads=[a1, a2], writes=[(CQ, h, l0)])

        def l1_attn_win(b, CQ, CK, CV, catT, st):
            PT = [sb(st, [128, 512], BF16, "PTw") for _ in range(2)]
            Yw = [sb(st, [128, 512], F32, "Yw") for _ in range(2)]
            den = sb(st, [128, 8], F32, "denw")
            ring = [0]

            def rb():
                r = PB[ring[0] % 6]
                ring[0] += 1
                return r
            seq = []
            for qt in range(16):
                for g in range(2):
                    keys = [(0, None), (1, None)]
                    if qt >= 1:
                        keys.append((qt + 1, 10))
                    keys.append((qt + 2, None))
                    if qt <= 14:
                        keys.append((qt + 3, 11))
                    for ki, (kt, mk) in enumerate(keys):
                        seq.append((qt, g, kt, mk, ki == 0, ki == len(keys) - 1))
            pss = {}

            def issue_S(i):
                qt, g, kt, mk, first, last = seq[i]
                ps = rb()
                S.mm(ps[:, 0:512], CK[:, g, kt * 128:(kt + 1) * 128], CQ[:, 4 * g:4 * g + 4, qt * 128:(qt + 1) * 128], True, True, reads=[], writes=[ps])
                pss[i] = ps
            issue_S(0)
            io = 0
            po = None; po3 = None
            for i, (qt, g, kt, mk, first, last) in enumerate(seq):
                yw = Yw[qt % 2]
                if first:
                    po = PB[6 + io % 2]; io += 1
                    po3 = po[:, 0:260].rearrange("p (a b) -> p a b", a=4)
                    S.memset("dve", po[:, 0:260], 0.0, writes=[po])
                if i + 1 < len(seq):
                    issue_S(i + 1)
                ps = pss.pop(i)
                pt = PT[i % 2]
                S.act(pt[:], ps[:, 0:512], AF.Exp, scale=0.125, reads=[ps], writes=[pt])
                if mk is not None:
                    pt3 = pt[:].rearrange("p (a b) -> p a b", a=4)
                    S.tt("dve", pt3, pt3, MK[:, mk, :].unsqueeze(1).to_broadcast([128, 4, 128]), ALU.mult, reads=[pt], writes=[pt])
                for hh in range(4):
                    S.op("pe", lambda e, hh=hh, pt=pt, kt=kt, po=po, g=g: e.matmul(po[:, hh * 65:(hh + 1) * 65], pt[:, hh * 128:(hh + 1) * 128], CV[:, kt, g, :],
                                                                                 start=False, stop=False, skip_group_check=True), reads=[pt], writes=[po])
                if last:
                    S.tt("dve", den[:, 0:4], po3[:, :, 64], expsink[:, 4 * g:4 * g + 4], ALU.add, reads=[po], writes=[den])
                    S.op("dve", lambda e: e.reciprocal(den[:, 4:8], den[:, 0:4]), reads=[den], writes=[den])
                    S.tt("dve", yw[:, g * 256:(g + 1) * 256].rearrange("p (a b) -> p a b", a=4), po3[:, :, 0:64],
                         den[:, 4:8].unsqueeze(2).to_broadcast([128, 4, 64]), ALU.mult, reads=[po, den], writes=[yw])
                    if g == 1:
                        pT = rb()
                        for j in range(4):
                            S.tr(pT[:, j * 128:(j + 1) * 128], yw[:, j * 128:(j + 1) * 128], ident[:], reads=[yw], writes=[pT])
                        S.copy("act", catT[:, 0:4, qt * 128:(qt + 1) * 128], pT[:, 0:512].rearrange("p (a b) -> p a b", a=4), reads=[pT], writes=[(catT, "w", qt)])

        def l1_up_mla(b, dqn, dkvn, KR, MQ, MKk, MV, st):
            wuq = sb(st, [128, 3, 768], BF16, "wuq"); S.dma(wuq[:], wuq_b.rearrange("(c p) n -> p c n", p=128), writes=[wuq])
            wuqs = sb(st, [128, 3, 768], BF16, "wuqs"); S.dma(wuqs[:], wuqsw_b.rearrange("(c p) n -> p c n", p=128), writes=[wuqs])
            wukv = sb(st, [128, 2, 1024], BF16, "wukv"); S.dma(wukv[:], wukv_b.rearrange("(c p) n -> p c n", p=128), writes=[wukv])
            r32 = sb(st, [96, 2, LT], F32, "r32b"); S.dma(r32[:], rope32_d.rearrange("a p t -> p a t"), writes=[r32])
            t1 = [sb(st, [96, 512], F32, "t1m") for _ in range(2)]; t2 = [sb(st, [96, 512], F32, "t2m") for _ in range(2)]
            S.memset("pool", MV[:, :, :, 64:65], 1.0, writes=[(MV, "ones")])
            ii = 0
            for (c0, w, t0, l0) in LBLK:
                for h in range(8):
                    pq = pb(); pqs = pb()
                    for c in range(3):
                        S.mm(pq[0:96, 0:w], wuq[:, c, h * 96:(h + 1) * 96], dqn[:, c, l0:l0 + w], c == 0, c == 2, reads=[wuq], writes=[pq])
                    for c in range(3):
                        S.mm(pqs[0:96, 0:w], wuqs[:, c, h * 96:(h + 1) * 96], dqn[:, c, l0:l0 + w], c == 0, c == 2, reads=[wuqs], writes=[pqs])
                    S.copy("act", MQ[0:64, h, l0:l0 + w], pq[0:64, 0:w], reads=[pq], writes=[(MQ, h, l0, 0)])
                    a1 = t1[ii % 2]; a2 = t2[ii % 2]; ii += 1
                    S.tt("dve", a1[64:96, 0:w], pq[64:96, 0:w], r32[64:96, 0, l0:l0 + w], ALU.mult, reads=[pq, r32], writes=[a1])
                    S.tt("dve", a2[64:96, 0:w], pqs[64:96, 0:w], r32[64:96, 1, l0:l0 + w], ALU.mult, reads=[pqs, r32], writes=[a2])
                    S.tt("dve", MQ[64:96, h, l0:l0 + w], a1[64:96, 0:w], a2[64:96, 0:w], ALU.add, reads=[a1, a2], writes=[(MQ, h, l0, 1)])
            for (c0, w, t0) in BLOCKS:
                for h in range(8):
                    pk = pb()
                    for c in range(2):
                        S.mm(pk[0:64, 0:w], wukv[:, c, h * 128:h * 128 + 64], dkvn[:, c, t0:t0 + w], c == 0, c == 1, reads=[wukv], writes=[pk])
                    if h % 2 == 0:
                        S.copy("act", MKk[0:64, h, t0:t0 + w], pk[0:64, 0:w], reads=[pk], writes=[(MKk, h, t0, 0)])
                    else:
                        S.copy("dve", MKk[0:64, h, t0:t0 + w], pk[0:64, 0:w], reads=[pk], writes=[(MKk, h, t0, 0)])
                    S.copy("pool", MKk[64:96, h, t0:t0 + w], KR[64:96, t0:t0 + w], reads=[], writes=[(MKk, h, t0, 1)])
                for j in range(w // 128):
                    ti = t0 // 128 + j
                    pv = pb()
                    wv3 = wukv[:].rearrange("p c (h x) -> p c h x", h=8)
                    for c in range(2):
                        S.mm(pv[:, 0:512], dkvn[:, c, ti * 128:(ti + 1) * 128], wv3[:, c, :, 64:128], c == 0, c == 1, reads=[wukv], writes=[pv])
                    S.copy("act", MV[:, ti, :, 0:64], pv[:, 0:512].rearrange("p (h x) -> p h x", h=8), reads=[pv], writes=[(MV, ti)])

        def l1_attn_mla(b, MQ, MKk, MV, catT, st):
            PT = [sb(st, [128, 1024], BF16, "PTm") for _ in range(3)]
            Ym = sb(st, [128, 4, 512], F32, "Ym")
            den = sb(st, [128, 8], F32, "denm")
            ring = [0]

            def rb():
                r = PB[ring[0] % 6]
                ring[0] += 1
                return r
            scale = 96.0 ** -0.5
            seq = [(qb, h, kp) for qb in range(4) for h in range(8) for kp in range(9)]
            pss = {}
            r2 = [0]

            def issue_S(i):
                qb, h, kp = seq[i]
                j = r2[0] % 3
                r2[0] += 1
                for half in range(2):
                    kt = 2 * kp + half
                    ps = PB[2 * j + half]
                    S.mm(ps[:, 0:512], MKk[:, h, kt * 128:(kt + 1) * 128], MQ[:, h, qb * 512:(qb + 1) * 512], True, True, reads=[], writes=[ps])
                pss[i] = j
            issue_S(0)
            io = 0
            po = None; po3 = None
            for i, (qb, h, kp) in enumerate(seq):
                if kp == 0:
                    po = PB[6 + io % 2]; io += 1
                    po3 = po[:, 0:260].rearrange("p (a b) -> p a b", a=4)
                    S.memset("dve", po[:, 0:260], 0.0, writes=[po])
                defer = (h == 7 and kp == 8)
                if i + 1 < len(seq) and not defer:
                    issue_S(i + 1)
                j = pss.pop(i)
                pt = PT[i % 3]
                S.act(pt[:], P2[j][:, 0:1024], AF.Exp, scale=scale, reads=[PB[2 * j], PB[2 * j + 1]], writes=[pt])
                for half in range(2):
                    kt = 2 * kp + half
                    for qs in range(4):
                        S.op("pe", lambda e, qs=qs, pt=pt, kt=kt, po=po, h=h, half=half: e.matmul(po[:, qs * 65:(qs + 1) * 65], pt[:, half * 512 + qs * 128:half * 512 + (qs + 1) * 128], MV[:, kt, h, :],
                                                                                                 start=False, stop=False, skip_group_check=True), reads=[pt], writes=[po])
                if kp == 8:
                    S.op("dve", lambda e, po3=po3: e.reciprocal(den[:, 0:4], po3[:, :, 64]), reads=[po], writes=[den])
                    S.tt("dve", Ym[:, :, h * 64:(h + 1) * 64], po3[:, :, 0:64], den[:, 0:4].unsqueeze(2).to_broadcast([128, 4, 64]), ALU.mult,
                         reads=[po, den], writes=[(Ym, h)])
                    if h == 7:
                        for qs in range(4):
                            pT = PB[2 * (r2[0] % 3)]
                            r2[0] += 1
                            for jj in range(4):
                                S.tr(pT[:, jj * 128:(jj + 1) * 128], Ym[:, qs, jj * 128:(jj + 1) * 128], ident[:], reads=[(Ym, hh) for hh in range(8)], writes=[pT])
                            qt = qb * 4 + qs
                            S.copy("act", catT[:, 4:8, qt * 128:(qt + 1) * 128], pT[:, 0:512].rearrange("p (a b) -> p a b", a=4), reads=[pT], writes=[(catT, "m", qt)])
                if defer and i + 1 < len(seq):
                    issue_S(i + 1)

        def layer1(b):
            tiles = list(range(2, 18))
            with ExitStack() as stL:
                gates = sb(stL, [128, 16, 16], F32, "gates1")
                with ExitStack() as stC:
                    catT = sb(stC, [128, 8, LT], BF16, "catT1")
                    dqn = sb(stC, [128, 3, LT], BF16, "dqn"); dkvn = sb(stC, [128, 2, T], BF16, "dkvn"); KR = sb(stC, [96, T], BF16, "KR")
                    with ExitStack() as stW:
                        CQ = sb(stW, [64, 8, LT], BF16, "CQ"); CK = sb(stW, [64, 2, T], BF16, "CK"); CV = sb(stW, [128, 18, 2, 65], BF16, "CV")
                        with ExitStack() as st:
                            hT = sb(st, [128, 8, 2307], BF16, "hT1")
                            with ExitStack() as st2:
                                phase_A(1, b, hT, list(range(18)), st2)
                                S.barrier()
                            if want('L1P'):
                              with ExitStack() as st2:
                                l1_proj_mla(b, hT, dqn, dkvn, KR, st2)
                                S.barrier()
                            if want('L1W'):
                              with ExitStack() as st2:
                                l1_proj_win(b, hT, CQ, CK, CV, st2)
                                S.barrier()
                        if want('L1WA'):
                          with ExitStack() as st2:
                            l1_attn_win(b, CQ, CK, CV, catT, st2)
                            S.barrier()
                    with ExitStack() as stM:
                        MQ = sb(stM, [96, 8, LT], BF16, "MQ"); MKk = sb(stM, [96, 8, T], BF16, "MKk"); MV = sb(stM, [128, 18, 8, 65], BF16, "MV")
                        if want('L1U'):
                          with ExitStack() as st2:
                            l1_up_mla(b, dqn, dkvn, KR, MQ, MKk, MV, st2)
                            S.barrier()
                        if want('L1MA'):
                          with ExitStack() as st2:
                            l1_attn_mla(b, MQ, MKk, MV, catT, st2)
                            S.barrier()
                    if dbg:
                        S.dma(cat_dbg, catT[:], reads=[])
                        S.barrier()
                    if want('L1E'):
                        post_E(1, b, catT, tiles, gates)
                if want('L1F'):
                    post_FG(1, b, tiles, gates)


        for b in range(NB):
            layer0(b)
            if not only0 and want('L1A'):
                layer1(b)
        S.finish([])
    return nc


NEGV = -30000.0


def make_consts():
    p = np.arange(128)
    ch = p // 64
    same = ch[:, None] == ch[None, :]
    a = p[:, None]; bb = p[None, :]
    M = np.zeros((14, 128, 128), np.float32)
    M[0] = same & (a <= bb)
    M[1] = same & (a > bb)
    M[2] = same & (a >= bb)
    M[3] = same & (a < bb)
    M[4] = np.where(same & (a > bb), 0.0, NEGV)
    M[5] = np.where(same & (bb >= a), 0.0, NEGV)
    M[6] = np.where(same & (a < bb), 0.0, NEGV)
    M[7] = np.where(same & (bb <= a), 0.0, NEGV)
    M[8] = (a < 64) & (bb >= 0)
    M[9] = (a >= 64) & (bb >= 0)
    M[10] = bb <= a
    M[11] = a <= bb
    t = np.arange(LT)
    rows = (t // 64).astype(np.float32); cols = (t % 64).astype(np.float32)

    def ang(rot):
        nf = rot // 4
        inv = (10000.0 ** (-np.arange(nf, dtype=np.float32) / nf)).astype(np.float32)
        return np.concatenate([rows[:, None] * inv, cols[:, None] * inv], -1).astype(np.float32)
    a64 = ang(64); a32 = ang(32)
    r64 = np.zeros((2, 64, LT), np.float32)
    r64[0] = np.concatenate([np.cos(a64), np.cos(a64)], 1).T
    r64[1] = np.concatenate([-np.sin(a64), np.sin(a64)], 1).T
    r32 = np.zeros((2, 96, LT), np.float32)
    r32[0, :64] = 1.0
    r32[0, 64:] = np.concatenate([np.cos(a32), np.cos(a32)], 1).T
    r32[1, 64:] = np.concatenate([-np.sin(a32), np.sin(a32)], 1).T
    return M, r64, r32


def prep_shared(inp):
    f = lambda a: np.ascontiguousarray(a, dtype=np.float32)
    M, r64, r32 = make_consts()
    w1 = inp["od_w_in"][0]
    w1s = w1.copy()
    for h in range(8):
        b0 = h * 64
        w1s[:, b0:b0 + 32] = w1[:, b0 + 32:b0 + 64]; w1s[:, b0 + 32:b0 + 64] = w1[:, b0:b0 + 32]
    for g in range(2):
        b0 = 512 + g * 64
        w1s[:, b0:b0 + 32] = w1[:, b0 + 32:b0 + 64]; w1s[:, b0 + 32:b0 + 64] = w1[:, b0:b0 + 32]
    wkr = np.zeros((1024, 96), np.float32); wkrs = np.zeros((1024, 96), np.float32)
    wkr[:, 64:96] = w1[:, 1408:1440]
    wkrs[:, 64:80] = w1[:, 1424:1440]; wkrs[:, 80:96] = w1[:, 1408:1424]
    wuq = inp["od_d_wuq"][0]
    wuqs = wuq.copy()
    for h in range(8):
        b0 = h * 96 + 64
        wuqs[:, b0:b0 + 16] = wuq[:, b0 + 16:b0 + 32]; wuqs[:, b0 + 16:b0 + 32] = wuq[:, b0:b0 + 16]
    d = {
        "ada_w": f(inp["ada_w"]), "ada_b": f(inp["ada_b"]),
        "ln_g": f(inp["ln_g"].reshape(4, 1024)), "ln_b": f(inp["ln_b"].reshape(4, 1024)),
        "ev_w_in": f(inp["ev_w_in"][0]),
        "a_convT": f(inp["ev_a_conv"][0].reshape(3, 4, 128).transpose(2, 1, 0)),
        "ev_b_conv": f(inp["ev_b_conv"][0]),
        "alog": f(inp["ev_b_alog"].reshape(1, 8)), "dtbias": f(inp["ev_b_dtbias"].reshape(1, 8)),
        "bnorm": f(inp["ev_b_norm"].reshape(1, 128)), "ev_w_out": f(inp["ev_w_out"][0]),
        "od_w_in": f(w1), "od_w_in_sw": f(w1s), "wkr": f(wkr), "wkr_sw": f(wkrs),
        "sink": f(inp["od_c_sink"].reshape(1, 8)),
        "qnormT": f(inp["od_d_qnorm"][0].reshape(3, 128).T), "kvnormT": f(inp["od_d_kvnorm"][0].reshape(2, 128).T),
        "wuq": f(wuq), "wuq_sw": f(wuqs), "wukv": f(inp["od_d_wukv"][0]), "od_w_out": f(inp["od_w_out"][0]),
        "router_w": f(inp["router_w"]), "router_bias": f(inp["router_bias"].reshape(1, 16)),
        "moe_g": f(inp["moe_w_gate"].reshape(32768, 512)), "moe_u": f(inp["moe_w_up"].reshape(32768, 512)),
        "moe_d": f(inp["moe_w_down"].reshape(16384, 1024)),
        "idn": np.eye(128, dtype=np.float32), "masks": M, "rope64": r64, "rope32": r32,
    }
    return d


def prep_core(inp, shared, b0, NB):
    f = lambda a: np.ascontiguousarray(a, dtype=np.float32)
    cs = np.concatenate([inp["c"][b0:b0 + NB], inp["c_ctx"][None, :]], 0)
    csT = cs.reshape(NB + 1, 8, 128).transpose(2, 1, 0)
    d = dict(shared)
    d["x"] = f(inp["x"][b0:b0 + NB]); d["ctx"] = f(inp["ctx"][b0:b0 + NB]); d["csT"] = f(csT)
    return d


_NC_CACHE = {}


def kernel(**inputs):
    inp = {k: np.asarray(v) for k, v in inputs.items()}
    NBC = 4
    if "nc" not in _NC_CACHE:
        _NC_CACHE["nc"] = build(NBC)
    nc = _NC_CACHE["nc"]
    shared = prep_shared(inp)
    in_maps = [prep_core(inp, shared, NBC * i, NBC) for i in range(8)]
    res = run_bass_kernel_spmd(nc, in_maps, core_ids=list(range(8)))
    return np.ascontiguousarray(np.concatenate([np.asarray(r["out"]) for r in res.results], 0).astype(np.float32))
```
